# Optimizing a Trainium2 kernel written in Bass

```python
import math
import jax, jax.numpy as jnp
from jax import lax
import numpy as np

D_MODEL = 1024
BATCH = 16
SEQ = 2048
DEPTH = 2

GRID_W = 64
CTX_LEN = 256
D_MIX = D_MODEL
N_GROUPS = 4
GROUP_W = D_MIX // N_GROUPS
CONV_W = 4
LRU_HEADS = 4
LRU_HD = GROUP_W // LRU_HEADS
LRU_C = 8.0
HGRN_HEADS = 4
HGRN_HD = GROUP_W // HGRN_HEADS
HGRN_CHUNK = 64
SSD_HEADS = 4
SSD_HD = GROUP_W // SSD_HEADS
SSD_BC_GROUPS = 2
SSD_STATE = 64
SSD_CHUNK = 64
SSD_XBC = GROUP_W + 2 * SSD_BC_GROUPS * SSD_STATE
MLA_HEADS = 4
MLA_Q_RANK = 192
MLA_KV_RANK = 128
MLA_NOPE = 64
MLA_ROPE = 32
MLA_V = GROUP_W // MLA_HEADS
ROPE_BASE = 10000.0
Q_BLOCK = 128
N_EXPERTS = 16
EXPERT_FF = 512
EC_FACTOR = 2
N_MOD = 6
EPS = 1e-6
LRU_COLS = 2 * GROUP_W
HGRN_COLS = 5 * GROUP_W
SSD_COLS = GROUP_W + SSD_XBC + 2 * SSD_HEADS
MLA_COLS = MLA_Q_RANK + MLA_KV_RANK + MLA_ROPE
IN_COLS = LRU_COLS + HGRN_COLS + SSD_COLS + MLA_COLS
SPLITS = [LRU_COLS, LRU_COLS + HGRN_COLS, LRU_COLS + HGRN_COLS + SSD_COLS]

kernel_name = 'hybrid_dit_rglru_hgrn2_ssd_mla_ecmoe'


def rmsnorm(x, w):
    xf = x.astype(jnp.float32)
    y = xf * lax.rsqrt(jnp.mean(xf * xf, axis=-1, keepdims=True) + EPS)
    return (y * w).astype(x.dtype)


def modulate(h, shift, scale):
    return h * (1.0 + scale) + shift


def flip(t, d):
    return t[:, ::-1] if d else t


def to_chunks(t, chunk):
    b, n = t.shape[:2]
    return t.reshape(b, n // chunk, chunk, *t.shape[2:]).swapaxes(0, 1)


def from_chunks(t):
    nc, b, ch = t.shape[:3]
    return t.swapaxes(0, 1).reshape(b, nc * ch, *t.shape[3:])


def dwconv(x, w, b):
    k = w.shape[0]
    n = x.shape[1]
    xp = jnp.pad(x, ((0, 0), (k // 2, k - 1 - k // 2), (0, 0)))
    return sum(xp[:, j:j + n] * w[j] for j in range(k)) + b


def linear_scan(a, b, h0):
    b = b.at[:, 0].add(a[:, 0] * h0)

    def combine(left, right):
        a_l, b_l = left
        a_r, b_r = right
        return a_l * a_r, a_r * b_l + b_r

    _, h = lax.associative_scan(combine, (a, b), axis=1)
    return h


def masked_decay(seg, mask):
    return jnp.where(mask, jnp.exp(jnp.where(mask, seg, 0.0)), 0.0)


def rglru_coeffs(xc, w_r, b_r, w_i, b_i, lam):
    bn, nn, _ = xc.shape
    xh = xc.reshape(bn, nn, LRU_HEADS, LRU_HD)
    r = jax.nn.sigmoid((jnp.einsum('bnhd,hde->bnhe', xh, w_r).reshape(bn, nn, GROUP_W) + b_r).astype(jnp.float32))
    i = jax.nn.sigmoid((jnp.einsum('bnhd,hde->bnhe', xh, w_i).reshape(bn, nn, GROUP_W) + b_i).astype(jnp.float32))
    log_a = -LRU_C * r * jax.nn.softplus(-lam.astype(jnp.float32))
    a = jnp.exp(log_a)
    b = jnp.sqrt(-jnp.expm1(2.0 * log_a)) * (i * xc.astype(jnp.float32))
    return a, b


def rglru_mixer(u, u_ctx, conv_w, conv_b, w_r, b_r, w_i, b_i, lam, need_ctx):
    x_l, gate_l = jnp.split(u, 2, axis=-1)
    x_c, gate_c = jnp.split(u_ctx, 2, axis=-1)
    x_l = dwconv(x_l, conv_w, conv_b)
    x_c = dwconv(x_c, conv_w, conv_b)
    h_l, h_c = [], []
    for d in range(2):
        a, b = rglru_coeffs(flip(x_c, d), w_r[d], b_r[d], w_i[d], b_i[d], lam[d])
        hc = linear_scan(a, b, jnp.zeros((u_ctx.shape[0], GROUP_W), jnp.float32))
        a, b = rglru_coeffs(flip(x_l, d), w_r[d], b_r[d], w_i[d], b_i[d], lam[d])
        hl = linear_scan(a, b, hc[:, -1])
        h_l.append(flip(hl, d))
        h_c.append(flip(hc, d))
    y_l = ((h_l[0] + h_l[1]) * jax.nn.gelu(gate_l.astype(jnp.float32))).astype(u.dtype)
    y_c = ((h_c[0] + h_c[1]) * jax.nn.gelu(gate_c.astype(jnp.float32))).astype(u.dtype) if need_ctx else None
    return y_l, y_c


def gla_chunk_scan(q, k, v, log_f, s0):
    f32 = jnp.float32
    mask = jnp.tril(jnp.ones((HGRN_CHUNK, HGRN_CHUNK), bool))[None, :, :, None, None]

    def step(s, blk):
        qc, kc, vc, gc = blk
        cum = jnp.cumsum(gc, axis=1)
        seg = cum[:, :, None] - cum[:, None]
        decay = masked_decay(seg, mask)
        scores = jnp.einsum('bthk,bshk,btshk->btsh', qc, kc, decay)
        y = jnp.einsum('btsh,bshv->bthv', scores, vc) + jnp.einsum('bthk,bhkv->bthv', qc * jnp.exp(cum), s)
        k_end = kc * jnp.exp(cum[:, -1:] - cum)
        s_new = s * jnp.exp(cum[:, -1])[..., None] + jnp.einsum('bshk,bshv->bhkv', k_end, vc)
        return s_new, y

    blks = tuple(to_chunks(t.astype(f32), HGRN_CHUNK) for t in (q, k, v, log_f))
    s_fin, ys = lax.scan(step, s0.astype(f32), blks)
    return from_chunks(ys), s_fin


def hgrn2_mixer(u, u_ctx, lb, norm_w, need_ctx):
    lb = lb.astype(jnp.float32).reshape(HGRN_HEADS, HGRN_HD)

    def heads(t):
        return t.reshape(t.shape[0], t.shape[1], HGRN_HEADS, HGRN_HD)

    def prep(v):
        q, f_fwd, f_bwd, inp, g = jnp.split(v, 5, axis=-1)
        return heads(jax.nn.silu(q)), (heads(f_fwd), heads(f_bwd)), heads(inp), g

    def gates(f_raw):
        sig = jax.nn.sigmoid(f_raw.astype(jnp.float32))
        f = lb + (1.0 - lb) * sig
        return jnp.log(f), 1.0 - f

    q_l, f_l, v_l, g_l = prep(u)
    q_c, f_c, v_c, g_c = prep(u_ctx)
    o_l, o_c = [], []
    for d in range(2):
        log_f, k = gates(f_c[d])
        s0 = jnp.zeros((u_ctx.shape[0], HGRN_HEADS, HGRN_HD, HGRN_HD), jnp.float32)
        oc, s_c = gla_chunk_scan(flip(q_c, d), flip(k, d), flip(v_c, d), flip(log_f, d), s0)
        log_f, k = gates(f_l[d])
        ol, _ = gla_chunk_scan(flip(q_l, d), flip(k, d), flip(v_l, d), flip(log_f, d), s_c)
        o_l.append(flip(ol, d))
        o_c.append(flip(oc, d))

    def out(o, g):
        o = rmsnorm(o, norm_w.reshape(HGRN_HEADS, HGRN_HD)).reshape(o.shape[0], o.shape[1], GROUP_W)
        return (o * jax.nn.silu(g.astype(jnp.float32))).astype(u.dtype)

    y_l = out(o_l[0] + o_l[1], g_l)
    y_c = out(o_c[0] + o_c[1], g_c) if need_ctx else None
    return y_l, y_c


def ssd_chunk_scan(xs, bm, cm, dt, log_a, s0):
    f32 = jnp.float32
    mask = jnp.tril(jnp.ones((SSD_CHUNK, SSD_CHUNK), bool))[None, :, :, None]

    def step(s, blk):
        xc, bc, cc, dtc, lac = blk
        cum = jnp.cumsum(lac, axis=1)
        seg = cum[:, :, None, :] - cum[:, None, :, :]
        decay = masked_decay(seg, mask)
        scores = jnp.einsum('bthn,bshn->btsh', cc, bc) * decay
        y = jnp.einsum('btsh,bsh,bshp->bthp', scores, dtc, xc)
        y = y + jnp.einsum('bthn,bhpn->bthp', cc, s) * jnp.exp(cum)[..., None]
        w_end = dtc * jnp.exp(cum[:, -1:, :] - cum)
        s_new = s * jnp.exp(cum[:, -1])[:, :, None, None] + jnp.einsum('bshn,bsh,bshp->bhpn', bc, w_end, xc)
        return s_new, y

    blks = tuple(to_chunks(t.astype(f32), SSD_CHUNK) for t in (xs, bm, cm, dt, log_a))
    s_fin, ys = lax.scan(step, s0.astype(f32), blks)
    return from_chunks(ys), s_fin


def ssd_mixer(u, u_ctx, conv_w, conv_b, a_log, dt_bias, d_skip, norm_w, need_ctx):
    rep = SSD_HEADS // SSD_BC_GROUPS

    def prep(v):
        bn, nn = v.shape[:2]
        z, xbc, dt_raw = jnp.split(v, [GROUP_W, GROUP_W + SSD_XBC], axis=-1)
        xbc = jax.nn.silu(dwconv(xbc, conv_w, conv_b))
        xs, bm, cm = jnp.split(xbc, [GROUP_W, GROUP_W + SSD_BC_GROUPS * SSD_STATE], axis=-1)
        xs = xs.reshape(bn, nn, SSD_HEADS, SSD_HD)
        bm = jnp.repeat(bm.reshape(bn, nn, SSD_BC_GROUPS, SSD_STATE), rep, axis=2)
        cm = jnp.repeat(cm.reshape(bn, nn, SSD_BC_GROUPS, SSD_STATE), rep, axis=2)
        return z, xs, bm, cm, dt_raw.reshape(bn, nn, 2, SSD_HEADS)

    z_l, x_l, b_l, c_l, dt_l = prep(u)
    z_c, x_c, b_c, c_c, dt_c = prep(u_ctx)
    y_l, y_c = [], []
    for d in range(2):
        a = -jnp.exp(a_log[d].astype(jnp.float32))
        dt = jax.nn.softplus(dt_c[:, :, d].astype(jnp.float32) + dt_bias[d])
        s0 = jnp.zeros((u_ctx.shape[0], SSD_HEADS, SSD_HD, SSD_STATE), jnp.float32)
        yc, s_c = ssd_chunk_scan(flip(x_c, d), flip(b_c, d), flip(c_c, d), flip(dt, d), flip(dt * a, d), s0)
        dt = jax.nn.softplus(dt_l[:, :, d].astype(jnp.float32) + dt_bias[d])
        yl, _ = ssd_chunk_scan(flip(x_l, d), flip(b_l, d), flip(c_l, d), flip(dt, d), flip(dt * a, d), s_c)
        y_l.append(flip(yl, d))
        y_c.append(flip(yc, d))

    def out(y, xs, z):
        y = (y + d_skip[:, None] * xs).reshape(y.shape[0], y.shape[1], GROUP_W)
        return rmsnorm(y * jax.nn.silu(z.astype(jnp.float32)), norm_w).astype(u.dtype)

    out_l = out(y_l[0] + y_l[1], x_l, z_l)
    out_c = out(y_c[0] + y_c[1], x_c, z_c) if need_ctx else None
    return out_l, out_c


def axial_rope_tables(n_tokens):
    rows = n_tokens // GRID_W
    row = jnp.repeat(jnp.arange(rows, dtype=jnp.float32), GRID_W)
    col = jnp.tile(jnp.arange(GRID_W, dtype=jnp.float32), rows)
    half = MLA_ROPE // 2
    inv = ROPE_BASE ** (-jnp.arange(0, half, 2, dtype=jnp.float32) / half)
    ang = jnp.concatenate([row[:, None] * inv, col[:, None] * inv], axis=-1)
    return jnp.cos(ang), jnp.sin(ang)


def apply_rope(t, cos, sin):
    t1, t2 = t[..., 0::2], t[..., 1::2]
    cos = cos[:, None, :]
    sin = sin[:, None, :]
    return jnp.stack([t1 * cos - t2 * sin, t1 * sin + t2 * cos], axis=-1).reshape(t.shape)


def block_attention(q, k, v):
    scale = q.shape[-1] ** -0.5

    def one_block(qi):
        s = jnp.einsum('bqhd,bkhd->bhqk', qi, k).astype(jnp.float32) * scale
        p = jax.nn.softmax(s, axis=-1).astype(v.dtype)
        return jnp.einsum('bhqk,bkhd->bqhd', p, v)

    return from_chunks(lax.map(one_block, to_chunks(q, Q_BLOCK)))


def mla_mixer(u, u_ctx, cos, sin, q_a_norm, w_q_up, kv_a_norm, w_kv_up, q_norm, k_norm, need_ctx):
    def project(v, rotary):
        bn, nn = v.shape[:2]
        cq, ckv, kr = jnp.split(v, [MLA_Q_RANK, MLA_Q_RANK + MLA_KV_RANK], axis=-1)
        q = (rmsnorm(cq, q_a_norm) @ w_q_up).reshape(bn, nn, MLA_HEADS, MLA_NOPE + MLA_ROPE)
        kv = (rmsnorm(ckv, kv_a_norm) @ w_kv_up).reshape(bn, nn, MLA_HEADS, MLA_NOPE + MLA_V)
        q_nope = rmsnorm(q[..., :MLA_NOPE], q_norm[:MLA_NOPE])
        q_rope = rmsnorm(q[..., MLA_NOPE:], q_norm[MLA_NOPE:])
        k_nope = rmsnorm(kv[..., :MLA_NOPE], k_norm[:MLA_NOPE])
        k_rope = rmsnorm(kr, k_norm[MLA_NOPE:])[:, :, None, :]
        if rotary:
            q_rope = apply_rope(q_rope, cos, sin)
            k_rope = apply_rope(k_rope, cos, sin)
        k = jnp.concatenate([k_nope, jnp.broadcast_to(k_rope, k_nope.shape[:-1] + (MLA_ROPE,))], axis=-1)
        q = jnp.concatenate([q_nope, q_rope], axis=-1)
        return q, k, kv[..., MLA_NOPE:]

    q_l, k_l, v_l = project(u, True)
    q_c, k_c, v_c = project(u_ctx, False)
    k_all = jnp.concatenate([k_c, k_l], axis=1)
    v_all = jnp.concatenate([v_c, v_l], axis=1)
    o_l = block_attention(q_l, k_all, v_all)
    y_l = o_l.reshape(o_l.shape[0], o_l.shape[1], GROUP_W)
    if need_ctx:
        o_c = block_attention(q_c, k_c, v_c)
        return y_l, o_c.reshape(o_c.shape[0], o_c.shape[1], GROUP_W)
    return y_l, None


def expert_choice_ffn(h, w_router, w_gate, w_up, w_down):
    n, dm = h.shape[1], h.shape[2]
    cap = EC_FACTOR * n // N_EXPERTS
    aff = jax.nn.softmax(jnp.einsum('bnd,de->bne', h, w_router).astype(jnp.float32), axis=-1)
    g, idx = lax.top_k(aff.swapaxes(1, 2), cap)
    xs = jax.vmap(lambda hb, ib: hb[ib])(h, idx)
    act = jax.nn.silu(jnp.einsum('becd,edf->becf', xs, w_gate)) * jnp.einsum('becd,edf->becf', xs, w_up)
    ys = jnp.einsum('becf,efd->becd', act, w_down) * g[..., None].astype(h.dtype)

    def combine(ib, yb):
        return jnp.zeros((n, dm), yb.dtype).at[ib.reshape(-1)].add(yb.reshape(-1, dm))

    return jax.vmap(combine)(idx, ys)


def setup_inputs(seed: int = 0) -> dict:
    key = jax.random.key(seed)
    keys = iter(jax.random.split(key, 48))
    f32 = jnp.float32
    L = DEPTH

    def normal(shape, scale):
        return jax.random.normal(next(keys), shape, f32) * scale

    def gain(shape):
        return 1.0 + normal(shape, 0.02)

    a0 = jax.random.uniform(next(keys), (L, 2, GROUP_W), f32, 0.9, 0.999)
    dt0 = jnp.exp(jax.random.uniform(next(keys), (L, 2, SSD_HEADS), f32, math.log(1e-3), math.log(1e-1)))
    a_init = jax.random.uniform(next(keys), (L, 2, SSD_HEADS), f32, 1.0, 16.0)
    return {
        'x': normal((BATCH, SEQ, D_MODEL), 1.0),
        'c': normal((BATCH, D_MODEL), 1.0),
        'ctx': normal((BATCH, CTX_LEN, D_MODEL), 1.0),
        'c_ctx': normal((D_MODEL,), 1.0),
        'ada_w': normal((L, D_MODEL, N_MOD * D_MODEL), 0.5 * D_MODEL ** -0.5),
        'ada_b': normal((L, N_MOD * D_MODEL), 0.02),
        'norm1_w': gain((L, D_MODEL)),
        'norm2_w': gain((L, D_MODEL)),
        'w_in': normal((L, D_MODEL, IN_COLS), D_MODEL ** -0.5),
        'w_out': normal((L, D_MIX, D_MODEL), D_MIX ** -0.5),
        'lru_conv_w': normal((L, CONV_W, GROUP_W), CONV_W ** -0.5),
        'lru_conv_b': normal((L, GROUP_W), 0.02),
        'lru_w_r': normal((L, 2, LRU_HEADS, LRU_HD, LRU_HD), LRU_HD ** -0.5),
        'lru_b_r': normal((L, 2, GROUP_W), 0.02),
        'lru_w_i': normal((L, 2, LRU_HEADS, LRU_HD, LRU_HD), LRU_HD ** -0.5),
        'lru_b_i': normal((L, 2, GROUP_W), 0.02),
        'lru_lam': jnp.log(a0) - jnp.log1p(-a0),
        'hgrn_lb_logits': normal((L, GROUP_W), 0.5),
        'hgrn_norm_w': gain((L, GROUP_W)),
        'ssd_conv_w': normal((L, CONV_W, SSD_XBC), CONV_W ** -0.5),
        'ssd_conv_b': normal((L, SSD_XBC), 0.02),
        'ssd_a_log': jnp.log(a_init),
        'ssd_dt_bias': dt0 + jnp.log(-jnp.expm1(-dt0)),
        'ssd_d_skip': gain((L, SSD_HEADS)),
        'ssd_norm_w': gain((L, GROUP_W)),
        'mla_q_a_norm': gain((L, MLA_Q_RANK)),
        'mla_w_q_up': normal((L, MLA_Q_RANK, MLA_HEADS * (MLA_NOPE + MLA_ROPE)), MLA_Q_RANK ** -0.5),
        'mla_kv_a_norm': gain((L, MLA_KV_RANK)),
        'mla_w_kv_up': normal((L, MLA_KV_RANK, MLA_HEADS * (MLA_NOPE + MLA_V)), MLA_KV_RANK ** -0.5),
        'mla_q_norm': gain((L, MLA_NOPE + MLA_ROPE)),
        'mla_k_norm': gain((L, MLA_NOPE + MLA_ROPE)),
        'moe_router': normal((L, D_MODEL, N_EXPERTS), D_MODEL ** -0.5),
        'moe_w_gate': normal((L, N_EXPERTS, D_MODEL, EXPERT_FF), D_MODEL ** -0.5),
        'moe_w_up': normal((L, N_EXPERTS, D_MODEL, EXPERT_FF), D_MODEL ** -0.5),
        'moe_w_down': normal((L, N_EXPERTS, EXPERT_FF, D_MODEL), EXPERT_FF ** -0.5),
    }


def reference(x, c, ctx, c_ctx, ada_w, ada_b, norm1_w, norm2_w, w_in, w_out,
              lru_conv_w, lru_conv_b, lru_w_r, lru_b_r, lru_w_i, lru_b_i, lru_lam,
              hgrn_lb_logits, hgrn_norm_w,
              ssd_conv_w, ssd_conv_b, ssd_a_log, ssd_dt_bias, ssd_d_skip, ssd_norm_w,
              mla_q_a_norm, mla_w_q_up, mla_kv_a_norm, mla_w_kv_up, mla_q_norm, mla_k_norm,
              moe_router, moe_w_gate, moe_w_up, moe_w_down):
    cos, sin = axial_rope_tables(x.shape[1])
    lb_w = jax.nn.softmax(hgrn_lb_logits.astype(jnp.float32), axis=0)
    lb_all = jnp.cumsum(lb_w, axis=0) - lb_w[0]
    s_c = jax.nn.silu(c)
    s_cc = jax.nn.silu(c_ctx)
    for l in range(DEPTH):
        need_ctx = l < DEPTH - 1
        mod_l = jnp.split((s_c @ ada_w[l] + ada_b[l])[:, None, :], N_MOD, axis=-1)
        mod_c = jnp.split((s_cc @ ada_w[l] + ada_b[l])[None, None, :], N_MOD, axis=-1)
        h = modulate(rmsnorm(x, norm1_w[l]), mod_l[0], mod_l[1])
        hc = modulate(rmsnorm(ctx, norm1_w[l]), mod_c[0], mod_c[1])
        u_a, u_b, u_s, u_m = jnp.split(h @ w_in[l], SPLITS, axis=-1)
        c_a, c_b, c_s, c_m = jnp.split(hc @ w_in[l], SPLITS, axis=-1)
        ya, yca = rglru_mixer(u_a, c_a, lru_conv_w[l], lru_conv_b[l], lru_w_r[l], lru_b_r[l],
                              lru_w_i[l], lru_b_i[l], lru_lam[l], need_ctx)
        yb, ycb = hgrn2_mixer(u_b, c_b, lb_all[l], hgrn_norm_w[l], need_ctx)
        ys, ycs = ssd_mixer(u_s, c_s, ssd_conv_w[l], ssd_conv_b[l], ssd_a_log[l], ssd_dt_bias[l],
                            ssd_d_skip[l], ssd_norm_w[l], need_ctx)
        ym, ycm = mla_mixer(u_m, c_m, cos, sin, mla_q_a_norm[l], mla_w_q_up[l], mla_kv_a_norm[l],
                            mla_w_kv_up[l], mla_q_norm[l], mla_k_norm[l], need_ctx)
        x = x + mod_l[2] * (jnp.concatenate([ya, yb, ys, ym], axis=-1) @ w_out[l])
        h = modulate(rmsnorm(x, norm2_w[l]), mod_l[3], mod_l[4])
        x = x + mod_l[5] * expert_choice_ffn(h, moe_router[l], moe_w_gate[l], moe_w_up[l], moe_w_down[l])
        if need_ctx:
            ctx = ctx + mod_c[2] * (jnp.concatenate([yca, ycb, ycs, ycm], axis=-1) @ w_out[l])
            hc = modulate(rmsnorm(ctx, norm2_w[l]), mod_c[3], mod_c[4])
            ctx = ctx + mod_c[5] * expert_choice_ffn(hc, moe_router[l], moe_w_gate[l], moe_w_up[l], moe_w_down[l])
    return x
```

```python
import math
from contextlib import ExitStack
import numpy as np
import ml_dtypes
import concourse.bass as bass
import concourse.mybir as mybir
from concourse.bass_utils import run_bass_kernel_spmd

F32 = mybir.dt.float32
BF16 = mybir.dt.bfloat16
I32 = mybir.dt.int32
U32 = mybir.dt.uint32
ALU = mybir.AluOpType
AF = mybir.ActivationFunctionType
AX = mybir.AxisListType

L = 2
D = 1024
NLAT = 2048
NCTX = 256
S = NCTX + NLAT
NT = S // 128
IN_COLS = 2920
EPS = 1e-6
NEXP = 16
FF = 512
TM_RANGES = [(1280, 1536), (1792, 2048), (2560, 2920)]
TM_COLS = sum(b - a for a, b in TM_RANGES)
FM_CHUNKS = list(range(0, 10)) + [12, 13] + [16, 17, 18, 19]
BLOCKS = [(0, 256)] + [(256 + 512 * i, 512) for i in range(4)]


class T:
    __slots__ = ("name", "w", "r", "x")

    def __init__(self, name="", x=False):
        self.name = name
        self.w = None
        self.r = []
        self.x = x


def PT():
    return T("psum", True)


class K:
    N_DMA_SEMS = 24

    def __init__(self, nc, stack):
        self.nc = nc
        self.stack = stack
        self.eng = {"pe": nc.tensor, "dve": nc.vector, "act": nc.scalar,
                    "pool": nc.gpsimd, "sp": nc.sync}
        self.sems = {}
        self.count = {}
        self.seen = {e: {} for e in self.eng}
        for e in self.eng:
            self.sems[e] = stack.enter_context(nc.semaphore("s_" + e))
            self.count[e] = 0
        for i in range(self.N_DMA_SEMS):
            k = "d%d" % i
            self.sems[k] = stack.enter_context(nc.semaphore("s_" + k))
            self.count[k] = 0
        self.dma_rr = 0
        self.n_inst = 0
        self.scope = stack

    def sb(self, name, shape, dtype=F32):
        self.n_alloc = getattr(self, "n_alloc", 0) + 1
        return self.scope.enter_context(self.nc.sbuf_tensor("sb%d_%s" % (self.n_alloc, name), list(shape), dtype))

    def ps(self, name, shape, dtype=F32):
        self.n_alloc = getattr(self, "n_alloc", 0) + 1
        nel = 512 if dtype == F32 else 1024
        scope = getattr(self, "pscope", None) or self.scope
        full = scope.enter_context(self.nc.psum_tensor("ps%d_%s" % (self.n_alloc, name), [128, nel], dtype))
        n = 1
        for d_ in shape[1:]:
            n *= d_
        assert n <= nel, (name, shape)
        v = full[0:shape[0], 0:n]
        if len(shape) == 3:
            v = v.rearrange("p (a b) -> p a b", b=shape[2])
        elif len(shape) == 4:
            v = v.rearrange("p (a b c) -> p a b c", b=shape[2], c=shape[3])
        return v

    def _waits(self, e, reads, writes):
        need = {}
        for t in reads:
            if t.w is not None:
                k, v, pe = t.w
                if not (pe == "pe" and e == "pe"):
                    need[k] = max(need.get(k, 0), v)
        for t in writes:
            if t.w is not None:
                k, v, pe = t.w
                if not (pe == "pe" and e == "pe"):
                    need[k] = max(need.get(k, 0), v)
            for (k, v, pe) in t.r:
                if pe == "pe" and e == "pe":
                    continue
                need[k] = max(need.get(k, 0), v)
        seen = self.seen[e]
        h = self.eng[e]
        for k, v in need.items():
            if seen.get(k, 0) < v:
                h.wait_ge(self.sems[k], v)
                seen[k] = v

    def _commit(self, tok, reads, writes):
        for t in writes:
            t.w = tok
            t.r = []
        for t in reads:
            if t not in writes:
                t.r.append(tok)
                if len(t.r) > 16:
                    best = {}
                    for (k, v, pe) in t.r:
                        if k not in best or best[k][1] < v:
                            best[k] = (k, v, pe)
                    t.r = list(best.values())

    def op(self, e, fn, reads=(), writes=()):
        reads = list(reads)
        writes = list(writes)
        xr = [t for t in reads if t.x]
        if xr:
            reads = [t for t in reads if not t.x]
            writes = writes + [t for t in xr if t not in writes]
        self._waits(e, reads, writes)
        ins = fn(self.eng[e])
        self.count[e] += 1
        ins.then_inc(self.sems[e], 1)
        self._commit((e, self.count[e], e), reads, writes)
        self.n_inst += 1
        return ins

    def dma(self, e, out, in_, reads=(), writes=(), **kw):
        reads = list(reads)
        writes = list(writes)
        self._waits(e, reads, writes)
        k = "d%d" % self.dma_rr
        self.dma_rr = (self.dma_rr + 1) % self.N_DMA_SEMS
        ins = self.eng[e].dma_start(out=out, in_=in_, **kw)
        self.count[k] += 16
        ins.then_inc(self.sems[k], 16)
        self._commit((k, self.count[k], "dma"), reads, writes)
        self.n_inst += 1
        return ins

    def barrier(self):
        for e, h in self.eng.items():
            seen = self.seen[e]
            for k, v in self.count.items():
                if v > 0 and seen.get(k, 0) < v and k != e:
                    h.wait_ge(self.sems[k], v)
                    seen[k] = v


class PScope:
    def __init__(self, k):
        self.k = k

    def __enter__(self):
        self.prev = getattr(self.k, "pscope", None)
        self.st = ExitStack()
        self.st.__enter__()
        self.k.pscope = self.st
        return self

    def __exit__(self, *a):
        self.k.barrier()
        self.k.pscope = self.prev
        return self.st.__exit__(*a)


class Caster:
    def __init__(self, k, npart, nfree, nbuf=2, eng="act"):
        self.k = k
        self.eng = eng
        self.st = [k.sb("stg%d" % i, [npart, nfree]) for i in range(nbuf)]
        self.T = [T() for _ in range(nbuf)]
        self.i = 0

    def load(self, dst, src, npart, nfree, writes, reads=()):
        k = self.k
        j = self.i % len(self.st)
        self.i += 1
        st = self.st[j][0:npart, 0:nfree]
        k.dma("sp", st, src, reads=list(reads), writes=[self.T[j]])
        if self.eng == "act":
            k.op("act", lambda e: e.activation(out=dst, in_=st, func=AF.Copy), reads=[self.T[j]], writes=list(writes))
        else:
            k.op(self.eng, lambda e: e.tensor_copy(out=dst, in_=st), reads=[self.T[j]], writes=list(writes))


class Stage:
    def __init__(self, k):
        self.k = k

    def __enter__(self):
        self.prev = self.k.scope
        self.st = ExitStack()
        self.st.__enter__()
        self.k.scope = self.st
        return self

    def __exit__(self, *a):
        self.k.barrier()
        self.k.scope = self.prev
        return self.st.__exit__(*a)


def fm(v, nch):
    v = np.asarray(v, np.float32)
    lead = v.shape[:-1]
    r = v.reshape(lead + (nch, 128))
    r = np.moveaxis(r, -1, 0)
    return np.ascontiguousarray(r)


def host_consts():
    c = {}
    c["ident_f"] = np.eye(128, dtype=np.float32)
    c["ident_b"] = np.eye(128, dtype=np.float32).astype(ml_dtypes.bfloat16)
    c["ones_b"] = np.ones((128, 128), np.float32).astype(ml_dtypes.bfloat16)
    c["ones_f"] = np.ones((128, 128), np.float32)
    t = np.arange(S)
    c["mfwd"] = np.ascontiguousarray(np.broadcast_to((t % 128 != 0).astype(np.float32), (128, S)))
    c["mbwd"] = np.ascontiguousarray(np.broadcast_to((t % 128 != 127).astype(np.float32), (128, S)))
    i = np.arange(128)
    c["triU"] = (i[:, None] <= i[None, :]).astype(np.uint32)
    c["triL"] = (i[:, None] >= i[None, :]).astype(np.uint32)
    c["triUf"] = (i[:, None] <= i[None, :]).astype(np.float32)
    c["triLf"] = (i[:, None] >= i[None, :]).astype(np.float32)
    c["strLf"] = (i[:, None] > i[None, :]).astype(np.float32)
    c["strUf"] = (i[:, None] < i[None, :]).astype(np.float32)
    tt = np.arange(NLAT)
    inv = 10000.0 ** (-np.arange(0, 16, 2, dtype=np.float32) / 16)
    ang = np.concatenate([(tt // 64).astype(np.float32)[:, None] * inv, (tt % 64).astype(np.float32)[:, None] * inv], axis=-1).astype(np.float32)
    cs = np.stack([np.cos(ang), np.sin(ang)], axis=1).astype(np.float32)
    c["rope"] = np.ascontiguousarray(cs.reshape(16, 128, 2, 16).transpose(1, 0, 2, 3))
    c["invn3"] = np.ascontiguousarray(np.broadcast_to(np.array([1 / 192, 1 / 128, 1 / 32], np.float32), (128, 3)))
    c["invn8"] = np.ascontiguousarray(np.broadcast_to(np.array([1 / 64] * 4 + [1 / 32] * 4, np.float32), (128, 8)))
    c["iotaf"] = np.ascontiguousarray(np.broadcast_to(np.arange(256, dtype=np.float32), (128, 256)))
    c["iotap"] = np.stack([i.astype(np.float32), i.astype(np.float32) + 128], axis=1)
    oh = np.zeros((16, 16, 128), np.float32)
    for e_ in range(16):
        oh[e_, e_, :] = 1.0
    c["oneh"] = oh
    c["ones16"] = np.ones((16, NLAT), np.float32)
    rep4 = lambda a_: np.ascontiguousarray(np.broadcast_to(a_[:, None, :], (128, 4, 128))).astype(np.float32)
    c["triUf4"] = rep4(c["triUf"]); c["triLf4"] = rep4(c["triLf"])
    c["negmf"] = rep4(-1.0e4 * c["strLf"]); c["negmb"] = rep4(-1.0e4 * c["strUf"])
    c["blk64"] = (i[:, None] // 64 == i[None, :] // 64).astype(np.float32).astype(ml_dtypes.bfloat16)
    return c


def prep_lru(inp):
    m = {}
    cw = np.asarray(inp["lru_conv_w"], np.float32)
    m["lru_cw"] = np.ascontiguousarray(cw.reshape(L, 4, 2, 128).transpose(3, 0, 2, 1))
    m["lru_cb"] = fm(inp["lru_conv_b"], 2)
    for nm, key in (("lru_wr", "lru_w_r"), ("lru_wi", "lru_w_i")):
        w = np.asarray(inp[key], np.float32)
        o = np.zeros((128, L, 2, 2, 128), np.float32)
        for ch in range(2):
            for hh in range(2):
                o[hh * 64:(hh + 1) * 64, :, :, ch, hh * 64:(hh + 1) * 64] = w[:, :, 2 * ch + hh].transpose(2, 0, 1, 3)
        m[nm] = o
    m["lru_br"] = fm(inp["lru_b_r"], 2)
    m["lru_bi"] = fm(inp["lru_b_i"], 2)
    m["lru_lam"] = fm(inp["lru_lam"], 2)
    return m


class Cfg:
    def __init__(self, nb=2, upto=99, debug=False, layers=2):
        self.layers = layers
        self.nb = nb
        self.upto = upto
        self.debug = debug


def build(cfg):
    nc = bass.Bass("TRN2", target_bir_lowering=False)
    NB = cfg.nb
    dbg_kind = "ExternalOutput" if cfg.debug else "Internal"

    def din(name, shape, dt=F32):
        return nc.dram_tensor(name, list(shape), dt, kind="ExternalInput").ap()

    def dscr(name, shape, dt=F32):
        return nc.dram_tensor(name, list(shape), dt, kind=dbg_kind).ap()

    x_d = din("x", [NB, NLAT, D])
    ctx_d = din("ctx", [NB, NCTX, D])
    cT_d = din("cT", [128, 8, 3])
    ada_w_d = din("ada_w", [L, D, 6 * D])
    ada_bT_d = din("ada_bT", [128, L, 48])
    n1T_d = din("n1T", [128, L, 8])
    n2T_d = din("n2T", [128, L, 8])
    w_in_d = din("w_in", [L, D, IN_COLS])
    lru_cw_d = din("lru_cw", [128, L, 2, 4])
    lru_cb_d = din("lru_cb", [128, L, 2])
    lru_wr_d = din("lru_wr", [128, L, 2, 2, 128])
    lru_wi_d = din("lru_wi", [128, L, 2, 2, 128])
    lru_br_d = din("lru_br", [128, L, 2, 2])
    lru_bi_d = din("lru_bi", [128, L, 2, 2])
    lru_lam_d = din("lru_lam", [128, L, 2, 2])
    hg_lbT_d = din("hg_lbT", [128, L, 2])
    hg_nwT_d = din("hg_nwT", [128, L, 2])
    mfwd_d = din("mfwd", [128, S])
    mbwd_d = din("mbwd", [128, S])
    triU_d = din("triU", [128, 128], U32)
    triL_d = din("triL", [128, 128], U32)
    blk64_d = din("blk64", [128, 128], BF16)
    sd_cw_d = din("sd_cw", [128, L, 4, 4])
    sd_cb_d = din("sd_cb", [128, L, 4])
    sd_alog_d = din("sd_alog", [128, L, 8])
    sd_dtb_d = din("sd_dtb", [128, L, 8])
    sd_dsk_d = din("sd_dsk", [128, L, 256])
    sd_nw_d = din("sd_nw", [128, L, 256])
    triUf_d = din("triUf", [128, 128])
    triLf_d = din("triLf", [128, 128])
    strLf_d = din("strLf", [128, 128])
    strUf_d = din("strUf", [128, 128])
    ml_qan_d = din("ml_qan", [128, L, 192])
    ml_kvan_d = din("ml_kvan", [128, L, 128])
    ml_qn_d = din("ml_qn", [128, L, 96])
    ml_kn_d = din("ml_kn", [128, L, 96])
    ml_wq_d = din("ml_wq", [L, 192, 384])
    ml_wkv_d = din("ml_wkv", [L, 128, 512])
    rope_d = din("rope", [128, 16, 2, 16])
    invn3_d = din("invn3", [128, 3])
    invn8_d = din("invn8", [128, 8])
    w_out_d = din("w_out", [L, D, D])
    w_rt_d = din("w_rt", [L, D, NEXP])
    w_gate_d = din("w_gate", [L, NEXP, D, FF])
    w_up_d = din("w_up", [L, NEXP, D, FF])
    w_down_d = din("w_down", [L, NEXP, FF, D])
    iotaf_d = din("iotaf", [128, 256])
    iotap_d = din("iotap", [128, 2])
    oneh_d = din("oneh", [16, 16, 128])
    ones16_d = din("ones16", [16, NLAT])
    triUf4_d = din("triUf4", [128, 4, 128])
    triLf4_d = din("triLf4", [128, 4, 128])
    negmf_d = din("negmf", [128, 4, 128])
    negmb_d = din("negmb", [128, 4, 128])
    ident_f_d = din("ident_f", [128, 128])
    ident_b_d = din("ident_b", [128, 128], BF16)
    ones_b_d = din("ones_b", [128, 128], BF16)
    ones_f_d = din("ones_f", [128, 128])
    out_d = nc.dram_tensor("out", [NB, NLAT, D], F32, kind="ExternalOutput").ap()

    xT_d = dscr("xT", [NB, D, S])
    uT_d = dscr("uT", [NB, IN_COLS, S])
    ut_d = dscr("ut", [NB, S, TM_COLS])
    yT_d = dscr("yT", [NB, D, S], BF16)
    TyT = [T() for b in range(NB)]
    h2t_d = dscr("h2t", [NB, S, D], BF16)
    aff_d = dscr("aff", [NB, NEXP, S])
    Th2 = [T() for b in range(NB)]
    Taff = [T() for b in range(NB)]
    Tx = [T("xT%d" % b) for b in range(NB)]
    TuT = [T() for b in range(NB)]
    Tut = [T() for b in range(NB)]
    Tout = T("out")

    with ExitStack() as root:
        k = K(nc, root)
        ident_f = k.sb("ident_f", [128, 128]); ident_b = k.sb("ident_b", [128, 128], BF16)
        ones_b = k.sb("ones_b", [128, 128], BF16); ones_f = k.sb("ones_f", [128, 128])
        modT = k.sb("modT", [128, L, 48, 3])
        n1T = k.sb("n1T", [128, L, 8]); n2T = k.sb("n2T", [128, L, 8])
        Tc = T("consts")
        Tmod = T("mod")
        k.dma("sp", ident_f[:], ident_f_d, writes=[Tc])
        k.dma("sp", ident_b[:], ident_b_d, writes=[Tc])
        k.dma("sp", ones_b[:], ones_b_d, writes=[Tc])
        k.dma("sp", ones_f[:], ones_f_d, writes=[Tc])
        k.dma("sp", n1T[:], n1T_d, writes=[Tc])
        k.dma("sp", n2T[:], n2T_d, writes=[Tc])

        with Stage(k):
            cT = k.sb("cT", [128, 8, 3]); sT = k.sb("sT", [128, 8, 3])
            abT = k.sb("abT", [128, L, 48])
            Tct = T(); Tst = T()
            k.dma("sp", cT[:], cT_d, writes=[Tct])
            k.dma("sp", abT[:], ada_bT_d, writes=[Tct])
            k.op("act", lambda e: e.activation(out=sT[:], in_=cT[:], func=AF.Silu), reads=[Tct], writes=[Tst])
            wbuf = [k.sb("adaw%d" % i, [128, 8, 512]) for i in range(2)]
            Tw = [T(), T()]
            pm = [k.ps("pm%d" % i, [128, 4, 4]) for i in range(2)]
            Tpm = [PT(), PT()]
            it = 0
            for l in range(L):
                for j in range(12):
                    wb = wbuf[it % 2]; tw = Tw[it % 2]
                    k.dma("sp", wb[:], ada_w_d[l, :, j * 512:(j + 1) * 512].rearrange("(kc p) n -> p kc n", p=128), writes=[tw])
                    pp = pm[it % 2]; tp = Tpm[it % 2]
                    for sub in range(4):
                        for kc in range(8):
                            k.op("pe", lambda e: e.matmul(pp[:, sub, 0:3], lhsT=wb[:, kc, sub * 128:(sub + 1) * 128],
                                                          rhs=sT[:, kc, :], start=(kc == 0), stop=(kc == 7)),
                                 reads=[tw, Tst], writes=[tp])
                    for sub in range(4):
                        ch = j * 4 + sub
                        k.op("dve", lambda e: e.tensor_scalar(out=modT[:, l, ch, :], in0=pp[:, sub, 0:3],
                                                              scalar1=abT[:, l, ch:ch + 1], scalar2=None, op0=ALU.add),
                             reads=[tp, Tct], writes=[Tmod])
                    it += 1

        with Stage(k):
            xin = [k.sb("xin%d" % i, [128, D]) for i in range(3)]
            Txin = [T() for _ in range(3)]
            xo = [k.sb("xo%d" % i, [128, 8, 128]) for i in range(3)]
            Txo = [T() for _ in range(3)]
            pt = [k.ps("pt%d" % i, [128, 4, 128]) for i in range(4)]
            Tpt = [PT() for _ in range(4)]
            it = 0
            for b in range(NB):
                for ti in range(NT):
                    src = ctx_d[b, ti * 128:(ti + 1) * 128, :] if ti < 2 else x_d[b, (ti - 2) * 128:(ti - 1) * 128, :]
                    xi = xin[it % 3]; txi = Txin[it % 3]
                    k.dma("sp", xi[:], src, writes=[txi])
                    xx = xo[it % 3]; txo = Txo[it % 3]
                    for half in range(2):
                        pp = pt[(2 * it + half) % 4]; tp = Tpt[(2 * it + half) % 4]
                        for q in range(4):
                            kc = half * 4 + q
                            k.op("pe", lambda e: e.transpose(pp[:, q, :], xi[:, kc * 128:(kc + 1) * 128], ident_f[:]),
                                 reads=[txi, Tc], writes=[tp])
                        if half == 0:
                            k.op("act", lambda e: e.activation(out=xx[:, 0:4, :], in_=pp[:], func=AF.Copy), reads=[tp], writes=[txo])
                        else:
                            k.op("dve", lambda e: e.tensor_copy(out=xx[:, 4:8, :], in_=pp[:]), reads=[tp], writes=[txo])
                    k.dma("sp", xT_d[b, :, ti * 128:(ti + 1) * 128].rearrange("(kc p) t -> p kc t", p=128), xx[:],
                          reads=[txo], writes=[Tx[b]])
                    it += 1

        for l in range(cfg.layers):
            if cfg.upto < 1:
                break
            for b in range(NB):
                with Stage(k):
                    w_in = k.sb("w_in", [128, 8, IN_COLS], BF16); Tw = T()
                    cst = Caster(k, 128, IN_COLS)
                    for kc in range(8):
                        cst.load(w_in[:, kc, :], w_in_d[l, kc * 128:(kc + 1) * 128, :], 128, IN_COLS, [Tw])
                    G = k.sb("G", [128, 2, 8]); Tg = T()
                    for i, mi in enumerate((b, 2)):
                        k.op("dve", lambda e: e.scalar_tensor_tensor(out=G[:, i, :], in0=modT[:, l, 8:16, mi], scalar=1.0,
                                                                    in1=n1T[:, l, :], op0=ALU.add, op1=ALU.mult),
                             reads=[Tmod, Tc], writes=[Tg])
                    xb = [k.sb("xb%d" % i, [128, 8, 512]) for i in range(2)]; Txb = [T(), T()]
                    sq = [k.sb("sq%d" % i, [128, 8, 512], BF16) for i in range(2)]; Tsq = [T(), T()]
                    rs = [k.sb("rs%d" % i, [128, 512]) for i in range(2)]; Trs = [T(), T()]
                    tmp = [k.sb("tmp%d" % i, [128, 512]) for i in range(2)]; Ttmp = [T(), T()]
                    hT = [k.sb("hT%d" % i, [128, 8, 512], BF16) for i in range(2)]; ThT = [T(), T()]
                    ev = [k.sb("ev%d" % i, [128, 512]) for i in range(4)]; Tev = [T() for _ in range(4)]
                    evt = [k.sb("evt%d" % i, [128, TM_COLS]) for i in range(2)]; Tevt = [T(), T()]
                    pss = k.ps("pss", [128, 512]); Tpss = PT()
                    pu = [k.ps("pu%d" % i, [128, 512]) for i in range(4)]; Tpu = [PT() for _ in range(4)]
                    pv = [k.ps("pv%d" % i, [128, 512]) for i in range(3)]; Tpv = [PT() for _ in range(3)]
                    nev = 0
                    for bi, (t0, n) in enumerate(BLOCKS):
                        seg = 1 if bi == 0 else 0
                        mi = 2 if bi == 0 else b
                        X = xb[bi % 2]; tX = Txb[bi % 2]
                        k.dma("sp", X[:, :, 0:n], xT_d[b, :, t0:t0 + n].rearrange("(kc p) t -> p kc t", p=128),
                              reads=[Tx[b]], writes=[tX])
                        Q = sq[bi % 2]; tQ = Tsq[bi % 2]
                        k.op("act", lambda e: e.activation(out=Q[:, :, 0:n], in_=X[:, :, 0:n], func=AF.Square), reads=[tX], writes=[tQ])
                        for kc in range(8):
                            k.op("pe", lambda e: e.matmul(pss[:, 0:n], lhsT=ones_b[:], rhs=Q[:, kc, 0:n], start=(kc == 0), stop=(kc == 7)),
                                 reads=[tQ, Tc], writes=[Tpss])
                        R = rs[bi % 2]; tR = Trs[bi % 2]
                        k.op("act", lambda e: e.activation(out=R[:, 0:n], in_=pss[:, 0:n], func=AF.Sqrt, scale=1.0 / D, bias=EPS),
                             reads=[Tpss], writes=[tR])
                        k.op("dve", lambda e: e.reciprocal(out=R[:, 0:n], in_=R[:, 0:n]), reads=[tR], writes=[tR])
                        H = hT[bi % 2]; tH = ThT[bi % 2]
                        for kc in range(8):
                            tm = tmp[kc % 2]; ttm = Ttmp[kc % 2]
                            k.op("dve", lambda e: e.tensor_tensor(out=tm[:, 0:n], in0=X[:, kc, 0:n], in1=R[:, 0:n], op=ALU.mult),
                                 reads=[tX, tR], writes=[ttm])
                            k.op("act", lambda e: e.activation(out=H[:, kc, 0:n], in_=tm[:, 0:n], func=AF.Identity,
                                                               scale=G[:, seg, kc:kc + 1], bias=modT[:, l, kc, mi:mi + 1]),
                                 reads=[ttm, Tg, Tmod], writes=[tH])
                        for ci, ch in enumerate(FM_CHUNKS):
                            c0 = ch * 128
                            pp = pu[ci % 4]; tp = Tpu[ci % 4]
                            for kc in range(8):
                                k.op("pe", lambda e: e.matmul(pp[:, 0:n], lhsT=w_in[:, kc, c0:c0 + 128], rhs=H[:, kc, 0:n],
                                                              start=(kc == 0), stop=(kc == 7)), reads=[Tw, tH], writes=[tp])
                            E = ev[nev % 4]; tE = Tev[nev % 4]
                            if nev % 2 == 0:
                                k.op("act", lambda e: e.activation(out=E[:, 0:n], in_=pp[:, 0:n], func=AF.Copy), reads=[tp], writes=[tE])
                            else:
                                k.op("dve", lambda e: e.tensor_copy(out=E[:, 0:n], in_=pp[:, 0:n]), reads=[tp], writes=[tE])
                            k.dma("sp", uT_d[b, c0:c0 + 128, t0:t0 + n], E[:, 0:n], reads=[tE], writes=[TuT[b]])
                            nev += 1
                        for tt in range(n // 128):
                            ET = evt[tt % 2]; tET = Tevt[tt % 2]
                            off = 0
                            for ri, (a, bnd) in enumerate(TM_RANGES):
                                w = bnd - a
                                pp = pv[ri]; tp = Tpv[ri]
                                for kc in range(8):
                                    k.op("pe", lambda e: e.matmul(pp[:, 0:w], lhsT=H[:, kc, tt * 128:(tt + 1) * 128], rhs=w_in[:, kc, a:bnd],
                                                                  start=(kc == 0), stop=(kc == 7)), reads=[Tw, tH], writes=[tp])
                                if ri == 1:
                                    k.op("act", lambda e: e.activation(out=ET[:, off:off + w], in_=pp[:, 0:w], func=AF.Copy), reads=[tp], writes=[tET])
                                else:
                                    k.op("dve", lambda e: e.tensor_copy(out=ET[:, off:off + w], in_=pp[:, 0:w]), reads=[tp], writes=[tET])
                                off += w
                            k.dma("sp", ut_d[b, t0 + tt * 128:t0 + (tt + 1) * 128, :], ET[:], reads=[tET], writes=[Tut[b]])
                if cfg.upto < 2:
                    continue
                with Stage(k):
                    cw = k.sb("cw", [128, 2, 4]); cb = k.sb("cb", [128, 2])
                    wr = k.sb("wr", [128, 2, 2, 128], BF16); wi = k.sb("wi", [128, 2, 2, 128], BF16)
                    br = k.sb("br", [128, 2, 2]); bi_ = k.sb("bi", [128, 2, 2]); lam = k.sb("lam", [128, 2, 2])
                    cl = k.sb("cl", [128, 2, 2]); cl2 = k.sb("cl2", [128, 2, 2])
                    Tp2 = T()
                    k.dma("sp", cw[:], lru_cw_d[:, l], writes=[Tp2]); k.dma("sp", cb[:], lru_cb_d[:, l], writes=[Tp2])
                    cst = Caster(k, 128, 512)
                    cst.load(wr[:].rearrange("p a b c -> p (a b c)"), lru_wr_d[:, l].rearrange("p a b c -> p (a b c)"), 128, 512, [Tp2])
                    cst.load(wi[:].rearrange("p a b c -> p (a b c)"), lru_wi_d[:, l].rearrange("p a b c -> p (a b c)"), 128, 512, [Tp2])
                    k.dma("sp", br[:], lru_br_d[:, l], writes=[Tp2]); k.dma("sp", bi_[:], lru_bi_d[:, l], writes=[Tp2])
                    k.dma("sp", lam[:], lru_lam_d[:, l], writes=[Tp2])
                    k.op("act", lambda e: e.activation(out=cl[:], in_=lam[:], func=AF.Exp, scale=-1.0), reads=[Tp2], writes=[Tp2])
                    k.op("act", lambda e: e.activation(out=cl[:], in_=cl[:], func=AF.Ln, bias=1.0), reads=[Tp2], writes=[Tp2])
                    k.op("dve", lambda e: e.tensor_scalar(out=cl2[:], in0=cl[:], scalar1=-16.0, scalar2=None, op0=ALU.mult), reads=[Tp2], writes=[Tp2])
                    k.op("dve", lambda e: e.tensor_scalar(out=cl[:], in0=cl[:], scalar1=-8.0, scalar2=None, op0=ALU.mult), reads=[Tp2], writes=[Tp2])
                    xp = k.sb("xp", [128, S + 6]); Txp = T()
                    xc = k.sb("xc", [128, S]); Txc = T()
                    xcb = k.sb("xcb", [128, S], BF16); Txcb = T()
                    gt = k.sb("gt", [128, S]); Tgt = T()
                    Rr = k.sb("Rr", [128, S]); TR = T()
                    Ii = k.sb("Ii", [128, S]); TI = T()
                    Aa = k.sb("Aa", [128, S]); TA = T()
                    Bb = k.sb("Bb", [128, S]); TB = T()
                    Hh = [k.sb("Hh%d" % i, [128, S]) for i in range(2)]; TH = [T(), T()]
                    yo = k.sb("yo", [128, S], BF16); Tyo = T()
                    pg = [k.ps("pg%d" % i, [128, 512]) for i in range(4)]; Tpg = [PT() for _ in range(4)]
                    npg = 0
                    segs = [(0, NCTX, 2), (NCTX, NLAT, NCTX + 5)]
                    for ch in range(2):
                        k.op("pool", lambda e: e.memset(xp[:], 0.0), writes=[Txp])
                        for (t0, n, o) in segs:
                            k.dma("sp", xp[:, o:o + n], uT_d[b, ch * 128:(ch + 1) * 128, t0:t0 + n], reads=[TuT[b]], writes=[Txp])
                        k.dma("sp", gt[:], uT_d[b, 256 + ch * 128:256 + (ch + 1) * 128, :], reads=[TuT[b]], writes=[Tgt])
                        for (t0, n, o) in segs:
                            k.op("dve", lambda e: e.tensor_scalar(out=xc[:, t0:t0 + n], in0=xp[:, o - 2:o - 2 + n], scalar1=cw[:, ch, 0:1],
                                                                  scalar2=cb[:, ch:ch + 1], op0=ALU.mult, op1=ALU.add),
                                 reads=[Txp, Tp2], writes=[Txc])
                            for j in range(1, 4):
                                k.op("dve", lambda e: e.scalar_tensor_tensor(out=xc[:, t0:t0 + n], in0=xp[:, o - 2 + j:o - 2 + j + n],
                                                                            scalar=cw[:, ch, j:j + 1], in1=xc[:, t0:t0 + n],
                                                                            op0=ALU.mult, op1=ALU.add),
                                     reads=[Txp, Tp2, Txc], writes=[Txc])
                        k.op("pool", lambda e: e.tensor_copy(out=xcb[:], in_=xc[:]), reads=[Txc], writes=[Txcb])
                        for d in range(2):
                            for (t0, n) in BLOCKS:
                                for (W, bias, dst, tdst) in ((wr, br, Rr, TR), (wi, bi_, Ii, TI)):
                                    pp = pg[npg % 4]; tp = Tpg[npg % 4]; npg += 1
                                    k.op("pe", lambda e: e.matmul(pp[:, 0:n], lhsT=W[:, d, ch, :], rhs=xcb[:, t0:t0 + n], start=True, stop=True),
                                         reads=[Tp2, Txcb], writes=[tp])
                                    k.op("act", lambda e: e.activation(out=dst[:, t0:t0 + n], in_=pp[:, 0:n], func=AF.Sigmoid,
                                                                       bias=bias[:, d, ch:ch + 1]), reads=[tp, Tp2], writes=[tdst])
                            k.op("act", lambda e: e.activation(out=Aa[:], in_=Rr[:], func=AF.Exp, scale=cl[:, d, ch:ch + 1]),
                                 reads=[TR, Tp2], writes=[TA])
                            k.op("act", lambda e: e.activation(out=Bb[:], in_=Rr[:], func=AF.Exp, scale=cl2[:, d, ch:ch + 1]),
                                 reads=[TR, Tp2], writes=[TB])
                            k.op("act", lambda e: e.activation(out=Bb[:], in_=Bb[:], func=AF.Sqrt, scale=-1.0, bias=1.0), reads=[TB], writes=[TB])
                            k.op("dve", lambda e: e.tensor_tensor(out=Ii[:], in0=Ii[:], in1=xc[:], op=ALU.mult), reads=[TI, Txc], writes=[TI])
                            k.op("dve", lambda e: e.tensor_tensor(out=Bb[:], in0=Bb[:], in1=Ii[:], op=ALU.mult), reads=[TB, TI], writes=[TB])
                            H = Hh[d]
                            if d == 0:
                                k.op("dve", lambda e: e.tensor_tensor_scan(out=H[:], data0=Aa[:], data1=Bb[:], initial=0.0,
                                                                          op0=ALU.mult, op1=ALU.add), reads=[TA, TB], writes=[TH[d]])
                            else:
                                k.op("dve", lambda e: e.tensor_tensor_scan(out=H[:, NCTX - 1::-1], data0=Aa[:, NCTX - 1::-1], data1=Bb[:, NCTX - 1::-1],
                                                                          initial=0.0, op0=ALU.mult, op1=ALU.add), reads=[TA, TB], writes=[TH[d]])
                                k.op("dve", lambda e: e.tensor_tensor_scan(out=H[:, S - 1:NCTX - 1:-1], data0=Aa[:, S - 1:NCTX - 1:-1],
                                                                          data1=Bb[:, S - 1:NCTX - 1:-1], initial=H[:, 0:1],
                                                                          op0=ALU.mult, op1=ALU.add), reads=[TA, TB, TH[d]], writes=[TH[d]])
                        k.op("act", lambda e: e.activation(out=gt[:], in_=gt[:], func=AF.Gelu_apprx_tanh), reads=[Tgt], writes=[Tgt])
                        k.op("dve", lambda e: e.tensor_tensor(out=Hh[0][:], in0=Hh[0][:], in1=Hh[1][:], op=ALU.add), reads=TH, writes=[TH[0]])
                        k.op("dve", lambda e: e.tensor_tensor(out=yo[:], in0=Hh[0][:], in1=gt[:], op=ALU.mult), reads=[TH[0], Tgt], writes=[Tyo])
                        k.dma("sp", yT_d[b, ch * 128:(ch + 1) * 128, :], yo[:], reads=[Tyo], writes=[TyT[b]])
                if cfg.upto < 3:
                    continue
                with Stage(k):
                    lbz = k.sb("lbz", [128, L, 2]); lbe = k.sb("lbe", [128, L, 2]); lbs = k.sb("lbs", [128, 2]); lb = k.sb("lb", [128, 2])
                    oml = k.sb("oml", [128, 2]); hnw = k.sb("hnw", [128, 2]); Tp3 = T()
                    mf = k.sb("mf", [128, S]); mb = k.sb("mb", [128, S]); triU = k.sb("triU", [128, 128], U32); triL = k.sb("triL", [128, 128], U32)
                    blk = k.sb("blk", [128, 128], BF16)
                    k.dma("sp", lbz[:], hg_lbT_d, writes=[Tp3]); k.dma("sp", hnw[:], hg_nwT_d[:, l], writes=[Tp3])
                    k.dma("sp", mf[:], mfwd_d, writes=[Tp3]); k.dma("sp", mb[:], mbwd_d, writes=[Tp3])
                    k.dma("sp", triU[:], triU_d, writes=[Tp3]); k.dma("sp", triL[:], triL_d, writes=[Tp3]); k.dma("sp", blk[:], blk64_d, writes=[Tp3])
                    k.op("act", lambda e: e.activation(out=lbe[:], in_=lbz[:], func=AF.Exp), reads=[Tp3], writes=[Tp3])
                    k.op("dve", lambda e: e.tensor_tensor(out=lbs[:], in0=lbe[:, 0, :], in1=lbe[:, 1, :], op=ALU.add), reads=[Tp3], writes=[Tp3])
                    k.op("dve", lambda e: e.reciprocal(out=lbs[:], in_=lbs[:]), reads=[Tp3], writes=[Tp3])
                    for ll in range(L):
                        k.op("dve", lambda e: e.tensor_tensor(out=lbe[:, ll, :], in0=lbe[:, ll, :], in1=lbs[:], op=ALU.mult), reads=[Tp3], writes=[Tp3])
                    k.op("dve", lambda e: e.tensor_copy(out=lb[:], in_=lbe[:, 0, :]), reads=[Tp3], writes=[Tp3])
                    for ll in range(1, l + 1):
                        k.op("dve", lambda e: e.tensor_tensor(out=lb[:], in0=lb[:], in1=lbe[:, ll, :], op=ALU.add), reads=[Tp3], writes=[Tp3])
                    k.op("dve", lambda e: e.tensor_tensor(out=lb[:], in0=lb[:], in1=lbe[:, 0, :], op=ALU.subtract), reads=[Tp3], writes=[Tp3])
                    k.op("dve", lambda e: e.tensor_scalar(out=oml[:], in0=lb[:], scalar1=-1.0, scalar2=1.0, op0=ALU.mult, op1=ALU.add), reads=[Tp3], writes=[Tp3])
                    vt = k.sb("vt", [128, NT, 256], BF16); Tvt = T()
                    vstg = k.sb("vstg", [128, NT // 2, 256]); Tvstg = T()
                    for hf in range(2):
                        k.dma("sp", vstg[:], ut_d[b, hf * (S // 2):(hf + 1) * (S // 2), 0:256].rearrange("(n p) c -> p n c", p=128), reads=[Tut[b]], writes=[Tvstg])
                        k.op("pool", lambda e: e.tensor_copy(out=vt[:, hf * (NT // 2):(hf + 1) * (NT // 2), :], in_=vstg[:]), reads=[Tvstg], writes=[Tvt])
                    qh = k.sb("qh", [128, S]); Tqh = T()
                    gg = k.sb("gg", [128, S]); Tgg = T()
                    ff = k.sb("ff", [128, S]); Tff = T()
                    lf = k.sb("lf", [128, S]); Tlf = T()
                    cum = k.sb("cum", [128, S]); Tcum = T()
                    dd = k.sb("dd", [128, S]); Tdd = T()
                    EE = k.sb("EE", [128, S]); TEE = T()
                    qt = k.sb("qt", [128, S], BF16); Tqt = T()
                    kt = k.sb("kt", [128, S], BF16); Tkt = T()
                    qs = k.sb("qs", [128, S], BF16); Tqs = T()
                    ke = k.sb("ke", [128, S], BF16); Tke = T()
                    etot = k.sb("etot", [128, NT]); Tet = T()
                    OO = k.sb("OO", [128, S]); TOO = T()
                    ket = [k.sb("ket%d" % i, [128, 128], BF16) for i in range(2)]; Tket = [T(), T()]
                    Am = [[k.sb("Am%d_%d" % (d, i), [128, 128], BF16) for i in range(2)] for d in range(2)]
                    TAm = [[T(), T()] for d in range(2)]
                    S32 = k.sb("S32", [128, 64]); TS32 = T()
                    Sb = k.sb("Sb", [128, 64], BF16); TSb = T()
                    sqb = k.sb("sqb", [128, 512], BF16); Tsqb = T()
                    rsd = k.sb("rsd", [128, 512]); Trsd = T()
                    yo = k.sb("yo3", [128, S], BF16); Tyo = T()
                    p_sc = [k.ps("p_sc%d" % i, [128, 128]) for i in range(2)]; Tp_sc = [PT(), PT()]
                    p_y = [k.ps("p_y%d" % i, [128, 128]) for i in range(2)]; Tp_y = [PT(), PT()]
                    p_st = k.ps("p_st", [128, 64]); Tp_st = PT()
                    p_tr = k.ps("p_tr", [128, 128], BF16); Tp_tr = PT()
                    p_ss = k.ps("p_ss", [128, 512]); Tp_ss = PT()
                    for d in range(2):
                        for i in range(2):
                            k.op("pool", lambda e: e.memset(Am[d][i][:], 0.0), writes=[TAm[d][i]])
                    for i in range(2):
                        k.op("dve", lambda e: e.memset(p_sc[i][:], 0.0), writes=[Tp_sc[i]])
                    cum3 = cum[:].rearrange("p (n t) -> p n t", t=128)
                    dd3 = dd[:].rearrange("p (n t) -> p n t", t=128)
                    cum4 = cum[:].rearrange("p (n t) -> p n t", t=32)
                    dd4 = dd[:].rearrange("p (n t) -> p n t", t=32)
                    qx = k.sb("qx", [128, S], BF16); Tqx = T()
                    kx = [None] + [k.sb("kx%d" % i, [128, NT, 96], BF16) for i in range(1, 4)]; Tkx = T()
                    for hp in range(2):
                        r0 = 512 + hp * 128
                        k.dma("sp", qh[:], uT_d[b, r0:r0 + 128, :], reads=[TuT[b]], writes=[Tqh])
                        k.dma("sp", gg[:], uT_d[b, r0 + 1024:r0 + 1024 + 128, :], reads=[TuT[b]], writes=[Tgg])
                        k.op("act", lambda e: e.activation(out=qh[:], in_=qh[:], func=AF.Silu), reads=[Tqh], writes=[Tqh])
                        k.op("act", lambda e: e.activation(out=gg[:], in_=gg[:], func=AF.Silu), reads=[Tgg], writes=[Tgg])
                        for d in range(2):
                            k.dma("sp", ff[:], uT_d[b, r0 + 256 * (d + 1):r0 + 256 * (d + 1) + 128, :], reads=[TuT[b]], writes=[Tff])
                            k.op("act", lambda e: e.activation(out=ff[:], in_=ff[:], func=AF.Sigmoid), reads=[Tff], writes=[Tff])
                            k.op("dve", lambda e: e.tensor_scalar(out=ff[:], in0=ff[:], scalar1=oml[:, hp:hp + 1], scalar2=lb[:, hp:hp + 1],
                                                                  op0=ALU.mult, op1=ALU.add), reads=[Tff, Tp3], writes=[Tff])
                            k.op("act", lambda e: e.activation(out=lf[:], in_=ff[:], func=AF.Ln), reads=[Tff], writes=[Tlf])
                            k.op("dve", lambda e: e.tensor_scalar(out=ff[:], in0=ff[:], scalar1=-1.0, scalar2=1.0, op0=ALU.mult, op1=ALU.add),
                                 reads=[Tff], writes=[Tff])
                            if d == 0:
                                k.op("dve", lambda e: e.tensor_tensor_scan(out=cum[:], data0=mf[:], data1=lf[:], initial=0.0, op0=ALU.mult, op1=ALU.add),
                                     reads=[Tp3, Tlf], writes=[Tcum])
                                mid, end = 63, 127
                            else:
                                k.op("dve", lambda e: e.tensor_tensor_scan(out=cum[:, ::-1], data0=mb[:, ::-1], data1=lf[:, ::-1], initial=0.0,
                                                                          op0=ALU.mult, op1=ALU.add), reads=[Tp3, Tlf], writes=[Tcum])
                                mid, end = 64, 0
                            mid4, first4 = (15, 0) if d == 0 else (16, 31)
                            k.op("dve", lambda e: e.tensor_tensor(out=dd4, in0=cum4, in1=cum4[:, :, mid4:mid4 + 1].to_broadcast([128, S // 32, 32]), op=ALU.subtract),
                                 reads=[Tcum], writes=[Tdd])
                            k.op("act", lambda e: e.activation(out=EE[:], in_=dd[:], func=AF.Exp), reads=[Tdd], writes=[TEE])
                            k.op("dve", lambda e: e.tensor_tensor(out=qt[:], in0=qh[:], in1=EE[:], op=ALU.mult), reads=[Tqh, TEE], writes=[Tqt])
                            k.op("act", lambda e: e.activation(out=EE[:], in_=dd[:], func=AF.Exp, scale=-1.0), reads=[Tdd], writes=[TEE])
                            k.op("dve", lambda e: e.tensor_tensor(out=kt[:], in0=ff[:], in1=EE[:], op=ALU.mult), reads=[Tff, TEE], writes=[Tkt])
                            k.op("dve", lambda e: e.tensor_tensor(out=dd4, in0=cum4, in1=cum4[:, :, first4:first4 + 1].to_broadcast([128, S // 32, 32]), op=ALU.subtract),
                                 reads=[Tcum], writes=[Tdd])
                            k.op("act", lambda e: e.activation(out=EE[:], in_=dd[:], func=AF.Exp), reads=[Tdd], writes=[TEE])
                            k.op("dve", lambda e: e.tensor_tensor(out=qx[:], in0=qh[:], in1=EE[:], op=ALU.mult), reads=[Tqh, TEE], writes=[Tqx])
                            ff3 = ff[:].rearrange("p (n t) -> p n t", t=128)
                            EE3 = EE[:].rearrange("p (n t) -> p n t", t=128)
                            for i in range(1, 4):
                                w_ = 32 * i
                                if d == 0:
                                    srcs = slice(0, w_); refi = w_
                                else:
                                    srcs = slice(128 - w_, 128); refi = 127 - w_
                                k.op("dve", lambda e: e.tensor_tensor(out=dd3[:, :, 0:w_], in0=cum3[:, :, refi:refi + 1].to_broadcast([128, NT, w_]),
                                                                      in1=cum3[:, :, srcs], op=ALU.subtract), reads=[Tcum], writes=[Tdd])
                                k.op("act", lambda e: e.activation(out=EE3[:, :, 0:w_], in_=dd3[:, :, 0:w_], func=AF.Exp), reads=[Tdd], writes=[TEE])
                                k.op("dve", lambda e: e.tensor_tensor(out=kx[i][:, :, 0:w_], in0=ff3[:, :, srcs], in1=EE3[:, :, 0:w_], op=ALU.mult),
                                     reads=[Tff, TEE], writes=[Tkx])
                            k.op("act", lambda e: e.activation(out=EE[:], in_=cum[:], func=AF.Exp), reads=[Tcum], writes=[TEE])
                            k.op("dve", lambda e: e.tensor_tensor(out=qs[:], in0=qh[:], in1=EE[:], op=ALU.mult), reads=[Tqh, TEE], writes=[Tqs])
                            k.op("act", lambda e: e.activation(out=etot[:], in_=cum3[:, :, end], func=AF.Exp), reads=[Tcum], writes=[Tet])
                            k.op("dve", lambda e: e.tensor_tensor(out=dd3, in0=cum3[:, :, end:end + 1].to_broadcast([128, NT, 128]), in1=cum3, op=ALU.subtract),
                                 reads=[Tcum], writes=[Tdd])
                            k.op("act", lambda e: e.activation(out=EE[:], in_=dd[:], func=AF.Exp), reads=[Tdd], writes=[TEE])
                            k.op("dve", lambda e: e.tensor_tensor(out=ke[:], in0=ff[:], in1=EE[:], op=ALU.mult), reads=[Tff, TEE], writes=[Tke])
                            order = list(range(NT)) if d == 0 else [1, 0] + list(range(NT - 1, 1, -1))
                            tri = triU if d == 0 else triL
                            for oi, ti in enumerate(order):
                                ts_ = slice(ti * 128, (ti + 1) * 128)
                                k.op("pe", lambda e: e.transpose(p_tr[:], ke[:, ts_], ident_b[:]), reads=[Tke, Tc], writes=[Tp_tr])
                                KT = ket[oi % 2]; tKT = Tket[oi % 2]
                                k.op("act", lambda e: e.activation(out=KT[:], in_=p_tr[:], func=AF.Copy), reads=[Tp_tr], writes=[tKT])
                                py = p_y[oi % 2]; tpy = Tp_y[oi % 2]
                                for hh in range(2):
                                    bs = slice(hh * 64, (hh + 1) * 64)
                                    psc = p_sc[hh]; tps = Tp_sc[hh]
                                    for tb in range(4):
                                        for sb_ in (range(0, tb + 1) if d == 0 else range(tb, 4)):
                                            tq = slice(ti * 128 + 32 * tb, ti * 128 + 32 * tb + 32)
                                            if sb_ == tb:
                                                lw = kt[bs, tq]; rq = qt[bs, tq]
                                            else:
                                                i = tb if d == 0 else 3 - tb
                                                o_ = 32 * sb_ if d == 0 else 32 * sb_ - (128 - 32 * i)
                                                lw = kx[i][bs, ti, o_:o_ + 32]; rq = qx[bs, tq]
                                            k.op("pe", lambda e: e.matmul(psc[32 * sb_:32 * sb_ + 32, 32 * tb:32 * tb + 32], lhsT=lw, rhs=rq, start=True, stop=True,
                                                                          tile_position=(bs.start, 32 * sb_)),
                                                 reads=[Tkt, Tqt, Tkx, Tqx], writes=[tps])
                                    A = Am[d][hh]; tA = TAm[d][hh]
                                    k.op("dve", lambda e: e.copy_predicated(out=A[:], mask=tri[:], data=psc[:]), reads=[tps, Tp3], writes=[tA])
                                    vs = vt[:, ti, (2 * hp + hh) * 64:(2 * hp + hh + 1) * 64]
                                    k.op("pe", lambda e: e.matmul(py[bs, :], lhsT=vs, rhs=A[:], start=True, stop=(oi == 0)),
                                         reads=[Tvt, tA], writes=[tpy])
                                    if oi > 0:
                                        k.op("pe", lambda e: e.matmul(py[bs, :], lhsT=Sb[bs, :], rhs=qs[bs, ts_], start=False, stop=True),
                                             reads=[TSb, Tqs], writes=[tpy])
                                if d == 0:
                                    k.op("act", lambda e: e.activation(out=OO[:, ts_], in_=py[:], func=AF.Copy), reads=[tpy], writes=[TOO])
                                else:
                                    k.op("dve", lambda e: e.tensor_tensor(out=OO[:, ts_], in0=OO[:, ts_], in1=py[:], op=ALU.add), reads=[tpy, TOO], writes=[TOO])
                                if oi == NT - 1:
                                    continue
                                for hh in range(2):
                                    bs = slice(hh * 64, (hh + 1) * 64)
                                    vs = vt[:, ti, (2 * hp + hh) * 64:(2 * hp + hh + 1) * 64]
                                    k.op("pe", lambda e: e.matmul(p_st[bs, :], lhsT=KT[:, bs], rhs=vs, start=True, stop=True),
                                         reads=[tKT, Tvt], writes=[Tp_st])
                                if oi == 0:
                                    k.op("dve", lambda e: e.tensor_copy(out=S32[:], in_=p_st[:]), reads=[Tp_st], writes=[TS32])
                                else:
                                    k.op("dve", lambda e: e.scalar_tensor_tensor(out=S32[:], in0=S32[:], scalar=etot[:, ti:ti + 1], in1=p_st[:],
                                                                                op0=ALU.mult, op1=ALU.add), reads=[Tp_st, Tet, TS32], writes=[TS32])
                                k.op("pool", lambda e: e.tensor_copy(out=Sb[:], in_=S32[:]), reads=[TS32], writes=[TSb])
                        for (t0, n) in BLOCKS:
                            k.op("act", lambda e: e.activation(out=sqb[:, 0:n], in_=OO[:, t0:t0 + n], func=AF.Square), reads=[TOO], writes=[Tsqb])
                            k.op("pe", lambda e: e.matmul(p_ss[:, 0:n], lhsT=blk[:], rhs=sqb[:, 0:n], start=True, stop=True), reads=[Tsqb, Tp3], writes=[Tp_ss])
                            k.op("act", lambda e: e.activation(out=rsd[:, 0:n], in_=p_ss[:, 0:n], func=AF.Sqrt, scale=1.0 / 64, bias=EPS), reads=[Tp_ss], writes=[Trsd])
                            k.op("dve", lambda e: e.reciprocal(out=rsd[:, 0:n], in_=rsd[:, 0:n]), reads=[Trsd], writes=[Trsd])
                            k.op("dve", lambda e: e.tensor_tensor(out=rsd[:, 0:n], in0=rsd[:, 0:n], in1=OO[:, t0:t0 + n], op=ALU.mult), reads=[Trsd, TOO], writes=[Trsd])
                            k.op("dve", lambda e: e.scalar_tensor_tensor(out=yo[:, t0:t0 + n], in0=rsd[:, 0:n], scalar=hnw[:, hp:hp + 1], in1=gg[:, t0:t0 + n],
                                                                        op0=ALU.mult, op1=ALU.mult), reads=[Trsd, Tgg, Tp3], writes=[Tyo])
                        k.dma("sp", yT_d[b, 256 + hp * 128:256 + (hp + 1) * 128, :], yo[:], reads=[Tyo], writes=[TyT[b]])
                if cfg.upto < 4:
                    continue
                with Stage(k):
                    Tp4 = T()
                    cw = k.sb("scw", [128, 4, 4]); cb = k.sb("scb", [128, 4])
                    aneg = k.sb("aneg", [128, 8]); dtb = k.sb("dtb", [128, 8]); dsk = k.sb("dsk", [128, 256]); snw = k.sb("snw", [128, 256])
                    triUf = k.sb("triUf", [128, 128]); triLf = k.sb("triLf", [128, 128]); strLf = k.sb("strLf", [128, 128]); strUf = k.sb("strUf", [128, 128])
                    for dst, src in ((cw, sd_cw_d[:, l]), (cb, sd_cb_d[:, l]), (aneg, sd_alog_d[:, l]), (dtb, sd_dtb_d[:, l]), (dsk, sd_dsk_d[:, l]),
                                     (snw, sd_nw_d[:, l]), (triUf, triUf_d), (triLf, triLf_d), (strLf, strLf_d), (strUf, strUf_d)):
                        k.dma("sp", dst[:], src, writes=[Tp4])
                    k.op("act", lambda e: e.activation(out=aneg[:], in_=aneg[:], func=AF.Exp), reads=[Tp4], writes=[Tp4])
                    k.op("dve", lambda e: e.tensor_scalar(out=aneg[:], in0=aneg[:], scalar1=-1.0, scalar2=None, op0=ALU.mult), reads=[Tp4], writes=[Tp4])
                    xp = k.sb("sxp", [128, S + 6]); Txp = T()
                    xc = k.sb("sxc", [128, S]); Txc = T()
                    fmb = k.sb("fmb", [128, 4, S], BF16); Tfmb = T()
                    segs = [(0, NCTX, 2), (NCTX, NLAT, NCTX + 5)]
                    for ch in range(4):
                        k.op("pool", lambda e: e.memset(xp[:], 0.0), writes=[Txp])
                        for (t0, n, o) in segs:
                            k.dma("sp", xp[:, o:o + n], uT_d[b, 2048 + ch * 128:2048 + (ch + 1) * 128, t0:t0 + n], reads=[TuT[b]], writes=[Txp])
                        for (t0, n, o) in segs:
                            k.op("dve", lambda e: e.tensor_scalar(out=xc[:, t0:t0 + n], in0=xp[:, o - 2:o - 2 + n], scalar1=cw[:, ch, 0:1],
                                                                  scalar2=cb[:, ch:ch + 1], op0=ALU.mult, op1=ALU.add), reads=[Txp, Tp4], writes=[Txc])
                            for j in range(1, 4):
                                k.op("dve", lambda e: e.scalar_tensor_tensor(out=xc[:, t0:t0 + n], in0=xp[:, o - 2 + j:o - 2 + j + n], scalar=cw[:, ch, j:j + 1],
                                                                            in1=xc[:, t0:t0 + n], op0=ALU.mult, op1=ALU.add), reads=[Txp, Tp4, Txc], writes=[Txc])
                        k.op("act", lambda e: e.activation(out=fmb[:, ch, :], in_=xc[:], func=AF.Silu), reads=[Txc], writes=[Tfmb])
                    xst = k.sb("xst", [128, NT, 256], BF16); Txst = T()
                    Bt = k.sb("Bt", [128, NT, 128], BF16); TBt = T()
                    ps_pre = PScope(k); ps_pre.__enter__()
                    p_tr = [k.ps("p4tr%d" % i, [128, 128], BF16) for i in range(2)]; Tp_tr = [PT(), PT()]
                    ntr = 0
                    for ti in range(NT):
                        ts_ = slice(ti * 128, (ti + 1) * 128)
                        for ch in range(3):
                            pp = p_tr[ntr % 2]; tp = Tp_tr[ntr % 2]
                            k.op("pe", lambda e: e.transpose(pp[:], fmb[:, ch, ts_], ident_b[:]), reads=[Tfmb, Tc], writes=[tp])
                            dst = xst[:, ti, ch * 128:(ch + 1) * 128] if ch < 2 else Bt[:, ti, :]
                            tdst = Txst if ch < 2 else TBt
                            if ntr % 2 == 0:
                                k.op("act", lambda e: e.activation(out=dst, in_=pp[:], func=AF.Copy), reads=[tp], writes=[tdst])
                            else:
                                k.op("dve", lambda e: e.tensor_copy(out=dst, in_=pp[:]), reads=[tp], writes=[tdst])
                            ntr += 1
                    dt = k.sb("dt", [128, NT, 8]); Tdt = T()
                    la = k.sb("la", [128, NT, 8]); Tla = T()
                    k.dma("sp", dt[:], ut_d[b, :, 512:520].rearrange("(n p) c -> p n c", p=128), reads=[Tut[b]], writes=[Tdt])
                    k.op("dve", lambda e: e.tensor_tensor(out=dt[:], in0=dt[:], in1=dtb[:, None, :].to_broadcast([128, NT, 8]), op=ALU.add), reads=[Tdt, Tp4], writes=[Tdt])
                    k.op("act", lambda e: e.activation(out=dt[:], in_=dt[:], func=AF.Exp), reads=[Tdt], writes=[Tdt])
                    k.op("act", lambda e: e.activation(out=dt[:], in_=dt[:], func=AF.Ln, bias=1.0), reads=[Tdt], writes=[Tdt])
                    k.op("dve", lambda e: e.tensor_tensor(out=la[:], in0=dt[:], in1=aneg[:, None, :].to_broadcast([128, NT, 8]), op=ALU.mult), reads=[Tdt, Tp4], writes=[Tla])
                    p_ct = k.ps("p_ct", [128, 2, NT, 8]); Tp_ct = PT()
                    for ti in range(NT):
                        k.op("pe", lambda e: e.matmul(p_ct[:, 0, ti, 0:4], lhsT=triUf[:], rhs=la[:, ti, 0:4], start=True, stop=True), reads=[Tla, Tp4], writes=[Tp_ct])
                        k.op("pe", lambda e: e.matmul(p_ct[:, 0, ti, 4:8], lhsT=triLf[:], rhs=la[:, ti, 4:8], start=True, stop=True), reads=[Tla, Tp4], writes=[Tp_ct])
                        k.op("pe", lambda e: e.matmul(p_ct[:, 1, ti, :], lhsT=ones_f[:], rhs=la[:, ti, :], start=True, stop=True), reads=[Tla, Tc], writes=[Tp_ct])
                    cexp = k.sb("cexp", [128, NT, 8]); etot = k.sb("etot4", [128, NT, 8]); wend = k.sb("wend", [128, NT, 8]); Tce = T()
                    k.op("dve", lambda e: e.tensor_tensor(out=wend[:], in0=p_ct[:, 1], in1=p_ct[:, 0], op=ALU.subtract), reads=[Tp_ct], writes=[Tce]) if False else None
                    k.op("act", lambda e: e.activation(out=cexp[:], in_=p_ct[:, 0], func=AF.Copy), reads=[Tp_ct], writes=[Tce])
                    k.op("dve", lambda e: e.tensor_tensor(out=wend[:], in0=p_ct[:, 1], in1=cexp[:], op=ALU.subtract), reads=[Tp_ct, Tce], writes=[Tce])
                    k.op("act", lambda e: e.activation(out=wend[:], in_=wend[:], func=AF.Exp), reads=[Tce], writes=[Tce])
                    k.op("dve", lambda e: e.tensor_tensor(out=wend[:], in0=wend[:], in1=dt[:], op=ALU.mult), reads=[Tce, Tdt], writes=[Tce])
                    k.op("act", lambda e: e.activation(out=cexp[:], in_=cexp[:], func=AF.Exp), reads=[Tce], writes=[Tce])
                    k.op("act", lambda e: e.activation(out=etot[:], in_=p_ct[:, 1], func=AF.Exp), reads=[Tp_ct], writes=[Tce])
                    ps_pre.__exit__(None, None, None)
                    ps_loop = PScope(k); ps_loop.__enter__()
                    Yacc = k.sb("Yacc", [128, NT, 256]); TY = T()
                    inc4 = [k.sb("inc4_%d" % d, [128, 4, 128]) for d in range(2)]
                    ngm = [k.sb("ngm%d" % d, [128, 4, 128]) for d in range(2)]
                    k.dma("sp", inc4[0][:], triUf4_d, writes=[Tp4]); k.dma("sp", inc4[1][:], triLf4_d, writes=[Tp4])
                    k.dma("sp", ngm[0][:], negmf_d, writes=[Tp4]); k.dma("sp", ngm[1][:], negmb_d, writes=[Tp4])
                    etH = k.sb("etH", [128, NT, 2, 2]); TetH = T()
                    et4 = etot[:].rearrange("p n (d h) -> p n d h", d=2)
                    for g in range(2):
                        gs = slice(g * 64, (g + 1) * 64)
                        k.op("dve", lambda e: e.tensor_copy(out=etH[gs], in_=et4[gs, :, :, 2 * g:2 * g + 2]), reads=[Tce], writes=[TetH])
                    Rr4 = [k.sb("Rr4_%d" % i, [128, 4, 128]) for i in range(2)]; TRr4 = [T(), T()]
                    Es = [k.sb("Es%d" % i, [128, 4, 128]) for i in range(2)]; TEs = [T(), T()]
                    Ab = [k.sb("Ab%d" % i, [128, 4, 128], BF16) for i in range(2)]; TAb = [T(), T()]
                    Bw = [k.sb("Bw%d" % i, [128, 2, 2, 64], BF16) for i in range(2)]; TBw = [T(), T()]
                    tmpy = [k.sb("tmpy%d" % i, [128, 4, 64]) for i in range(2)]; Ttmpy = [T(), T()]
                    S32 = k.sb("S32_4", [128, 2, 64]); TS32 = T()
                    STb = k.sb("STb4", [128, 2, 64], BF16); TSTb = T()
                    p_g = [k.ps("p_g%d" % i, [128, 128]) for i in range(2)]; Tp_g = [PT(), PT()]
                    p_seg = [k.ps("p_seg%d" % i, [128, 4, 128]) for i in range(2)]; Tp_seg = [PT(), PT()]
                    p_y1 = k.ps("p_y1", [128, 4, 64]); Tp_y1 = PT()
                    p_y2 = [k.ps("p_y2%d" % i, [128, 2, 64]) for i in range(2)]; Tp_y2 = [PT(), PT()]
                    p_st = k.ps("p_st4", [128, 2, 64]); Tp_st = PT()
                    Bt4 = Bt[:].rearrange("p n (g c) -> p n g c", g=2)
                    def ssd_front(d, oi, ti, i2):
                        ts_ = slice(ti * 128, (ti + 1) * 128)
                        d4 = slice(d * 4, d * 4 + 4)
                        strm = strLf if d == 0 else strUf
                        for g in range(2):
                            gs = slice(g * 64, (g + 1) * 64)
                            k.op("pe", lambda e: e.matmul(p_g[g][:], lhsT=fmb[gs, 2, ts_], rhs=fmb[gs, 3, ts_], start=True, stop=True),
                                 reads=[Tfmb], writes=[Tp_g[g]])
                        k.op("pool", lambda e: e.tensor_tensor(out=Rr4[i2][:], in0=inc4[d][:], in1=la[:, ti, d4, None].to_broadcast([128, 4, 128]), op=ALU.mult),
                             reads=[Tla, Tp4], writes=[TRr4[i2]])
                        k.op("pe", lambda e: e.matmul(p_seg[i2][:].rearrange("p h t -> p (h t)"), lhsT=strm[:], rhs=Rr4[i2][:].rearrange("p h t -> p (h t)"),
                                                      start=True, stop=False), reads=[TRr4[i2], Tp4], writes=[Tp_seg[i2]])
                        k.op("pe", lambda e: e.matmul(p_seg[i2][:].rearrange("p h t -> p (h t)"), lhsT=ident_f[:], rhs=ngm[d][:].rearrange("p h t -> p (h t)"),
                                                      start=False, stop=True), reads=[Tc, Tp4], writes=[Tp_seg[i2]])
                        k.op("act", lambda e: e.activation(out=Es[i2][:], in_=p_seg[i2][:], func=AF.Exp), reads=[Tp_seg[i2]], writes=[TEs[i2]])
                        k.op("dve", lambda e: e.tensor_tensor(out=Es[i2][:], in0=Es[i2][:], in1=dt[:, ti, d4, None].to_broadcast([128, 4, 128]), op=ALU.mult),
                             reads=[TEs[i2], Tdt], writes=[TEs[i2]])
                        for g in range(2):
                            k.op("dve", lambda e: e.tensor_tensor(out=Ab[i2][:, 2 * g:2 * g + 2, :], in0=Es[i2][:, 2 * g:2 * g + 2, :],
                                                                  in1=p_g[g][:, None, :].to_broadcast([128, 2, 128]), op=ALU.mult),
                                 reads=[TEs[i2], Tp_g[g]], writes=[TAb[i2]])
                        if oi < NT - 1:
                            k.op("pool", lambda e: e.tensor_tensor(out=Bw[i2][:], in0=Bt4[:, ti, :, None, :].to_broadcast([128, 2, 2, 64]),
                                                                   in1=wend[:, ti, d4].rearrange("p (g j) -> p g j", g=2)[:, :, :, None].to_broadcast([128, 2, 2, 64]),
                                                                   op=ALU.mult), reads=[TBt, Tce], writes=[TBw[i2]])

                    def ssd_back(d, oi, ti, i2):
                        ts_ = slice(ti * 128, (ti + 1) * 128)
                        for h in range(4):
                            k.op("pe", lambda e: e.matmul(p_y1[:, h, :], lhsT=Ab[i2][:, h, :], rhs=xst[:, ti, h * 64:(h + 1) * 64], start=True, stop=True),
                                 reads=[TAb[i2], Txst], writes=[Tp_y1])
                        yacc = Yacc[:, ti, :].rearrange("p (h c) -> p h c", h=4)
                        if oi > 0:
                            for h in range(4):
                                g = h // 2; gs = slice(g * 64, (g + 1) * 64)
                                k.op("pe", lambda e: e.matmul(p_y2[g][:, h % 2, :], lhsT=fmb[gs, 3, ts_], rhs=STb[gs, h % 2, :], start=True, stop=True),
                                     reads=[Tfmb, TSTb], writes=[Tp_y2[g]])
                            for g in range(2):
                                k.op("dve", lambda e: e.tensor_tensor(out=tmpy[i2][:, 2 * g:2 * g + 2, :], in0=p_y2[g][:],
                                                                      in1=cexp[:, ti, d * 4 + 2 * g:d * 4 + 2 * g + 2, None].to_broadcast([128, 2, 64]), op=ALU.mult),
                                     reads=[Tp_y2[g], Tce], writes=[Ttmpy[i2]])
                            if d == 0:
                                k.op("dve", lambda e: e.tensor_tensor(out=yacc, in0=p_y1[:], in1=tmpy[i2][:], op=ALU.add), reads=[Tp_y1, Ttmpy[i2]], writes=[TY])
                            else:
                                k.op("pool", lambda e: e.tensor_tensor(out=yacc, in0=yacc, in1=tmpy[i2][:], op=ALU.add), reads=[TY, Ttmpy[i2]], writes=[TY])
                                k.op("dve", lambda e: e.tensor_tensor(out=yacc, in0=yacc, in1=p_y1[:], op=ALU.add), reads=[TY, Tp_y1], writes=[TY])
                        else:
                            if d == 0:
                                k.op("dve", lambda e: e.tensor_copy(out=yacc, in_=p_y1[:]), reads=[Tp_y1], writes=[TY])
                            else:
                                k.op("dve", lambda e: e.tensor_tensor(out=yacc, in0=yacc, in1=p_y1[:], op=ALU.add), reads=[TY, Tp_y1], writes=[TY])
                        if oi == NT - 1:
                            return
                        for h in range(4):
                            g = h // 2; gs = slice(g * 64, (g + 1) * 64)
                            k.op("pe", lambda e: e.matmul(p_st[gs, h % 2, :], lhsT=Bw[i2][:, g, h % 2, :], rhs=xst[:, ti, h * 64:(h + 1) * 64], start=True, stop=True,
                                                          tile_position=(0, g * 64)), reads=[TBw[i2], Txst], writes=[Tp_st])
                        if oi == 0:
                            k.op("dve", lambda e: e.tensor_copy(out=S32[:], in_=p_st[:]), reads=[Tp_st], writes=[TS32])
                        else:
                            k.op("pool", lambda e: e.tensor_tensor(out=S32[:], in0=S32[:], in1=etH[:, ti, d, :, None].to_broadcast([128, 2, 64]), op=ALU.mult),
                                 reads=[TS32, TetH], writes=[TS32])
                            k.op("dve", lambda e: e.tensor_tensor(out=S32[:], in0=S32[:], in1=p_st[:], op=ALU.add), reads=[TS32, Tp_st], writes=[TS32])
                        k.op("pool", lambda e: e.tensor_copy(out=STb[:], in_=S32[:]), reads=[TS32], writes=[TSTb])

                    seq = []
                    for d in range(2):
                        order = list(range(NT)) if d == 0 else [1, 0] + list(range(NT - 1, 1, -1))
                        for oi, ti in enumerate(order):
                            seq.append((d, oi, ti, len(seq) % 2))
                    ssd_front(*seq[0])
                    for i_ in range(len(seq)):
                        if i_ + 1 < len(seq):
                            ssd_front(*seq[i_ + 1])
                        ssd_back(*seq[i_])
                    zz = k.sb("zz", [128, NT, 256]); Tzz = T()
                    k.dma("sp", zz[:], ut_d[b, :, 256:512].rearrange("(n p) c -> p n c", p=128), reads=[Tut[b]], writes=[Tzz])
                    k.op("act", lambda e: e.activation(out=zz[:], in_=zz[:], func=AF.Silu), reads=[Tzz], writes=[Tzz])
                    tq = k.sb("tq", [128, NT, 256]); Ttq = T()
                    k.op("dve", lambda e: e.tensor_tensor(out=tq[:], in0=xst[:], in1=dsk[:, None, :].to_broadcast([128, NT, 256]), op=ALU.mult), reads=[Txst, Tp4], writes=[Ttq])
                    k.op("dve", lambda e: e.tensor_tensor(out=Yacc[:], in0=Yacc[:], in1=tq[:], op=ALU.add), reads=[TY, Ttq], writes=[TY])
                    k.op("dve", lambda e: e.tensor_tensor(out=Yacc[:], in0=Yacc[:], in1=zz[:], op=ALU.mult), reads=[TY, Tzz], writes=[TY])
                    k.op("pool", lambda e: e.tensor_tensor(out=tq[:], in0=Yacc[:], in1=Yacc[:], op=ALU.mult), reads=[TY], writes=[Ttq])
                    ssq = k.sb("ssq", [128, NT]); Tssq = T()
                    k.op("dve", lambda e: e.reduce_sum(out=ssq[:], in_=tq[:], axis=AX.X), reads=[Ttq], writes=[Tssq])
                    k.op("act", lambda e: e.activation(out=ssq[:], in_=ssq[:], func=AF.Sqrt, scale=1.0 / 256, bias=EPS), reads=[Tssq], writes=[Tssq])
                    k.op("dve", lambda e: e.reciprocal(out=ssq[:], in_=ssq[:]), reads=[Tssq], writes=[Tssq])
                    k.op("dve", lambda e: e.tensor_tensor(out=Yacc[:], in0=Yacc[:], in1=ssq[:, :, None].to_broadcast([128, NT, 256]), op=ALU.mult), reads=[TY, Tssq], writes=[TY])
                    yob = k.sb("yob", [128, NT, 256], BF16); Tyob = T()
                    k.op("dve", lambda e: e.tensor_tensor(out=yob[:], in0=Yacc[:], in1=snw[:, None, :].to_broadcast([128, NT, 256]), op=ALU.mult), reads=[TY, Tp4], writes=[Tyob])
                    ps_loop.__exit__(None, None, None)
                    ps_epi = PScope(k); ps_epi.__enter__()
                    p_tr = [k.ps("p4tre%d" % i, [128, 128], BF16) for i in range(2)]; Tp_tr = [PT(), PT()]
                    yoT = k.sb("yoT", [128, 2, S], BF16); TyoT = T()
                    for ti in range(NT):
                        for ch in range(2):
                            pp = p_tr[ntr % 2]; tp = Tp_tr[ntr % 2]
                            k.op("pe", lambda e: e.transpose(pp[:], yob[:, ti, ch * 128:(ch + 1) * 128], ident_b[:]), reads=[Tyob, Tc], writes=[tp])
                            if ntr % 2 == 0:
                                k.op("act", lambda e: e.activation(out=yoT[:, ch, ti * 128:(ti + 1) * 128], in_=pp[:], func=AF.Copy), reads=[tp], writes=[TyoT])
                            else:
                                k.op("dve", lambda e: e.tensor_copy(out=yoT[:, ch, ti * 128:(ti + 1) * 128], in_=pp[:]), reads=[tp], writes=[TyoT])
                            ntr += 1
                    k.dma("sp", yT_d[b, 512:768, :].rearrange("(c p) t -> p c t", p=128), yoT[:], reads=[TyoT], writes=[TyT[b]])
                    ps_epi.__exit__(None, None, None)
                if cfg.upto < 5:
                    continue
                need_ctx = l < L - 1
                with Stage(k):
                    Tp5 = T()
                    qan = k.sb("qan", [128, 192]); kvan = k.sb("kvan", [128, 128]); qnr = k.sb("qnr", [128, 96]); knr = k.sb("knr", [128, 96])
                    wq = k.sb("wq", [96, 2, 384], BF16); wkv = k.sb("wkv", [128, 512], BF16)
                    rope = k.sb("rope", [128, 16, 2, 16]); invn3 = k.sb("invn3", [128, 3]); invn8 = k.sb("invn8", [128, 8])
                    for dst, src in ((qan, ml_qan_d[:, l]), (kvan, ml_kvan_d[:, l]), (qnr, ml_qn_d[:, l]), (knr, ml_kn_d[:, l]), (rope, rope_d),
                                     (invn3, invn3_d), (invn8, invn8_d)):
                        k.dma("sp", dst[:], src, writes=[Tp5])
                    wqs = k.sb("wqs", [96, 2, 384]); wkvs = k.sb("wkvs", [128, 512]); Twqs = T()
                    k.dma("sp", wqs[:], ml_wq_d[l].rearrange("(c p) n -> p c n", p=96), writes=[Twqs])
                    k.dma("sp", wkvs[:], ml_wkv_d[l], writes=[Twqs])
                    k.op("pool", lambda e: e.tensor_copy(out=wq[:], in_=wqs[:]), reads=[Twqs], writes=[Tp5])
                    k.op("pool", lambda e: e.tensor_copy(out=wkv[:], in_=wkvs[:]), reads=[Twqs], writes=[Tp5])
                    QT = k.sb("QT", [96, 4, S], BF16); TQT = T()
                    KT = k.sb("KT", [96, 4, S], BF16); TKT = T()
                    Va = k.sb("Va", [128, NT, 4, 65], BF16); TVa = T()
                    k.op("pool", lambda e: e.memset(Va[:], 1.0), writes=[TVa])
                    um = [k.sb("um%d" % i, [128, 352]) for i in range(2)]; Tum = [T(), T()]
                    ps_prep = PScope(k); ps_prep.__enter__()
                    def dbl(name, shape, dt_=F32):
                        return [k.sb("%s_%d" % (name, i), shape, dt_) for i in range(2)], [T(), T()]
                    sqL, TsqL = dbl("sq5", [128, 512]); ss3L, Tss3L = dbl("ss3", [128, 3]); ss8L, Tss8L = dbl("ss8", [128, 12])
                    cnL, TcnL = dbl("cn", [128, 320], BF16); cTtL, TcTL = dbl("cTt", [128, 3, 128], BF16)
                    qfL, TqfL = dbl("qf", [128, 4, 96]); kvfL, TkvfL = dbl("kvf", [128, 4, 128])
                    rbL, TrbL = dbl("rb", [128, 5, 32]); raL, TraL = dbl("ra", [128, 4, 5, 16])
                    QbL, TQbL = dbl("Qb", [128, 4, 96], BF16); KbL, TKbL = dbl("Kb", [128, 4, 96], BF16)
                    p_trA = [k.ps("p5trA%d" % i, [128, 4, 128], BF16) for i in range(2)]; Tp_trA = [PT(), PT()]
                    p_trB = [k.ps("p5trB%d" % i, [128, 4, 128], BF16) for i in range(2)]; Tp_trB = [PT(), PT()]
                    p_qL = [k.ps("p_q%d" % i, [128, 384]) for i in range(2)]; Tp_qL = [PT(), PT()]
                    p_kvL = [k.ps("p_kv%d" % i, [128, 512]) for i in range(2)]; Tp_kvL = [PT(), PT()]
                    for ti in range(NT):
                        U = um[ti % 2]; tU = Tum[ti % 2]
                        j2 = ti % 2
                        sq = sqL[j2]; Tsq = TsqL[j2]; ss3 = ss3L[j2]; Tss3 = Tss3L[j2]; ss8 = ss8L[j2]; Tss8 = Tss8L[j2]
                        cn = cnL[j2]; Tcn = TcnL[j2]; cTt = cTtL[j2]; TcT = TcTL[j2]; qf = qfL[j2]; Tqf = TqfL[j2]; kvf = kvfL[j2]; Tkvf = TkvfL[j2]
                        rb = rbL[j2]; Trb = TrbL[j2]; ra = raL[j2]; Tra = TraL[j2]; Qb = QbL[j2]; TQb = TQbL[j2]; Kb = KbL[j2]; TKb = TKbL[j2]
                        p_q = p_qL[j2]; Tp_q = Tp_qL[j2]; p_kv = p_kvL[j2]; Tp_kv = Tp_kvL[j2]
                        p_tr = [p_trB[0], p_trB[1]]; Tp_tr = [Tp_trB[0], Tp_trB[1]]
                        k.dma("sp", U[:], ut_d[b, ti * 128:(ti + 1) * 128, 520:872], reads=[Tut[b]], writes=[tU])
                        k.op("pool", lambda e: e.tensor_tensor(out=sq[:, 0:352], in0=U[:], in1=U[:], op=ALU.mult), reads=[tU], writes=[Tsq])
                        for j, (a_, b_) in enumerate(((0, 192), (192, 320), (320, 352))):
                            k.op("dve", lambda e: e.reduce_sum(out=ss3[:, j:j + 1], in_=sq[:, a_:b_], axis=AX.X), reads=[Tsq], writes=[Tss3])
                        k.op("dve", lambda e: e.tensor_tensor(out=ss3[:], in0=ss3[:], in1=invn3[:], op=ALU.mult), reads=[Tss3, Tp5], writes=[Tss3])
                        k.op("act", lambda e: e.activation(out=ss3[:], in_=ss3[:], func=AF.Sqrt, bias=EPS), reads=[Tss3], writes=[Tss3])
                        k.op("dve", lambda e: e.reciprocal(out=ss3[:], in_=ss3[:]), reads=[Tss3], writes=[Tss3])
                        k.op("dve", lambda e: e.scalar_tensor_tensor(out=cn[:, 0:192], in0=U[:, 0:192], scalar=ss3[:, 0:1], in1=qan[:], op0=ALU.mult, op1=ALU.mult),
                             reads=[tU, Tss3, Tp5], writes=[Tcn])
                        k.op("dve", lambda e: e.scalar_tensor_tensor(out=cn[:, 192:320], in0=U[:, 192:320], scalar=ss3[:, 1:2], in1=kvan[:], op0=ALU.mult, op1=ALU.mult),
                             reads=[tU, Tss3, Tp5], writes=[Tcn])
                        k.op("dve", lambda e: e.scalar_tensor_tensor(out=rb[:, 4, :], in0=U[:, 320:352], scalar=ss3[:, 2:3], in1=knr[:, 64:96], op0=ALU.mult, op1=ALU.mult),
                             reads=[tU, Tss3, Tp5], writes=[Trb])
                        pt = p_trA[j2]; tpt = Tp_trA[j2]
                        k.op("pe", lambda e: e.transpose(pt[0:96, 0, :], cn[:, 0:96], ident_b[:]), reads=[Tcn, Tc], writes=[tpt])
                        k.op("pe", lambda e: e.transpose(pt[0:96, 1, :], cn[:, 96:192], ident_b[:]), reads=[Tcn, Tc], writes=[tpt])
                        k.op("pe", lambda e: e.transpose(pt[:, 2, :], cn[:, 192:320], ident_b[:]), reads=[Tcn, Tc], writes=[tpt])
                        k.op("act", lambda e: e.activation(out=cTt[0:96, 0:2, :], in_=pt[0:96, 0:2, :], func=AF.Copy), reads=[tpt], writes=[TcT])
                        k.op("act", lambda e: e.activation(out=cTt[:, 2, :], in_=pt[:, 2, :], func=AF.Copy), reads=[tpt], writes=[TcT])
                        for c_ in range(2):
                            k.op("pe", lambda e: e.matmul(p_q[:], lhsT=cTt[0:96, c_, :], rhs=wq[:, c_, :], start=(c_ == 0), stop=(c_ == 1)), reads=[TcT, Tp5], writes=[Tp_q])
                        k.op("pe", lambda e: e.matmul(p_kv[:], lhsT=cTt[:, 2, :], rhs=wkv[:], start=True, stop=True), reads=[TcT, Tp5], writes=[Tp_kv])
                        k.op("act", lambda e: e.activation(out=qf[:].rearrange("p h c -> p (h c)"), in_=p_q[:], func=AF.Copy), reads=[Tp_q], writes=[Tqf])
                        k.op("dve", lambda e: e.tensor_copy(out=kvf[:].rearrange("p h c -> p (h c)"), in_=p_kv[:]), reads=[Tp_kv], writes=[Tkvf])
                        sq4 = sq[:, 0:384].rearrange("p (h c) -> p h c", c=96)
                        k.op("pool", lambda e: e.tensor_tensor(out=sq4, in0=qf[:], in1=qf[:], op=ALU.mult), reads=[Tqf], writes=[Tsq])
                        k.op("dve", lambda e: e.reduce_sum(out=ss8[:, 0:4], in_=sq4[:, :, 0:64], axis=AX.X), reads=[Tsq], writes=[Tss8])
                        k.op("dve", lambda e: e.reduce_sum(out=ss8[:, 4:8], in_=sq4[:, :, 64:96], axis=AX.X), reads=[Tsq], writes=[Tss8])
                        sq5 = sq[:, 0:512].rearrange("p (h c) -> p h c", c=128)
                        k.op("pool", lambda e: e.tensor_tensor(out=sq5, in0=kvf[:], in1=kvf[:], op=ALU.mult), reads=[Tkvf, Tss8], writes=[Tsq])
                        k.op("dve", lambda e: e.reduce_sum(out=ss8[:, 8:12], in_=sq5[:, :, 0:64], axis=AX.X), reads=[Tsq], writes=[Tss8])
                        k.op("dve", lambda e: e.tensor_tensor(out=ss8[:, 0:8], in0=ss8[:, 0:8], in1=invn8[:], op=ALU.mult), reads=[Tss8, Tp5], writes=[Tss8])
                        k.op("dve", lambda e: e.tensor_tensor(out=ss8[:, 8:12], in0=ss8[:, 8:12], in1=invn8[:, 0:4], op=ALU.mult), reads=[Tss8, Tp5], writes=[Tss8])
                        k.op("act", lambda e: e.activation(out=ss8[:], in_=ss8[:], func=AF.Sqrt, bias=EPS), reads=[Tss8], writes=[Tss8])
                        k.op("dve", lambda e: e.reciprocal(out=ss8[:], in_=ss8[:]), reads=[Tss8], writes=[Tss8])
                        k.op("dve", lambda e: e.tensor_tensor(out=qf[:, :, 0:64], in0=qf[:, :, 0:64], in1=ss8[:, 0:4, None].to_broadcast([128, 4, 64]), op=ALU.mult),
                             reads=[Tqf, Tss8], writes=[Tqf])
                        k.op("dve", lambda e: e.tensor_tensor(out=Qb[:, :, 0:64], in0=qf[:, :, 0:64], in1=qnr[:, None, 0:64].to_broadcast([128, 4, 64]), op=ALU.mult),
                             reads=[Tqf, Tp5], writes=[TQb])
                        k.op("dve", lambda e: e.tensor_tensor(out=qf[:, :, 64:96], in0=qf[:, :, 64:96], in1=ss8[:, 4:8, None].to_broadcast([128, 4, 32]), op=ALU.mult),
                             reads=[Tqf, Tss8], writes=[Tqf])
                        k.op("dve", lambda e: e.tensor_tensor(out=rb[:, 0:4, :], in0=qf[:, :, 64:96], in1=qnr[:, None, 64:96].to_broadcast([128, 4, 32]), op=ALU.mult),
                             reads=[Tqf, Tp5], writes=[Trb])
                        k.op("dve", lambda e: e.tensor_tensor(out=kvf[:, :, 0:64], in0=kvf[:, :, 0:64], in1=ss8[:, 8:12, None].to_broadcast([128, 4, 64]), op=ALU.mult),
                             reads=[Tkvf, Tss8], writes=[Tkvf])
                        k.op("dve", lambda e: e.tensor_tensor(out=Kb[:, :, 0:64], in0=kvf[:, :, 0:64], in1=knr[:, None, 0:64].to_broadcast([128, 4, 64]), op=ALU.mult),
                             reads=[Tkvf, Tp5], writes=[TKb])
                        k.op("pool", lambda e: e.tensor_copy(out=Va[:, ti, :, 0:64], in_=kvf[:, :, 64:128]), reads=[Tkvf], writes=[TVa])
                        if ti >= 2:
                            rb4 = rb[:].rearrange("p h (j two) -> p h j two", two=2)
                            cs = rope[:, ti - 2, 0, None, :].to_broadcast([128, 5, 16]); sn = rope[:, ti - 2, 1, None, :].to_broadcast([128, 5, 16])
                            k.op("dve", lambda e: e.tensor_tensor(out=ra[:, 0], in0=rb4[:, :, :, 0], in1=cs, op=ALU.mult), reads=[Trb, Tp5], writes=[Tra])
                            k.op("dve", lambda e: e.tensor_tensor(out=ra[:, 1], in0=rb4[:, :, :, 1], in1=sn, op=ALU.mult), reads=[Trb, Tp5], writes=[Tra])
                            k.op("pool", lambda e: e.tensor_tensor(out=ra[:, 2], in0=rb4[:, :, :, 0], in1=sn, op=ALU.mult), reads=[Trb, Tp5], writes=[Tra])
                            k.op("pool", lambda e: e.tensor_tensor(out=ra[:, 3], in0=rb4[:, :, :, 1], in1=cs, op=ALU.mult), reads=[Trb, Tp5], writes=[Tra])
                            k.op("dve", lambda e: e.tensor_tensor(out=rb4[:, :, :, 0], in0=ra[:, 0], in1=ra[:, 1], op=ALU.subtract), reads=[Tra, Trb], writes=[Trb])
                            k.op("dve", lambda e: e.tensor_tensor(out=rb4[:, :, :, 1], in0=ra[:, 2], in1=ra[:, 3], op=ALU.add), reads=[Tra, Trb], writes=[Trb])
                        k.op("dve", lambda e: e.tensor_copy(out=Qb[:, :, 64:96], in_=rb[:, 0:4, :]), reads=[Trb], writes=[TQb])
                        k.op("dve", lambda e: e.tensor_copy(out=Kb[:, :, 64:96], in_=rb[:, 4:5, :].to_broadcast([128, 4, 32])), reads=[Trb], writes=[TKb])
                        for (src, tsrc, dstT, tdst, pi) in ((Qb, TQb, QT, TQT, 1), (Kb, TKb, KT, TKT, 0)):
                            pt = p_tr[pi]; tpt = Tp_tr[pi]
                            for h in range(4):
                                k.op("pe", lambda e: e.transpose(pt[0:96, h, :], src[:, h, :], ident_b[:]), reads=[tsrc, Tc], writes=[tpt])
                            if pi == 1:
                                k.op("act", lambda e: e.activation(out=dstT[:, :, ti * 128:(ti + 1) * 128], in_=pt[0:96, :, :], func=AF.Copy), reads=[tpt], writes=[tdst])
                            else:
                                k.op("dve", lambda e: e.tensor_copy(out=dstT[:, :, ti * 128:(ti + 1) * 128], in_=pt[0:96, :, :]), reads=[tpt], writes=[tdst])
                    ps_prep.__exit__(None, None, None)
                    ps_att = PScope(k); ps_att.__enter__()
                    p_tr = [k.ps("p5trC%d" % i, [128, 4, 128], BF16) for i in range(2)]; Tp_tr = [PT(), PT()]
                    PTt = [k.sb("PT%d" % i, [128, 512], BF16) for i in range(3)]; TPT = [T() for _ in range(3)]
                    p_s = [k.ps("p_s%d" % i, [128, 512]) for i in range(2)]; Tp_s = [PT(), PT()]
                    p_o = [k.ps("p_o%d" % i, [128, 4, 65]) for i in range(2)]; Tp_o = [PT(), PT()]
                    rec = k.sb("rec", [128, 4]); Trec = T()
                    ym = k.sb("ym", [128, NT, 256], BF16); Tym = T()
                    sc = 96.0 ** -0.5
                    nsc = 0; nh = 0
                    qblocks = BLOCKS if need_ctx else BLOCKS[1:]
                    its = []
                    for (q0, qn_) in qblocks:
                        keys = list(range(0, 2) if q0 == 0 else range(NT))
                        for h in range(4):
                            for ki, kt_ in enumerate(keys):
                                its.append((q0, qn_, h, ki, kt_, len(keys), len(its)))

                    def att_qk(q0, qn_, h, ki, kt_, nk, j):
                        ps_ = p_s[j % 2]; tps = Tp_s[j % 2]; P = PTt[j % 3]; tP = TPT[j % 3]
                        k.op("pe", lambda e: e.matmul(ps_[:, 0:qn_], lhsT=KT[:, h, kt_ * 128:(kt_ + 1) * 128], rhs=QT[:, h, q0:q0 + qn_], start=True, stop=True),
                             reads=[TKT, TQT], writes=[tps])
                        k.op("act", lambda e: e.activation(out=P[:, 0:qn_], in_=ps_[:, 0:qn_], func=AF.Exp, scale=sc), reads=[tps], writes=[tP])

                    def att_pv(q0, qn_, h, ki, kt_, nk, j):
                        P = PTt[j % 3]; tP = TPT[j % 3]
                        grp = j // 1
                        nq = qn_ // 128
                        gidx = (q0, h)
                        if ki == 0:
                            att_state["nh"] += 1
                        po = p_o[att_state["nh"] % 2]; tpo = Tp_o[att_state["nh"] % 2]
                        for qs_ in range(nq):
                            k.op("pe", lambda e: e.matmul(po[:, qs_, :], lhsT=P[:, qs_ * 128:(qs_ + 1) * 128], rhs=Va[:, kt_, h, :],
                                                          start=(ki == 0 and qs_ == 0), stop=(ki == nk - 1), skip_group_check=True),
                                 reads=[tP, TVa], writes=[tpo])
                        if ki == nk - 1:
                            k.op("dve", lambda e: e.reciprocal(out=rec[:, 0:nq], in_=po[:, 0:nq, 64]), reads=[tpo], writes=[Trec])
                            t_0 = q0 // 128
                            k.op("dve", lambda e: e.tensor_tensor(out=ym[:, t_0:t_0 + nq, h * 64:(h + 1) * 64], in0=po[:, 0:nq, 0:64],
                                                                  in1=rec[:, 0:nq, None].to_broadcast([128, nq, 64]), op=ALU.mult), reads=[tpo, Trec], writes=[Tym])

                    att_state = {"nh": 0}
                    att_qk(*its[0])
                    for j in range(len(its)):
                        if j + 1 < len(its):
                            att_qk(*its[j + 1])
                        att_pv(*its[j])
                    yoT = k.sb("yoT5", [128, 2, S], BF16); TyoT = T()
                    if not need_ctx:
                        k.op("pool", lambda e: e.memset(yoT[:, :, 0:NCTX], 0.0), writes=[TyoT])
                    ntr = 0
                    for ti in range(0 if need_ctx else 2, NT):
                        for ch in range(2):
                            pp = p_tr[ntr % 2]; tp = Tp_tr[ntr % 2]
                            k.op("pe", lambda e: e.transpose(pp[:, 0, :], ym[:, ti, ch * 128:(ch + 1) * 128], ident_b[:]), reads=[Tym, Tc], writes=[tp])
                            if ntr % 2 == 0:
                                k.op("act", lambda e: e.activation(out=yoT[:, ch, ti * 128:(ti + 1) * 128], in_=pp[:, 0, :], func=AF.Copy), reads=[tp], writes=[TyoT])
                            else:
                                k.op("dve", lambda e: e.tensor_copy(out=yoT[:, ch, ti * 128:(ti + 1) * 128], in_=pp[:, 0, :]), reads=[tp], writes=[TyoT])
                            ntr += 1
                    k.dma("sp", yT_d[b, 768:1024, :].rearrange("(c p) t -> p c t", p=128), yoT[:], reads=[TyoT], writes=[TyT[b]])
                    ps_att.__exit__(None, None, None)
                if cfg.upto < 6:
                    continue
                need_ctx = l < L - 1
                with Stage(k):
                    Tp6 = T()
                    wo = k.sb("wo", [128, 8, D], BF16); wrt = k.sb("wrt", [128, 8, NEXP], BF16)
                    cst = Caster(k, 128, D)
                    for kc in range(8):
                        cst.load(wo[:, kc, :], w_out_d[l, kc * 128:(kc + 1) * 128, :], 128, D, [Tp6])
                    wrs = k.sb("wrs", [128, 8, NEXP]); Twrs = T()
                    k.dma("sp", wrs[:], w_rt_d[l].rearrange("(c p) e -> p c e", p=128), writes=[Twrs])
                    k.op("pool", lambda e: e.tensor_copy(out=wrt[:], in_=wrs[:]), reads=[Twrs], writes=[Tp6])
                    G2 = k.sb("G2", [128, 2, 8]); Tg2 = T()
                    for i, mi in enumerate((b, 2)):
                        k.op("dve", lambda e: e.scalar_tensor_tensor(out=G2[:, i, :], in0=modT[:, l, 32:40, mi], scalar=1.0, in1=n2T[:, l, :],
                                                                    op0=ALU.add, op1=ALU.mult), reads=[Tmod, Tc], writes=[Tg2])
                    Yb = [k.sb("Yb%d" % i, [128, 8, 512], BF16) for i in range(2)]; TYb = [T(), T()]
                    Xb = [k.sb("Xb%d" % i, [128, 8, 512]) for i in range(2)]; TXb = [T(), T()]
                    Qs = k.sb("Qs", [128, 8, 512], BF16); TQs = T()
                    Rr6 = k.sb("Rr6", [128, 512]); TRr6 = T()
                    tm6 = [k.sb("tm6_%d" % i, [128, 512]) for i in range(2)]; Ttm6 = [T(), T()]
                    H2 = k.sb("H2", [128, 8, 512], BF16); TH2 = T()
                    h2o = [k.sb("h2o%d" % i, [128, D], BF16) for i in range(2)]; Th2o = [T(), T()]
                    Ee = k.sb("Ee", [16, 512]); TEe = T()
                    rc6 = k.sb("rc6", [16, 512]); Trc6 = T()
                    pwo = [k.ps("pwo%d" % i, [128, 512]) for i in range(2)]; Tpwo = [PT(), PT()]
                    pss = k.ps("pss6", [128, 512]); Tpss = PT()
                    ptr = [k.ps("ptr6_%d" % i, [128, 8, 128], BF16) for i in range(2)]; Tptr = [PT(), PT()]
                    prl = k.ps("prl", [16, 512]); Tprl = PT()
                    prs = k.ps("prs", [16, 512]); Tprs = PT()
                    ntr = 0
                    for bi, (t0, n) in enumerate(BLOCKS if need_ctx else BLOCKS[1:]):
                        isctx = (t0 == 0)
                        seg = 1 if isctx else 0
                        mi = 2 if isctx else b
                        Y = Yb[bi % 2]; tY = TYb[bi % 2]; X = Xb[bi % 2]; tX = TXb[bi % 2]
                        k.dma("sp", Y[:, :, 0:n], yT_d[b, :, t0:t0 + n].rearrange("(c p) t -> p c t", p=128), reads=[TyT[b]], writes=[tY])
                        k.dma("sp", X[:, :, 0:n], xT_d[b, :, t0:t0 + n].rearrange("(c p) t -> p c t", p=128), reads=[Tx[b]], writes=[tX])
                        for dch in range(8):
                            pp = pwo[dch % 2]; tp = Tpwo[dch % 2]
                            for c_ in range(8):
                                k.op("pe", lambda e: e.matmul(pp[:, 0:n], lhsT=wo[:, c_, dch * 128:(dch + 1) * 128], rhs=Y[:, c_, 0:n], start=(c_ == 0), stop=(c_ == 7)),
                                     reads=[Tp6, tY], writes=[tp])
                            k.op("dve", lambda e: e.scalar_tensor_tensor(out=X[:, dch, 0:n], in0=pp[:, 0:n], scalar=modT[:, l, 16 + dch, mi:mi + 1], in1=X[:, dch, 0:n],
                                                                        op0=ALU.mult, op1=ALU.add), reads=[tp, Tmod, tX], writes=[tX])
                        k.dma("sp", xT_d[b, :, t0:t0 + n].rearrange("(c p) t -> p c t", p=128), X[:, :, 0:n], reads=[tX], writes=[Tx[b]])
                        k.op("act", lambda e: e.activation(out=Qs[:, :, 0:n], in_=X[:, :, 0:n], func=AF.Square), reads=[tX], writes=[TQs])
                        for kc in range(8):
                            k.op("pe", lambda e: e.matmul(pss[:, 0:n], lhsT=ones_b[:], rhs=Qs[:, kc, 0:n], start=(kc == 0), stop=(kc == 7)), reads=[TQs, Tc], writes=[Tpss])
                        k.op("act", lambda e: e.activation(out=Rr6[:, 0:n], in_=pss[:, 0:n], func=AF.Sqrt, scale=1.0 / D, bias=EPS), reads=[Tpss], writes=[TRr6])
                        k.op("dve", lambda e: e.reciprocal(out=Rr6[:, 0:n], in_=Rr6[:, 0:n]), reads=[TRr6], writes=[TRr6])
                        for kc in range(8):
                            tm = tm6[kc % 2]; ttm = Ttm6[kc % 2]
                            k.op("dve", lambda e: e.tensor_tensor(out=tm[:, 0:n], in0=X[:, kc, 0:n], in1=Rr6[:, 0:n], op=ALU.mult), reads=[tX, TRr6], writes=[ttm])
                            k.op("act", lambda e: e.activation(out=H2[:, kc, 0:n], in_=tm[:, 0:n], func=AF.Identity, scale=G2[:, seg, kc:kc + 1],
                                                               bias=modT[:, l, 24 + kc, mi:mi + 1]), reads=[ttm, Tg2, Tmod], writes=[TH2])
                        for tt in range(n // 128):
                            pp = ptr[ntr % 2]; tp = Tptr[ntr % 2]; ho = h2o[ntr % 2]; tho = Th2o[ntr % 2]
                            for kc in range(8):
                                k.op("pe", lambda e: e.transpose(pp[:, kc, :], H2[:, kc, tt * 128:(tt + 1) * 128], ident_b[:]), reads=[TH2, Tc], writes=[tp])
                            if ntr % 2 == 0:
                                k.op("act", lambda e: e.activation(out=ho[:], in_=pp[:].rearrange("p c t -> p (c t)"), func=AF.Copy), reads=[tp], writes=[tho])
                            else:
                                k.op("dve", lambda e: e.tensor_copy(out=ho[:], in_=pp[:].rearrange("p c t -> p (c t)")), reads=[tp], writes=[tho])
                            k.dma("sp", h2t_d[b, t0 + tt * 128:t0 + (tt + 1) * 128, :], ho[:], reads=[tho], writes=[Th2[b]])
                            ntr += 1
                        for kc in range(8):
                            k.op("pe", lambda e: e.matmul(prl[:, 0:n], lhsT=wrt[:, kc, :], rhs=H2[:, kc, 0:n], start=(kc == 0), stop=(kc == 7)), reads=[Tp6, TH2], writes=[Tprl])
                        k.op("act", lambda e: e.activation(out=Ee[:, 0:n], in_=prl[:, 0:n], func=AF.Exp), reads=[Tprl], writes=[TEe])
                        k.op("pe", lambda e: e.matmul(prs[:, 0:n], lhsT=ones_f[0:16, 0:16], rhs=Ee[:, 0:n], start=True, stop=True), reads=[TEe, Tc], writes=[Tprs])
                        k.op("dve", lambda e: e.reciprocal(out=rc6[:, 0:n], in_=prs[:, 0:n]), reads=[Tprs], writes=[Trc6])
                        k.op("dve", lambda e: e.tensor_tensor(out=rc6[:, 0:n], in0=rc6[:, 0:n], in1=Ee[:, 0:n], op=ALU.mult), reads=[Trc6, TEe], writes=[Trc6])
                        k.dma("sp", aff_d[b, :, t0:t0 + n], rc6[:, 0:n], reads=[Trc6], writes=[Taff[b]])
                if cfg.upto < 7:
                    continue
                need_ctx = l < L - 1
                last = (l == cfg.layers - 1)
                segl = [(NCTX, NLAT, 256)] + ([(0, NCTX, 32)] if need_ctx else [])
                if l in getattr(cfg, 'skip_moe', ()) or b in getattr(cfg, 'skip_moe_b', ()):
                    segl = []
                segl = segl[:getattr(cfg, 'max_seg', 2)]
                for (t0, N, cap) in segl:
                  isctx = (t0 == 0)
                  mi = 2 if isctx else b
                  ntile = N // 128; nst = max(1, cap // 128); sp = min(cap, 128)
                  with Stage(k):
                    Tp7 = T()
                    iotaf = k.sb("iotaf", [128, 256]); iotap = k.sb("iotap", [128, 2])
                    for dst, src in ((iotaf, iotaf_d), (iotap, iotap_d)):
                        k.dma("sp", dst[:], src, writes=[Tp7])
                    slotm = k.sb("slotm", [16, N]); Tslot = T()
                    slotT = k.sb("slotT", [128, ntile, 16]); TslotT = T()
                    ghlT = k.sb("ghlT", [128, ntile, 16, 2], BF16); TghlT = T()
                    ysb = k.sb("ysb", [128, NEXP, nst, D], BF16); Tysb = T()
                    with Stage(k):
                        ones16 = k.sb("ones16", [16, NLAT])
                        k.dma("sp", ones16[:], ones16_d, writes=[Tp7])
                        affs = k.sb("affs", [16, N]); Taffs = T()
                        work = k.sb("work", [16, N]); Twork = T()
                        m8 = k.sb("m8", [16, 8]); Tm8 = T()
                        mask = k.sb("mask", [16, N]); Tmask = T()
                        ghi = k.sb("ghi", [16, N], BF16); glo = k.sb("glo", [16, N], BF16); Tg = T()
                        k.dma("sp", affs[:], aff_d[b, :, t0:t0 + N], reads=[Taff[b]], writes=[Taffs])
                        k.op("act", lambda e: e.activation(out=work[:], in_=affs[:], func=AF.Copy), reads=[Taffs], writes=[Twork])
                        for it_ in range(cap // 8):
                            k.op("dve", lambda e: e.max(out=m8[:], in_=work[:]), reads=[Twork], writes=[Tm8])
                            k.op("dve", lambda e: e.match_replace(out=work[:], in_to_replace=m8[:], in_values=work[:], imm_value=-1.0), reads=[Tm8, Twork], writes=[Twork])
                        k.op("dve", lambda e: e.tensor_scalar(out=mask[:], in0=work[:], scalar1=0.0, scalar2=None, op0=ALU.is_lt), reads=[Twork], writes=[Tmask])
                        k.op("dve", lambda e: e.tensor_tensor_scan(out=slotm[:], data0=ones16[:, 0:N], data1=mask[:], initial=0.0, op0=ALU.mult, op1=ALU.add),
                             reads=[Tmask, Tp7], writes=[Tslot])
                        k.op("dve", lambda e: e.tensor_tensor(out=slotm[:], in0=slotm[:], in1=mask[:], op=ALU.mult), reads=[Tslot, Tmask], writes=[Tslot])
                        k.op("dve", lambda e: e.tensor_scalar(out=slotm[:], in0=slotm[:], scalar1=-1.0, scalar2=None, op0=ALU.add), reads=[Tslot], writes=[Tslot])
                        k.op("dve", lambda e: e.tensor_tensor(out=affs[:], in0=affs[:], in1=mask[:], op=ALU.mult), reads=[Taffs, Tmask], writes=[Taffs])
                        k.op("dve", lambda e: e.tensor_copy(out=ghi[:], in_=affs[:]), reads=[Taffs], writes=[Tg])
                        k.op("dve", lambda e: e.tensor_tensor(out=affs[:], in0=affs[:], in1=ghi[:], op=ALU.subtract), reads=[Taffs, Tg], writes=[Taffs])
                        k.op("dve", lambda e: e.tensor_copy(out=glo[:], in_=affs[:]), reads=[Taffs], writes=[Tg])
                        pst = k.ps("pst", [128, ntile, 16]); Tpst = PT()
                        pgh = k.ps("pgh", [128, ntile, 2, 16], BF16); Tpgh = PT()
                        for ti in range(ntile):
                            cs_ = slice(ti * 128, (ti + 1) * 128)
                            k.op("pe", lambda e: e.transpose(pst[:, ti, :], slotm[:, cs_], ident_f[0:16, 0:16]), reads=[Tslot, Tc], writes=[Tpst])
                            k.op("pe", lambda e: e.transpose(pgh[:, ti, 0, :], ghi[:, cs_], ident_b[0:16, 0:16]), reads=[Tg, Tc], writes=[Tpgh])
                            k.op("pe", lambda e: e.transpose(pgh[:, ti, 1, :], glo[:, cs_], ident_b[0:16, 0:16]), reads=[Tg, Tc], writes=[Tpgh])
                        k.op("dve", lambda e: e.tensor_copy(out=slotT[:], in_=pst[:]), reads=[Tpst], writes=[TslotT])
                        k.op("dve", lambda e: e.tensor_copy(out=ghlT[:].rearrange("p n e h -> p n h e"), in_=pgh[:]), reads=[Tpgh], writes=[TghlT])
                    with Stage(k):
                        h2k = k.sb("h2k", [128, ntile, D], BF16); Th2k = T()
                        k.dma("sp", h2k[:], h2t_d[b, t0:t0 + N, :].rearrange("(n p) d -> p n d", p=128), reads=[Th2[b]], writes=[Th2k])
                        wg = [k.sb("wg%d" % i, [128, 8, FF], BF16) for i in range(2)]
                        wu = [k.sb("wu%d" % i, [128, 8, FF], BF16) for i in range(2)]
                        wd = [k.sb("wd%d" % i, [128, 4, D], BF16) for i in range(2)]
                        Tw = [T(), T()]
                        Sel = [k.sb("Sel%d" % i, [128, ntile, cap], BF16) for i in range(1)] * 2; TSel = [T()] * 2
                        cst7 = Caster(k, 128, 2048, nbuf=4)
                        xsTL = [k.sb("xsT%d" % i, [128, 8, cap], BF16) for i in range(1)] * 2; TxsTL = [T()] * 2
                        sg = [k.sb("sg%d" % i, [128, cap]) for i in range(2)]; Tsg = [T(), T()]
                        actTL = [k.sb("actT%d" % i, [128, 4, cap], BF16) for i in range(1)] * 2; TactTL = [T()] * 2
                        gs2L = [k.sb("gs2_%d" % i, [128, nst, 2]) for i in range(2)]; gsL = [k.sb("gs_%d" % i, [128, nst]) for i in range(2)]; TgsL = [T(), T()]
                        pg = [k.ps("pg7_%d" % i, [128, cap]) for i in range(3)]; Tpg = [PT() for _ in range(3)]
                        pG = k.ps("pG", [128, cap]); TpG = PT()
                        pU = k.ps("pU", [128, cap]); TpU = PT()
                        pY = [k.ps("pY%d" % i, [128, 512]) for i in range(2)]; TpY = [PT(), PT()]
                        pgs = k.ps("pgs", [128, nst, 2]); Tpgs = PT()
                        nev = 0
                        for ex in range(NEXP):
                            i2 = ex % 2
                            xsT = xsTL[i2]; TxsT = TxsTL[i2]; actT = actTL[i2]; TactT = TactTL[i2]
                            gs2 = gs2L[i2]; gs = gsL[i2]; Tgs = TgsL[i2]
                            for (wt_, src_, c_) in ((wg[i2], w_gate_d[l, ex], 8), (wu[i2], w_up_d[l, ex], 8), (wd[i2], w_down_d[l, ex], 4)):
                                dflat = wt_[:].rearrange("p c n -> p (c n)")
                                sflat = src_.rearrange("(p c) n -> p (c n)", c=c_)
                                for pc_ in range(2):
                                    cst7.load(dflat[:, pc_ * 2048:(pc_ + 1) * 2048], sflat[:, pc_ * 2048:(pc_ + 1) * 2048], 128, 2048, [Tw[i2]])
                            SL = Sel[i2]; tSL = TSel[i2]
                            for ti in range(ntile):
                                k.op("dve", lambda e: e.tensor_scalar(out=SL[:, ti, :], in0=iotaf[:, 0:cap], scalar1=slotT[:, ti, ex:ex + 1], scalar2=None, op0=ALU.is_equal),
                                     reads=[TslotT, Tp7], writes=[tSL])
                            for dc in range(8):
                                pp = pg[dc % 3]; tp = Tpg[dc % 3]
                                for ti in range(ntile):
                                    k.op("pe", lambda e: e.matmul(pp[:], lhsT=h2k[:, ti, dc::8], rhs=SL[:, ti, :], start=(ti == 0), stop=(ti == ntile - 1)),
                                         reads=[Th2k, tSL], writes=[tp])
                                if dc % 2 == 0:
                                    k.op("act", lambda e: e.activation(out=xsT[:, dc, :], in_=pp[:], func=AF.Copy), reads=[tp], writes=[TxsT])
                                else:
                                    k.op("dve", lambda e: e.tensor_copy(out=xsT[:, dc, :], in_=pp[:]), reads=[tp], writes=[TxsT])
                            for st in range(nst):
                                for ti in range(ntile):
                                    k.op("pe", lambda e: e.matmul(pgs[0:sp, st, :], lhsT=SL[:, ti, st * 128:st * 128 + sp], rhs=ghlT[:, ti, ex, :], start=(ti == 0), stop=(ti == ntile - 1)),
                                         reads=[tSL, TghlT], writes=[Tpgs])
                            k.op("act", lambda e: e.activation(out=gs2[0:sp], in_=pgs[0:sp], func=AF.Copy), reads=[Tpgs], writes=[Tgs])
                            k.op("dve", lambda e: e.tensor_tensor(out=gs[0:sp], in0=gs2[0:sp, :, 0], in1=gs2[0:sp, :, 1], op=ALU.add), reads=[Tgs], writes=[Tgs])
                            for fc in range(4):
                                for kc in range(8):
                                    k.op("pe", lambda e: e.matmul(pG[:], lhsT=wg[i2][:, kc, fc::4], rhs=xsT[:, kc, :], start=(kc == 0), stop=(kc == 7)),
                                         reads=[Tw[i2], TxsT], writes=[TpG])
                                for kc in range(8):
                                    k.op("pe", lambda e: e.matmul(pU[:], lhsT=wu[i2][:, kc, fc::4], rhs=xsT[:, kc, :], start=(kc == 0), stop=(kc == 7)),
                                         reads=[Tw[i2], TxsT], writes=[TpU])
                                k.op("act", lambda e: e.activation(out=sg[fc % 2][:], in_=pG[:], func=AF.Silu), reads=[TpG], writes=[Tsg[fc % 2]])
                                k.op("dve", lambda e: e.tensor_tensor(out=actT[:, fc, :], in0=sg[fc % 2][:], in1=pU[:], op=ALU.mult), reads=[Tsg[fc % 2], TpU], writes=[TactT])
                            for st in range(nst):
                                for dh in range(2):
                                    pp = pY[nev % 2]; tp = TpY[nev % 2]
                                    for fc in range(4):
                                        k.op("pe", lambda e: e.matmul(pp[0:sp, :], lhsT=actT[:, fc, st * 128:st * 128 + sp], rhs=wd[i2][:, fc, dh * 512:(dh + 1) * 512],
                                                                      start=(fc == 0), stop=(fc == 3)), reads=[TactT, Tw[i2]], writes=[tp])
                                    dsto = ysb[0:sp, ex, st, dh * 512:(dh + 1) * 512]
                                    if nev % 2 == 0:
                                        k.op("act", lambda e: e.activation(out=dsto, in_=pp[0:sp, :], func=AF.Copy, scale=gs[0:sp, st:st + 1]), reads=[tp, Tgs], writes=[Tysb])
                                    else:
                                        k.op("dve", lambda e: e.tensor_scalar(out=dsto, in0=pp[0:sp, :], scalar1=gs[0:sp, st:st + 1], scalar2=None, op0=ALU.mult),
                                             reads=[tp, Tgs], writes=[Tysb])
                                    nev += 1
                    with Stage(k):
                        oneh = k.sb("oneh", [16, 16, 128]); Toneh = T()
                        k.dma("sp", oneh[:], oneh_d, writes=[Toneh])
                        SelT = k.sb("SelT", [128, NEXP, nst, 512], BF16); TSelT = T()
                        Xc = [k.sb("Xc%d" % i, [128, 8, 512]) for i in range(2)]; TXc = [T(), T()]
                        ot = [k.sb("ot%d" % i, [128, D]) for i in range(2)]; Tot = [T(), T()]
                        pb = [k.ps("pb%d" % i, [128, 512]) for i in range(2)]; Tpb = [PT(), PT()]
                        pc = [k.ps("pc%d" % i, [128, 512]) for i in range(4)]; Tpc = [PT() for _ in range(4)]
                        po_ = [k.ps("po7_%d" % i, [128, 4, 128]) for i in range(2)]; Tpo_ = [PT(), PT()]
                        nto = 0
                        nblk = max(1, N // 512)
                        for tb in range(nblk):
                            n = min(N, 512)
                            c0 = tb * 512
                            X = Xc[tb % 2]; tX = TXc[tb % 2]
                            k.dma("sp", X[:, :, 0:n], xT_d[b, :, t0 + c0:t0 + c0 + n].rearrange("(c p) t -> p c t", p=128), reads=[Tx[b]], writes=[tX])
                            for ex in range(NEXP):
                                pp = pb[ex % 2]; tp = Tpb[ex % 2]
                                k.op("pe", lambda e: e.matmul(pp[:, 0:n], lhsT=oneh[:, ex, :], rhs=slotm[:, c0:c0 + n], start=True, stop=True), reads=[Tslot, Toneh], writes=[tp])
                                for st in range(nst):
                                    k.op("dve", lambda e: e.tensor_scalar(out=SelT[:, ex, st, 0:n], in0=pp[:, 0:n], scalar1=iotap[:, st:st + 1], scalar2=None, op0=ALU.is_equal),
                                         reads=[tp, Tp7], writes=[TSelT])
                            for dh in range(2):
                                for dcl in range(4):
                                    dc = dh * 4 + dcl
                                    pp = pc[dcl]; tp = Tpc[dcl]
                                    for ex in range(NEXP):
                                        for st in range(nst):
                                            k.op("pe", lambda e: e.matmul(pp[:, 0:n], lhsT=ysb[0:sp, ex, st, dc * 128:(dc + 1) * 128], rhs=SelT[0:sp, ex, st, 0:n],
                                                                          start=(ex == 0 and st == 0), stop=(ex == NEXP - 1 and st == nst - 1)),
                                                 reads=[Tysb, TSelT], writes=[tp])
                                    k.op("dve", lambda e: e.scalar_tensor_tensor(out=X[:, dc, 0:n], in0=pp[:, 0:n], scalar=modT[:, l, 40 + dc, mi:mi + 1], in1=X[:, dc, 0:n],
                                                                                op0=ALU.mult, op1=ALU.add), reads=[tp, Tmod, tX], writes=[tX])
                            if not last:
                                k.dma("sp", xT_d[b, :, t0 + c0:t0 + c0 + n].rearrange("(c p) t -> p c t", p=128), X[:, :, 0:n], reads=[tX], writes=[Tx[b]])
                            elif not isctx:
                                for tt in range(n // 128):
                                    O = ot[nto % 2]; tO = Tot[nto % 2]
                                    for half in range(2):
                                        pp = po_[half]; tp = Tpo_[half]
                                        for q in range(4):
                                            dc = half * 4 + q
                                            k.op("pe", lambda e: e.transpose(pp[:, q, :], X[:, dc, tt * 128:(tt + 1) * 128], ident_f[:]), reads=[tX, Tc], writes=[tp])
                                        if half == 0:
                                            k.op("act", lambda e: e.activation(out=O[:, 0:512], in_=pp[:].rearrange("p q t -> p (q t)"), func=AF.Copy), reads=[tp], writes=[tO])
                                        else:
                                            k.op("dve", lambda e: e.tensor_copy(out=O[:, 512:1024], in_=pp[:].rearrange("p q t -> p (q t)")), reads=[tp], writes=[tO])
                                    tok0 = c0 + tt * 128
                                    k.dma("sp", out_d[b, tok0:tok0 + 128, :], O[:], reads=[tO], writes=[Tout])
                                    nto += 1

        k.barrier()
        global LAST_K
        LAST_K = k
    return nc


def prep_inputs(inp, core, nb=2):
    b0 = core * 2
    m = {}
    m["x"] = np.ascontiguousarray(inp["x"][b0:b0 + nb])
    m["ctx"] = np.ascontiguousarray(inp["ctx"][b0:b0 + nb])
    cc = np.stack([inp["c"][b0], inp["c"][b0 + 1], inp["c_ctx"]], axis=0)
    m["cT"] = np.ascontiguousarray(fm(cc, 8).transpose(0, 2, 1))
    m["ada_w"] = inp["ada_w"]
    m["ada_bT"] = fm(inp["ada_b"], 48)
    m["n1T"] = fm(inp["norm1_w"], 8)
    m["n2T"] = fm(inp["norm2_w"], 8)
    m["w_in"] = inp["w_in"]
    m.update(prep_lru(inp))
    m["hg_lbT"] = fm(inp["hgrn_lb_logits"], 2)
    m["hg_nwT"] = fm(inp["hgrn_norm_w"], 2)
    scw = np.asarray(inp["ssd_conv_w"], np.float32)
    m["sd_cw"] = np.ascontiguousarray(scw.reshape(L, 4, 4, 128).transpose(3, 0, 2, 1))
    m["sd_cb"] = fm(inp["ssd_conv_b"], 4)
    rep = lambda v: np.ascontiguousarray(np.broadcast_to(np.asarray(v, np.float32)[None], (128,) + tuple(np.shape(v))))
    m["sd_alog"] = rep(np.asarray(inp["ssd_a_log"]).reshape(L, 8))
    m["sd_dtb"] = rep(np.asarray(inp["ssd_dt_bias"]).reshape(L, 8))
    m["sd_dsk"] = rep(np.repeat(np.asarray(inp["ssd_d_skip"]), 64, axis=-1))
    m["sd_nw"] = rep(inp["ssd_norm_w"])
    m["ml_qan"] = rep(inp["mla_q_a_norm"]); m["ml_kvan"] = rep(inp["mla_kv_a_norm"])
    m["ml_qn"] = rep(inp["mla_q_norm"]); m["ml_kn"] = rep(inp["mla_k_norm"])
    m["ml_wq"] = inp["mla_w_q_up"]; m["ml_wkv"] = inp["mla_w_kv_up"]
    m["w_out"] = inp["w_out"]; m["w_rt"] = inp["moe_router"]
    m["w_gate"] = inp["moe_w_gate"]; m["w_up"] = inp["moe_w_up"]; m["w_down"] = inp["moe_w_down"]
    m.update(host_consts())
    return m


def kernel(**inputs):
    inp = {k_: np.asarray(v) for k_, v in inputs.items()}
    cfg = Cfg(nb=2)
    nc = build(cfg)
    in_maps = [prep_inputs(inp, c) for c in range(8)]
    res = run_bass_kernel_spmd(nc, in_maps, core_ids=list(range(8)))
    out = np.concatenate([r["out"] for r in res.results], axis=0)
    return out.astype(np.float32)
```

```python
import math
from contextlib import ExitStack
import numpy as np
import ml_dtypes
import concourse.bass as bass
import concourse.mybir as mybir
from concourse.bass_utils import run_bass_kernel_spmd

F32 = mybir.dt.float32
BF16 = mybir.dt.bfloat16
I32 = mybir.dt.int32
U32 = mybir.dt.uint32
ALU = mybir.AluOpType
AF = mybir.ActivationFunctionType
AX = mybir.AxisListType

L = 2
D = 1024
NLAT = 2048
NCTX = 256
S = NCTX + NLAT
NT = S // 128
IN_COLS = 2920
EPS = 1e-6
NEXP = 16
FF = 512
TM_RANGES = [(1280, 1536), (1792, 2048), (2560, 2920)]
TM_COLS = sum(b - a for a, b in TM_RANGES)
FM_CHUNKS = list(range(0, 10)) + [12, 13] + [16, 17, 18, 19]
BLOCKS = [(0, 256)] + [(256 + 512 * i, 512) for i in range(4)]


class T:
    __slots__ = ("name", "w", "r", "x")

    def __init__(self, name="", x=False):
        self.name = name
        self.w = None
        self.r = []
        self.x = x


def PT():
    return T("psum", True)


class _Rec:
    def __init__(self):
        self.call = None

    def __getattr__(self, name):
        def f(*a, **kw):
            self.call = (name, a, kw)
            return self
        return f


class K:
    N_DMA_SEMS = 24

    def __init__(self, nc, stack):
        self.nc = nc
        self.stack = stack
        self.eng = {"pe": nc.tensor, "dve": nc.vector, "act": nc.scalar,
                    "pool": nc.gpsimd, "sp": nc.sync}
        self.sems = {}
        self.count = {}
        self.seen = {e: {} for e in self.eng}
        for e in self.eng:
            self.sems[e] = stack.enter_context(nc.semaphore("s_" + e))
            self.count[e] = 0
        for i in range(self.N_DMA_SEMS):
            k = "d%d" % i
            self.sems[k] = stack.enter_context(nc.semaphore("s_" + k))
            self.count[k] = 0
        self.dma_rr = 0
        self.n_inst = 0
        self.scope = stack

    def sb(self, name, shape, dtype=F32):
        self.n_alloc = getattr(self, "n_alloc", 0) + 1
        return self.scope.enter_context(self.nc.sbuf_tensor("sb%d_%s" % (self.n_alloc, name), list(shape), dtype))

    def ps(self, name, shape, dtype=F32):
        self.n_alloc = getattr(self, "n_alloc", 0) + 1
        nel = 512 if dtype == F32 else 1024
        scope = getattr(self, "pscope", None) or self.scope
        full = scope.enter_context(self.nc.psum_tensor("ps%d_%s" % (self.n_alloc, name), [128, nel], dtype))
        n = 1
        for d_ in shape[1:]:
            n *= d_
        assert n <= nel, (name, shape)
        v = full[0:shape[0], 0:n]
        if len(shape) == 3:
            v = v.rearrange("p (a b) -> p a b", b=shape[2])
        elif len(shape) == 4:
            v = v.rearrange("p (a b c) -> p a b c", b=shape[2], c=shape[3])
        return v

    def _waits(self, e, reads, writes):
        need = {}
        for t in reads:
            if t.w is not None:
                k, v, pe = t.w
                if not (pe == "pe" and e == "pe"):
                    need[k] = max(need.get(k, 0), v)
        for t in writes:
            if t.w is not None:
                k, v, pe = t.w
                if not (pe == "pe" and e == "pe"):
                    need[k] = max(need.get(k, 0), v)
            for (k, v, pe) in t.r:
                if pe == "pe" and e == "pe":
                    continue
                need[k] = max(need.get(k, 0), v)
        seen = self.seen[e]
        h = self.eng[e]
        for k, v in need.items():
            if seen.get(k, 0) < v:
                h.wait_ge(self.sems[k], v)
                seen[k] = v

    def _commit(self, tok, reads, writes):
        for t in writes:
            t.w = tok
            t.r = []
        for t in reads:
            if t not in writes:
                t.r.append(tok)
                if len(t.r) > 16:
                    best = {}
                    for (k, v, pe) in t.r:
                        if k not in best or best[k][1] < v:
                            best[k] = (k, v, pe)
                    t.r = list(best.values())

    def flush(self, pend, n=None):
        n = len(pend) if n is None else min(n, len(pend))
        for _ in range(n):
            it = pend.pop(0)
            if it[0] == "op":
                _, e, (name, a, kw), reads, writes = it
                self.op(e, lambda eng: getattr(eng, name)(*a, **kw), reads, writes)
            else:
                _, e, out, in_, reads, writes, kw = it
                self.dma(e, out, in_, reads, writes, **kw)

    def op(self, e, fn, reads=(), writes=()):
        reads = list(reads)
        writes = list(writes)
        if getattr(self, "defer", None) is not None:
            rec = _Rec()
            fn(rec)
            self.defer.append(("op", e, rec.call, reads, writes))
            return None
        xr = [t for t in reads if t.x]
        if xr:
            reads = [t for t in reads if not t.x]
            writes = writes + [t for t in xr if t not in writes]
        self._waits(e, reads, writes)
        ins = fn(self.eng[e])
        self.count[e] += 1
        ins.then_inc(self.sems[e], 1)
        self._commit((e, self.count[e], e), reads, writes)
        self.n_inst += 1
        return ins

    def dma(self, e, out, in_, reads=(), writes=(), **kw):
        reads = list(reads)
        writes = list(writes)
        if getattr(self, "defer", None) is not None:
            self.defer.append(("dma", e, out, in_, reads, writes, kw))
            return None
        self._waits(e, reads, writes)
        k = "d%d" % self.dma_rr
        self.dma_rr = (self.dma_rr + 1) % self.N_DMA_SEMS
        ins = self.eng[e].dma_start(out=out, in_=in_, **kw)
        self.count[k] += 16
        ins.then_inc(self.sems[k], 16)
        self._commit((k, self.count[k], "dma"), reads, writes)
        self.n_inst += 1
        return ins

    def barrier(self):
        for e, h in self.eng.items():
            seen = self.seen[e]
            for k, v in self.count.items():
                if v > 0 and seen.get(k, 0) < v and k != e:
                    h.wait_ge(self.sems[k], v)
                    seen[k] = v


class PScope:
    def __init__(self, k):
        self.k = k

    def __enter__(self):
        self.prev = getattr(self.k, "pscope", None)
        self.st = ExitStack()
        self.st.__enter__()
        self.k.pscope = self.st
        return self

    def __exit__(self, *a):
        self.k.barrier()
        self.k.pscope = self.prev
        return self.st.__exit__(*a)


class Caster:
    def __init__(self, k, npart, nfree, nbuf=2, eng="act"):
        self.k = k
        self.eng = eng
        self.st = [k.sb("stg%d" % i, [npart, nfree]) for i in range(nbuf)]
        self.T = [T() for _ in range(nbuf)]
        self.i = 0

    def load(self, dst, src, npart, nfree, writes, reads=()):
        k = self.k
        j = self.i % len(self.st)
        self.i += 1
        st = self.st[j][0:npart, 0:nfree]
        k.dma("sp", st, src, reads=list(reads), writes=[self.T[j]])
        if self.eng == "act":
            k.op("act", lambda e: e.activation(out=dst, in_=st, func=AF.Copy), reads=[self.T[j]], writes=list(writes))
        else:
            k.op(self.eng, lambda e: e.tensor_copy(out=dst, in_=st), reads=[self.T[j]], writes=list(writes))


class Stage:
    def __init__(self, k):
        self.k = k

    def __enter__(self):
        self.prev = self.k.scope
        self.st = ExitStack()
        self.st.__enter__()
        self.k.scope = self.st
        return self

    def __exit__(self, *a):
        self.k.barrier()
        self.k.scope = self.prev
        return self.st.__exit__(*a)


def fm(v, nch):
    v = np.asarray(v, np.float32)
    lead = v.shape[:-1]
    r = v.reshape(lead + (nch, 128))
    r = np.moveaxis(r, -1, 0)
    return np.ascontiguousarray(r)


def host_consts():
    c = {}
    c["ident_f"] = np.eye(128, dtype=np.float32)
    c["ident_b"] = np.eye(128, dtype=np.float32).astype(ml_dtypes.bfloat16)
    c["ones_b"] = np.ones((128, 128), np.float32).astype(ml_dtypes.bfloat16)
    c["ones_f"] = np.ones((128, 128), np.float32)
    t = np.arange(S)
    c["mfwd"] = np.ascontiguousarray(np.broadcast_to((t % 128 != 0).astype(np.float32), (128, S)))
    c["mbwd"] = np.ascontiguousarray(np.broadcast_to((t % 128 != 127).astype(np.float32), (128, S)))
    i = np.arange(128)
    c["triU"] = (i[:, None] <= i[None, :]).astype(np.uint32)
    c["triL"] = (i[:, None] >= i[None, :]).astype(np.uint32)
    c["triUf"] = (i[:, None] <= i[None, :]).astype(np.float32)
    c["triLf"] = (i[:, None] >= i[None, :]).astype(np.float32)
    c["strLf"] = (i[:, None] > i[None, :]).astype(np.float32)
    c["strUf"] = (i[:, None] < i[None, :]).astype(np.float32)
    tt = np.arange(NLAT)
    inv = 10000.0 ** (-np.arange(0, 16, 2, dtype=np.float32) / 16)
    ang = np.concatenate([(tt // 64).astype(np.float32)[:, None] * inv, (tt % 64).astype(np.float32)[:, None] * inv], axis=-1).astype(np.float32)
    cs = np.stack([np.cos(ang), np.sin(ang)], axis=1).astype(np.float32)
    c["rope"] = np.ascontiguousarray(cs.reshape(16, 128, 2, 16).transpose(1, 0, 2, 3))
    c["invn3"] = np.ascontiguousarray(np.broadcast_to(np.array([1 / 192, 1 / 128, 1 / 32], np.float32), (128, 3)))
    c["invn8"] = np.ascontiguousarray(np.broadcast_to(np.array([1 / 64] * 4 + [1 / 32] * 4, np.float32), (128, 8)))
    c["iotaf"] = np.ascontiguousarray(np.broadcast_to(np.arange(256, dtype=np.float32), (128, 256)))
    c["iotap"] = np.stack([i.astype(np.float32), i.astype(np.float32) + 128], axis=1)
    oh = np.zeros((16, 16, 128), np.float32)
    for e_ in range(16):
        oh[e_, e_, :] = 1.0
    c["oneh"] = oh
    c["ones16"] = np.ones((16, NLAT), np.float32)
    rep4 = lambda a_: np.ascontiguousarray(np.broadcast_to(a_[:, None, :], (128, 4, 128))).astype(np.float32)
    c["triUf4"] = rep4(c["triUf"]); c["triLf4"] = rep4(c["triLf"])
    c["negmf"] = rep4(-1.0e4 * c["strLf"]); c["negmb"] = rep4(-1.0e4 * c["strUf"])
    c["blk64"] = (i[:, None] // 64 == i[None, :] // 64).astype(np.float32).astype(ml_dtypes.bfloat16)
    return c


def prep_lru(inp):
    m = {}
    cw = np.asarray(inp["lru_conv_w"], np.float32)
    m["lru_cw"] = np.ascontiguousarray(cw.reshape(L, 4, 2, 128).transpose(3, 0, 2, 1))
    m["lru_cb"] = fm(inp["lru_conv_b"], 2)
    for nm, key in (("lru_wr", "lru_w_r"), ("lru_wi", "lru_w_i")):
        w = np.asarray(inp[key], np.float32)
        o = np.zeros((128, L, 2, 2, 128), np.float32)
        for ch in range(2):
            for hh in range(2):
                o[hh * 64:(hh + 1) * 64, :, :, ch, hh * 64:(hh + 1) * 64] = w[:, :, 2 * ch + hh].transpose(2, 0, 1, 3)
        m[nm] = o
    m["lru_br"] = fm(inp["lru_b_r"], 2)
    m["lru_bi"] = fm(inp["lru_b_i"], 2)
    m["lru_lam"] = fm(inp["lru_lam"], 2)
    return m


class Cfg:
    def __init__(self, nb=2, upto=99, debug=False, layers=2):
        self.layers = layers
        self.nb = nb
        self.upto = upto
        self.debug = debug


def build(cfg):
    nc = bass.Bass("TRN2", target_bir_lowering=False)
    NB = cfg.nb
    dbg_kind = "ExternalOutput" if cfg.debug else "Internal"

    def din(name, shape, dt=F32):
        return nc.dram_tensor(name, list(shape), dt, kind="ExternalInput").ap()

    def dscr(name, shape, dt=F32):
        return nc.dram_tensor(name, list(shape), dt, kind=dbg_kind).ap()

    x_d = din("x", [NB, NLAT, D])
    ctx_d = din("ctx", [NB, NCTX, D])
    cT_d = din("cT", [128, 8, 3])
    ada_w_d = din("ada_w", [L, D, 6 * D])
    ada_bT_d = din("ada_bT", [128, L, 48])
    n1T_d = din("n1T", [128, L, 8])
    n2T_d = din("n2T", [128, L, 8])
    w_in_d = din("w_in", [L, D, IN_COLS])
    lru_cw_d = din("lru_cw", [128, L, 2, 4])
    lru_cb_d = din("lru_cb", [128, L, 2])
    lru_wr_d = din("lru_wr", [128, L, 2, 2, 128])
    lru_wi_d = din("lru_wi", [128, L, 2, 2, 128])
    lru_br_d = din("lru_br", [128, L, 2, 2])
    lru_bi_d = din("lru_bi", [128, L, 2, 2])
    lru_lam_d = din("lru_lam", [128, L, 2, 2])
    hg_lbT_d = din("hg_lbT", [128, L, 2])
    hg_nwT_d = din("hg_nwT", [128, L, 2])
    mfwd_d = din("mfwd", [128, S])
    mbwd_d = din("mbwd", [128, S])
    triU_d = din("triU", [128, 128], U32)
    triL_d = din("triL", [128, 128], U32)
    blk64_d = din("blk64", [128, 128], BF16)
    sd_cw_d = din("sd_cw", [128, L, 4, 4])
    sd_cb_d = din("sd_cb", [128, L, 4])
    sd_alog_d = din("sd_alog", [128, L, 8])
    sd_dtb_d = din("sd_dtb", [128, L, 8])
    sd_dsk_d = din("sd_dsk", [128, L, 256])
    sd_nw_d = din("sd_nw", [128, L, 256])
    triUf_d = din("triUf", [128, 128])
    triLf_d = din("triLf", [128, 128])
    strLf_d = din("strLf", [128, 128])
    strUf_d = din("strUf", [128, 128])
    ml_qan_d = din("ml_qan", [128, L, 192])
    ml_kvan_d = din("ml_kvan", [128, L, 128])
    ml_qn_d = din("ml_qn", [128, L, 96])
    ml_kn_d = din("ml_kn", [128, L, 96])
    ml_wq_d = din("ml_wq", [L, 192, 384])
    ml_wkv_d = din("ml_wkv", [L, 128, 512])
    rope_d = din("rope", [128, 16, 2, 16])
    invn3_d = din("invn3", [128, 3])
    invn8_d = din("invn8", [128, 8])
    w_out_d = din("w_out", [L, D, D])
    w_rt_d = din("w_rt", [L, D, NEXP])
    w_gate_d = din("w_gate", [L, NEXP, D, FF])
    w_up_d = din("w_up", [L, NEXP, D, FF])
    w_down_d = din("w_down", [L, NEXP, FF, D])
    iotaf_d = din("iotaf", [128, 256])
    iotap_d = din("iotap", [128, 2])
    oneh_d = din("oneh", [16, 16, 128])
    ones16_d = din("ones16", [16, NLAT])
    triUf4_d = din("triUf4", [128, 4, 128])
    triLf4_d = din("triLf4", [128, 4, 128])
    negmf_d = din("negmf", [128, 4, 128])
    negmb_d = din("negmb", [128, 4, 128])
    ident_f_d = din("ident_f", [128, 128])
    ident_b_d = din("ident_b", [128, 128], BF16)
    ones_b_d = din("ones_b", [128, 128], BF16)
    ones_f_d = din("ones_f", [128, 128])
    out_d = nc.dram_tensor("out", [NB, NLAT, D], F32, kind="ExternalOutput").ap()

    xT_d = dscr("xT", [NB, D, S])
    uT_d = dscr("uT", [NB, IN_COLS, S])
    ut_d = dscr("ut", [NB, S, TM_COLS])
    yT_d = dscr("yT", [NB, D, S], BF16)
    TyT = [T() for b in range(NB)]
    h2t_d = dscr("h2t", [NB, S, D], BF16)
    aff_d = dscr("aff", [NB, NEXP, S])
    Th2 = [T() for b in range(NB)]
    Taff = [T() for b in range(NB)]
    Tx = [T("xT%d" % b) for b in range(NB)]
    TuT = [T() for b in range(NB)]
    Tut = [T() for b in range(NB)]
    Tout = T("out")

    with ExitStack() as root:
        k = K(nc, root)
        ident_f = k.sb("ident_f", [128, 128]); ident_b = k.sb("ident_b", [128, 128], BF16)
        ones_b = k.sb("ones_b", [128, 128], BF16); ones_f = k.sb("ones_f", [128, 128])
        modT = k.sb("modT", [128, L, 48, 3])
        n1T = k.sb("n1T", [128, L, 8]); n2T = k.sb("n2T", [128, L, 8])
        Tc = T("consts")
        Tmod = T("mod")
        k.dma("sp", ident_f[:], ident_f_d, writes=[Tc])
        k.dma("sp", ident_b[:], ident_b_d, writes=[Tc])
        k.dma("sp", ones_b[:], ones_b_d, writes=[Tc])
        k.dma("sp", ones_f[:], ones_f_d, writes=[Tc])
        k.dma("sp", n1T[:], n1T_d, writes=[Tc])
        k.dma("sp", n2T[:], n2T_d, writes=[Tc])

        with Stage(k):
            cT = k.sb("cT", [128, 8, 3]); sT = k.sb("sT", [128, 8, 3])
            abT = k.sb("abT", [128, L, 48])
            Tct = T(); Tst = T()
            k.dma("sp", cT[:], cT_d, writes=[Tct])
            k.dma("sp", abT[:], ada_bT_d, writes=[Tct])
            k.op("act", lambda e: e.activation(out=sT[:], in_=cT[:], func=AF.Silu), reads=[Tct], writes=[Tst])
            wbuf = [k.sb("adaw%d" % i, [128, 8, 512]) for i in range(2)]
            Tw = [T(), T()]
            pm = [k.ps("pm%d" % i, [128, 4, 4]) for i in range(2)]
            Tpm = [PT(), PT()]
            it = 0
            for l in range(L):
                for j in range(12):
                    wb = wbuf[it % 2]; tw = Tw[it % 2]
                    k.dma("sp", wb[:], ada_w_d[l, :, j * 512:(j + 1) * 512].rearrange("(kc p) n -> p kc n", p=128), writes=[tw])
                    pp = pm[it % 2]; tp = Tpm[it % 2]
                    for sub in range(4):
                        for kc in range(8):
                            k.op("pe", lambda e: e.matmul(pp[:, sub, 0:3], lhsT=wb[:, kc, sub * 128:(sub + 1) * 128],
                                                          rhs=sT[:, kc, :], start=(kc == 0), stop=(kc == 7)),
                                 reads=[tw, Tst], writes=[tp])
                    for sub in range(4):
                        ch = j * 4 + sub
                        k.op("dve", lambda e: e.tensor_scalar(out=modT[:, l, ch, :], in0=pp[:, sub, 0:3],
                                                              scalar1=abT[:, l, ch:ch + 1], scalar2=None, op0=ALU.add),
                             reads=[tp, Tct], writes=[Tmod])
                    it += 1

        with Stage(k):
            xin = [k.sb("xin%d" % i, [128, D]) for i in range(3)]
            Txin = [T() for _ in range(3)]
            xo = [k.sb("xo%d" % i, [128, 8, 128]) for i in range(3)]
            Txo = [T() for _ in range(3)]
            pt = [k.ps("pt%d" % i, [128, 4, 128]) for i in range(4)]
            Tpt = [PT() for _ in range(4)]
            it = 0
            for b in range(NB):
                for ti in range(NT):
                    src = ctx_d[b, ti * 128:(ti + 1) * 128, :] if ti < 2 else x_d[b, (ti - 2) * 128:(ti - 1) * 128, :]
                    xi = xin[it % 3]; txi = Txin[it % 3]
                    k.dma("sp", xi[:], src, writes=[txi])
                    xx = xo[it % 3]; txo = Txo[it % 3]
                    for half in range(2):
                        pp = pt[(2 * it + half) % 4]; tp = Tpt[(2 * it + half) % 4]
                        for q in range(4):
                            kc = half * 4 + q
                            k.op("pe", lambda e: e.transpose(pp[:, q, :], xi[:, kc * 128:(kc + 1) * 128], ident_f[:]),
                                 reads=[txi, Tc], writes=[tp])
                        if half == 0:
                            k.op("act", lambda e: e.activation(out=xx[:, 0:4, :], in_=pp[:], func=AF.Copy), reads=[tp], writes=[txo])
                        else:
                            k.op("dve", lambda e: e.tensor_copy(out=xx[:, 4:8, :], in_=pp[:]), reads=[tp], writes=[txo])
                    k.dma("sp", xT_d[b, :, ti * 128:(ti + 1) * 128].rearrange("(kc p) t -> p kc t", p=128), xx[:],
                          reads=[txo], writes=[Tx[b]])
                    it += 1

        for l in range(cfg.layers):
            if cfg.upto < 1:
                break
            for b in range(NB):
                with Stage(k):
                    w_in = k.sb("w_in", [128, 8, IN_COLS], BF16); Tw = T()
                    cst = Caster(k, 128, IN_COLS)
                    for kc in range(8):
                        cst.load(w_in[:, kc, :], w_in_d[l, kc * 128:(kc + 1) * 128, :], 128, IN_COLS, [Tw])
                    G = k.sb("G", [128, 2, 8]); Tg = T()
                    for i, mi in enumerate((b, 2)):
                        k.op("dve", lambda e: e.scalar_tensor_tensor(out=G[:, i, :], in0=modT[:, l, 8:16, mi], scalar=1.0,
                                                                    in1=n1T[:, l, :], op0=ALU.add, op1=ALU.mult),
                             reads=[Tmod, Tc], writes=[Tg])
                    xb = [k.sb("xb%d" % i, [128, 8, 512]) for i in range(2)]; Txb = [T(), T()]
                    sq = [k.sb("sq%d" % i, [128, 8, 512], BF16) for i in range(2)]; Tsq = [T(), T()]
                    rs = [k.sb("rs%d" % i, [128, 512]) for i in range(2)]; Trs = [T(), T()]
                    tmp = [k.sb("tmp%d" % i, [128, 512]) for i in range(2)]; Ttmp = [T(), T()]
                    hT = [k.sb("hT%d" % i, [128, 8, 512], BF16) for i in range(2)]; ThT = [T(), T()]
                    ev = [k.sb("ev%d" % i, [128, 512]) for i in range(4)]; Tev = [T() for _ in range(4)]
                    evt = [k.sb("evt%d" % i, [128, TM_COLS]) for i in range(2)]; Tevt = [T(), T()]
                    pss = k.ps("pss", [128, 512]); Tpss = PT()
                    pu = [k.ps("pu%d" % i, [128, 512]) for i in range(4)]; Tpu = [PT() for _ in range(4)]
                    pv = [k.ps("pv%d" % i, [128, 512]) for i in range(3)]; Tpv = [PT() for _ in range(3)]
                    nev = 0
                    for bi, (t0, n) in enumerate(BLOCKS):
                        seg = 1 if bi == 0 else 0
                        mi = 2 if bi == 0 else b
                        X = xb[bi % 2]; tX = Txb[bi % 2]
                        k.dma("sp", X[:, :, 0:n], xT_d[b, :, t0:t0 + n].rearrange("(kc p) t -> p kc t", p=128),
                              reads=[Tx[b]], writes=[tX])
                        Q = sq[bi % 2]; tQ = Tsq[bi % 2]
                        k.op("act", lambda e: e.activation(out=Q[:, :, 0:n], in_=X[:, :, 0:n], func=AF.Square), reads=[tX], writes=[tQ])
                        for kc in range(8):
                            k.op("pe", lambda e: e.matmul(pss[:, 0:n], lhsT=ones_b[:], rhs=Q[:, kc, 0:n], start=(kc == 0), stop=(kc == 7)),
                                 reads=[tQ, Tc], writes=[Tpss])
                        R = rs[bi % 2]; tR = Trs[bi % 2]
                        k.op("act", lambda e: e.activation(out=R[:, 0:n], in_=pss[:, 0:n], func=AF.Sqrt, scale=1.0 / D, bias=EPS),
                             reads=[Tpss], writes=[tR])
                        k.op("dve", lambda e: e.reciprocal(out=R[:, 0:n], in_=R[:, 0:n]), reads=[tR], writes=[tR])
                        H = hT[bi % 2]; tH = ThT[bi % 2]
                        for kc in range(8):
                            tm = tmp[kc % 2]; ttm = Ttmp[kc % 2]
                            k.op("dve", lambda e: e.tensor_tensor(out=tm[:, 0:n], in0=X[:, kc, 0:n], in1=R[:, 0:n], op=ALU.mult),
                                 reads=[tX, tR], writes=[ttm])
                            k.op("act", lambda e: e.activation(out=H[:, kc, 0:n], in_=tm[:, 0:n], func=AF.Identity,
                                                               scale=G[:, seg, kc:kc + 1], bias=modT[:, l, kc, mi:mi + 1]),
                                 reads=[ttm, Tg, Tmod], writes=[tH])
                        for ci, ch in enumerate(FM_CHUNKS):
                            c0 = ch * 128
                            pp = pu[ci % 4]; tp = Tpu[ci % 4]
                            for kc in range(8):
                                k.op("pe", lambda e: e.matmul(pp[:, 0:n], lhsT=w_in[:, kc, c0:c0 + 128], rhs=H[:, kc, 0:n],
                                                              start=(kc == 0), stop=(kc == 7)), reads=[Tw, tH], writes=[tp])
                            E = ev[nev % 4]; tE = Tev[nev % 4]
                            if nev % 2 == 0:
                                k.op("act", lambda e: e.activation(out=E[:, 0:n], in_=pp[:, 0:n], func=AF.Copy), reads=[tp], writes=[tE])
                            else:
                                k.op("dve", lambda e: e.tensor_copy(out=E[:, 0:n], in_=pp[:, 0:n]), reads=[tp], writes=[tE])
                            k.dma("sp", uT_d[b, c0:c0 + 128, t0:t0 + n], E[:, 0:n], reads=[tE], writes=[TuT[b]])
                            nev += 1
                        for tt in range(n // 128):
                            ET = evt[tt % 2]; tET = Tevt[tt % 2]
                            off = 0
                            for ri, (a, bnd) in enumerate(TM_RANGES):
                                w = bnd - a
                                pp = pv[ri]; tp = Tpv[ri]
                                for kc in range(8):
                                    k.op("pe", lambda e: e.matmul(pp[:, 0:w], lhsT=H[:, kc, tt * 128:(tt + 1) * 128], rhs=w_in[:, kc, a:bnd],
                                                                  start=(kc == 0), stop=(kc == 7)), reads=[Tw, tH], writes=[tp])
                                if ri == 1:
                                    k.op("act", lambda e: e.activation(out=ET[:, off:off + w], in_=pp[:, 0:w], func=AF.Copy), reads=[tp], writes=[tET])
                                else:
                                    k.op("dve", lambda e: e.tensor_copy(out=ET[:, off:off + w], in_=pp[:, 0:w]), reads=[tp], writes=[tET])
                                off += w
                            k.dma("sp", ut_d[b, t0 + tt * 128:t0 + (tt + 1) * 128, :], ET[:], reads=[tET], writes=[Tut[b]])
                if cfg.upto < 2:
                    continue
                with Stage(k):
                    cw = k.sb("cw", [128, 2, 4]); cb = k.sb("cb", [128, 2])
                    wr = k.sb("wr", [128, 2, 2, 128], BF16); wi = k.sb("wi", [128, 2, 2, 128], BF16)
                    br = k.sb("br", [128, 2, 2]); bi_ = k.sb("bi", [128, 2, 2]); lam = k.sb("lam", [128, 2, 2])
                    cl = k.sb("cl", [128, 2, 2]); cl2 = k.sb("cl2", [128, 2, 2])
                    Tp2 = T()
                    k.dma("sp", cw[:], lru_cw_d[:, l], writes=[Tp2]); k.dma("sp", cb[:], lru_cb_d[:, l], writes=[Tp2])
                    cst = Caster(k, 128, 512)
                    cst.load(wr[:].rearrange("p a b c -> p (a b c)"), lru_wr_d[:, l].rearrange("p a b c -> p (a b c)"), 128, 512, [Tp2])
                    cst.load(wi[:].rearrange("p a b c -> p (a b c)"), lru_wi_d[:, l].rearrange("p a b c -> p (a b c)"), 128, 512, [Tp2])
                    k.dma("sp", br[:], lru_br_d[:, l], writes=[Tp2]); k.dma("sp", bi_[:], lru_bi_d[:, l], writes=[Tp2])
                    k.dma("sp", lam[:], lru_lam_d[:, l], writes=[Tp2])
                    k.op("act", lambda e: e.activation(out=cl[:], in_=lam[:], func=AF.Exp, scale=-1.0), reads=[Tp2], writes=[Tp2])
                    k.op("act", lambda e: e.activation(out=cl[:], in_=cl[:], func=AF.Ln, bias=1.0), reads=[Tp2], writes=[Tp2])
                    k.op("dve", lambda e: e.tensor_scalar(out=cl2[:], in0=cl[:], scalar1=-16.0, scalar2=None, op0=ALU.mult), reads=[Tp2], writes=[Tp2])
                    k.op("dve", lambda e: e.tensor_scalar(out=cl[:], in0=cl[:], scalar1=-8.0, scalar2=None, op0=ALU.mult), reads=[Tp2], writes=[Tp2])
                    xp = k.sb("xp", [128, S + 6]); Txp = T()
                    xc = k.sb("xc", [128, S]); Txc = T()
                    xcb = k.sb("xcb", [128, S], BF16); Txcb = T()
                    gt = k.sb("gt", [128, S]); Tgt = T()
                    Rr = k.sb("Rr", [128, S]); TR = T()
                    Ii = k.sb("Ii", [128, S]); TI = T()
                    Aa = k.sb("Aa", [128, S]); TA = T()
                    Bb = k.sb("Bb", [128, S]); TB = T()
                    Hh = [k.sb("Hh%d" % i, [128, S]) for i in range(2)]; TH = [T(), T()]
                    yo = k.sb("yo", [128, S], BF16); Tyo = T()
                    pg = [k.ps("pg%d" % i, [128, 512]) for i in range(4)]; Tpg = [PT() for _ in range(4)]
                    npg = 0
                    segs = [(0, NCTX, 2), (NCTX, NLAT, NCTX + 5)]
                    for ch in range(2):
                        k.op("pool", lambda e: e.memset(xp[:], 0.0), writes=[Txp])
                        for (t0, n, o) in segs:
                            k.dma("sp", xp[:, o:o + n], uT_d[b, ch * 128:(ch + 1) * 128, t0:t0 + n], reads=[TuT[b]], writes=[Txp])
                        k.dma("sp", gt[:], uT_d[b, 256 + ch * 128:256 + (ch + 1) * 128, :], reads=[TuT[b]], writes=[Tgt])
                        for (t0, n, o) in segs:
                            k.op("dve", lambda e: e.tensor_scalar(out=xc[:, t0:t0 + n], in0=xp[:, o - 2:o - 2 + n], scalar1=cw[:, ch, 0:1],
                                                                  scalar2=cb[:, ch:ch + 1], op0=ALU.mult, op1=ALU.add),
                                 reads=[Txp, Tp2], writes=[Txc])
                            for j in range(1, 4):
                                k.op("dve", lambda e: e.scalar_tensor_tensor(out=xc[:, t0:t0 + n], in0=xp[:, o - 2 + j:o - 2 + j + n],
                                                                            scalar=cw[:, ch, j:j + 1], in1=xc[:, t0:t0 + n],
                                                                            op0=ALU.mult, op1=ALU.add),
                                     reads=[Txp, Tp2, Txc], writes=[Txc])
                        k.op("pool", lambda e: e.tensor_copy(out=xcb[:], in_=xc[:]), reads=[Txc], writes=[Txcb])
                        for d in range(2):
                            for (t0, n) in BLOCKS:
                                for (W, bias, dst, tdst) in ((wr, br, Rr, TR), (wi, bi_, Ii, TI)):
                                    pp = pg[npg % 4]; tp = Tpg[npg % 4]; npg += 1
                                    k.op("pe", lambda e: e.matmul(pp[:, 0:n], lhsT=W[:, d, ch, :], rhs=xcb[:, t0:t0 + n], start=True, stop=True),
                                         reads=[Tp2, Txcb], writes=[tp])
                                    k.op("act", lambda e: e.activation(out=dst[:, t0:t0 + n], in_=pp[:, 0:n], func=AF.Sigmoid,
                                                                       bias=bias[:, d, ch:ch + 1]), reads=[tp, Tp2], writes=[tdst])
                            k.op("act", lambda e: e.activation(out=Aa[:], in_=Rr[:], func=AF.Exp, scale=cl[:, d, ch:ch + 1]),
                                 reads=[TR, Tp2], writes=[TA])
                            k.op("act", lambda e: e.activation(out=Bb[:], in_=Rr[:], func=AF.Exp, scale=cl2[:, d, ch:ch + 1]),
                                 reads=[TR, Tp2], writes=[TB])
                            k.op("act", lambda e: e.activation(out=Bb[:], in_=Bb[:], func=AF.Sqrt, scale=-1.0, bias=1.0), reads=[TB], writes=[TB])
                            k.op("dve", lambda e: e.tensor_tensor(out=Ii[:], in0=Ii[:], in1=xc[:], op=ALU.mult), reads=[TI, Txc], writes=[TI])
                            k.op("dve", lambda e: e.tensor_tensor(out=Bb[:], in0=Bb[:], in1=Ii[:], op=ALU.mult), reads=[TB, TI], writes=[TB])
                            H = Hh[d]
                            if d == 0:
                                k.op("dve", lambda e: e.tensor_tensor_scan(out=H[:], data0=Aa[:], data1=Bb[:], initial=0.0,
                                                                          op0=ALU.mult, op1=ALU.add), reads=[TA, TB], writes=[TH[d]])
                            else:
                                k.op("dve", lambda e: e.tensor_tensor_scan(out=H[:, NCTX - 1::-1], data0=Aa[:, NCTX - 1::-1], data1=Bb[:, NCTX - 1::-1],
                                                                          initial=0.0, op0=ALU.mult, op1=ALU.add), reads=[TA, TB], writes=[TH[d]])
                                k.op("dve", lambda e: e.tensor_tensor_scan(out=H[:, S - 1:NCTX - 1:-1], data0=Aa[:, S - 1:NCTX - 1:-1],
                                                                          data1=Bb[:, S - 1:NCTX - 1:-1], initial=H[:, 0:1],
                                                                          op0=ALU.mult, op1=ALU.add), reads=[TA, TB, TH[d]], writes=[TH[d]])
                        k.op("act", lambda e: e.activation(out=gt[:], in_=gt[:], func=AF.Gelu_apprx_tanh), reads=[Tgt], writes=[Tgt])
                        k.op("dve", lambda e: e.tensor_tensor(out=Hh[0][:], in0=Hh[0][:], in1=Hh[1][:], op=ALU.add), reads=TH, writes=[TH[0]])
                        k.op("dve", lambda e: e.tensor_tensor(out=yo[:], in0=Hh[0][:], in1=gt[:], op=ALU.mult), reads=[TH[0], Tgt], writes=[Tyo])
                        k.dma("sp", yT_d[b, ch * 128:(ch + 1) * 128, :], yo[:], reads=[Tyo], writes=[TyT[b]])
                if cfg.upto < 3:
                    continue
                with Stage(k):
                    lbz = k.sb("lbz", [128, L, 2]); lbe = k.sb("lbe", [128, L, 2]); lbs = k.sb("lbs", [128, 2]); lb = k.sb("lb", [128, 2])
                    oml = k.sb("oml", [128, 2]); hnw = k.sb("hnw", [128, 2]); Tp3 = T()
                    mf = k.sb("mf", [128, S]); mb = k.sb("mb", [128, S]); triU = k.sb("triU", [128, 128], U32); triL = k.sb("triL", [128, 128], U32)
                    blk = k.sb("blk", [128, 128], BF16)
                    k.dma("sp", lbz[:], hg_lbT_d, writes=[Tp3]); k.dma("sp", hnw[:], hg_nwT_d[:, l], writes=[Tp3])
                    k.dma("sp", mf[:], mfwd_d, writes=[Tp3]); k.dma("sp", mb[:], mbwd_d, writes=[Tp3])
                    k.dma("sp", triU[:], triU_d, writes=[Tp3]); k.dma("sp", triL[:], triL_d, writes=[Tp3]); k.dma("sp", blk[:], blk64_d, writes=[Tp3])
                    k.op("act", lambda e: e.activation(out=lbe[:], in_=lbz[:], func=AF.Exp), reads=[Tp3], writes=[Tp3])
                    k.op("dve", lambda e: e.tensor_tensor(out=lbs[:], in0=lbe[:, 0, :], in1=lbe[:, 1, :], op=ALU.add), reads=[Tp3], writes=[Tp3])
                    k.op("dve", lambda e: e.reciprocal(out=lbs[:], in_=lbs[:]), reads=[Tp3], writes=[Tp3])
                    for ll in range(L):
                        k.op("dve", lambda e: e.tensor_tensor(out=lbe[:, ll, :], in0=lbe[:, ll, :], in1=lbs[:], op=ALU.mult), reads=[Tp3], writes=[Tp3])
                    k.op("dve", lambda e: e.tensor_copy(out=lb[:], in_=lbe[:, 0, :]), reads=[Tp3], writes=[Tp3])
                    for ll in range(1, l + 1):
                        k.op("dve", lambda e: e.tensor_tensor(out=lb[:], in0=lb[:], in1=lbe[:, ll, :], op=ALU.add), reads=[Tp3], writes=[Tp3])
                    k.op("dve", lambda e: e.tensor_tensor(out=lb[:], in0=lb[:], in1=lbe[:, 0, :], op=ALU.subtract), reads=[Tp3], writes=[Tp3])
                    k.op("dve", lambda e: e.tensor_scalar(out=oml[:], in0=lb[:], scalar1=-1.0, scalar2=1.0, op0=ALU.mult, op1=ALU.add), reads=[Tp3], writes=[Tp3])
                    vt = k.sb("vt", [128, NT, 256], BF16); Tvt = T()
                    vstg = k.sb("vstg", [128, NT // 2, 256]); Tvstg = T()
                    for hf in range(2):
                        k.dma("sp", vstg[:], ut_d[b, hf * (S // 2):(hf + 1) * (S // 2), 0:256].rearrange("(n p) c -> p n c", p=128), reads=[Tut[b]], writes=[Tvstg])
                        k.op("pool", lambda e: e.tensor_copy(out=vt[:, hf * (NT // 2):(hf + 1) * (NT // 2), :], in_=vstg[:]), reads=[Tvstg], writes=[Tvt])
                    qh = k.sb("qh", [128, S]); Tqh = T()
                    gg = k.sb("gg", [128, S]); Tgg = T()
                    ff = k.sb("ff", [128, S]); Tff = T()
                    lf = k.sb("lf", [128, S]); Tlf = T()
                    cum = k.sb("cum", [128, S]); Tcum = T()
                    dd = k.sb("dd", [128, S]); Tdd = T()
                    EE = k.sb("EE", [128, S]); TEE = T()
                    qt = k.sb("qt", [128, S], BF16); Tqt = T()
                    kt = k.sb("kt", [128, S], BF16); Tkt = T()
                    qs = k.sb("qs", [128, S], BF16); Tqs = T()
                    ke = k.sb("ke", [128, S], BF16); Tke = T()
                    etot = k.sb("etot", [128, NT]); Tet = T()
                    OO = k.sb("OO", [128, S]); TOO = T()
                    ket = [k.sb("ket%d" % i, [128, 128], BF16) for i in range(2)]; Tket = [T(), T()]
                    Am = [[k.sb("Am%d_%d" % (d, i), [128, 128], BF16) for i in range(2)] for d in range(2)]
                    TAm = [[T(), T()] for d in range(2)]
                    S32 = k.sb("S32", [128, 64]); TS32 = T()
                    Sb = k.sb("Sb", [128, 64], BF16); TSb = T()
                    sqb = k.sb("sqb", [128, 512], BF16); Tsqb = T()
                    rsd = k.sb("rsd", [128, 512]); Trsd = T()
                    yo = k.sb("yo3", [128, S], BF16); Tyo = T()
                    p_sc = [k.ps("p_sc%d" % i, [128, 128]) for i in range(2)]; Tp_sc = [PT(), PT()]
                    p_y = [k.ps("p_y%d" % i, [128, 128]) for i in range(2)]; Tp_y = [PT(), PT()]
                    p_st = k.ps("p_st", [128, 64]); Tp_st = PT()
                    p_tr = k.ps("p_tr", [128, 128], BF16); Tp_tr = PT()
                    p_ss = k.ps("p_ss", [128, 512]); Tp_ss = PT()
                    for d in range(2):
                        for i in range(2):
                            k.op("pool", lambda e: e.memset(Am[d][i][:], 0.0), writes=[TAm[d][i]])
                    for i in range(2):
                        k.op("dve", lambda e: e.memset(p_sc[i][:], 0.0), writes=[Tp_sc[i]])
                    cum3 = cum[:].rearrange("p (n t) -> p n t", t=128)
                    dd3 = dd[:].rearrange("p (n t) -> p n t", t=128)
                    cum4 = cum[:].rearrange("p (n t) -> p n t", t=32)
                    dd4 = dd[:].rearrange("p (n t) -> p n t", t=32)
                    qx = k.sb("qx", [128, S], BF16); Tqx = T()
                    kx = [None] + [k.sb("kx%d" % i, [128, NT, 96], BF16) for i in range(1, 4)]; Tkx = T()
                    def mkset(i_):
                        d_ = {}
                        for nm in ("qt", "kt", "qx", "qs", "ke"):
                            d_[nm] = k.sb("%s_b%d" % (nm, i_), [128, S], BF16); d_["T" + nm] = T()
                        d_["kx"] = [None] + [k.sb("kx%d_b%d" % (j, i_), [128, NT, 96], BF16) for j in range(1, 4)]; d_["Tkx"] = T()
                        d_["etot"] = k.sb("etot_b%d" % i_, [128, NT]); d_["Tet"] = T()
                        return d_
                    sets = [dict(qt=qt, Tqt=Tqt, kt=kt, Tkt=Tkt, qx=qx, Tqx=Tqx, qs=qs, Tqs=Tqs, ke=ke, Tke=Tke, kx=kx, Tkx=Tkx, etot=etot, Tet=Tet), mkset(1)]

                    def hg_prep(hp, d, B):
                        r0 = 512 + hp * 128
                        qt, kt, qx, qs, ke, kx, etot = B["qt"], B["kt"], B["qx"], B["qs"], B["ke"], B["kx"], B["etot"]
                        Tqt, Tkt, Tqx, Tqs, Tke, Tkx, Tet = B["Tqt"], B["Tkt"], B["Tqx"], B["Tqs"], B["Tke"], B["Tkx"], B["Tet"]
                        if d == 0:
                            k.dma("sp", qh[:], uT_d[b, r0:r0 + 128, :], reads=[TuT[b]], writes=[Tqh])
                            k.op("act", lambda e: e.activation(out=qh[:], in_=qh[:], func=AF.Silu), reads=[Tqh], writes=[Tqh])
                        k.dma("sp", ff[:], uT_d[b, r0 + 256 * (d + 1):r0 + 256 * (d + 1) + 128, :], reads=[TuT[b]], writes=[Tff])
                        k.op("act", lambda e: e.activation(out=ff[:], in_=ff[:], func=AF.Sigmoid), reads=[Tff], writes=[Tff])
                        k.op("dve", lambda e: e.tensor_scalar(out=ff[:], in0=ff[:], scalar1=oml[:, hp:hp + 1], scalar2=lb[:, hp:hp + 1],
                                                              op0=ALU.mult, op1=ALU.add), reads=[Tff, Tp3], writes=[Tff])
                        k.op("act", lambda e: e.activation(out=lf[:], in_=ff[:], func=AF.Ln), reads=[Tff], writes=[Tlf])
                        k.op("dve", lambda e: e.tensor_scalar(out=ff[:], in0=ff[:], scalar1=-1.0, scalar2=1.0, op0=ALU.mult, op1=ALU.add),
                             reads=[Tff], writes=[Tff])
                        if d == 0:
                            k.op("dve", lambda e: e.tensor_tensor_scan(out=cum[:], data0=mf[:], data1=lf[:], initial=0.0, op0=ALU.mult, op1=ALU.add),
                                 reads=[Tp3, Tlf], writes=[Tcum])
                            mid, end = 63, 127
                        else:
                            k.op("dve", lambda e: e.tensor_tensor_scan(out=cum[:, ::-1], data0=mb[:, ::-1], data1=lf[:, ::-1], initial=0.0,
                                                                      op0=ALU.mult, op1=ALU.add), reads=[Tp3, Tlf], writes=[Tcum])
                            mid, end = 64, 0
                        mid4, first4 = (15, 0) if d == 0 else (16, 31)
                        k.op("dve", lambda e: e.tensor_tensor(out=dd4, in0=cum4, in1=cum4[:, :, mid4:mid4 + 1].to_broadcast([128, S // 32, 32]), op=ALU.subtract),
                             reads=[Tcum], writes=[Tdd])
                        k.op("act", lambda e: e.activation(out=EE[:], in_=dd[:], func=AF.Exp), reads=[Tdd], writes=[TEE])
                        k.op("dve", lambda e: e.tensor_tensor(out=qt[:], in0=qh[:], in1=EE[:], op=ALU.mult), reads=[Tqh, TEE], writes=[Tqt])
                        k.op("act", lambda e: e.activation(out=EE[:], in_=dd[:], func=AF.Exp, scale=-1.0), reads=[Tdd], writes=[TEE])
                        k.op("dve", lambda e: e.tensor_tensor(out=kt[:], in0=ff[:], in1=EE[:], op=ALU.mult), reads=[Tff, TEE], writes=[Tkt])
                        k.op("dve", lambda e: e.tensor_tensor(out=dd4, in0=cum4, in1=cum4[:, :, first4:first4 + 1].to_broadcast([128, S // 32, 32]), op=ALU.subtract),
                             reads=[Tcum], writes=[Tdd])
                        k.op("act", lambda e: e.activation(out=EE[:], in_=dd[:], func=AF.Exp), reads=[Tdd], writes=[TEE])
                        k.op("dve", lambda e: e.tensor_tensor(out=qx[:], in0=qh[:], in1=EE[:], op=ALU.mult), reads=[Tqh, TEE], writes=[Tqx])
                        ff3 = ff[:].rearrange("p (n t) -> p n t", t=128)
                        EE3 = EE[:].rearrange("p (n t) -> p n t", t=128)
                        for i in range(1, 4):
                            w_ = 32 * i
                            if d == 0:
                                srcs = slice(0, w_); refi = w_
                            else:
                                srcs = slice(128 - w_, 128); refi = 127 - w_
                            k.op("dve", lambda e: e.tensor_tensor(out=dd3[:, :, 0:w_], in0=cum3[:, :, refi:refi + 1].to_broadcast([128, NT, w_]),
                                                                  in1=cum3[:, :, srcs], op=ALU.subtract), reads=[Tcum], writes=[Tdd])
                            k.op("act", lambda e: e.activation(out=EE3[:, :, 0:w_], in_=dd3[:, :, 0:w_], func=AF.Exp), reads=[Tdd], writes=[TEE])
                            k.op("dve", lambda e: e.tensor_tensor(out=kx[i][:, :, 0:w_], in0=ff3[:, :, srcs], in1=EE3[:, :, 0:w_], op=ALU.mult),
                                 reads=[Tff, TEE], writes=[Tkx])
                        k.op("act", lambda e: e.activation(out=EE[:], in_=cum[:], func=AF.Exp), reads=[Tcum], writes=[TEE])
                        k.op("dve", lambda e: e.tensor_tensor(out=qs[:], in0=qh[:], in1=EE[:], op=ALU.mult), reads=[Tqh, TEE], writes=[Tqs])
                        k.op("act", lambda e: e.activation(out=etot[:], in_=cum3[:, :, end], func=AF.Exp), reads=[Tcum], writes=[Tet])
                        k.op("dve", lambda e: e.tensor_tensor(out=dd3, in0=cum3[:, :, end:end + 1].to_broadcast([128, NT, 128]), in1=cum3, op=ALU.subtract),
                             reads=[Tcum], writes=[Tdd])
                        k.op("act", lambda e: e.activation(out=EE[:], in_=dd[:], func=AF.Exp), reads=[Tdd], writes=[TEE])
                        k.op("dve", lambda e: e.tensor_tensor(out=ke[:], in0=ff[:], in1=EE[:], op=ALU.mult), reads=[Tff, TEE], writes=[Tke])

                    def hg_loop(hp, d, B, pend):
                        qt, kt, qx, qs, ke, kx, etot = B["qt"], B["kt"], B["qx"], B["qs"], B["ke"], B["kx"], B["etot"]
                        Tqt, Tkt, Tqx, Tqs, Tke, Tkx, Tet = B["Tqt"], B["Tkt"], B["Tqx"], B["Tqs"], B["Tke"], B["Tkx"], B["Tet"]
                        order = list(range(NT)) if d == 0 else [1, 0] + list(range(NT - 1, 1, -1))
                        tri = triU if d == 0 else triL
                        per = (len(pend) + NT - 3) // (NT - 2) if pend else 0
                        for oi, ti in enumerate(order):
                            ts_ = slice(ti * 128, (ti + 1) * 128)
                            k.op("pe", lambda e: e.transpose(p_tr[:], ke[:, ts_], ident_b[:]), reads=[Tke, Tc], writes=[Tp_tr])
                            KT = ket[oi % 2]; tKT = Tket[oi % 2]
                            k.op("act", lambda e: e.activation(out=KT[:], in_=p_tr[:], func=AF.Copy), reads=[Tp_tr], writes=[tKT])
                            py = p_y[oi % 2]; tpy = Tp_y[oi % 2]
                            for hh in range(2):
                                bs = slice(hh * 64, (hh + 1) * 64)
                                psc = p_sc[hh]; tps = Tp_sc[hh]
                                for tb in range(4):
                                    for sb_ in (range(0, tb + 1) if d == 0 else range(tb, 4)):
                                        tq = slice(ti * 128 + 32 * tb, ti * 128 + 32 * tb + 32)
                                        if sb_ == tb:
                                            lw = kt[bs, tq]; rq = qt[bs, tq]
                                        else:
                                            i = tb if d == 0 else 3 - tb
                                            o_ = 32 * sb_ if d == 0 else 32 * sb_ - (128 - 32 * i)
                                            lw = kx[i][bs, ti, o_:o_ + 32]; rq = qx[bs, tq]
                                        k.op("pe", lambda e: e.matmul(psc[32 * sb_:32 * sb_ + 32, 32 * tb:32 * tb + 32], lhsT=lw, rhs=rq, start=True, stop=True,
                                                                      tile_position=(bs.start, 32 * sb_)),
                                             reads=[Tkt, Tqt, Tkx, Tqx], writes=[tps])
                                A = Am[d][hh]; tA = TAm[d][hh]
                                k.op("dve", lambda e: e.copy_predicated(out=A[:], mask=tri[:], data=psc[:]), reads=[tps, Tp3], writes=[tA])
                                vs = vt[:, ti, (2 * hp + hh) * 64:(2 * hp + hh + 1) * 64]
                                k.op("pe", lambda e: e.matmul(py[bs, :], lhsT=vs, rhs=A[:], start=True, stop=(oi == 0)),
                                     reads=[Tvt, tA], writes=[tpy])
                                if oi > 0:
                                    k.op("pe", lambda e: e.matmul(py[bs, :], lhsT=Sb[bs, :], rhs=qs[bs, ts_], start=False, stop=True),
                                         reads=[TSb, Tqs], writes=[tpy])
                            if d == 0:
                                k.op("act", lambda e: e.activation(out=OO[:, ts_], in_=py[:], func=AF.Copy), reads=[tpy], writes=[TOO])
                            else:
                                k.op("dve", lambda e: e.tensor_tensor(out=OO[:, ts_], in0=OO[:, ts_], in1=py[:], op=ALU.add), reads=[tpy, TOO], writes=[TOO])
                            if oi < NT - 1:
                                for hh in range(2):
                                    bs = slice(hh * 64, (hh + 1) * 64)
                                    vs = vt[:, ti, (2 * hp + hh) * 64:(2 * hp + hh + 1) * 64]
                                    k.op("pe", lambda e: e.matmul(p_st[bs, :], lhsT=KT[:, bs], rhs=vs, start=True, stop=True),
                                         reads=[tKT, Tvt], writes=[Tp_st])
                                if oi == 0:
                                    k.op("dve", lambda e: e.tensor_copy(out=S32[:], in_=p_st[:]), reads=[Tp_st], writes=[TS32])
                                else:
                                    k.op("dve", lambda e: e.scalar_tensor_tensor(out=S32[:], in0=S32[:], scalar=etot[:, ti:ti + 1], in1=p_st[:],
                                                                                op0=ALU.mult, op1=ALU.add), reads=[Tp_st, Tet, TS32], writes=[TS32])
                                k.op("pool", lambda e: e.tensor_copy(out=Sb[:], in_=S32[:]), reads=[TS32], writes=[TSb])
                            if pend:
                                k.flush(pend, per)

                    chains = [(0, 0), (0, 1), (1, 0), (1, 1)]
                    hg_prep(0, 0, sets[0])
                    for ci, (hp, d) in enumerate(chains):
                        if d == 0:
                            r0 = 512 + hp * 128
                            k.dma("sp", gg[:], uT_d[b, r0 + 1024:r0 + 1024 + 128, :], reads=[TuT[b]], writes=[Tgg])
                            k.op("act", lambda e: e.activation(out=gg[:], in_=gg[:], func=AF.Silu), reads=[Tgg], writes=[Tgg])
                        pend = []
                        if ci + 1 < len(chains):
                            k.defer = pend
                            hg_prep(chains[ci + 1][0], chains[ci + 1][1], sets[(ci + 1) % 2])
                            k.defer = None
                        hg_loop(hp, d, sets[ci % 2], pend)
                        k.flush(pend)
                        if d == 1:
                            for (t0, n) in BLOCKS:
                                k.op("act", lambda e: e.activation(out=sqb[:, 0:n], in_=OO[:, t0:t0 + n], func=AF.Square), reads=[TOO], writes=[Tsqb])
                                k.op("pe", lambda e: e.matmul(p_ss[:, 0:n], lhsT=blk[:], rhs=sqb[:, 0:n], start=True, stop=True), reads=[Tsqb, Tp3], writes=[Tp_ss])
                                k.op("act", lambda e: e.activation(out=rsd[:, 0:n], in_=p_ss[:, 0:n], func=AF.Sqrt, scale=1.0 / 64, bias=EPS), reads=[Tp_ss], writes=[Trsd])
                                k.op("dve", lambda e: e.reciprocal(out=rsd[:, 0:n], in_=rsd[:, 0:n]), reads=[Trsd], writes=[Trsd])
                                k.op("dve", lambda e: e.tensor_tensor(out=rsd[:, 0:n], in0=rsd[:, 0:n], in1=OO[:, t0:t0 + n], op=ALU.mult), reads=[Trsd, TOO], writes=[Trsd])
                                k.op("dve", lambda e: e.scalar_tensor_tensor(out=yo[:, t0:t0 + n], in0=rsd[:, 0:n], scalar=hnw[:, hp:hp + 1], in1=gg[:, t0:t0 + n],
                                                                            op0=ALU.mult, op1=ALU.mult), reads=[Trsd, Tgg, Tp3], writes=[Tyo])
                            k.dma("sp", yT_d[b, 256 + hp * 128:256 + (hp + 1) * 128, :], yo[:], reads=[Tyo], writes=[TyT[b]])
                if cfg.upto < 4:
                    continue
                with Stage(k):
                    Tp4 = T()
                    cw = k.sb("scw", [128, 4, 4]); cb = k.sb("scb", [128, 4])
                    aneg = k.sb("aneg", [128, 8]); dtb = k.sb("dtb", [128, 8]); dsk = k.sb("dsk", [128, 256]); snw = k.sb("snw", [128, 256])
                    triUf = k.sb("triUf", [128, 128]); triLf = k.sb("triLf", [128, 128]); strLf = k.sb("strLf", [128, 128]); strUf = k.sb("strUf", [128, 128])
                    for dst, src in ((cw, sd_cw_d[:, l]), (cb, sd_cb_d[:, l]), (aneg, sd_alog_d[:, l]), (dtb, sd_dtb_d[:, l]), (dsk, sd_dsk_d[:, l]),
                                     (snw, sd_nw_d[:, l]), (triUf, triUf_d), (triLf, triLf_d), (strLf, strLf_d), (strUf, strUf_d)):
                        k.dma("sp", dst[:], src, writes=[Tp4])
                    k.op("act", lambda e: e.activation(out=aneg[:], in_=aneg[:], func=AF.Exp), reads=[Tp4], writes=[Tp4])
                    k.op("dve", lambda e: e.tensor_scalar(out=aneg[:], in0=aneg[:], scalar1=-1.0, scalar2=None, op0=ALU.mult), reads=[Tp4], writes=[Tp4])
                    xp = k.sb("sxp", [128, S + 6]); Txp = T()
                    xc = k.sb("sxc", [128, S]); Txc = T()
                    fmb = k.sb("fmb", [128, 4, S], BF16); Tfmb = T()
                    segs = [(0, NCTX, 2), (NCTX, NLAT, NCTX + 5)]
                    for ch in range(4):
                        k.op("pool", lambda e: e.memset(xp[:], 0.0), writes=[Txp])
                        for (t0, n, o) in segs:
                            k.dma("sp", xp[:, o:o + n], uT_d[b, 2048 + ch * 128:2048 + (ch + 1) * 128, t0:t0 + n], reads=[TuT[b]], writes=[Txp])
                        for (t0, n, o) in segs:
                            k.op("dve", lambda e: e.tensor_scalar(out=xc[:, t0:t0 + n], in0=xp[:, o - 2:o - 2 + n], scalar1=cw[:, ch, 0:1],
                                                                  scalar2=cb[:, ch:ch + 1], op0=ALU.mult, op1=ALU.add), reads=[Txp, Tp4], writes=[Txc])
                            for j in range(1, 4):
                                k.op("dve", lambda e: e.scalar_tensor_tensor(out=xc[:, t0:t0 + n], in0=xp[:, o - 2 + j:o - 2 + j + n], scalar=cw[:, ch, j:j + 1],
                                                                            in1=xc[:, t0:t0 + n], op0=ALU.mult, op1=ALU.add), reads=[Txp, Tp4, Txc], writes=[Txc])
                        k.op("act", lambda e: e.activation(out=fmb[:, ch, :], in_=xc[:], func=AF.Silu), reads=[Txc], writes=[Tfmb])
                    xst = k.sb("xst", [128, NT, 256], BF16); Txst = T()
                    Bt = k.sb("Bt", [128, NT, 128], BF16); TBt = T()
                    ps_pre = PScope(k); ps_pre.__enter__()
                    p_tr = [k.ps("p4tr%d" % i, [128, 128], BF16) for i in range(2)]; Tp_tr = [PT(), PT()]
                    ntr = 0
                    for ti in range(NT):
                        ts_ = slice(ti * 128, (ti + 1) * 128)
                        for ch in range(3):
                            pp = p_tr[ntr % 2]; tp = Tp_tr[ntr % 2]
                            k.op("pe", lambda e: e.transpose(pp[:], fmb[:, ch, ts_], ident_b[:]), reads=[Tfmb, Tc], writes=[tp])
                            dst = xst[:, ti, ch * 128:(ch + 1) * 128] if ch < 2 else Bt[:, ti, :]
                            tdst = Txst if ch < 2 else TBt
                            if ntr % 2 == 0:
                                k.op("act", lambda e: e.activation(out=dst, in_=pp[:], func=AF.Copy), reads=[tp], writes=[tdst])
                            else:
                                k.op("dve", lambda e: e.tensor_copy(out=dst, in_=pp[:]), reads=[tp], writes=[tdst])
                            ntr += 1
                    dt = k.sb("dt", [128, NT, 8]); Tdt = T()
                    la = k.sb("la", [128, NT, 8]); Tla = T()
                    k.dma("sp", dt[:], ut_d[b, :, 512:520].rearrange("(n p) c -> p n c", p=128), reads=[Tut[b]], writes=[Tdt])
                    k.op("dve", lambda e: e.tensor_tensor(out=dt[:], in0=dt[:], in1=dtb[:, None, :].to_broadcast([128, NT, 8]), op=ALU.add), reads=[Tdt, Tp4], writes=[Tdt])
                    k.op("act", lambda e: e.activation(out=dt[:], in_=dt[:], func=AF.Exp), reads=[Tdt], writes=[Tdt])
                    k.op("act", lambda e: e.activation(out=dt[:], in_=dt[:], func=AF.Ln, bias=1.0), reads=[Tdt], writes=[Tdt])
                    k.op("dve", lambda e: e.tensor_tensor(out=la[:], in0=dt[:], in1=aneg[:, None, :].to_broadcast([128, NT, 8]), op=ALU.mult), reads=[Tdt, Tp4], writes=[Tla])
                    p_ct = k.ps("p_ct", [128, 2, NT, 8]); Tp_ct = PT()
                    for ti in range(NT):
                        k.op("pe", lambda e: e.matmul(p_ct[:, 0, ti, 0:4], lhsT=triUf[:], rhs=la[:, ti, 0:4], start=True, stop=True), reads=[Tla, Tp4], writes=[Tp_ct])
                        k.op("pe", lambda e: e.matmul(p_ct[:, 0, ti, 4:8], lhsT=triLf[:], rhs=la[:, ti, 4:8], start=True, stop=True), reads=[Tla, Tp4], writes=[Tp_ct])
                        k.op("pe", lambda e: e.matmul(p_ct[:, 1, ti, :], lhsT=ones_f[:], rhs=la[:, ti, :], start=True, stop=True), reads=[Tla, Tc], writes=[Tp_ct])
                    cexp = k.sb("cexp", [128, NT, 8]); etot = k.sb("etot4", [128, NT, 8]); wend = k.sb("wend", [128, NT, 8]); Tce = T()
                    k.op("dve", lambda e: e.tensor_tensor(out=wend[:], in0=p_ct[:, 1], in1=p_ct[:, 0], op=ALU.subtract), reads=[Tp_ct], writes=[Tce]) if False else None
                    k.op("act", lambda e: e.activation(out=cexp[:], in_=p_ct[:, 0], func=AF.Copy), reads=[Tp_ct], writes=[Tce])
                    k.op("dve", lambda e: e.tensor_tensor(out=wend[:], in0=p_ct[:, 1], in1=cexp[:], op=ALU.subtract), reads=[Tp_ct, Tce], writes=[Tce])
                    k.op("act", lambda e: e.activation(out=wend[:], in_=wend[:], func=AF.Exp), reads=[Tce], writes=[Tce])
                    k.op("dve", lambda e: e.tensor_tensor(out=wend[:], in0=wend[:], in1=dt[:], op=ALU.mult), reads=[Tce, Tdt], writes=[Tce])
                    k.op("act", lambda e: e.activation(out=cexp[:], in_=cexp[:], func=AF.Exp), reads=[Tce], writes=[Tce])
                    k.op("act", lambda e: e.activation(out=etot[:], in_=p_ct[:, 1], func=AF.Exp), reads=[Tp_ct], writes=[Tce])
                    ps_pre.__exit__(None, None, None)
                    ps_loop = PScope(k); ps_loop.__enter__()
                    Yacc = k.sb("Yacc", [128, NT, 256]); TY = T()
                    inc4 = [k.sb("inc4_%d" % d, [128, 4, 128]) for d in range(2)]
                    ngm = [k.sb("ngm%d" % d, [128, 4, 128]) for d in range(2)]
                    k.dma("sp", inc4[0][:], triUf4_d, writes=[Tp4]); k.dma("sp", inc4[1][:], triLf4_d, writes=[Tp4])
                    k.dma("sp", ngm[0][:], negmf_d, writes=[Tp4]); k.dma("sp", ngm[1][:], negmb_d, writes=[Tp4])
                    etH = k.sb("etH", [128, NT, 2, 2]); TetH = T()
                    et4 = etot[:].rearrange("p n (d h) -> p n d h", d=2)
                    for g in range(2):
                        gs = slice(g * 64, (g + 1) * 64)
                        k.op("dve", lambda e: e.tensor_copy(out=etH[gs], in_=et4[gs, :, :, 2 * g:2 * g + 2]), reads=[Tce], writes=[TetH])
                    Rr4 = [k.sb("Rr4_%d" % i, [128, 4, 128]) for i in range(2)]; TRr4 = [T(), T()]
                    Es = [k.sb("Es%d" % i, [128, 4, 128]) for i in range(2)]; TEs = [T(), T()]
                    Ab = [k.sb("Ab%d" % i, [128, 4, 128], BF16) for i in range(2)]; TAb = [T(), T()]
                    Bw = [k.sb("Bw%d" % i, [128, 2, 2, 64], BF16) for i in range(2)]; TBw = [T(), T()]
                    tmpy = [k.sb("tmpy%d" % i, [128, 4, 64]) for i in range(2)]; Ttmpy = [T(), T()]
                    S32 = k.sb("S32_4", [128, 2, 64]); TS32 = T()
                    STb = k.sb("STb4", [128, 2, 64], BF16); TSTb = T()
                    p_g = [k.ps("p_g%d" % i, [128, 128]) for i in range(2)]; Tp_g = [PT(), PT()]
                    p_seg = [k.ps("p_seg%d" % i, [128, 4, 128]) for i in range(2)]; Tp_seg = [PT(), PT()]
                    p_y1 = k.ps("p_y1", [128, 4, 64]); Tp_y1 = PT()
                    p_y2 = [k.ps("p_y2%d" % i, [128, 2, 64]) for i in range(2)]; Tp_y2 = [PT(), PT()]
                    p_st = k.ps("p_st4", [128, 2, 64]); Tp_st = PT()
                    Bt4 = Bt[:].rearrange("p n (g c) -> p n g c", g=2)
                    def ssd_front(d, oi, ti, i2):
                        ts_ = slice(ti * 128, (ti + 1) * 128)
                        d4 = slice(d * 4, d * 4 + 4)
                        strm = strLf if d == 0 else strUf
                        for g in range(2):
                            gs = slice(g * 64, (g + 1) * 64)
                            k.op("pe", lambda e: e.matmul(p_g[g][:], lhsT=fmb[gs, 2, ts_], rhs=fmb[gs, 3, ts_], start=True, stop=True),
                                 reads=[Tfmb], writes=[Tp_g[g]])
                        k.op("pool", lambda e: e.tensor_tensor(out=Rr4[i2][:], in0=inc4[d][:], in1=la[:, ti, d4, None].to_broadcast([128, 4, 128]), op=ALU.mult),
                             reads=[Tla, Tp4], writes=[TRr4[i2]])
                        k.op("pe", lambda e: e.matmul(p_seg[i2][:].rearrange("p h t -> p (h t)"), lhsT=strm[:], rhs=Rr4[i2][:].rearrange("p h t -> p (h t)"),
                                                      start=True, stop=False), reads=[TRr4[i2], Tp4], writes=[Tp_seg[i2]])
                        k.op("pe", lambda e: e.matmul(p_seg[i2][:].rearrange("p h t -> p (h t)"), lhsT=ident_f[:], rhs=ngm[d][:].rearrange("p h t -> p (h t)"),
                                                      start=False, stop=True), reads=[Tc, Tp4], writes=[Tp_seg[i2]])
                        k.op("act", lambda e: e.activation(out=Es[i2][:], in_=p_seg[i2][:], func=AF.Exp), reads=[Tp_seg[i2]], writes=[TEs[i2]])
                        k.op("dve", lambda e: e.tensor_tensor(out=Es[i2][:], in0=Es[i2][:], in1=dt[:, ti, d4, None].to_broadcast([128, 4, 128]), op=ALU.mult),
                             reads=[TEs[i2], Tdt], writes=[TEs[i2]])
                        for g in range(2):
                            k.op("dve", lambda e: e.tensor_tensor(out=Ab[i2][:, 2 * g:2 * g + 2, :], in0=Es[i2][:, 2 * g:2 * g + 2, :],
                                                                  in1=p_g[g][:, None, :].to_broadcast([128, 2, 128]), op=ALU.mult),
                                 reads=[TEs[i2], Tp_g[g]], writes=[TAb[i2]])
                        if oi < NT - 1:
                            k.op("pool", lambda e: e.tensor_tensor(out=Bw[i2][:], in0=Bt4[:, ti, :, None, :].to_broadcast([128, 2, 2, 64]),
                                                                   in1=wend[:, ti, d4].rearrange("p (g j) -> p g j", g=2)[:, :, :, None].to_broadcast([128, 2, 2, 64]),
                                                                   op=ALU.mult), reads=[TBt, Tce], writes=[TBw[i2]])

                    def ssd_back(d, oi, ti, i2):
                        ts_ = slice(ti * 128, (ti + 1) * 128)
                        for h in range(4):
                            k.op("pe", lambda e: e.matmul(p_y1[:, h, :], lhsT=Ab[i2][:, h, :], rhs=xst[:, ti, h * 64:(h + 1) * 64], start=True, stop=True),
                                 reads=[TAb[i2], Txst], writes=[Tp_y1])
                        yacc = Yacc[:, ti, :].rearrange("p (h c) -> p h c", h=4)
                        if oi > 0:
                            for h in range(4):
                                g = h // 2; gs = slice(g * 64, (g + 1) * 64)
                                k.op("pe", lambda e: e.matmul(p_y2[g][:, h % 2, :], lhsT=fmb[gs, 3, ts_], rhs=STb[gs, h % 2, :], start=True, stop=True),
                                     reads=[Tfmb, TSTb], writes=[Tp_y2[g]])
                            for g in range(2):
                                k.op("dve", lambda e: e.tensor_tensor(out=tmpy[i2][:, 2 * g:2 * g + 2, :], in0=p_y2[g][:],
                                                                      in1=cexp[:, ti, d * 4 + 2 * g:d * 4 + 2 * g + 2, None].to_broadcast([128, 2, 64]), op=ALU.mult),
                                     reads=[Tp_y2[g], Tce], writes=[Ttmpy[i2]])
                            if d == 0:
                                k.op("dve", lambda e: e.tensor_tensor(out=yacc, in0=p_y1[:], in1=tmpy[i2][:], op=ALU.add), reads=[Tp_y1, Ttmpy[i2]], writes=[TY])
                            else:
                                k.op("pool", lambda e: e.tensor_tensor(out=yacc, in0=yacc, in1=tmpy[i2][:], op=ALU.add), reads=[TY, Ttmpy[i2]], writes=[TY])
                                k.op("dve", lambda e: e.tensor_tensor(out=yacc, in0=yacc, in1=p_y1[:], op=ALU.add), reads=[TY, Tp_y1], writes=[TY])
                        else:
                            if d == 0:
                                k.op("dve", lambda e: e.tensor_copy(out=yacc, in_=p_y1[:]), reads=[Tp_y1], writes=[TY])
                            else:
                                k.op("dve", lambda e: e.tensor_tensor(out=yacc, in0=yacc, in1=p_y1[:], op=ALU.add), reads=[TY, Tp_y1], writes=[TY])
                        if oi == NT - 1:
                            return
                        for h in range(4):
                            g = h // 2; gs = slice(g * 64, (g + 1) * 64)
                            k.op("pe", lambda e: e.matmul(p_st[gs, h % 2, :], lhsT=Bw[i2][:, g, h % 2, :], rhs=xst[:, ti, h * 64:(h + 1) * 64], start=True, stop=True,
                                                          tile_position=(0, g * 64)), reads=[TBw[i2], Txst], writes=[Tp_st])
                        if oi == 0:
                            k.op("dve", lambda e: e.tensor_copy(out=S32[:], in_=p_st[:]), reads=[Tp_st], writes=[TS32])
                        else:
                            k.op("pool", lambda e: e.tensor_tensor(out=S32[:], in0=S32[:], in1=etH[:, ti, d, :, None].to_broadcast([128, 2, 64]), op=ALU.mult),
                                 reads=[TS32, TetH], writes=[TS32])
                            k.op("dve", lambda e: e.tensor_tensor(out=S32[:], in0=S32[:], in1=p_st[:], op=ALU.add), reads=[TS32, Tp_st], writes=[TS32])
                        k.op("pool", lambda e: e.tensor_copy(out=STb[:], in_=S32[:]), reads=[TS32], writes=[TSTb])

                    seq = []
                    for d in range(2):
                        order = list(range(NT)) if d == 0 else [1, 0] + list(range(NT - 1, 1, -1))
                        for oi, ti in enumerate(order):
                            seq.append((d, oi, ti, len(seq) % 2))
                    ssd_front(*seq[0])
                    for i_ in range(len(seq)):
                        if i_ + 1 < len(seq):
                            ssd_front(*seq[i_ + 1])
                        ssd_back(*seq[i_])
                    zz = k.sb("zz", [128, NT, 256]); Tzz = T()
                    k.dma("sp", zz[:], ut_d[b, :, 256:512].rearrange("(n p) c -> p n c", p=128), reads=[Tut[b]], writes=[Tzz])
                    k.op("act", lambda e: e.activation(out=zz[:], in_=zz[:], func=AF.Silu), reads=[Tzz], writes=[Tzz])
                    tq = k.sb("tq", [128, NT, 256]); Ttq = T()
                    k.op("dve", lambda e: e.tensor_tensor(out=tq[:], in0=xst[:], in1=dsk[:, None, :].to_broadcast([128, NT, 256]), op=ALU.mult), reads=[Txst, Tp4], writes=[Ttq])
                    k.op("dve", lambda e: e.tensor_tensor(out=Yacc[:], in0=Yacc[:], in1=tq[:], op=ALU.add), reads=[TY, Ttq], writes=[TY])
                    k.op("dve", lambda e: e.tensor_tensor(out=Yacc[:], in0=Yacc[:], in1=zz[:], op=ALU.mult), reads=[TY, Tzz], writes=[TY])
                    k.op("pool", lambda e: e.tensor_tensor(out=tq[:], in0=Yacc[:], in1=Yacc[:], op=ALU.mult), reads=[TY], writes=[Ttq])
                    ssq = k.sb("ssq", [128, NT]); Tssq = T()
                    k.op("dve", lambda e: e.reduce_sum(out=ssq[:], in_=tq[:], axis=AX.X), reads=[Ttq], writes=[Tssq])
                    k.op("act", lambda e: e.activation(out=ssq[:], in_=ssq[:], func=AF.Sqrt, scale=1.0 / 256, bias=EPS), reads=[Tssq], writes=[Tssq])
                    k.op("dve", lambda e: e.reciprocal(out=ssq[:], in_=ssq[:]), reads=[Tssq], writes=[Tssq])
                    k.op("dve", lambda e: e.tensor_tensor(out=Yacc[:], in0=Yacc[:], in1=ssq[:, :, None].to_broadcast([128, NT, 256]), op=ALU.mult), reads=[TY, Tssq], writes=[TY])
                    yob = k.sb("yob", [128, NT, 256], BF16); Tyob = T()
                    k.op("dve", lambda e: e.tensor_tensor(out=yob[:], in0=Yacc[:], in1=snw[:, None, :].to_broadcast([128, NT, 256]), op=ALU.mult), reads=[TY, Tp4], writes=[Tyob])
                    ps_loop.__exit__(None, None, None)
                    ps_epi = PScope(k); ps_epi.__enter__()
                    p_tr = [k.ps("p4tre%d" % i, [128, 128], BF16) for i in range(2)]; Tp_tr = [PT(), PT()]
                    yoT = k.sb("yoT", [128, 2, S], BF16); TyoT = T()
                    for ti in range(NT):
                        for ch in range(2):
                            pp = p_tr[ntr % 2]; tp = Tp_tr[ntr % 2]
                            k.op("pe", lambda e: e.transpose(pp[:], yob[:, ti, ch * 128:(ch + 1) * 128], ident_b[:]), reads=[Tyob, Tc], writes=[tp])
                            if ntr % 2 == 0:
                                k.op("act", lambda e: e.activation(out=yoT[:, ch, ti * 128:(ti + 1) * 128], in_=pp[:], func=AF.Copy), reads=[tp], writes=[TyoT])
                            else:
                                k.op("dve", lambda e: e.tensor_copy(out=yoT[:, ch, ti * 128:(ti + 1) * 128], in_=pp[:]), reads=[tp], writes=[TyoT])
                            ntr += 1
                    k.dma("sp", yT_d[b, 512:768, :].rearrange("(c p) t -> p c t", p=128), yoT[:], reads=[TyoT], writes=[TyT[b]])
                    ps_epi.__exit__(None, None, None)
                if cfg.upto < 5:
                    continue
                need_ctx = l < L - 1
                with Stage(k):
                    Tp5 = T()
                    qan = k.sb("qan", [128, 192]); kvan = k.sb("kvan", [128, 128]); qnr = k.sb("qnr", [128, 96]); knr = k.sb("knr", [128, 96])
                    wq = k.sb("wq", [96, 2, 384], BF16); wkv = k.sb("wkv", [128, 512], BF16)
                    rope = k.sb("rope", [128, 16, 2, 16]); invn3 = k.sb("invn3", [128, 3]); invn8 = k.sb("invn8", [128, 8])
                    for dst, src in ((qan, ml_qan_d[:, l]), (kvan, ml_kvan_d[:, l]), (qnr, ml_qn_d[:, l]), (knr, ml_kn_d[:, l]), (rope, rope_d),
                                     (invn3, invn3_d), (invn8, invn8_d)):
                        k.dma("sp", dst[:], src, writes=[Tp5])
                    wqs = k.sb("wqs", [96, 2, 384]); wkvs = k.sb("wkvs", [128, 512]); Twqs = T()
                    k.dma("sp", wqs[:], ml_wq_d[l].rearrange("(c p) n -> p c n", p=96), writes=[Twqs])
                    k.dma("sp", wkvs[:], ml_wkv_d[l], writes=[Twqs])
                    k.op("pool", lambda e: e.tensor_copy(out=wq[:], in_=wqs[:]), reads=[Twqs], writes=[Tp5])
                    k.op("pool", lambda e: e.tensor_copy(out=wkv[:], in_=wkvs[:]), reads=[Twqs], writes=[Tp5])
                    QT = k.sb("QT", [96, 4, S], BF16); TQT = T()
                    KT = k.sb("KT", [96, 4, S], BF16); TKT = T()
                    Va = k.sb("Va", [128, NT, 4, 65], BF16); TVa = T()
                    k.op("pool", lambda e: e.memset(Va[:], 1.0), writes=[TVa])
                    um = [k.sb("um%d" % i, [128, 352]) for i in range(2)]; Tum = [T(), T()]
                    ps_prep = PScope(k); ps_prep.__enter__()
                    def dbl(name, shape, dt_=F32):
                        return [k.sb("%s_%d" % (name, i), shape, dt_) for i in range(2)], [T(), T()]
                    sqL, TsqL = dbl("sq5", [128, 512]); ss3L, Tss3L = dbl("ss3", [128, 3]); ss8L, Tss8L = dbl("ss8", [128, 12])
                    cnL, TcnL = dbl("cn", [128, 320], BF16); cTtL, TcTL = dbl("cTt", [128, 3, 128], BF16)
                    qfL, TqfL = dbl("qf", [128, 4, 96]); kvfL, TkvfL = dbl("kvf", [128, 4, 128])
                    rbL, TrbL = dbl("rb", [128, 5, 32]); raL, TraL = dbl("ra", [128, 4, 5, 16])
                    QbL, TQbL = dbl("Qb", [128, 4, 96], BF16); KbL, TKbL = dbl("Kb", [128, 4, 96], BF16)
                    p_trA = [k.ps("p5trA%d" % i, [128, 4, 128], BF16) for i in range(2)]; Tp_trA = [PT(), PT()]
                    p_trB = [k.ps("p5trB%d" % i, [128, 4, 128], BF16) for i in range(2)]; Tp_trB = [PT(), PT()]
                    p_qL = [k.ps("p_q%d" % i, [128, 384]) for i in range(2)]; Tp_qL = [PT(), PT()]
                    p_kvL = [k.ps("p_kv%d" % i, [128, 512]) for i in range(2)]; Tp_kvL = [PT(), PT()]
                    for ti in range(NT):
                        U = um[ti % 2]; tU = Tum[ti % 2]
                        j2 = ti % 2
                        sq = sqL[j2]; Tsq = TsqL[j2]; ss3 = ss3L[j2]; Tss3 = Tss3L[j2]; ss8 = ss8L[j2]; Tss8 = Tss8L[j2]
                        cn = cnL[j2]; Tcn = TcnL[j2]; cTt = cTtL[j2]; TcT = TcTL[j2]; qf = qfL[j2]; Tqf = TqfL[j2]; kvf = kvfL[j2]; Tkvf = TkvfL[j2]
                        rb = rbL[j2]; Trb = TrbL[j2]; ra = raL[j2]; Tra = TraL[j2]; Qb = QbL[j2]; TQb = TQbL[j2]; Kb = KbL[j2]; TKb = TKbL[j2]
                        p_q = p_qL[j2]; Tp_q = Tp_qL[j2]; p_kv = p_kvL[j2]; Tp_kv = Tp_kvL[j2]
                        p_tr = [p_trB[0], p_trB[1]]; Tp_tr = [Tp_trB[0], Tp_trB[1]]
                        k.dma("sp", U[:], ut_d[b, ti * 128:(ti + 1) * 128, 520:872], reads=[Tut[b]], writes=[tU])
                        k.op("pool", lambda e: e.tensor_tensor(out=sq[:, 0:352], in0=U[:], in1=U[:], op=ALU.mult), reads=[tU], writes=[Tsq])
                        for j, (a_, b_) in enumerate(((0, 192), (192, 320), (320, 352))):
                            k.op("dve", lambda e: e.reduce_sum(out=ss3[:, j:j + 1], in_=sq[:, a_:b_], axis=AX.X), reads=[Tsq], writes=[Tss3])
                        k.op("dve", lambda e: e.tensor_tensor(out=ss3[:], in0=ss3[:], in1=invn3[:], op=ALU.mult), reads=[Tss3, Tp5], writes=[Tss3])
                        k.op("act", lambda e: e.activation(out=ss3[:], in_=ss3[:], func=AF.Sqrt, bias=EPS), reads=[Tss3], writes=[Tss3])
                        k.op("dve", lambda e: e.reciprocal(out=ss3[:], in_=ss3[:]), reads=[Tss3], writes=[Tss3])
                        k.op("dve", lambda e: e.scalar_tensor_tensor(out=cn[:, 0:192], in0=U[:, 0:192], scalar=ss3[:, 0:1], in1=qan[:], op0=ALU.mult, op1=ALU.mult),
                             reads=[tU, Tss3, Tp5], writes=[Tcn])
                        k.op("dve", lambda e: e.scalar_tensor_tensor(out=cn[:, 192:320], in0=U[:, 192:320], scalar=ss3[:, 1:2], in1=kvan[:], op0=ALU.mult, op1=ALU.mult),
                             reads=[tU, Tss3, Tp5], writes=[Tcn])
                        k.op("dve", lambda e: e.scalar_tensor_tensor(out=rb[:, 4, :], in0=U[:, 320:352], scalar=ss3[:, 2:3], in1=knr[:, 64:96], op0=ALU.mult, op1=ALU.mult),
                             reads=[tU, Tss3, Tp5], writes=[Trb])
                        pt = p_trA[j2]; tpt = Tp_trA[j2]
                        k.op("pe", lambda e: e.transpose(pt[0:96, 0, :], cn[:, 0:96], ident_b[:]), reads=[Tcn, Tc], writes=[tpt])
                        k.op("pe", lambda e: e.transpose(pt[0:96, 1, :], cn[:, 96:192], ident_b[:]), reads=[Tcn, Tc], writes=[tpt])
                        k.op("pe", lambda e: e.transpose(pt[:, 2, :], cn[:, 192:320], ident_b[:]), reads=[Tcn, Tc], writes=[tpt])
                        k.op("act", lambda e: e.activation(out=cTt[0:96, 0:2, :], in_=pt[0:96, 0:2, :], func=AF.Copy), reads=[tpt], writes=[TcT])
                        k.op("act", lambda e: e.activation(out=cTt[:, 2, :], in_=pt[:, 2, :], func=AF.Copy), reads=[tpt], writes=[TcT])
                        for c_ in range(2):
                            k.op("pe", lambda e: e.matmul(p_q[:], lhsT=cTt[0:96, c_, :], rhs=wq[:, c_, :], start=(c_ == 0), stop=(c_ == 1)), reads=[TcT, Tp5], writes=[Tp_q])
                        k.op("pe", lambda e: e.matmul(p_kv[:], lhsT=cTt[:, 2, :], rhs=wkv[:], start=True, stop=True), reads=[TcT, Tp5], writes=[Tp_kv])
                        k.op("act", lambda e: e.activation(out=qf[:].rearrange("p h c -> p (h c)"), in_=p_q[:], func=AF.Copy), reads=[Tp_q], writes=[Tqf])
                        k.op("dve", lambda e: e.tensor_copy(out=kvf[:].rearrange("p h c -> p (h c)"), in_=p_kv[:]), reads=[Tp_kv], writes=[Tkvf])
                        sq4 = sq[:, 0:384].rearrange("p (h c) -> p h c", c=96)
                        k.op("pool", lambda e: e.tensor_tensor(out=sq4, in0=qf[:], in1=qf[:], op=ALU.mult), reads=[Tqf], writes=[Tsq])
                        k.op("dve", lambda e: e.reduce_sum(out=ss8[:, 0:4], in_=sq4[:, :, 0:64], axis=AX.X), reads=[Tsq], writes=[Tss8])
                        k.op("dve", lambda e: e.reduce_sum(out=ss8[:, 4:8], in_=sq4[:, :, 64:96], axis=AX.X), reads=[Tsq], writes=[Tss8])
                        sq5 = sq[:, 0:512].rearrange("p (h c) -> p h c", c=128)
                        k.op("pool", lambda e: e.tensor_tensor(out=sq5, in0=kvf[:], in1=kvf[:], op=ALU.mult), reads=[Tkvf, Tss8], writes=[Tsq])
                        k.op("dve", lambda e: e.reduce_sum(out=ss8[:, 8:12], in_=sq5[:, :, 0:64], axis=AX.X), reads=[Tsq], writes=[Tss8])
                        k.op("dve", lambda e: e.tensor_tensor(out=ss8[:, 0:8], in0=ss8[:, 0:8], in1=invn8[:], op=ALU.mult), reads=[Tss8, Tp5], writes=[Tss8])
                        k.op("dve", lambda e: e.tensor_tensor(out=ss8[:, 8:12], in0=ss8[:, 8:12], in1=invn8[:, 0:4], op=ALU.mult), reads=[Tss8, Tp5], writes=[Tss8])
                        k.op("act", lambda e: e.activation(out=ss8[:], in_=ss8[:], func=AF.Sqrt, bias=EPS), reads=[Tss8], writes=[Tss8])
                        k.op("dve", lambda e: e.reciprocal(out=ss8[:], in_=ss8[:]), reads=[Tss8], writes=[Tss8])
                        k.op("dve", lambda e: e.tensor_tensor(out=qf[:, :, 0:64], in0=qf[:, :, 0:64], in1=ss8[:, 0:4, None].to_broadcast([128, 4, 64]), op=ALU.mult),
                             reads=[Tqf, Tss8], writes=[Tqf])
                        k.op("dve", lambda e: e.tensor_tensor(out=Qb[:, :, 0:64], in0=qf[:, :, 0:64], in1=qnr[:, None, 0:64].to_broadcast([128, 4, 64]), op=ALU.mult),
                             reads=[Tqf, Tp5], writes=[TQb])
                        k.op("dve", lambda e: e.tensor_tensor(out=qf[:, :, 64:96], in0=qf[:, :, 64:96], in1=ss8[:, 4:8, None].to_broadcast([128, 4, 32]), op=ALU.mult),
                             reads=[Tqf, Tss8], writes=[Tqf])
                        k.op("dve", lambda e: e.tensor_tensor(out=rb[:, 0:4, :], in0=qf[:, :, 64:96], in1=qnr[:, None, 64:96].to_broadcast([128, 4, 32]), op=ALU.mult),
                             reads=[Tqf, Tp5], writes=[Trb])
                        k.op("dve", lambda e: e.tensor_tensor(out=kvf[:, :, 0:64], in0=kvf[:, :, 0:64], in1=ss8[:, 8:12, None].to_broadcast([128, 4, 64]), op=ALU.mult),
                             reads=[Tkvf, Tss8], writes=[Tkvf])
                        k.op("dve", lambda e: e.tensor_tensor(out=Kb[:, :, 0:64], in0=kvf[:, :, 0:64], in1=knr[:, None, 0:64].to_broadcast([128, 4, 64]), op=ALU.mult),
                             reads=[Tkvf, Tp5], writes=[TKb])
                        k.op("pool", lambda e: e.tensor_copy(out=Va[:, ti, :, 0:64], in_=kvf[:, :, 64:128]), reads=[Tkvf], writes=[TVa])
                        if ti >= 2:
                            rb4 = rb[:].rearrange("p h (j two) -> p h j two", two=2)
                            cs = rope[:, ti - 2, 0, None, :].to_broadcast([128, 5, 16]); sn = rope[:, ti - 2, 1, None, :].to_broadcast([128, 5, 16])
                            k.op("dve", lambda e: e.tensor_tensor(out=ra[:, 0], in0=rb4[:, :, :, 0], in1=cs, op=ALU.mult), reads=[Trb, Tp5], writes=[Tra])
                            k.op("dve", lambda e: e.tensor_tensor(out=ra[:, 1], in0=rb4[:, :, :, 1], in1=sn, op=ALU.mult), reads=[Trb, Tp5], writes=[Tra])
                            k.op("pool", lambda e: e.tensor_tensor(out=ra[:, 2], in0=rb4[:, :, :, 0], in1=sn, op=ALU.mult), reads=[Trb, Tp5], writes=[Tra])
                            k.op("pool", lambda e: e.tensor_tensor(out=ra[:, 3], in0=rb4[:, :, :, 1], in1=cs, op=ALU.mult), reads=[Trb, Tp5], writes=[Tra])
                            k.op("dve", lambda e: e.tensor_tensor(out=rb4[:, :, :, 0], in0=ra[:, 0], in1=ra[:, 1], op=ALU.subtract), reads=[Tra, Trb], writes=[Trb])
                            k.op("dve", lambda e: e.tensor_tensor(out=rb4[:, :, :, 1], in0=ra[:, 2], in1=ra[:, 3], op=ALU.add), reads=[Tra, Trb], writes=[Trb])
                        k.op("dve", lambda e: e.tensor_copy(out=Qb[:, :, 64:96], in_=rb[:, 0:4, :]), reads=[Trb], writes=[TQb])
                        k.op("dve", lambda e: e.tensor_copy(out=Kb[:, :, 64:96], in_=rb[:, 4:5, :].to_broadcast([128, 4, 32])), reads=[Trb], writes=[TKb])
                        for (src, tsrc, dstT, tdst, pi) in ((Qb, TQb, QT, TQT, 1), (Kb, TKb, KT, TKT, 0)):
                            pt = p_tr[pi]; tpt = Tp_tr[pi]
                            for h in range(4):
                                k.op("pe", lambda e: e.transpose(pt[0:96, h, :], src[:, h, :], ident_b[:]), reads=[tsrc, Tc], writes=[tpt])
                            if pi == 1:
                                k.op("act", lambda e: e.activation(out=dstT[:, :, ti * 128:(ti + 1) * 128], in_=pt[0:96, :, :], func=AF.Copy), reads=[tpt], writes=[tdst])
                            else:
                                k.op("dve", lambda e: e.tensor_copy(out=dstT[:, :, ti * 128:(ti + 1) * 128], in_=pt[0:96, :, :]), reads=[tpt], writes=[tdst])
                    ps_prep.__exit__(None, None, None)
                    ps_att = PScope(k); ps_att.__enter__()
                    p_tr = [k.ps("p5trC%d" % i, [128, 4, 128], BF16) for i in range(2)]; Tp_tr = [PT(), PT()]
                    PTt = [k.sb("PT%d" % i, [128, 512], BF16) for i in range(3)]; TPT = [T() for _ in range(3)]
                    p_s = [k.ps("p_s%d" % i, [128, 512]) for i in range(2)]; Tp_s = [PT(), PT()]
                    p_o = [k.ps("p_o%d" % i, [128, 4, 65]) for i in range(2)]; Tp_o = [PT(), PT()]
                    rec = k.sb("rec", [128, 4]); Trec = T()
                    ym = k.sb("ym", [128, NT, 256], BF16); Tym = T()
                    sc = 96.0 ** -0.5
                    nsc = 0; nh = 0
                    qblocks = BLOCKS if need_ctx else BLOCKS[1:]
                    its = []
                    for (q0, qn_) in qblocks:
                        keys = list(range(0, 2) if q0 == 0 else range(NT))
                        for h in range(4):
                            for ki, kt_ in enumerate(keys):
                                its.append((q0, qn_, h, ki, kt_, len(keys), len(its)))

                    def att_qk(q0, qn_, h, ki, kt_, nk, j):
                        ps_ = p_s[j % 2]; tps = Tp_s[j % 2]; P = PTt[j % 3]; tP = TPT[j % 3]
                        k.op("pe", lambda e: e.matmul(ps_[:, 0:qn_], lhsT=KT[:, h, kt_ * 128:(kt_ + 1) * 128], rhs=QT[:, h, q0:q0 + qn_], start=True, stop=True),
                             reads=[TKT, TQT], writes=[tps])
                        k.op("act", lambda e: e.activation(out=P[:, 0:qn_], in_=ps_[:, 0:qn_], func=AF.Exp, scale=sc), reads=[tps], writes=[tP])

                    def att_pv(q0, qn_, h, ki, kt_, nk, j):
                        P = PTt[j % 3]; tP = TPT[j % 3]
                        grp = j // 1
                        nq = qn_ // 128
                        gidx = (q0, h)
                        if ki == 0:
                            att_state["nh"] += 1
                        po = p_o[att_state["nh"] % 2]; tpo = Tp_o[att_state["nh"] % 2]
                        for qs_ in range(nq):
                            k.op("pe", lambda e: e.matmul(po[:, qs_, :], lhsT=P[:, qs_ * 128:(qs_ + 1) * 128], rhs=Va[:, kt_, h, :],
                                                          start=(ki == 0 and qs_ == 0), stop=(ki == nk - 1), skip_group_check=True),
                                 reads=[tP, TVa], writes=[tpo])
                        if ki == nk - 1:
                            k.op("dve", lambda e: e.reciprocal(out=rec[:, 0:nq], in_=po[:, 0:nq, 64]), reads=[tpo], writes=[Trec])
                            t_0 = q0 // 128
                            k.op("dve", lambda e: e.tensor_tensor(out=ym[:, t_0:t_0 + nq, h * 64:(h + 1) * 64], in0=po[:, 0:nq, 0:64],
                                                                  in1=rec[:, 0:nq, None].to_broadcast([128, nq, 64]), op=ALU.mult), reads=[tpo, Trec], writes=[Tym])

                    att_state = {"nh": 0}
                    att_qk(*its[0])
                    for j in range(len(its)):
                        if j + 1 < len(its):
                            att_qk(*its[j + 1])
                        att_pv(*its[j])
                    yoT = k.sb("yoT5", [128, 2, S], BF16); TyoT = T()
                    if not need_ctx:
                        k.op("pool", lambda e: e.memset(yoT[:, :, 0:NCTX], 0.0), writes=[TyoT])
                    ntr = 0
                    for ti in range(0 if need_ctx else 2, NT):
                        for ch in range(2):
                            pp = p_tr[ntr % 2]; tp = Tp_tr[ntr % 2]
                            k.op("pe", lambda e: e.transpose(pp[:, 0, :], ym[:, ti, ch * 128:(ch + 1) * 128], ident_b[:]), reads=[Tym, Tc], writes=[tp])
                            if ntr % 2 == 0:
                                k.op("act", lambda e: e.activation(out=yoT[:, ch, ti * 128:(ti + 1) * 128], in_=pp[:, 0, :], func=AF.Copy), reads=[tp], writes=[TyoT])
                            else:
                                k.op("dve", lambda e: e.tensor_copy(out=yoT[:, ch, ti * 128:(ti + 1) * 128], in_=pp[:, 0, :]), reads=[tp], writes=[TyoT])
                            ntr += 1
                    k.dma("sp", yT_d[b, 768:1024, :].rearrange("(c p) t -> p c t", p=128), yoT[:], reads=[TyoT], writes=[TyT[b]])
                    ps_att.__exit__(None, None, None)
                if cfg.upto < 6:
                    continue
                need_ctx = l < L - 1
                with Stage(k):
                    Tp6 = T()
                    wo = k.sb("wo", [128, 8, D], BF16); wrt = k.sb("wrt", [128, 8, NEXP], BF16)
                    cst = Caster(k, 128, D)
                    for kc in range(8):
                        cst.load(wo[:, kc, :], w_out_d[l, kc * 128:(kc + 1) * 128, :], 128, D, [Tp6])
                    wrs = k.sb("wrs", [128, 8, NEXP]); Twrs = T()
                    k.dma("sp", wrs[:], w_rt_d[l].rearrange("(c p) e -> p c e", p=128), writes=[Twrs])
                    k.op("pool", lambda e: e.tensor_copy(out=wrt[:], in_=wrs[:]), reads=[Twrs], writes=[Tp6])
                    G2 = k.sb("G2", [128, 2, 8]); Tg2 = T()
                    for i, mi in enumerate((b, 2)):
                        k.op("dve", lambda e: e.scalar_tensor_tensor(out=G2[:, i, :], in0=modT[:, l, 32:40, mi], scalar=1.0, in1=n2T[:, l, :],
                                                                    op0=ALU.add, op1=ALU.mult), reads=[Tmod, Tc], writes=[Tg2])
                    Yb = [k.sb("Yb%d" % i, [128, 8, 512], BF16) for i in range(2)]; TYb = [T(), T()]
                    Xb = [k.sb("Xb%d" % i, [128, 8, 512]) for i in range(2)]; TXb = [T(), T()]
                    Qs = k.sb("Qs", [128, 8, 512], BF16); TQs = T()
                    Rr6 = k.sb("Rr6", [128, 512]); TRr6 = T()
                    tm6 = [k.sb("tm6_%d" % i, [128, 512]) for i in range(2)]; Ttm6 = [T(), T()]
                    H2 = k.sb("H2", [128, 8, 512], BF16); TH2 = T()
                    h2o = [k.sb("h2o%d" % i, [128, D], BF16) for i in range(2)]; Th2o = [T(), T()]
                    Ee = k.sb("Ee", [16, 512]); TEe = T()
                    rc6 = k.sb("rc6", [16, 512]); Trc6 = T()
                    pwo = [k.ps("pwo%d" % i, [128, 512]) for i in range(2)]; Tpwo = [PT(), PT()]
                    pss = k.ps("pss6", [128, 512]); Tpss = PT()
                    ptr = [k.ps("ptr6_%d" % i, [128, 8, 128], BF16) for i in range(2)]; Tptr = [PT(), PT()]
                    prl = k.ps("prl", [16, 512]); Tprl = PT()
                    prs = k.ps("prs", [16, 512]); Tprs = PT()
                    ntr = 0
                    for bi, (t0, n) in enumerate(BLOCKS if need_ctx else BLOCKS[1:]):
                        isctx = (t0 == 0)
                        seg = 1 if isctx else 0
                        mi = 2 if isctx else b
                        Y = Yb[bi % 2]; tY = TYb[bi % 2]; X = Xb[bi % 2]; tX = TXb[bi % 2]
                        k.dma("sp", Y[:, :, 0:n], yT_d[b, :, t0:t0 + n].rearrange("(c p) t -> p c t", p=128), reads=[TyT[b]], writes=[tY])
                        k.dma("sp", X[:, :, 0:n], xT_d[b, :, t0:t0 + n].rearrange("(c p) t -> p c t", p=128), reads=[Tx[b]], writes=[tX])
                        for dch in range(8):
                            pp = pwo[dch % 2]; tp = Tpwo[dch % 2]
                            for c_ in range(8):
                                k.op("pe", lambda e: e.matmul(pp[:, 0:n], lhsT=wo[:, c_, dch * 128:(dch + 1) * 128], rhs=Y[:, c_, 0:n], start=(c_ == 0), stop=(c_ == 7)),
                                     reads=[Tp6, tY], writes=[tp])
                            k.op("dve", lambda e: e.scalar_tensor_tensor(out=X[:, dch, 0:n], in0=pp[:, 0:n], scalar=modT[:, l, 16 + dch, mi:mi + 1], in1=X[:, dch, 0:n],
                                                                        op0=ALU.mult, op1=ALU.add), reads=[tp, Tmod, tX], writes=[tX])
                        k.dma("sp", xT_d[b, :, t0:t0 + n].rearrange("(c p) t -> p c t", p=128), X[:, :, 0:n], reads=[tX], writes=[Tx[b]])
                        k.op("act", lambda e: e.activation(out=Qs[:, :, 0:n], in_=X[:, :, 0:n], func=AF.Square), reads=[tX], writes=[TQs])
                        for kc in range(8):
                            k.op("pe", lambda e: e.matmul(pss[:, 0:n], lhsT=ones_b[:], rhs=Qs[:, kc, 0:n], start=(kc == 0), stop=(kc == 7)), reads=[TQs, Tc], writes=[Tpss])
                        k.op("act", lambda e: e.activation(out=Rr6[:, 0:n], in_=pss[:, 0:n], func=AF.Sqrt, scale=1.0 / D, bias=EPS), reads=[Tpss], writes=[TRr6])
                        k.op("dve", lambda e: e.reciprocal(out=Rr6[:, 0:n], in_=Rr6[:, 0:n]), reads=[TRr6], writes=[TRr6])
                        for kc in range(8):
                            tm = tm6[kc % 2]; ttm = Ttm6[kc % 2]
                            k.op("dve", lambda e: e.tensor_tensor(out=tm[:, 0:n], in0=X[:, kc, 0:n], in1=Rr6[:, 0:n], op=ALU.mult), reads=[tX, TRr6], writes=[ttm])
                            k.op("act", lambda e: e.activation(out=H2[:, kc, 0:n], in_=tm[:, 0:n], func=AF.Identity, scale=G2[:, seg, kc:kc + 1],
                                                               bias=modT[:, l, 24 + kc, mi:mi + 1]), reads=[ttm, Tg2, Tmod], writes=[TH2])
                        for tt in range(n // 128):
                            pp = ptr[ntr % 2]; tp = Tptr[ntr % 2]; ho = h2o[ntr % 2]; tho = Th2o[ntr % 2]
                            for kc in range(8):
                                k.op("pe", lambda e: e.transpose(pp[:, kc, :], H2[:, kc, tt * 128:(tt + 1) * 128], ident_b[:]), reads=[TH2, Tc], writes=[tp])
                            if ntr % 2 == 0:
                                k.op("act", lambda e: e.activation(out=ho[:], in_=pp[:].rearrange("p c t -> p (c t)"), func=AF.Copy), reads=[tp], writes=[tho])
                            else:
                                k.op("dve", lambda e: e.tensor_copy(out=ho[:], in_=pp[:].rearrange("p c t -> p (c t)")), reads=[tp], writes=[tho])
                            k.dma("sp", h2t_d[b, t0 + tt * 128:t0 + (tt + 1) * 128, :], ho[:], reads=[tho], writes=[Th2[b]])
                            ntr += 1
                        for kc in range(8):
                            k.op("pe", lambda e: e.matmul(prl[:, 0:n], lhsT=wrt[:, kc, :], rhs=H2[:, kc, 0:n], start=(kc == 0), stop=(kc == 7)), reads=[Tp6, TH2], writes=[Tprl])
                        k.op("act", lambda e: e.activation(out=Ee[:, 0:n], in_=prl[:, 0:n], func=AF.Exp), reads=[Tprl], writes=[TEe])
                        k.op("pe", lambda e: e.matmul(prs[:, 0:n], lhsT=ones_f[0:16, 0:16], rhs=Ee[:, 0:n], start=True, stop=True), reads=[TEe, Tc], writes=[Tprs])
                        k.op("dve", lambda e: e.reciprocal(out=rc6[:, 0:n], in_=prs[:, 0:n]), reads=[Tprs], writes=[Trc6])
                        k.op("dve", lambda e: e.tensor_tensor(out=rc6[:, 0:n], in0=rc6[:, 0:n], in1=Ee[:, 0:n], op=ALU.mult), reads=[Trc6, TEe], writes=[Trc6])
                        k.dma("sp", aff_d[b, :, t0:t0 + n], rc6[:, 0:n], reads=[Trc6], writes=[Taff[b]])
                if cfg.upto < 7:
                    continue
                need_ctx = l < L - 1
                last = (l == cfg.layers - 1)
                segl = [(NCTX, NLAT, 256)] + ([(0, NCTX, 32)] if need_ctx else [])
                if l in getattr(cfg, 'skip_moe', ()) or b in getattr(cfg, 'skip_moe_b', ()):
                    segl = []
                segl = segl[:getattr(cfg, 'max_seg', 2)]
                for (t0, N, cap) in segl:
                  isctx = (t0 == 0)
                  mi = 2 if isctx else b
                  ntile = N // 128; nst = max(1, cap // 128); sp = min(cap, 128)
                  with Stage(k):
                    Tp7 = T()
                    iotaf = k.sb("iotaf", [128, 256]); iotap = k.sb("iotap", [128, 2])
                    for dst, src in ((iotaf, iotaf_d), (iotap, iotap_d)):
                        k.dma("sp", dst[:], src, writes=[Tp7])
                    slotm = k.sb("slotm", [16, N]); Tslot = T()
                    slotT = k.sb("slotT", [128, ntile, 16]); TslotT = T()
                    ghlT = k.sb("ghlT", [128, ntile, 16, 2], BF16); TghlT = T()
                    ysb = k.sb("ysb", [128, NEXP, nst, D], BF16); Tysb = T()
                    with Stage(k):
                        ones16 = k.sb("ones16", [16, NLAT])
                        k.dma("sp", ones16[:], ones16_d, writes=[Tp7])
                        affs = k.sb("affs", [16, N]); Taffs = T()
                        work = k.sb("work", [16, N]); Twork = T()
                        m8 = k.sb("m8", [16, 8]); Tm8 = T()
                        mask = k.sb("mask", [16, N]); Tmask = T()
                        ghi = k.sb("ghi", [16, N], BF16); glo = k.sb("glo", [16, N], BF16); Tg = T()
                        k.dma("sp", affs[:], aff_d[b, :, t0:t0 + N], reads=[Taff[b]], writes=[Taffs])
                        k.op("act", lambda e: e.activation(out=work[:], in_=affs[:], func=AF.Copy), reads=[Taffs], writes=[Twork])
                        for it_ in range(cap // 8):
                            k.op("dve", lambda e: e.max(out=m8[:], in_=work[:]), reads=[Twork], writes=[Tm8])
                            k.op("dve", lambda e: e.match_replace(out=work[:], in_to_replace=m8[:], in_values=work[:], imm_value=-1.0), reads=[Tm8, Twork], writes=[Twork])
                        k.op("dve", lambda e: e.tensor_scalar(out=mask[:], in0=work[:], scalar1=0.0, scalar2=None, op0=ALU.is_lt), reads=[Twork], writes=[Tmask])
                        k.op("dve", lambda e: e.tensor_tensor_scan(out=slotm[:], data0=ones16[:, 0:N], data1=mask[:], initial=0.0, op0=ALU.mult, op1=ALU.add),
                             reads=[Tmask, Tp7], writes=[Tslot])
                        k.op("dve", lambda e: e.tensor_tensor(out=slotm[:], in0=slotm[:], in1=mask[:], op=ALU.mult), reads=[Tslot, Tmask], writes=[Tslot])
                        k.op("dve", lambda e: e.tensor_scalar(out=slotm[:], in0=slotm[:], scalar1=-1.0, scalar2=None, op0=ALU.add), reads=[Tslot], writes=[Tslot])
                        k.op("dve", lambda e: e.tensor_tensor(out=affs[:], in0=affs[:], in1=mask[:], op=ALU.mult), reads=[Taffs, Tmask], writes=[Taffs])
                        k.op("dve", lambda e: e.tensor_copy(out=ghi[:], in_=affs[:]), reads=[Taffs], writes=[Tg])
                        k.op("dve", lambda e: e.tensor_tensor(out=affs[:], in0=affs[:], in1=ghi[:], op=ALU.subtract), reads=[Taffs, Tg], writes=[Taffs])
                        k.op("dve", lambda e: e.tensor_copy(out=glo[:], in_=affs[:]), reads=[Taffs], writes=[Tg])
                        pst = k.ps("pst", [128, ntile, 16]); Tpst = PT()
                        pgh = k.ps("pgh", [128, ntile, 2, 16], BF16); Tpgh = PT()
                        for ti in range(ntile):
                            cs_ = slice(ti * 128, (ti + 1) * 128)
                            k.op("pe", lambda e: e.transpose(pst[:, ti, :], slotm[:, cs_], ident_f[0:16, 0:16]), reads=[Tslot, Tc], writes=[Tpst])
                            k.op("pe", lambda e: e.transpose(pgh[:, ti, 0, :], ghi[:, cs_], ident_b[0:16, 0:16]), reads=[Tg, Tc], writes=[Tpgh])
                            k.op("pe", lambda e: e.transpose(pgh[:, ti, 1, :], glo[:, cs_], ident_b[0:16, 0:16]), reads=[Tg, Tc], writes=[Tpgh])
                        k.op("dve", lambda e: e.tensor_copy(out=slotT[:], in_=pst[:]), reads=[Tpst], writes=[TslotT])
                        k.op("dve", lambda e: e.tensor_copy(out=ghlT[:].rearrange("p n e h -> p n h e"), in_=pgh[:]), reads=[Tpgh], writes=[TghlT])
                    with Stage(k):
                        h2k = k.sb("h2k", [128, ntile, D], BF16); Th2k = T()
                        k.dma("sp", h2k[:], h2t_d[b, t0:t0 + N, :].rearrange("(n p) d -> p n d", p=128), reads=[Th2[b]], writes=[Th2k])
                        wg = [k.sb("wg%d" % i, [128, 8, FF], BF16) for i in range(2)]
                        wu = [k.sb("wu%d" % i, [128, 8, FF], BF16) for i in range(2)]
                        wd = [k.sb("wd%d" % i, [128, 4, D], BF16) for i in range(2)]
                        Tw = [T(), T()]
                        Sel = [k.sb("Sel%d" % i, [128, ntile, cap], BF16) for i in range(1)] * 2; TSel = [T()] * 2
                        cst7 = Caster(k, 128, 2048, nbuf=4)
                        xsTL = [k.sb("xsT%d" % i, [128, 8, cap], BF16) for i in range(1)] * 2; TxsTL = [T()] * 2
                        sg = [k.sb("sg%d" % i, [128, cap]) for i in range(2)]; Tsg = [T(), T()]
                        actTL = [k.sb("actT%d" % i, [128, 4, cap], BF16) for i in range(1)] * 2; TactTL = [T()] * 2
                        gs2L = [k.sb("gs2_%d" % i, [128, nst, 2]) for i in range(2)]; gsL = [k.sb("gs_%d" % i, [128, nst]) for i in range(2)]; TgsL = [T(), T()]
                        pg = [k.ps("pg7_%d" % i, [128, cap]) for i in range(3)]; Tpg = [PT() for _ in range(3)]
                        pG = k.ps("pG", [128, cap]); TpG = PT()
                        pU = k.ps("pU", [128, cap]); TpU = PT()
                        pY = [k.ps("pY%d" % i, [128, 512]) for i in range(2)]; TpY = [PT(), PT()]
                        pgs = k.ps("pgs", [128, nst, 2]); Tpgs = PT()
                        nev = 0
                        for ex in range(NEXP):
                            i2 = ex % 2
                            xsT = xsTL[i2]; TxsT = TxsTL[i2]; actT = actTL[i2]; TactT = TactTL[i2]
                            gs2 = gs2L[i2]; gs = gsL[i2]; Tgs = TgsL[i2]
                            for (wt_, src_, c_) in ((wg[i2], w_gate_d[l, ex], 8), (wu[i2], w_up_d[l, ex], 8), (wd[i2], w_down_d[l, ex], 4)):
                                dflat = wt_[:].rearrange("p c n -> p (c n)")
                                sflat = src_.rearrange("(p c) n -> p (c n)", c=c_)
                                for pc_ in range(2):
                                    cst7.load(dflat[:, pc_ * 2048:(pc_ + 1) * 2048], sflat[:, pc_ * 2048:(pc_ + 1) * 2048], 128, 2048, [Tw[i2]])
                            SL = Sel[i2]; tSL = TSel[i2]
                            for ti in range(ntile):
                                k.op("dve", lambda e: e.tensor_scalar(out=SL[:, ti, :], in0=iotaf[:, 0:cap], scalar1=slotT[:, ti, ex:ex + 1], scalar2=None, op0=ALU.is_equal),
                                     reads=[TslotT, Tp7], writes=[tSL])
                            for dc in range(8):
                                pp = pg[dc % 3]; tp = Tpg[dc % 3]
                                for ti in range(ntile):
                                    k.op("pe", lambda e: e.matmul(pp[:], lhsT=h2k[:, ti, dc::8], rhs=SL[:, ti, :], start=(ti == 0), stop=(ti == ntile - 1)),
                                         reads=[Th2k, tSL], writes=[tp])
                                if dc % 2 == 0:
                                    k.op("act", lambda e: e.activation(out=xsT[:, dc, :], in_=pp[:], func=AF.Copy), reads=[tp], writes=[TxsT])
                                else:
                                    k.op("dve", lambda e: e.tensor_copy(out=xsT[:, dc, :], in_=pp[:]), reads=[tp], writes=[TxsT])
                            for st in range(nst):
                                for ti in range(ntile):
                                    k.op("pe", lambda e: e.matmul(pgs[0:sp, st, :], lhsT=SL[:, ti, st * 128:st * 128 + sp], rhs=ghlT[:, ti, ex, :], start=(ti == 0), stop=(ti == ntile - 1)),
                                         reads=[tSL, TghlT], writes=[Tpgs])
                            k.op("act", lambda e: e.activation(out=gs2[0:sp], in_=pgs[0:sp], func=AF.Copy), reads=[Tpgs], writes=[Tgs])
                            k.op("dve", lambda e: e.tensor_tensor(out=gs[0:sp], in0=gs2[0:sp, :, 0], in1=gs2[0:sp, :, 1], op=ALU.add), reads=[Tgs], writes=[Tgs])
                            for fc in range(4):
                                for kc in range(8):
                                    k.op("pe", lambda e: e.matmul(pG[:], lhsT=wg[i2][:, kc, fc::4], rhs=xsT[:, kc, :], start=(kc == 0), stop=(kc == 7)),
                                         reads=[Tw[i2], TxsT], writes=[TpG])
                                for kc in range(8):
                                    k.op("pe", lambda e: e.matmul(pU[:], lhsT=wu[i2][:, kc, fc::4], rhs=xsT[:, kc, :], start=(kc == 0), stop=(kc == 7)),
                                         reads=[Tw[i2], TxsT], writes=[TpU])
                                k.op("act", lambda e: e.activation(out=sg[fc % 2][:], in_=pG[:], func=AF.Silu), reads=[TpG], writes=[Tsg[fc % 2]])
                                k.op("dve", lambda e: e.tensor_tensor(out=actT[:, fc, :], in0=sg[fc % 2][:], in1=pU[:], op=ALU.mult), reads=[Tsg[fc % 2], TpU], writes=[TactT])
                            for st in range(nst):
                                for dh in range(2):
                                    pp = pY[nev % 2]; tp = TpY[nev % 2]
                                    for fc in range(4):
                                        k.op("pe", lambda e: e.matmul(pp[0:sp, :], lhsT=actT[:, fc, st * 128:st * 128 + sp], rhs=wd[i2][:, fc, dh * 512:(dh + 1) * 512],
                                                                      start=(fc == 0), stop=(fc == 3)), reads=[TactT, Tw[i2]], writes=[tp])
                                    dsto = ysb[0:sp, ex, st, dh * 512:(dh + 1) * 512]
                                    if nev % 2 == 0:
                                        k.op("act", lambda e: e.activation(out=dsto, in_=pp[0:sp, :], func=AF.Copy, scale=gs[0:sp, st:st + 1]), reads=[tp, Tgs], writes=[Tysb])
                                    else:
                                        k.op("dve", lambda e: e.tensor_scalar(out=dsto, in0=pp[0:sp, :], scalar1=gs[0:sp, st:st + 1], scalar2=None, op0=ALU.mult),
                                             reads=[tp, Tgs], writes=[Tysb])
                                    nev += 1
                    with Stage(k):
                        oneh = k.sb("oneh", [16, 16, 128]); Toneh = T()
                        k.dma("sp", oneh[:], oneh_d, writes=[Toneh])
                        SelT = k.sb("SelT", [128, NEXP, nst, 512], BF16); TSelT = T()
                        Xc = [k.sb("Xc%d" % i, [128, 8, 512]) for i in range(2)]; TXc = [T(), T()]
                        ot = [k.sb("ot%d" % i, [128, D]) for i in range(2)]; Tot = [T(), T()]
                        pb = [k.ps("pb%d" % i, [128, 512]) for i in range(2)]; Tpb = [PT(), PT()]
                        pc = [k.ps("pc%d" % i, [128, 512]) for i in range(4)]; Tpc = [PT() for _ in range(4)]
                        po_ = [k.ps("po7_%d" % i, [128, 4, 128]) for i in range(2)]; Tpo_ = [PT(), PT()]
                        nto = 0
                        nblk = max(1, N // 512)
                        for tb in range(nblk):
                            n = min(N, 512)
                            c0 = tb * 512
                            X = Xc[tb % 2]; tX = TXc[tb % 2]
                            k.dma("sp", X[:, :, 0:n], xT_d[b, :, t0 + c0:t0 + c0 + n].rearrange("(c p) t -> p c t", p=128), reads=[Tx[b]], writes=[tX])
                            for ex in range(NEXP):
                                pp = pb[ex % 2]; tp = Tpb[ex % 2]
                                k.op("pe", lambda e: e.matmul(pp[:, 0:n], lhsT=oneh[:, ex, :], rhs=slotm[:, c0:c0 + n], start=True, stop=True), reads=[Tslot, Toneh], writes=[tp])
                                for st in range(nst):
                                    k.op("dve", lambda e: e.tensor_scalar(out=SelT[:, ex, st, 0:n], in0=pp[:, 0:n], scalar1=iotap[:, st:st + 1], scalar2=None, op0=ALU.is_equal),
                                         reads=[tp, Tp7], writes=[TSelT])
                            for dh in range(2):
                                for dcl in range(4):
                                    dc = dh * 4 + dcl
                                    pp = pc[dcl]; tp = Tpc[dcl]
                                    for ex in range(NEXP):
                                        for st in range(nst):
                                            k.op("pe", lambda e: e.matmul(pp[:, 0:n], lhsT=ysb[0:sp, ex, st, dc * 128:(dc + 1) * 128], rhs=SelT[0:sp, ex, st, 0:n],
                                                                          start=(ex == 0 and st == 0), stop=(ex == NEXP - 1 and st == nst - 1)),
                                                 reads=[Tysb, TSelT], writes=[tp])
                                    k.op("dve", lambda e: e.scalar_tensor_tensor(out=X[:, dc, 0:n], in0=pp[:, 0:n], scalar=modT[:, l, 40 + dc, mi:mi + 1], in1=X[:, dc, 0:n],
                                                                                op0=ALU.mult, op1=ALU.add), reads=[tp, Tmod, tX], writes=[tX])
                            if not last:
                                k.dma("sp", xT_d[b, :, t0 + c0:t0 + c0 + n].rearrange("(c p) t -> p c t", p=128), X[:, :, 0:n], reads=[tX], writes=[Tx[b]])
                            elif not isctx:
                                for tt in range(n // 128):
                                    O = ot[nto % 2]; tO = Tot[nto % 2]
                                    for half in range(2):
                                        pp = po_[half]; tp = Tpo_[half]
                                        for q in range(4):
                                            dc = half * 4 + q
                                            k.op("pe", lambda e: e.transpose(pp[:, q, :], X[:, dc, tt * 128:(tt + 1) * 128], ident_f[:]), reads=[tX, Tc], writes=[tp])
                                        if half == 0:
                                            k.op("act", lambda e: e.activation(out=O[:, 0:512], in_=pp[:].rearrange("p q t -> p (q t)"), func=AF.Copy), reads=[tp], writes=[tO])
                                        else:
                                            k.op("dve", lambda e: e.tensor_copy(out=O[:, 512:1024], in_=pp[:].rearrange("p q t -> p (q t)")), reads=[tp], writes=[tO])
                                    tok0 = c0 + tt * 128
                                    k.dma("sp", out_d[b, tok0:tok0 + 128, :], O[:], reads=[tO], writes=[Tout])
                                    nto += 1

        k.barrier()
        global LAST_K
        LAST_K = k
    return nc


def prep_inputs(inp, core, nb=2):
    b0 = core * 2
    m = {}
    m["x"] = np.ascontiguousarray(inp["x"][b0:b0 + nb])
    m["ctx"] = np.ascontiguousarray(inp["ctx"][b0:b0 + nb])
    cc = np.stack([inp["c"][b0], inp["c"][b0 + 1], inp["c_ctx"]], axis=0)
    m["cT"] = np.ascontiguousarray(fm(cc, 8).transpose(0, 2, 1))
    m["ada_w"] = inp["ada_w"]
    m["ada_bT"] = fm(inp["ada_b"], 48)
    m["n1T"] = fm(inp["norm1_w"], 8)
    m["n2T"] = fm(inp["norm2_w"], 8)
    m["w_in"] = inp["w_in"]
    m.update(prep_lru(inp))
    m["hg_lbT"] = fm(inp["hgrn_lb_logits"], 2)
    m["hg_nwT"] = fm(inp["hgrn_norm_w"], 2)
    scw = np.asarray(inp["ssd_conv_w"], np.float32)
    m["sd_cw"] = np.ascontiguousarray(scw.reshape(L, 4, 4, 128).transpose(3, 0, 2, 1))
    m["sd_cb"] = fm(inp["ssd_conv_b"], 4)
    rep = lambda v: np.ascontiguousarray(np.broadcast_to(np.asarray(v, np.float32)[None], (128,) + tuple(np.shape(v))))
    m["sd_alog"] = rep(np.asarray(inp["ssd_a_log"]).reshape(L, 8))
    m["sd_dtb"] = rep(np.asarray(inp["ssd_dt_bias"]).reshape(L, 8))
    m["sd_dsk"] = rep(np.repeat(np.asarray(inp["ssd_d_skip"]), 64, axis=-1))
    m["sd_nw"] = rep(inp["ssd_norm_w"])
    m["ml_qan"] = rep(inp["mla_q_a_norm"]); m["ml_kvan"] = rep(inp["mla_kv_a_norm"])
    m["ml_qn"] = rep(inp["mla_q_norm"]); m["ml_kn"] = rep(inp["mla_k_norm"])
    m["ml_wq"] = inp["mla_w_q_up"]; m["ml_wkv"] = inp["mla_w_kv_up"]
    m["w_out"] = inp["w_out"]; m["w_rt"] = inp["moe_router"]
    m["w_gate"] = inp["moe_w_gate"]; m["w_up"] = inp["moe_w_up"]; m["w_down"] = inp["moe_w_down"]
    m.update(host_consts())
    return m


def kernel(**inputs):
    inp = {k_: np.asarray(v) for k_, v in inputs.items()}
    cfg = Cfg(nb=2)
    nc = build(cfg)
    in_maps = [prep_inputs(inp, c) for c in range(8)]
    res = run_bass_kernel_spmd(nc, in_maps, core_ids=list(range(8)))
    out = np.concatenate([r["out"] for r in res.results], axis=0)
    return out.astype(np.float32)
```

```python
import math
from contextlib import ExitStack
import numpy as np
import ml_dtypes
import concourse.bass as bass
import concourse.mybir as mybir
from concourse.bass_utils import run_bass_kernel_spmd

F32 = mybir.dt.float32
BF16 = mybir.dt.bfloat16
I32 = mybir.dt.int32
U32 = mybir.dt.uint32
ALU = mybir.AluOpType
AF = mybir.ActivationFunctionType
AX = mybir.AxisListType

L = 2
D = 1024
NLAT = 2048
NCTX = 256
S = NCTX + NLAT
NT = S // 128
IN_COLS = 2920
EPS = 1e-6
NEXP = 16
FF = 512
TM_RANGES = [(1280, 1536), (1792, 2048), (2560, 2920)]
TM_COLS = sum(b - a for a, b in TM_RANGES)
FM_CHUNKS = list(range(0, 10)) + [12, 13] + [16, 17, 18, 19]
BLOCKS = [(0, 256)] + [(256 + 512 * i, 512) for i in range(4)]


class T:
    __slots__ = ("name", "w", "r", "x")

    def __init__(self, name="", x=False):
        self.name = name
        self.w = None
        self.r = []
        self.x = x


def PT():
    return T("psum", True)


class _Rec:
    def __init__(self):
        self.call = None

    def __getattr__(self, name):
        def f(*a, **kw):
            self.call = (name, a, kw)
            return self
        return f


class K:
    N_DMA_SEMS = 24

    def __init__(self, nc, stack):
        self.nc = nc
        self.stack = stack
        self.eng = {"pe": nc.tensor, "dve": nc.vector, "act": nc.scalar,
                    "pool": nc.gpsimd, "sp": nc.sync}
        self.sems = {}
        self.count = {}
        self.seen = {e: {} for e in self.eng}
        for e in self.eng:
            self.sems[e] = stack.enter_context(nc.semaphore("s_" + e))
            self.count[e] = 0
        for i in range(self.N_DMA_SEMS):
            k = "d%d" % i
            self.sems[k] = stack.enter_context(nc.semaphore("s_" + k))
            self.count[k] = 0
        self.dma_rr = 0
        self.n_inst = 0
        self.scope = stack

    def sb(self, name, shape, dtype=F32):
        self.n_alloc = getattr(self, "n_alloc", 0) + 1
        return self.scope.enter_context(self.nc.sbuf_tensor("sb%d_%s" % (self.n_alloc, name), list(shape), dtype))

    def ps(self, name, shape, dtype=F32):
        self.n_alloc = getattr(self, "n_alloc", 0) + 1
        nel = 512 if dtype == F32 else 1024
        scope = getattr(self, "pscope", None) or self.scope
        full = scope.enter_context(self.nc.psum_tensor("ps%d_%s" % (self.n_alloc, name), [128, nel], dtype))
        n = 1
        for d_ in shape[1:]:
            n *= d_
        assert n <= nel, (name, shape)
        v = full[0:shape[0], 0:n]
        if len(shape) == 3:
            v = v.rearrange("p (a b) -> p a b", b=shape[2])
        elif len(shape) == 4:
            v = v.rearrange("p (a b c) -> p a b c", b=shape[2], c=shape[3])
        return v

    def _waits(self, e, reads, writes):
        need = {}
        for t in reads:
            if t.w is not None:
                k, v, pe = t.w
                if not (pe == "pe" and e == "pe"):
                    need[k] = max(need.get(k, 0), v)
        for t in writes:
            if t.w is not None:
                k, v, pe = t.w
                if not (pe == "pe" and e == "pe"):
                    need[k] = max(need.get(k, 0), v)
            for (k, v, pe) in t.r:
                if pe == "pe" and e == "pe":
                    continue
                need[k] = max(need.get(k, 0), v)
        seen = self.seen[e]
        h = self.eng[e]
        for k, v in need.items():
            if seen.get(k, 0) < v:
                h.wait_ge(self.sems[k], v)
                seen[k] = v

    def _commit(self, tok, reads, writes):
        for t in writes:
            t.w = tok
            t.r = []
        for t in reads:
            if t not in writes:
                t.r.append(tok)
                if len(t.r) > 16:
                    best = {}
                    for (k, v, pe) in t.r:
                        if k not in best or best[k][1] < v:
                            best[k] = (k, v, pe)
                    t.r = list(best.values())

    def flush(self, pend, n=None):
        n = len(pend) if n is None else min(n, len(pend))
        for _ in range(n):
            it = pend.pop(0)
            if it[0] == "op":
                _, e, (name, a, kw), reads, writes = it
                self.op(e, lambda eng: getattr(eng, name)(*a, **kw), reads, writes)
            else:
                _, e, out, in_, reads, writes, kw = it
                self.dma(e, out, in_, reads, writes, **kw)

    def op(self, e, fn, reads=(), writes=()):
        reads = list(reads)
        writes = list(writes)
        if getattr(self, "defer", None) is not None:
            rec = _Rec()
            fn(rec)
            self.defer.append(("op", e, rec.call, reads, writes))
            return None
        xr = [t for t in reads if t.x]
        if xr:
            reads = [t for t in reads if not t.x]
            writes = writes + [t for t in xr if t not in writes]
        self._waits(e, reads, writes)
        ins = fn(self.eng[e])
        self.count[e] += 1
        ins.then_inc(self.sems[e], 1)
        self._commit((e, self.count[e], e), reads, writes)
        self.n_inst += 1
        return ins

    def dma(self, e, out, in_, reads=(), writes=(), **kw):
        reads = list(reads)
        writes = list(writes)
        if getattr(self, "defer", None) is not None:
            self.defer.append(("dma", e, out, in_, reads, writes, kw))
            return None
        self._waits(e, reads, writes)
        k = "d%d" % self.dma_rr
        self.dma_rr = (self.dma_rr + 1) % self.N_DMA_SEMS
        ins = self.eng[e].dma_start(out=out, in_=in_, **kw)
        self.count[k] += 16
        ins.then_inc(self.sems[k], 16)
        self._commit((k, self.count[k], "dma"), reads, writes)
        self.n_inst += 1
        return ins

    def barrier(self):
        for e, h in self.eng.items():
            seen = self.seen[e]
            for k, v in self.count.items():
                if v > 0 and seen.get(k, 0) < v and k != e:
                    h.wait_ge(self.sems[k], v)
                    seen[k] = v


class PScope:
    def __init__(self, k):
        self.k = k

    def __enter__(self):
        self.prev = getattr(self.k, "pscope", None)
        self.st = ExitStack()
        self.st.__enter__()
        self.k.pscope = self.st
        return self

    def __exit__(self, *a):
        self.k.barrier()
        self.k.pscope = self.prev
        return self.st.__exit__(*a)


class Caster:
    def __init__(self, k, npart, nfree, nbuf=2, eng="act"):
        self.k = k
        self.eng = eng
        self.st = [k.sb("stg%d" % i, [npart, nfree]) for i in range(nbuf)]
        self.T = [T() for _ in range(nbuf)]
        self.i = 0

    def load(self, dst, src, npart, nfree, writes, reads=(), split=None):
        k = self.k
        j = self.i % len(self.st)
        self.i += 1
        st = self.st[j][0:npart, 0:nfree]
        if split is not None:
            st = st.rearrange("p (a b) -> p a b", b=split)
        if getattr(self, "alt", False) and self.i % 2 == 0:
            k.dma("sp", st, src, reads=list(reads), writes=[self.T[j]])
            k.op("dve", lambda e: e.tensor_copy(out=dst, in_=st), reads=[self.T[j]], writes=list(writes))
            return
        k.dma("sp", st, src, reads=list(reads), writes=[self.T[j]])
        if self.eng == "act":
            k.op("act", lambda e: e.activation(out=dst, in_=st, func=AF.Copy), reads=[self.T[j]], writes=list(writes))
        else:
            k.op(self.eng, lambda e: e.tensor_copy(out=dst, in_=st), reads=[self.T[j]], writes=list(writes))


class Stage:
    def __init__(self, k):
        self.k = k

    def __enter__(self):
        self.prev = self.k.scope
        self.st = ExitStack()
        self.st.__enter__()
        self.k.scope = self.st
        return self

    def __exit__(self, *a):
        self.k.barrier()
        self.k.scope = self.prev
        return self.st.__exit__(*a)


def fm(v, nch):
    v = np.asarray(v, np.float32)
    lead = v.shape[:-1]
    r = v.reshape(lead + (nch, 128))
    r = np.moveaxis(r, -1, 0)
    return np.ascontiguousarray(r)


def host_consts():
    c = {}
    c["ident_f"] = np.eye(128, dtype=np.float32)
    c["ident_b"] = np.eye(128, dtype=np.float32).astype(ml_dtypes.bfloat16)
    c["ones_b"] = np.ones((128, 128), np.float32).astype(ml_dtypes.bfloat16)
    c["ones_f"] = np.ones((128, 128), np.float32)
    t = np.arange(S)
    c["mfwd"] = np.ascontiguousarray(np.broadcast_to((t % 128 != 0).astype(np.float32), (128, S)))
    c["mbwd"] = np.ascontiguousarray(np.broadcast_to((t % 128 != 127).astype(np.float32), (128, S)))
    i = np.arange(128)
    c["triU"] = (i[:, None] <= i[None, :]).astype(np.uint32)
    c["triL"] = (i[:, None] >= i[None, :]).astype(np.uint32)
    c["triUf"] = (i[:, None] <= i[None, :]).astype(np.float32)
    c["triLf"] = (i[:, None] >= i[None, :]).astype(np.float32)
    c["strLf"] = (i[:, None] > i[None, :]).astype(np.float32)
    c["strUf"] = (i[:, None] < i[None, :]).astype(np.float32)
    tt = np.arange(NLAT)
    inv = 10000.0 ** (-np.arange(0, 16, 2, dtype=np.float32) / 16)
    ang = np.concatenate([(tt // 64).astype(np.float32)[:, None] * inv, (tt % 64).astype(np.float32)[:, None] * inv], axis=-1).astype(np.float32)
    cs = np.stack([np.cos(ang), np.sin(ang)], axis=1).astype(np.float32)
    c["rope"] = np.ascontiguousarray(cs.reshape(16, 128, 2, 16).transpose(1, 0, 2, 3))
    c["invn3"] = np.ascontiguousarray(np.broadcast_to(np.array([1 / 192, 1 / 128, 1 / 32], np.float32), (128, 3)))
    c["invn8"] = np.ascontiguousarray(np.broadcast_to(np.array([1 / 64] * 4 + [1 / 32] * 4, np.float32), (128, 8)))
    c["iotaf"] = np.ascontiguousarray(np.broadcast_to(np.arange(256, dtype=np.float32), (128, 256)))
    c["iotap"] = np.stack([i.astype(np.float32), i.astype(np.float32) + 128], axis=1)
    oh = np.zeros((16, 16, 128), np.float32)
    for e_ in range(16):
        oh[e_, e_, :] = 1.0
    c["oneh"] = oh
    c["ones16"] = np.ones((16, NLAT), np.float32)
    rep4 = lambda a_: np.ascontiguousarray(np.broadcast_to(a_[:, None, :], (128, 4, 128))).astype(np.float32)
    c["triUf4"] = rep4(c["triUf"]); c["triLf4"] = rep4(c["triLf"])
    c["negmf"] = rep4(-1.0e4 * c["strLf"]); c["negmb"] = rep4(-1.0e4 * c["strUf"])
    c["blk64"] = (i[:, None] // 64 == i[None, :] // 64).astype(np.float32).astype(ml_dtypes.bfloat16)
    return c


def prep_lru(inp):
    m = {}
    cw = np.asarray(inp["lru_conv_w"], np.float32)
    m["lru_cw"] = np.ascontiguousarray(cw.reshape(L, 4, 2, 128).transpose(3, 0, 2, 1))
    m["lru_cb"] = fm(inp["lru_conv_b"], 2)
    for nm, key in (("lru_wr", "lru_w_r"), ("lru_wi", "lru_w_i")):
        w = np.asarray(inp[key], np.float32)
        o = np.zeros((128, L, 2, 2, 128), np.float32)
        for ch in range(2):
            for hh in range(2):
                o[hh * 64:(hh + 1) * 64, :, :, ch, hh * 64:(hh + 1) * 64] = w[:, :, 2 * ch + hh].transpose(2, 0, 1, 3)
        m[nm] = o
    m["lru_br"] = fm(inp["lru_b_r"], 2)
    m["lru_bi"] = fm(inp["lru_b_i"], 2)
    m["lru_lam"] = fm(inp["lru_lam"], 2)
    return m


class Cfg:
    def __init__(self, nb=2, upto=99, debug=False, layers=2):
        self.layers = layers
        self.nb = nb
        self.upto = upto
        self.debug = debug


def build(cfg):
    nc = bass.Bass("TRN2", target_bir_lowering=False)
    NB = cfg.nb
    dbg_kind = "ExternalOutput" if cfg.debug else "Internal"

    def din(name, shape, dt=F32):
        return nc.dram_tensor(name, list(shape), dt, kind="ExternalInput").ap()

    def dscr(name, shape, dt=F32):
        return nc.dram_tensor(name, list(shape), dt, kind=dbg_kind).ap()

    x_d = din("x", [NB, NLAT, D])
    ctx_d = din("ctx", [NB, NCTX, D])
    cT_d = din("cT", [128, 8, 3])
    ada_w_d = din("ada_w", [L, D, 6 * D])
    ada_bT_d = din("ada_bT", [128, L, 48])
    n1T_d = din("n1T", [128, L, 8])
    n2T_d = din("n2T", [128, L, 8])
    w_in_d = din("w_in", [L, D, IN_COLS])
    lru_cw_d = din("lru_cw", [128, L, 2, 4])
    lru_cb_d = din("lru_cb", [128, L, 2])
    lru_wr_d = din("lru_wr", [128, L, 2, 2, 128])
    lru_wi_d = din("lru_wi", [128, L, 2, 2, 128])
    lru_br_d = din("lru_br", [128, L, 2, 2])
    lru_bi_d = din("lru_bi", [128, L, 2, 2])
    lru_lam_d = din("lru_lam", [128, L, 2, 2])
    hg_lbT_d = din("hg_lbT", [128, L, 2])
    hg_nwT_d = din("hg_nwT", [128, L, 2])
    mfwd_d = din("mfwd", [128, S])
    mbwd_d = din("mbwd", [128, S])
    triU_d = din("triU", [128, 128], U32)
    triL_d = din("triL", [128, 128], U32)
    blk64_d = din("blk64", [128, 128], BF16)
    sd_cw_d = din("sd_cw", [128, L, 4, 4])
    sd_cb_d = din("sd_cb", [128, L, 4])
    sd_alog_d = din("sd_alog", [128, L, 8])
    sd_dtb_d = din("sd_dtb", [128, L, 8])
    sd_dsk_d = din("sd_dsk", [128, L, 256])
    sd_nw_d = din("sd_nw", [128, L, 256])
    triUf_d = din("triUf", [128, 128])
    triLf_d = din("triLf", [128, 128])
    strLf_d = din("strLf", [128, 128])
    strUf_d = din("strUf", [128, 128])
    ml_qan_d = din("ml_qan", [128, L, 192])
    ml_kvan_d = din("ml_kvan", [128, L, 128])
    ml_qn_d = din("ml_qn", [128, L, 96])
    ml_kn_d = din("ml_kn", [128, L, 96])
    ml_wq_d = din("ml_wq", [L, 192, 384])
    ml_wkv_d = din("ml_wkv", [L, 128, 512])
    rope_d = din("rope", [128, 16, 2, 16])
    invn3_d = din("invn3", [128, 3])
    invn8_d = din("invn8", [128, 8])
    w_out_d = din("w_out", [L, D, D])
    w_rt_d = din("w_rt", [L, D, NEXP])
    w_gate_d = din("w_gate", [L, NEXP, D, FF])
    w_up_d = din("w_up", [L, NEXP, D, FF])
    w_down_d = din("w_down", [L, NEXP, FF, D])
    iotaf_d = din("iotaf", [128, 256])
    iotap_d = din("iotap", [128, 2])
    oneh_d = din("oneh", [16, 16, 128])
    ones16_d = din("ones16", [16, NLAT])
    triUf4_d = din("triUf4", [128, 4, 128])
    triLf4_d = din("triLf4", [128, 4, 128])
    negmf_d = din("negmf", [128, 4, 128])
    negmb_d = din("negmb", [128, 4, 128])
    ident_f_d = din("ident_f", [128, 128])
    ident_b_d = din("ident_b", [128, 128], BF16)
    ones_b_d = din("ones_b", [128, 128], BF16)
    ones_f_d = din("ones_f", [128, 128])
    out_d = nc.dram_tensor("out", [NB, NLAT, D], F32, kind="ExternalOutput").ap()

    xT_d = dscr("xT", [NB, D, S])
    uT_d = dscr("uT", [NB, IN_COLS, S])
    ut_d = dscr("ut", [NB, S, TM_COLS])
    yT_d = dscr("yT", [NB, D, S], BF16)
    TyT = [T() for b in range(NB)]
    h2t_d = dscr("h2t", [NB, S, D], BF16)
    aff_d = dscr("aff", [NB, NEXP, S])
    Th2 = [T() for b in range(NB)]
    Taff = [T() for b in range(NB)]
    Tx = [T("xT%d" % b) for b in range(NB)]
    TuT = [T() for b in range(NB)]
    Tut = [T() for b in range(NB)]
    Tout = T("out")

    with ExitStack() as root:
        k = K(nc, root)
        ident_f = k.sb("ident_f", [128, 128]); ident_b = k.sb("ident_b", [128, 128], BF16)
        ones_b = k.sb("ones_b", [128, 128], BF16); ones_f = k.sb("ones_f", [128, 128])
        modT = k.sb("modT", [128, L, 48, 3])
        n1T = k.sb("n1T", [128, L, 8]); n2T = k.sb("n2T", [128, L, 8])
        Tc = T("consts")
        Tmod = T("mod")
        k.dma("sp", ident_f[:], ident_f_d, writes=[Tc])
        k.dma("sp", ident_b[:], ident_b_d, writes=[Tc])
        k.dma("sp", ones_b[:], ones_b_d, writes=[Tc])
        k.dma("sp", ones_f[:], ones_f_d, writes=[Tc])
        k.dma("sp", n1T[:], n1T_d, writes=[Tc])
        k.dma("sp", n2T[:], n2T_d, writes=[Tc])

        with Stage(k):
            cT = k.sb("cT", [128, 8, 3]); sT = k.sb("sT", [128, 8, 3])
            abT = k.sb("abT", [128, L, 48])
            Tct = T(); Tst = T()
            k.dma("sp", cT[:], cT_d, writes=[Tct])
            k.dma("sp", abT[:], ada_bT_d, writes=[Tct])
            k.op("act", lambda e: e.activation(out=sT[:], in_=cT[:], func=AF.Silu), reads=[Tct], writes=[Tst])
            wbuf = [k.sb("adaw%d" % i, [128, 8, 512]) for i in range(2)]
            Tw = [T(), T()]
            pm = [k.ps("pm%d" % i, [128, 4, 4]) for i in range(2)]
            Tpm = [PT(), PT()]
            it = 0
            for l in range(L):
                for j in range(12):
                    wb = wbuf[it % 2]; tw = Tw[it % 2]
                    k.dma("sp", wb[:], ada_w_d[l, :, j * 512:(j + 1) * 512].rearrange("(kc p) n -> p kc n", p=128), writes=[tw])
                    pp = pm[it % 2]; tp = Tpm[it % 2]
                    for sub in range(4):
                        for kc in range(8):
                            k.op("pe", lambda e: e.matmul(pp[:, sub, 0:3], lhsT=wb[:, kc, sub * 128:(sub + 1) * 128],
                                                          rhs=sT[:, kc, :], start=(kc == 0), stop=(kc == 7)),
                                 reads=[tw, Tst], writes=[tp])
                    for sub in range(4):
                        ch = j * 4 + sub
                        k.op("dve", lambda e: e.tensor_scalar(out=modT[:, l, ch, :], in0=pp[:, sub, 0:3],
                                                              scalar1=abT[:, l, ch:ch + 1], scalar2=None, op0=ALU.add),
                             reads=[tp, Tct], writes=[Tmod])
                    it += 1

        with Stage(k):
            xin = [k.sb("xin%d" % i, [128, D]) for i in range(3)]
            Txin = [T() for _ in range(3)]
            xo = [k.sb("xo%d" % i, [128, 8, 128]) for i in range(3)]
            Txo = [T() for _ in range(3)]
            pt = [k.ps("pt%d" % i, [128, 4, 128]) for i in range(4)]
            Tpt = [PT() for _ in range(4)]
            it = 0
            for b in range(NB):
                for ti in range(NT):
                    src = ctx_d[b, ti * 128:(ti + 1) * 128, :] if ti < 2 else x_d[b, (ti - 2) * 128:(ti - 1) * 128, :]
                    xi = xin[it % 3]; txi = Txin[it % 3]
                    k.dma("sp", xi[:], src, writes=[txi])
                    xx = xo[it % 3]; txo = Txo[it % 3]
                    for half in range(2):
                        pp = pt[(2 * it + half) % 4]; tp = Tpt[(2 * it + half) % 4]
                        for q in range(4):
                            kc = half * 4 + q
                            k.op("pe", lambda e: e.transpose(pp[:, q, :], xi[:, kc * 128:(kc + 1) * 128], ident_f[:]),
                                 reads=[txi, Tc], writes=[tp])
                        if half == 0:
                            k.op("act", lambda e: e.activation(out=xx[:, 0:4, :], in_=pp[:], func=AF.Copy), reads=[tp], writes=[txo])
                        else:
                            k.op("dve", lambda e: e.tensor_copy(out=xx[:, 4:8, :], in_=pp[:]), reads=[tp], writes=[txo])
                    k.dma("sp", xT_d[b, :, ti * 128:(ti + 1) * 128].rearrange("(kc p) t -> p kc t", p=128), xx[:],
                          reads=[txo], writes=[Tx[b]])
                    it += 1

        for l in range(cfg.layers):
            if cfg.upto < 1:
                break
            for b in range(NB):
                with Stage(k):
                    w_in = k.sb("w_in", [128, 8, IN_COLS], BF16); Tw = T()
                    cst = Caster(k, 128, IN_COLS)
                    for kc in range(8):
                        cst.load(w_in[:, kc, :], w_in_d[l, kc * 128:(kc + 1) * 128, :], 128, IN_COLS, [Tw])
                    G = k.sb("G", [128, 2, 8]); Tg = T()
                    for i, mi in enumerate((b, 2)):
                        k.op("dve", lambda e: e.scalar_tensor_tensor(out=G[:, i, :], in0=modT[:, l, 8:16, mi], scalar=1.0,
                                                                    in1=n1T[:, l, :], op0=ALU.add, op1=ALU.mult),
                             reads=[Tmod, Tc], writes=[Tg])
                    xb = [k.sb("xb%d" % i, [128, 8, 512]) for i in range(2)]; Txb = [T(), T()]
                    sq = [k.sb("sq%d" % i, [128, 8, 512], BF16) for i in range(2)]; Tsq = [T(), T()]
                    rs = [k.sb("rs%d" % i, [128, 512]) for i in range(2)]; Trs = [T(), T()]
                    tmp = [k.sb("tmp%d" % i, [128, 512]) for i in range(2)]; Ttmp = [T(), T()]
                    hT = [k.sb("hT%d" % i, [128, 8, 512], BF16) for i in range(2)]; ThT = [T(), T()]
                    ev = [k.sb("ev%d" % i, [128, 512]) for i in range(4)]; Tev = [T() for _ in range(4)]
                    evt = [k.sb("evt%d" % i, [128, TM_COLS]) for i in range(2)]; Tevt = [T(), T()]
                    pss = k.ps("pss", [128, 512]); Tpss = PT()
                    pu = [k.ps("pu%d" % i, [128, 512]) for i in range(4)]; Tpu = [PT() for _ in range(4)]
                    pv = [k.ps("pv%d" % i, [128, 512]) for i in range(3)]; Tpv = [PT() for _ in range(3)]
                    nev_box = [0]

                    def s1_norm(bi):
                        t0, n = BLOCKS[bi]
                        seg = 1 if bi == 0 else 0
                        mi = 2 if bi == 0 else b
                        X = xb[bi % 2]; tX = Txb[bi % 2]
                        k.dma("sp", X[:, :, 0:n], xT_d[b, :, t0:t0 + n].rearrange("(kc p) t -> p kc t", p=128),
                              reads=[Tx[b]], writes=[tX])
                        Q = sq[bi % 2]; tQ = Tsq[bi % 2]
                        k.op("act", lambda e: e.activation(out=Q[:, :, 0:n], in_=X[:, :, 0:n], func=AF.Square), reads=[tX], writes=[tQ])
                        for kc in range(8):
                            k.op("pe", lambda e: e.matmul(pss[:, 0:n], lhsT=ones_b[:], rhs=Q[:, kc, 0:n], start=(kc == 0), stop=(kc == 7)),
                                 reads=[tQ, Tc], writes=[Tpss])
                        R = rs[bi % 2]; tR = Trs[bi % 2]
                        k.op("act", lambda e: e.activation(out=R[:, 0:n], in_=pss[:, 0:n], func=AF.Sqrt, scale=1.0 / D, bias=EPS),
                             reads=[Tpss], writes=[tR])
                        k.op("dve", lambda e: e.reciprocal(out=R[:, 0:n], in_=R[:, 0:n]), reads=[tR], writes=[tR])
                        H = hT[bi % 2]; tH = ThT[bi % 2]
                        for kc in range(8):
                            tm = tmp[kc % 2]; ttm = Ttmp[kc % 2]
                            k.op("dve", lambda e: e.tensor_tensor(out=tm[:, 0:n], in0=X[:, kc, 0:n], in1=R[:, 0:n], op=ALU.mult),
                                 reads=[tX, tR], writes=[ttm])
                            k.op("act", lambda e: e.activation(out=H[:, kc, 0:n], in_=tm[:, 0:n], func=AF.Identity,
                                                               scale=G[:, seg, kc:kc + 1], bias=modT[:, l, kc, mi:mi + 1]),
                                 reads=[ttm, Tg, Tmod], writes=[tH])

                    def s1_proj(bi):
                        t0, n = BLOCKS[bi]
                        H = hT[bi % 2]; tH = ThT[bi % 2]
                        nev = nev_box[0]
                        for ci, ch in enumerate(FM_CHUNKS):
                            c0 = ch * 128
                            pp = pu[ci % 4]; tp = Tpu[ci % 4]
                            for kc in range(8):
                                k.op("pe", lambda e: e.matmul(pp[:, 0:n], lhsT=w_in[:, kc, c0:c0 + 128], rhs=H[:, kc, 0:n],
                                                              start=(kc == 0), stop=(kc == 7)), reads=[Tw, tH], writes=[tp])
                            E = ev[nev % 4]; tE = Tev[nev % 4]
                            if nev % 2 == 0:
                                k.op("act", lambda e: e.activation(out=E[:, 0:n], in_=pp[:, 0:n], func=AF.Copy), reads=[tp], writes=[tE])
                            else:
                                k.op("dve", lambda e: e.tensor_copy(out=E[:, 0:n], in_=pp[:, 0:n]), reads=[tp], writes=[tE])
                            k.dma("sp", uT_d[b, c0:c0 + 128, t0:t0 + n], E[:, 0:n], reads=[tE], writes=[TuT[b]])
                            nev += 1
                        for tt in range(n // 128):
                            ET = evt[tt % 2]; tET = Tevt[tt % 2]
                            off = 0
                            for ri, (a, bnd) in enumerate(TM_RANGES):
                                w = bnd - a
                                pp = pv[ri]; tp = Tpv[ri]
                                for kc in range(8):
                                    k.op("pe", lambda e: e.matmul(pp[:, 0:w], lhsT=H[:, kc, tt * 128:(tt + 1) * 128], rhs=w_in[:, kc, a:bnd],
                                                                  start=(kc == 0), stop=(kc == 7)), reads=[Tw, tH], writes=[tp])
                                if ri == 1:
                                    k.op("act", lambda e: e.activation(out=ET[:, off:off + w], in_=pp[:, 0:w], func=AF.Copy), reads=[tp], writes=[tET])
                                else:
                                    k.op("dve", lambda e: e.tensor_copy(out=ET[:, off:off + w], in_=pp[:, 0:w]), reads=[tp], writes=[tET])
                                off += w
                            k.dma("sp", ut_d[b, t0 + tt * 128:t0 + (tt + 1) * 128, :], ET[:], reads=[tET], writes=[Tut[b]])
                        nev_box[0] = nev

                    s1_norm(0)
                    for bi in range(len(BLOCKS)):
                        if bi + 1 < len(BLOCKS):
                            s1_norm(bi + 1)
                        s1_proj(bi)
                if cfg.upto < 2:
                    continue
                with Stage(k):
                    cw = k.sb("cw", [128, 2, 4]); cb = k.sb("cb", [128, 2])
                    wr = k.sb("wr", [128, 2, 2, 128], BF16); wi = k.sb("wi", [128, 2, 2, 128], BF16)
                    br = k.sb("br", [128, 2, 2]); bi_ = k.sb("bi", [128, 2, 2]); lam = k.sb("lam", [128, 2, 2])
                    cl = k.sb("cl", [128, 2, 2]); cl2 = k.sb("cl2", [128, 2, 2])
                    Tp2 = T()
                    k.dma("sp", cw[:], lru_cw_d[:, l], writes=[Tp2]); k.dma("sp", cb[:], lru_cb_d[:, l], writes=[Tp2])
                    cst = Caster(k, 128, 512)
                    cst.load(wr[:].rearrange("p a b c -> p (a b c)"), lru_wr_d[:, l].rearrange("p a b c -> p (a b c)"), 128, 512, [Tp2])
                    cst.load(wi[:].rearrange("p a b c -> p (a b c)"), lru_wi_d[:, l].rearrange("p a b c -> p (a b c)"), 128, 512, [Tp2])
                    k.dma("sp", br[:], lru_br_d[:, l], writes=[Tp2]); k.dma("sp", bi_[:], lru_bi_d[:, l], writes=[Tp2])
                    k.dma("sp", lam[:], lru_lam_d[:, l], writes=[Tp2])
                    k.op("act", lambda e: e.activation(out=cl[:], in_=lam[:], func=AF.Exp, scale=-1.0), reads=[Tp2], writes=[Tp2])
                    k.op("act", lambda e: e.activation(out=cl[:], in_=cl[:], func=AF.Ln, bias=1.0), reads=[Tp2], writes=[Tp2])
                    k.op("dve", lambda e: e.tensor_scalar(out=cl2[:], in0=cl[:], scalar1=-16.0, scalar2=None, op0=ALU.mult), reads=[Tp2], writes=[Tp2])
                    k.op("dve", lambda e: e.tensor_scalar(out=cl[:], in0=cl[:], scalar1=-8.0, scalar2=None, op0=ALU.mult), reads=[Tp2], writes=[Tp2])
                    xp = k.sb("xp", [128, S + 6]); Txp = T()
                    xc = k.sb("xc", [128, S]); Txc = T()
                    xcb = k.sb("xcb", [128, S], BF16); Txcb = T()
                    gt = k.sb("gt", [128, S]); Tgt = T()
                    Rr = k.sb("Rr", [128, S]); TR = T()
                    Ii = k.sb("Ii", [128, S]); TI = T()
                    Aa = k.sb("Aa", [128, S]); TA = T()
                    Bb = k.sb("Bb", [128, S]); TB = T()
                    Hh = [k.sb("Hh%d" % i, [128, S]) for i in range(2)]; TH = [T(), T()]
                    yo = k.sb("yo", [128, S], BF16); Tyo = T()
                    pg = [k.ps("pg%d" % i, [128, 512]) for i in range(4)]; Tpg = [PT() for _ in range(4)]
                    npg = 0
                    segs = [(0, NCTX, 2), (NCTX, NLAT, NCTX + 5)]
                    for ch in range(2):
                        k.op("pool", lambda e: e.memset(xp[:], 0.0), writes=[Txp])
                        for (t0, n, o) in segs:
                            k.dma("sp", xp[:, o:o + n], uT_d[b, ch * 128:(ch + 1) * 128, t0:t0 + n], reads=[TuT[b]], writes=[Txp])
                        k.dma("sp", gt[:], uT_d[b, 256 + ch * 128:256 + (ch + 1) * 128, :], reads=[TuT[b]], writes=[Tgt])
                        for (t0, n, o) in segs:
                            k.op("dve", lambda e: e.tensor_scalar(out=xc[:, t0:t0 + n], in0=xp[:, o - 2:o - 2 + n], scalar1=cw[:, ch, 0:1],
                                                                  scalar2=cb[:, ch:ch + 1], op0=ALU.mult, op1=ALU.add),
                                 reads=[Txp, Tp2], writes=[Txc])
                            for j in range(1, 4):
                                k.op("dve", lambda e: e.scalar_tensor_tensor(out=xc[:, t0:t0 + n], in0=xp[:, o - 2 + j:o - 2 + j + n],
                                                                            scalar=cw[:, ch, j:j + 1], in1=xc[:, t0:t0 + n],
                                                                            op0=ALU.mult, op1=ALU.add),
                                     reads=[Txp, Tp2, Txc], writes=[Txc])
                        k.op("pool", lambda e: e.tensor_copy(out=xcb[:], in_=xc[:]), reads=[Txc], writes=[Txcb])
                        for d in range(2):
                            for (t0, n) in BLOCKS:
                                for (W, bias, dst, tdst) in ((wr, br, Rr, TR), (wi, bi_, Ii, TI)):
                                    pp = pg[npg % 4]; tp = Tpg[npg % 4]; npg += 1
                                    k.op("pe", lambda e: e.matmul(pp[:, 0:n], lhsT=W[:, d, ch, :], rhs=xcb[:, t0:t0 + n], start=True, stop=True),
                                         reads=[Tp2, Txcb], writes=[tp])
                                    k.op("act", lambda e: e.activation(out=dst[:, t0:t0 + n], in_=pp[:, 0:n], func=AF.Sigmoid,
                                                                       bias=bias[:, d, ch:ch + 1]), reads=[tp, Tp2], writes=[tdst])
                            k.op("act", lambda e: e.activation(out=Aa[:], in_=Rr[:], func=AF.Exp, scale=cl[:, d, ch:ch + 1]),
                                 reads=[TR, Tp2], writes=[TA])
                            k.op("act", lambda e: e.activation(out=Bb[:], in_=Rr[:], func=AF.Exp, scale=cl2[:, d, ch:ch + 1]),
                                 reads=[TR, Tp2], writes=[TB])
                            k.op("act", lambda e: e.activation(out=Bb[:], in_=Bb[:], func=AF.Sqrt, scale=-1.0, bias=1.0), reads=[TB], writes=[TB])
                            k.op("dve", lambda e: e.tensor_tensor(out=Ii[:], in0=Ii[:], in1=xc[:], op=ALU.mult), reads=[TI, Txc], writes=[TI])
                            k.op("dve", lambda e: e.tensor_tensor(out=Bb[:], in0=Bb[:], in1=Ii[:], op=ALU.mult), reads=[TB, TI], writes=[TB])
                            H = Hh[d]
                            if d == 0:
                                k.op("dve", lambda e: e.tensor_tensor_scan(out=H[:], data0=Aa[:], data1=Bb[:], initial=0.0,
                                                                          op0=ALU.mult, op1=ALU.add), reads=[TA, TB], writes=[TH[d]])
                            else:
                                k.op("dve", lambda e: e.tensor_tensor_scan(out=H[:, NCTX - 1::-1], data0=Aa[:, NCTX - 1::-1], data1=Bb[:, NCTX - 1::-1],
                                                                          initial=0.0, op0=ALU.mult, op1=ALU.add), reads=[TA, TB], writes=[TH[d]])
                                k.op("dve", lambda e: e.tensor_tensor_scan(out=H[:, S - 1:NCTX - 1:-1], data0=Aa[:, S - 1:NCTX - 1:-1],
                                                                          data1=Bb[:, S - 1:NCTX - 1:-1], initial=H[:, 0:1],
                                                                          op0=ALU.mult, op1=ALU.add), reads=[TA, TB, TH[d]], writes=[TH[d]])
                        k.op("act", lambda e: e.activation(out=gt[:], in_=gt[:], func=AF.Gelu_apprx_tanh), reads=[Tgt], writes=[Tgt])
                        k.op("dve", lambda e: e.tensor_tensor(out=Hh[0][:], in0=Hh[0][:], in1=Hh[1][:], op=ALU.add), reads=TH, writes=[TH[0]])
                        k.op("dve", lambda e: e.tensor_tensor(out=yo[:], in0=Hh[0][:], in1=gt[:], op=ALU.mult), reads=[TH[0], Tgt], writes=[Tyo])
                        k.dma("sp", yT_d[b, ch * 128:(ch + 1) * 128, :], yo[:], reads=[Tyo], writes=[TyT[b]])
                if cfg.upto < 3:
                    continue
                with Stage(k):
                    lbz = k.sb("lbz", [128, L, 2]); lbe = k.sb("lbe", [128, L, 2]); lbs = k.sb("lbs", [128, 2]); lb = k.sb("lb", [128, 2])
                    oml = k.sb("oml", [128, 2]); hnw = k.sb("hnw", [128, 2]); Tp3 = T()
                    mf = k.sb("mf", [128, S]); mb = k.sb("mb", [128, S]); triU = k.sb("triU", [128, 128], U32); triL = k.sb("triL", [128, 128], U32)
                    blk = k.sb("blk", [128, 128], BF16)
                    k.dma("sp", lbz[:], hg_lbT_d, writes=[Tp3]); k.dma("sp", hnw[:], hg_nwT_d[:, l], writes=[Tp3])
                    k.dma("sp", mf[:], mfwd_d, writes=[Tp3]); k.dma("sp", mb[:], mbwd_d, writes=[Tp3])
                    k.dma("sp", triU[:], triU_d, writes=[Tp3]); k.dma("sp", triL[:], triL_d, writes=[Tp3]); k.dma("sp", blk[:], blk64_d, writes=[Tp3])
                    k.op("act", lambda e: e.activation(out=lbe[:], in_=lbz[:], func=AF.Exp), reads=[Tp3], writes=[Tp3])
                    k.op("dve", lambda e: e.tensor_tensor(out=lbs[:], in0=lbe[:, 0, :], in1=lbe[:, 1, :], op=ALU.add), reads=[Tp3], writes=[Tp3])
                    k.op("dve", lambda e: e.reciprocal(out=lbs[:], in_=lbs[:]), reads=[Tp3], writes=[Tp3])
                    for ll in range(L):
                        k.op("dve", lambda e: e.tensor_tensor(out=lbe[:, ll, :], in0=lbe[:, ll, :], in1=lbs[:], op=ALU.mult), reads=[Tp3], writes=[Tp3])
                    k.op("dve", lambda e: e.tensor_copy(out=lb[:], in_=lbe[:, 0, :]), reads=[Tp3], writes=[Tp3])
                    for ll in range(1, l + 1):
                        k.op("dve", lambda e: e.tensor_tensor(out=lb[:], in0=lb[:], in1=lbe[:, ll, :], op=ALU.add), reads=[Tp3], writes=[Tp3])
                    k.op("dve", lambda e: e.tensor_tensor(out=lb[:], in0=lb[:], in1=lbe[:, 0, :], op=ALU.subtract), reads=[Tp3], writes=[Tp3])
                    k.op("dve", lambda e: e.tensor_scalar(out=oml[:], in0=lb[:], scalar1=-1.0, scalar2=1.0, op0=ALU.mult, op1=ALU.add), reads=[Tp3], writes=[Tp3])
                    vt = k.sb("vt", [128, NT, 256], BF16); Tvt = T()
                    vstg = k.sb("vstg", [128, NT // 2, 256]); Tvstg = T()
                    for hf in range(2):
                        k.dma("sp", vstg[:], ut_d[b, hf * (S // 2):(hf + 1) * (S // 2), 0:256].rearrange("(n p) c -> p n c", p=128), reads=[Tut[b]], writes=[Tvstg])
                        k.op("pool", lambda e: e.tensor_copy(out=vt[:, hf * (NT // 2):(hf + 1) * (NT // 2), :], in_=vstg[:]), reads=[Tvstg], writes=[Tvt])
                    qh = k.sb("qh", [128, S]); Tqh = T()
                    gg = k.sb("gg", [128, S]); Tgg = T()
                    ff = k.sb("ff", [128, S]); Tff = T()
                    lf = k.sb("lf", [128, S]); Tlf = T()
                    cum = k.sb("cum", [128, S]); Tcum = T()
                    dd = k.sb("dd", [128, S]); Tdd = T()
                    EE = k.sb("EE", [128, S]); TEE = T()
                    qt = k.sb("qt", [128, S], BF16); Tqt = T()
                    kt = k.sb("kt", [128, S], BF16); Tkt = T()
                    qs = k.sb("qs", [128, S], BF16); Tqs = T()
                    ke = k.sb("ke", [128, S], BF16); Tke = T()
                    etot = k.sb("etot", [128, NT]); Tet = T()
                    OO = k.sb("OO", [128, S]); TOO = T()
                    ket = [k.sb("ket%d" % i, [128, 128], BF16) for i in range(2)]; Tket = [T(), T()]
                    Am = [[k.sb("Am%d_%d" % (d, i), [128, 128], BF16) for i in range(2)] for d in range(2)]
                    TAm = [[T(), T()] for d in range(2)]
                    S32 = k.sb("S32", [128, 64]); TS32 = T()
                    Sb = k.sb("Sb", [128, 64], BF16); TSb = T()
                    sqb = k.sb("sqb", [128, 512], BF16); Tsqb = T()
                    rsd = k.sb("rsd", [128, 512]); Trsd = T()
                    yo = k.sb("yo3", [128, S], BF16); Tyo = T()
                    p_sc = [k.ps("p_sc%d" % i, [128, 128]) for i in range(2)]; Tp_sc = [PT(), PT()]
                    p_y = [k.ps("p_y%d" % i, [128, 128]) for i in range(2)]; Tp_y = [PT(), PT()]
                    p_st = k.ps("p_st", [128, 64]); Tp_st = PT()
                    p_tr = k.ps("p_tr", [128, 128], BF16); Tp_tr = PT()
                    p_ss = k.ps("p_ss", [128, 512]); Tp_ss = PT()
                    for d in range(2):
                        for i in range(2):
                            k.op("pool", lambda e: e.memset(Am[d][i][:], 0.0), writes=[TAm[d][i]])
                    for i in range(2):
                        k.op("dve", lambda e: e.memset(p_sc[i][:], 0.0), writes=[Tp_sc[i]])
                    cum3 = cum[:].rearrange("p (n t) -> p n t", t=128)
                    dd3 = dd[:].rearrange("p (n t) -> p n t", t=128)
                    cum4 = cum[:].rearrange("p (n t) -> p n t", t=32)
                    dd4 = dd[:].rearrange("p (n t) -> p n t", t=32)
                    qx = k.sb("qx", [128, S], BF16); Tqx = T()
                    kx = [None] + [k.sb("kx%d" % i, [128, NT, 96], BF16) for i in range(1, 4)]; Tkx = T()
                    def mkset(i_):
                        d_ = {}
                        for nm in ("qt", "kt", "qx", "qs", "ke"):
                            d_[nm] = k.sb("%s_b%d" % (nm, i_), [128, S], BF16); d_["T" + nm] = T()
                        d_["kx"] = [None] + [k.sb("kx%d_b%d" % (j, i_), [128, NT, 96], BF16) for j in range(1, 4)]; d_["Tkx"] = T()
                        d_["etot"] = k.sb("etot_b%d" % i_, [128, NT]); d_["Tet"] = T()
                        return d_
                    sets = [dict(qt=qt, Tqt=Tqt, kt=kt, Tkt=Tkt, qx=qx, Tqx=Tqx, qs=qs, Tqs=Tqs, ke=ke, Tke=Tke, kx=kx, Tkx=Tkx, etot=etot, Tet=Tet), mkset(1)]

                    def hg_prep(hp, d, B):
                        r0 = 512 + hp * 128
                        qt, kt, qx, qs, ke, kx, etot = B["qt"], B["kt"], B["qx"], B["qs"], B["ke"], B["kx"], B["etot"]
                        Tqt, Tkt, Tqx, Tqs, Tke, Tkx, Tet = B["Tqt"], B["Tkt"], B["Tqx"], B["Tqs"], B["Tke"], B["Tkx"], B["Tet"]
                        if d == 0:
                            k.dma("sp", qh[:], uT_d[b, r0:r0 + 128, :], reads=[TuT[b]], writes=[Tqh])
                            k.op("act", lambda e: e.activation(out=qh[:], in_=qh[:], func=AF.Silu), reads=[Tqh], writes=[Tqh])
                        k.dma("sp", ff[:], uT_d[b, r0 + 256 * (d + 1):r0 + 256 * (d + 1) + 128, :], reads=[TuT[b]], writes=[Tff])
                        k.op("act", lambda e: e.activation(out=ff[:], in_=ff[:], func=AF.Sigmoid), reads=[Tff], writes=[Tff])
                        k.op("dve", lambda e: e.tensor_scalar(out=ff[:], in0=ff[:], scalar1=oml[:, hp:hp + 1], scalar2=lb[:, hp:hp + 1],
                                                              op0=ALU.mult, op1=ALU.add), reads=[Tff, Tp3], writes=[Tff])
                        k.op("act", lambda e: e.activation(out=lf[:], in_=ff[:], func=AF.Ln), reads=[Tff], writes=[Tlf])
                        k.op("dve", lambda e: e.tensor_scalar(out=ff[:], in0=ff[:], scalar1=-1.0, scalar2=1.0, op0=ALU.mult, op1=ALU.add),
                             reads=[Tff], writes=[Tff])
                        if d == 0:
                            k.op("dve", lambda e: e.tensor_tensor_scan(out=cum[:], data0=mf[:], data1=lf[:], initial=0.0, op0=ALU.mult, op1=ALU.add),
                                 reads=[Tp3, Tlf], writes=[Tcum])
                            mid, end = 63, 127
                        else:
                            k.op("dve", lambda e: e.tensor_tensor_scan(out=cum[:, ::-1], data0=mb[:, ::-1], data1=lf[:, ::-1], initial=0.0,
                                                                      op0=ALU.mult, op1=ALU.add), reads=[Tp3, Tlf], writes=[Tcum])
                            mid, end = 64, 0
                        mid4, first4 = (15, 0) if d == 0 else (16, 31)
                        k.op("dve", lambda e: e.tensor_tensor(out=dd4, in0=cum4, in1=cum4[:, :, mid4:mid4 + 1].to_broadcast([128, S // 32, 32]), op=ALU.subtract),
                             reads=[Tcum], writes=[Tdd])
                        k.op("act", lambda e: e.activation(out=EE[:], in_=dd[:], func=AF.Exp), reads=[Tdd], writes=[TEE])
                        k.op("dve", lambda e: e.tensor_tensor(out=qt[:], in0=qh[:], in1=EE[:], op=ALU.mult), reads=[Tqh, TEE], writes=[Tqt])
                        k.op("act", lambda e: e.activation(out=EE[:], in_=dd[:], func=AF.Exp, scale=-1.0), reads=[Tdd], writes=[TEE])
                        k.op("dve", lambda e: e.tensor_tensor(out=kt[:], in0=ff[:], in1=EE[:], op=ALU.mult), reads=[Tff, TEE], writes=[Tkt])
                        k.op("dve", lambda e: e.tensor_tensor(out=dd4, in0=cum4, in1=cum4[:, :, first4:first4 + 1].to_broadcast([128, S // 32, 32]), op=ALU.subtract),
                             reads=[Tcum], writes=[Tdd])
                        k.op("act", lambda e: e.activation(out=EE[:], in_=dd[:], func=AF.Exp), reads=[Tdd], writes=[TEE])
                        k.op("dve", lambda e: e.tensor_tensor(out=qx[:], in0=qh[:], in1=EE[:], op=ALU.mult), reads=[Tqh, TEE], writes=[Tqx])
                        ff3 = ff[:].rearrange("p (n t) -> p n t", t=128)
                        EE3 = EE[:].rearrange("p (n t) -> p n t", t=128)
                        for i in range(1, 4):
                            w_ = 32 * i
                            if d == 0:
                                srcs = slice(0, w_); refi = w_
                            else:
                                srcs = slice(128 - w_, 128); refi = 127 - w_
                            k.op("dve", lambda e: e.tensor_tensor(out=dd3[:, :, 0:w_], in0=cum3[:, :, refi:refi + 1].to_broadcast([128, NT, w_]),
                                                                  in1=cum3[:, :, srcs], op=ALU.subtract), reads=[Tcum], writes=[Tdd])
                            k.op("act", lambda e: e.activation(out=EE3[:, :, 0:w_], in_=dd3[:, :, 0:w_], func=AF.Exp), reads=[Tdd], writes=[TEE])
                            k.op("dve", lambda e: e.tensor_tensor(out=kx[i][:, :, 0:w_], in0=ff3[:, :, srcs], in1=EE3[:, :, 0:w_], op=ALU.mult),
                                 reads=[Tff, TEE], writes=[Tkx])
                        k.op("act", lambda e: e.activation(out=EE[:], in_=cum[:], func=AF.Exp), reads=[Tcum], writes=[TEE])
                        k.op("dve", lambda e: e.tensor_tensor(out=qs[:], in0=qh[:], in1=EE[:], op=ALU.mult), reads=[Tqh, TEE], writes=[Tqs])
                        k.op("act", lambda e: e.activation(out=etot[:], in_=cum3[:, :, end], func=AF.Exp), reads=[Tcum], writes=[Tet])
                        k.op("dve", lambda e: e.tensor_tensor(out=dd3, in0=cum3[:, :, end:end + 1].to_broadcast([128, NT, 128]), in1=cum3, op=ALU.subtract),
                             reads=[Tcum], writes=[Tdd])
                        k.op("act", lambda e: e.activation(out=EE[:], in_=dd[:], func=AF.Exp), reads=[Tdd], writes=[TEE])
                        k.op("dve", lambda e: e.tensor_tensor(out=ke[:], in0=ff[:], in1=EE[:], op=ALU.mult), reads=[Tff, TEE], writes=[Tke])

                    def hg_loop(hp, d, B, pend):
                        qt, kt, qx, qs, ke, kx, etot = B["qt"], B["kt"], B["qx"], B["qs"], B["ke"], B["kx"], B["etot"]
                        Tqt, Tkt, Tqx, Tqs, Tke, Tkx, Tet = B["Tqt"], B["Tkt"], B["Tqx"], B["Tqs"], B["Tke"], B["Tkx"], B["Tet"]
                        order = list(range(NT)) if d == 0 else [1, 0] + list(range(NT - 1, 1, -1))
                        tri = triU if d == 0 else triL
                        per = (len(pend) + NT - 3) // (NT - 2) if pend else 0
                        for oi, ti in enumerate(order):
                            ts_ = slice(ti * 128, (ti + 1) * 128)
                            k.op("pe", lambda e: e.transpose(p_tr[:], ke[:, ts_], ident_b[:]), reads=[Tke, Tc], writes=[Tp_tr])
                            KT = ket[oi % 2]; tKT = Tket[oi % 2]
                            k.op("act", lambda e: e.activation(out=KT[:], in_=p_tr[:], func=AF.Copy), reads=[Tp_tr], writes=[tKT])
                            py = p_y[oi % 2]; tpy = Tp_y[oi % 2]
                            for hh in range(2):
                                bs = slice(hh * 64, (hh + 1) * 64)
                                psc = p_sc[hh]; tps = Tp_sc[hh]
                                for tb in range(4):
                                    for sb_ in (range(0, tb + 1) if d == 0 else range(tb, 4)):
                                        tq = slice(ti * 128 + 32 * tb, ti * 128 + 32 * tb + 32)
                                        if sb_ == tb:
                                            lw = kt[bs, tq]; rq = qt[bs, tq]
                                        else:
                                            i = tb if d == 0 else 3 - tb
                                            o_ = 32 * sb_ if d == 0 else 32 * sb_ - (128 - 32 * i)
                                            lw = kx[i][bs, ti, o_:o_ + 32]; rq = qx[bs, tq]
                                        k.op("pe", lambda e: e.matmul(psc[32 * sb_:32 * sb_ + 32, 32 * tb:32 * tb + 32], lhsT=lw, rhs=rq, start=True, stop=True,
                                                                      tile_position=(bs.start, 32 * sb_)),
                                             reads=[Tkt, Tqt, Tkx, Tqx], writes=[tps])
                                A = Am[d][hh]; tA = TAm[d][hh]
                                k.op("dve", lambda e: e.copy_predicated(out=A[:], mask=tri[:], data=psc[:]), reads=[tps, Tp3], writes=[tA])
                                vs = vt[:, ti, (2 * hp + hh) * 64:(2 * hp + hh + 1) * 64]
                                k.op("pe", lambda e: e.matmul(py[bs, :], lhsT=vs, rhs=A[:], start=True, stop=(oi == 0)),
                                     reads=[Tvt, tA], writes=[tpy])
                                if oi > 0:
                                    k.op("pe", lambda e: e.matmul(py[bs, :], lhsT=Sb[bs, :], rhs=qs[bs, ts_], start=False, stop=True),
                                         reads=[TSb, Tqs], writes=[tpy])
                            if d == 0:
                                k.op("act", lambda e: e.activation(out=OO[:, ts_], in_=py[:], func=AF.Copy), reads=[tpy], writes=[TOO])
                            else:
                                k.op("dve", lambda e: e.tensor_tensor(out=OO[:, ts_], in0=OO[:, ts_], in1=py[:], op=ALU.add), reads=[tpy, TOO], writes=[TOO])
                            if oi < NT - 1:
                                for hh in range(2):
                                    bs = slice(hh * 64, (hh + 1) * 64)
                                    vs = vt[:, ti, (2 * hp + hh) * 64:(2 * hp + hh + 1) * 64]
                                    k.op("pe", lambda e: e.matmul(p_st[bs, :], lhsT=KT[:, bs], rhs=vs, start=True, stop=True),
                                         reads=[tKT, Tvt], writes=[Tp_st])
                                if oi == 0:
                                    k.op("dve", lambda e: e.tensor_copy(out=S32[:], in_=p_st[:]), reads=[Tp_st], writes=[TS32])
                                else:
                                    k.op("dve", lambda e: e.scalar_tensor_tensor(out=S32[:], in0=S32[:], scalar=etot[:, ti:ti + 1], in1=p_st[:],
                                                                                op0=ALU.mult, op1=ALU.add), reads=[Tp_st, Tet, TS32], writes=[TS32])
                                k.op("pool", lambda e: e.tensor_copy(out=Sb[:], in_=S32[:]), reads=[TS32], writes=[TSb])
                            if pend:
                                k.flush(pend, per)

                    chains = [(0, 0), (0, 1), (1, 0), (1, 1)]
                    hg_prep(0, 0, sets[0])
                    for ci, (hp, d) in enumerate(chains):
                        if d == 0:
                            r0 = 512 + hp * 128
                            k.dma("sp", gg[:], uT_d[b, r0 + 1024:r0 + 1024 + 128, :], reads=[TuT[b]], writes=[Tgg])
                            k.op("act", lambda e: e.activation(out=gg[:], in_=gg[:], func=AF.Silu), reads=[Tgg], writes=[Tgg])
                        pend = []
                        if ci + 1 < len(chains):
                            k.defer = pend
                            hg_prep(chains[ci + 1][0], chains[ci + 1][1], sets[(ci + 1) % 2])
                            k.defer = None
                        hg_loop(hp, d, sets[ci % 2], pend)
                        k.flush(pend)
                        if d == 1:
                            for (t0, n) in BLOCKS:
                                k.op("act", lambda e: e.activation(out=sqb[:, 0:n], in_=OO[:, t0:t0 + n], func=AF.Square), reads=[TOO], writes=[Tsqb])
                                k.op("pe", lambda e: e.matmul(p_ss[:, 0:n], lhsT=blk[:], rhs=sqb[:, 0:n], start=True, stop=True), reads=[Tsqb, Tp3], writes=[Tp_ss])
                                k.op("act", lambda e: e.activation(out=rsd[:, 0:n], in_=p_ss[:, 0:n], func=AF.Sqrt, scale=1.0 / 64, bias=EPS), reads=[Tp_ss], writes=[Trsd])
                                k.op("dve", lambda e: e.reciprocal(out=rsd[:, 0:n], in_=rsd[:, 0:n]), reads=[Trsd], writes=[Trsd])
                                k.op("dve", lambda e: e.tensor_tensor(out=rsd[:, 0:n], in0=rsd[:, 0:n], in1=OO[:, t0:t0 + n], op=ALU.mult), reads=[Trsd, TOO], writes=[Trsd])
                                k.op("dve", lambda e: e.scalar_tensor_tensor(out=yo[:, t0:t0 + n], in0=rsd[:, 0:n], scalar=hnw[:, hp:hp + 1], in1=gg[:, t0:t0 + n],
                                                                            op0=ALU.mult, op1=ALU.mult), reads=[Trsd, Tgg, Tp3], writes=[Tyo])
                            k.dma("sp", yT_d[b, 256 + hp * 128:256 + (hp + 1) * 128, :], yo[:], reads=[Tyo], writes=[TyT[b]])
                if cfg.upto < 4:
                    continue
                with Stage(k):
                    Tp4 = T()
                    cw = k.sb("scw", [128, 4, 4]); cb = k.sb("scb", [128, 4])
                    aneg = k.sb("aneg", [128, 8]); dtb = k.sb("dtb", [128, 8]); dsk = k.sb("dsk", [128, 256]); snw = k.sb("snw", [128, 256])
                    triUf = k.sb("triUf", [128, 128]); triLf = k.sb("triLf", [128, 128]); strLf = k.sb("strLf", [128, 128]); strUf = k.sb("strUf", [128, 128])
                    for dst, src in ((cw, sd_cw_d[:, l]), (cb, sd_cb_d[:, l]), (aneg, sd_alog_d[:, l]), (dtb, sd_dtb_d[:, l]), (dsk, sd_dsk_d[:, l]),
                                     (snw, sd_nw_d[:, l]), (triUf, triUf_d), (triLf, triLf_d), (strLf, strLf_d), (strUf, strUf_d)):
                        k.dma("sp", dst[:], src, writes=[Tp4])
                    k.op("act", lambda e: e.activation(out=aneg[:], in_=aneg[:], func=AF.Exp), reads=[Tp4], writes=[Tp4])
                    k.op("dve", lambda e: e.tensor_scalar(out=aneg[:], in0=aneg[:], scalar1=-1.0, scalar2=None, op0=ALU.mult), reads=[Tp4], writes=[Tp4])
                    xp = k.sb("sxp", [128, S + 6]); Txp = T()
                    xc = k.sb("sxc", [128, S]); Txc = T()
                    fmb = k.sb("fmb", [128, 4, S], BF16); Tfmb = T()
                    segs = [(0, NCTX, 2), (NCTX, NLAT, NCTX + 5)]
                    for ch in range(4):
                        k.op("pool", lambda e: e.memset(xp[:], 0.0), writes=[Txp])
                        for (t0, n, o) in segs:
                            k.dma("sp", xp[:, o:o + n], uT_d[b, 2048 + ch * 128:2048 + (ch + 1) * 128, t0:t0 + n], reads=[TuT[b]], writes=[Txp])
                        for (t0, n, o) in segs:
                            k.op("dve", lambda e: e.tensor_scalar(out=xc[:, t0:t0 + n], in0=xp[:, o - 2:o - 2 + n], scalar1=cw[:, ch, 0:1],
                                                                  scalar2=cb[:, ch:ch + 1], op0=ALU.mult, op1=ALU.add), reads=[Txp, Tp4], writes=[Txc])
                            for j in range(1, 4):
                                k.op("dve", lambda e: e.scalar_tensor_tensor(out=xc[:, t0:t0 + n], in0=xp[:, o - 2 + j:o - 2 + j + n], scalar=cw[:, ch, j:j + 1],
                                                                            in1=xc[:, t0:t0 + n], op0=ALU.mult, op1=ALU.add), reads=[Txp, Tp4, Txc], writes=[Txc])
                        k.op("act", lambda e: e.activation(out=fmb[:, ch, :], in_=xc[:], func=AF.Silu), reads=[Txc], writes=[Tfmb])
                    xst = k.sb("xst", [128, NT, 256], BF16); Txst = T()
                    Bt = k.sb("Bt", [128, NT, 128], BF16); TBt = T()
                    ps_pre = PScope(k); ps_pre.__enter__()
                    p_tr = [k.ps("p4tr%d" % i, [128, 128], BF16) for i in range(2)]; Tp_tr = [PT(), PT()]
                    ntr = 0
                    for ti in range(NT):
                        ts_ = slice(ti * 128, (ti + 1) * 128)
                        for ch in range(3):
                            pp = p_tr[ntr % 2]; tp = Tp_tr[ntr % 2]
                            k.op("pe", lambda e: e.transpose(pp[:], fmb[:, ch, ts_], ident_b[:]), reads=[Tfmb, Tc], writes=[tp])
                            dst = xst[:, ti, ch * 128:(ch + 1) * 128] if ch < 2 else Bt[:, ti, :]
                            tdst = Txst if ch < 2 else TBt
                            if ntr % 2 == 0:
                                k.op("act", lambda e: e.activation(out=dst, in_=pp[:], func=AF.Copy), reads=[tp], writes=[tdst])
                            else:
                                k.op("dve", lambda e: e.tensor_copy(out=dst, in_=pp[:]), reads=[tp], writes=[tdst])
                            ntr += 1
                    dt = k.sb("dt", [128, NT, 8]); Tdt = T()
                    la = k.sb("la", [128, NT, 8]); Tla = T()
                    k.dma("sp", dt[:], ut_d[b, :, 512:520].rearrange("(n p) c -> p n c", p=128), reads=[Tut[b]], writes=[Tdt])
                    k.op("dve", lambda e: e.tensor_tensor(out=dt[:], in0=dt[:], in1=dtb[:, None, :].to_broadcast([128, NT, 8]), op=ALU.add), reads=[Tdt, Tp4], writes=[Tdt])
                    k.op("act", lambda e: e.activation(out=dt[:], in_=dt[:], func=AF.Exp), reads=[Tdt], writes=[Tdt])
                    k.op("act", lambda e: e.activation(out=dt[:], in_=dt[:], func=AF.Ln, bias=1.0), reads=[Tdt], writes=[Tdt])
                    k.op("dve", lambda e: e.tensor_tensor(out=la[:], in0=dt[:], in1=aneg[:, None, :].to_broadcast([128, NT, 8]), op=ALU.mult), reads=[Tdt, Tp4], writes=[Tla])
                    p_ct = k.ps("p_ct", [128, 2, NT, 8]); Tp_ct = PT()
                    for ti in range(NT):
                        k.op("pe", lambda e: e.matmul(p_ct[:, 0, ti, 0:4], lhsT=triUf[:], rhs=la[:, ti, 0:4], start=True, stop=True), reads=[Tla, Tp4], writes=[Tp_ct])
                        k.op("pe", lambda e: e.matmul(p_ct[:, 0, ti, 4:8], lhsT=triLf[:], rhs=la[:, ti, 4:8], start=True, stop=True), reads=[Tla, Tp4], writes=[Tp_ct])
                        k.op("pe", lambda e: e.matmul(p_ct[:, 1, ti, :], lhsT=ones_f[:], rhs=la[:, ti, :], start=True, stop=True), reads=[Tla, Tc], writes=[Tp_ct])
                    cexp = k.sb("cexp", [128, NT, 8]); etot = k.sb("etot4", [128, NT, 8]); wend = k.sb("wend", [128, NT, 8]); Tce = T()
                    k.op("dve", lambda e: e.tensor_tensor(out=wend[:], in0=p_ct[:, 1], in1=p_ct[:, 0], op=ALU.subtract), reads=[Tp_ct], writes=[Tce]) if False else None
                    k.op("act", lambda e: e.activation(out=cexp[:], in_=p_ct[:, 0], func=AF.Copy), reads=[Tp_ct], writes=[Tce])
                    k.op("dve", lambda e: e.tensor_tensor(out=wend[:], in0=p_ct[:, 1], in1=cexp[:], op=ALU.subtract), reads=[Tp_ct, Tce], writes=[Tce])
                    k.op("act", lambda e: e.activation(out=wend[:], in_=wend[:], func=AF.Exp), reads=[Tce], writes=[Tce])
                    k.op("dve", lambda e: e.tensor_tensor(out=wend[:], in0=wend[:], in1=dt[:], op=ALU.mult), reads=[Tce, Tdt], writes=[Tce])
                    k.op("act", lambda e: e.activation(out=cexp[:], in_=cexp[:], func=AF.Exp), reads=[Tce], writes=[Tce])
                    k.op("act", lambda e: e.activation(out=etot[:], in_=p_ct[:, 1], func=AF.Exp), reads=[Tp_ct], writes=[Tce])
                    ps_pre.__exit__(None, None, None)
                    ps_loop = PScope(k); ps_loop.__enter__()
                    Yacc = k.sb("Yacc", [128, NT, 256]); TY = T()
                    inc4 = [k.sb("inc4_%d" % d, [128, 4, 128]) for d in range(2)]
                    ngm = [k.sb("ngm%d" % d, [128, 4, 128]) for d in range(2)]
                    k.dma("sp", inc4[0][:], triUf4_d, writes=[Tp4]); k.dma("sp", inc4[1][:], triLf4_d, writes=[Tp4])
                    k.dma("sp", ngm[0][:], negmf_d, writes=[Tp4]); k.dma("sp", ngm[1][:], negmb_d, writes=[Tp4])
                    etH = k.sb("etH", [128, NT, 2, 2]); TetH = T()
                    et4 = etot[:].rearrange("p n (d h) -> p n d h", d=2)
                    for g in range(2):
                        gs = slice(g * 64, (g + 1) * 64)
                        k.op("dve", lambda e: e.tensor_copy(out=etH[gs], in_=et4[gs, :, :, 2 * g:2 * g + 2]), reads=[Tce], writes=[TetH])
                    Rr4 = [k.sb("Rr4_%d" % i, [128, 4, 128]) for i in range(2)]; TRr4 = [T(), T()]
                    Es = [k.sb("Es%d" % i, [128, 4, 128]) for i in range(2)]; TEs = [T(), T()]
                    Ab = [k.sb("Ab%d" % i, [128, 4, 128], BF16) for i in range(2)]; TAb = [T(), T()]
                    Bw = [k.sb("Bw%d" % i, [128, 2, 2, 64], BF16) for i in range(2)]; TBw = [T(), T()]
                    tmpy = [k.sb("tmpy%d" % i, [128, 4, 64]) for i in range(2)]; Ttmpy = [T(), T()]
                    S32 = k.sb("S32_4", [128, 2, 64]); TS32 = T()
                    STb = k.sb("STb4", [128, 2, 64], BF16); TSTb = T()
                    p_g = [k.ps("p_g%d" % i, [128, 128]) for i in range(2)]; Tp_g = [PT(), PT()]
                    p_seg = [k.ps("p_seg%d" % i, [128, 4, 128]) for i in range(2)]; Tp_seg = [PT(), PT()]
                    p_y1 = k.ps("p_y1", [128, 4, 64]); Tp_y1 = PT()
                    p_y2 = [k.ps("p_y2%d" % i, [128, 2, 64]) for i in range(2)]; Tp_y2 = [PT(), PT()]
                    p_st = k.ps("p_st4", [128, 2, 64]); Tp_st = PT()
                    Bt4 = Bt[:].rearrange("p n (g c) -> p n g c", g=2)
                    def ssd_front(d, oi, ti, i2):
                        ts_ = slice(ti * 128, (ti + 1) * 128)
                        d4 = slice(d * 4, d * 4 + 4)
                        strm = strLf if d == 0 else strUf
                        for g in range(2):
                            gs = slice(g * 64, (g + 1) * 64)
                            k.op("pe", lambda e: e.matmul(p_g[g][:], lhsT=fmb[gs, 2, ts_], rhs=fmb[gs, 3, ts_], start=True, stop=True),
                                 reads=[Tfmb], writes=[Tp_g[g]])
                        k.op("pool", lambda e: e.tensor_tensor(out=Rr4[i2][:], in0=inc4[d][:], in1=la[:, ti, d4, None].to_broadcast([128, 4, 128]), op=ALU.mult),
                             reads=[Tla, Tp4], writes=[TRr4[i2]])
                        k.op("pe", lambda e: e.matmul(p_seg[i2][:].rearrange("p h t -> p (h t)"), lhsT=strm[:], rhs=Rr4[i2][:].rearrange("p h t -> p (h t)"),
                                                      start=True, stop=False), reads=[TRr4[i2], Tp4], writes=[Tp_seg[i2]])
                        k.op("pe", lambda e: e.matmul(p_seg[i2][:].rearrange("p h t -> p (h t)"), lhsT=ident_f[:], rhs=ngm[d][:].rearrange("p h t -> p (h t)"),
                                                      start=False, stop=True), reads=[Tc, Tp4], writes=[Tp_seg[i2]])
                        k.op("act", lambda e: e.activation(out=Es[i2][:], in_=p_seg[i2][:], func=AF.Exp), reads=[Tp_seg[i2]], writes=[TEs[i2]])
                        k.op("dve", lambda e: e.tensor_tensor(out=Es[i2][:], in0=Es[i2][:], in1=dt[:, ti, d4, None].to_broadcast([128, 4, 128]), op=ALU.mult),
                             reads=[TEs[i2], Tdt], writes=[TEs[i2]])
                        for g in range(2):
                            k.op("dve", lambda e: e.tensor_tensor(out=Ab[i2][:, 2 * g:2 * g + 2, :], in0=Es[i2][:, 2 * g:2 * g + 2, :],
                                                                  in1=p_g[g][:, None, :].to_broadcast([128, 2, 128]), op=ALU.mult),
                                 reads=[TEs[i2], Tp_g[g]], writes=[TAb[i2]])
                        if oi < NT - 1:
                            k.op("pool", lambda e: e.tensor_tensor(out=Bw[i2][:], in0=Bt4[:, ti, :, None, :].to_broadcast([128, 2, 2, 64]),
                                                                   in1=wend[:, ti, d4].rearrange("p (g j) -> p g j", g=2)[:, :, :, None].to_broadcast([128, 2, 2, 64]),
                                                                   op=ALU.mult), reads=[TBt, Tce], writes=[TBw[i2]])

                    def ssd_back(d, oi, ti, i2):
                        ts_ = slice(ti * 128, (ti + 1) * 128)
                        for h in range(4):
                            k.op("pe", lambda e: e.matmul(p_y1[:, h, :], lhsT=Ab[i2][:, h, :], rhs=xst[:, ti, h * 64:(h + 1) * 64], start=True, stop=True),
                                 reads=[TAb[i2], Txst], writes=[Tp_y1])
                        yacc = Yacc[:, ti, :].rearrange("p (h c) -> p h c", h=4)
                        if oi > 0:
                            for h in range(4):
                                g = h // 2; gs = slice(g * 64, (g + 1) * 64)
                                k.op("pe", lambda e: e.matmul(p_y2[g][:, h % 2, :], lhsT=fmb[gs, 3, ts_], rhs=STb[gs, h % 2, :], start=True, stop=True),
                                     reads=[Tfmb, TSTb], writes=[Tp_y2[g]])
                            for g in range(2):
                                k.op("dve", lambda e: e.tensor_tensor(out=tmpy[i2][:, 2 * g:2 * g + 2, :], in0=p_y2[g][:],
                                                                      in1=cexp[:, ti, d * 4 + 2 * g:d * 4 + 2 * g + 2, None].to_broadcast([128, 2, 64]), op=ALU.mult),
                                     reads=[Tp_y2[g], Tce], writes=[Ttmpy[i2]])
                            if d == 0:
                                k.op("dve", lambda e: e.tensor_tensor(out=yacc, in0=p_y1[:], in1=tmpy[i2][:], op=ALU.add), reads=[Tp_y1, Ttmpy[i2]], writes=[TY])
                            else:
                                k.op("pool", lambda e: e.tensor_tensor(out=yacc, in0=yacc, in1=tmpy[i2][:], op=ALU.add), reads=[TY, Ttmpy[i2]], writes=[TY])
                                k.op("dve", lambda e: e.tensor_tensor(out=yacc, in0=yacc, in1=p_y1[:], op=ALU.add), reads=[TY, Tp_y1], writes=[TY])
                        else:
                            if d == 0:
                                k.op("dve", lambda e: e.tensor_copy(out=yacc, in_=p_y1[:]), reads=[Tp_y1], writes=[TY])
                            else:
                                k.op("dve", lambda e: e.tensor_tensor(out=yacc, in0=yacc, in1=p_y1[:], op=ALU.add), reads=[TY, Tp_y1], writes=[TY])
                        if oi == NT - 1:
                            return
                        for h in range(4):
                            g = h // 2; gs = slice(g * 64, (g + 1) * 64)
                            k.op("pe", lambda e: e.matmul(p_st[gs, h % 2, :], lhsT=Bw[i2][:, g, h % 2, :], rhs=xst[:, ti, h * 64:(h + 1) * 64], start=True, stop=True,
                                                          tile_position=(0, g * 64)), reads=[TBw[i2], Txst], writes=[Tp_st])
                        if oi == 0:
                            k.op("dve", lambda e: e.tensor_copy(out=S32[:], in_=p_st[:]), reads=[Tp_st], writes=[TS32])
                        else:
                            k.op("pool", lambda e: e.tensor_tensor(out=S32[:], in0=S32[:], in1=etH[:, ti, d, :, None].to_broadcast([128, 2, 64]), op=ALU.mult),
                                 reads=[TS32, TetH], writes=[TS32])
                            k.op("dve", lambda e: e.tensor_tensor(out=S32[:], in0=S32[:], in1=p_st[:], op=ALU.add), reads=[TS32, Tp_st], writes=[TS32])
                        k.op("pool", lambda e: e.tensor_copy(out=STb[:], in_=S32[:]), reads=[TS32], writes=[TSTb])

                    seq = []
                    for d in range(2):
                        order = list(range(NT)) if d == 0 else [1, 0] + list(range(NT - 1, 1, -1))
                        for oi, ti in enumerate(order):
                            seq.append((d, oi, ti, len(seq) % 2))
                    ssd_front(*seq[0])
                    for i_ in range(len(seq)):
                        if i_ + 1 < len(seq):
                            ssd_front(*seq[i_ + 1])
                        ssd_back(*seq[i_])
                    zz = k.sb("zz", [128, NT, 256]); Tzz = T()
                    k.dma("sp", zz[:], ut_d[b, :, 256:512].rearrange("(n p) c -> p n c", p=128), reads=[Tut[b]], writes=[Tzz])
                    k.op("act", lambda e: e.activation(out=zz[:], in_=zz[:], func=AF.Silu), reads=[Tzz], writes=[Tzz])
                    tq = k.sb("tq", [128, NT, 256]); Ttq = T()
                    k.op("dve", lambda e: e.tensor_tensor(out=tq[:], in0=xst[:], in1=dsk[:, None, :].to_broadcast([128, NT, 256]), op=ALU.mult), reads=[Txst, Tp4], writes=[Ttq])
                    k.op("dve", lambda e: e.tensor_tensor(out=Yacc[:], in0=Yacc[:], in1=tq[:], op=ALU.add), reads=[TY, Ttq], writes=[TY])
                    k.op("dve", lambda e: e.tensor_tensor(out=Yacc[:], in0=Yacc[:], in1=zz[:], op=ALU.mult), reads=[TY, Tzz], writes=[TY])
                    k.op("pool", lambda e: e.tensor_tensor(out=tq[:], in0=Yacc[:], in1=Yacc[:], op=ALU.mult), reads=[TY], writes=[Ttq])
                    ssq = k.sb("ssq", [128, NT]); Tssq = T()
                    k.op("dve", lambda e: e.reduce_sum(out=ssq[:], in_=tq[:], axis=AX.X), reads=[Ttq], writes=[Tssq])
                    k.op("act", lambda e: e.activation(out=ssq[:], in_=ssq[:], func=AF.Sqrt, scale=1.0 / 256, bias=EPS), reads=[Tssq], writes=[Tssq])
                    k.op("dve", lambda e: e.reciprocal(out=ssq[:], in_=ssq[:]), reads=[Tssq], writes=[Tssq])
                    k.op("dve", lambda e: e.tensor_tensor(out=Yacc[:], in0=Yacc[:], in1=ssq[:, :, None].to_broadcast([128, NT, 256]), op=ALU.mult), reads=[TY, Tssq], writes=[TY])
                    yob = k.sb("yob", [128, NT, 256], BF16); Tyob = T()
                    k.op("dve", lambda e: e.tensor_tensor(out=yob[:], in0=Yacc[:], in1=snw[:, None, :].to_broadcast([128, NT, 256]), op=ALU.mult), reads=[TY, Tp4], writes=[Tyob])
                    ps_loop.__exit__(None, None, None)
                    ps_epi = PScope(k); ps_epi.__enter__()
                    p_tr = [k.ps("p4tre%d" % i, [128, 128], BF16) for i in range(2)]; Tp_tr = [PT(), PT()]
                    yoT = k.sb("yoT", [128, 2, S], BF16); TyoT = T()
                    for ti in range(NT):
                        for ch in range(2):
                            pp = p_tr[ntr % 2]; tp = Tp_tr[ntr % 2]
                            k.op("pe", lambda e: e.transpose(pp[:], yob[:, ti, ch * 128:(ch + 1) * 128], ident_b[:]), reads=[Tyob, Tc], writes=[tp])
                            if ntr % 2 == 0:
                                k.op("act", lambda e: e.activation(out=yoT[:, ch, ti * 128:(ti + 1) * 128], in_=pp[:], func=AF.Copy), reads=[tp], writes=[TyoT])
                            else:
                                k.op("dve", lambda e: e.tensor_copy(out=yoT[:, ch, ti * 128:(ti + 1) * 128], in_=pp[:]), reads=[tp], writes=[TyoT])
                            ntr += 1
                    k.dma("sp", yT_d[b, 512:768, :].rearrange("(c p) t -> p c t", p=128), yoT[:], reads=[TyoT], writes=[TyT[b]])
                    ps_epi.__exit__(None, None, None)
                if cfg.upto < 5:
                    continue
                need_ctx = l < L - 1
                with Stage(k):
                    Tp5 = T()
                    qan = k.sb("qan", [128, 192]); kvan = k.sb("kvan", [128, 128]); qnr = k.sb("qnr", [128, 96]); knr = k.sb("knr", [128, 96])
                    wq = k.sb("wq", [96, 2, 384], BF16); wkv = k.sb("wkv", [128, 512], BF16)
                    rope = k.sb("rope", [128, 16, 2, 16]); invn3 = k.sb("invn3", [128, 3]); invn8 = k.sb("invn8", [128, 8])
                    for dst, src in ((qan, ml_qan_d[:, l]), (kvan, ml_kvan_d[:, l]), (qnr, ml_qn_d[:, l]), (knr, ml_kn_d[:, l]), (rope, rope_d),
                                     (invn3, invn3_d), (invn8, invn8_d)):
                        k.dma("sp", dst[:], src, writes=[Tp5])
                    wqs = k.sb("wqs", [96, 2, 384]); wkvs = k.sb("wkvs", [128, 512]); Twqs = T()
                    k.dma("sp", wqs[:], ml_wq_d[l].rearrange("(c p) n -> p c n", p=96), writes=[Twqs])
                    k.dma("sp", wkvs[:], ml_wkv_d[l], writes=[Twqs])
                    k.op("pool", lambda e: e.tensor_copy(out=wq[:], in_=wqs[:]), reads=[Twqs], writes=[Tp5])
                    k.op("pool", lambda e: e.tensor_copy(out=wkv[:], in_=wkvs[:]), reads=[Twqs], writes=[Tp5])
                    QT = k.sb("QT", [96, 4, S], BF16); TQT = T()
                    KT = k.sb("KT", [96, 4, S], BF16); TKT = T()
                    Va = k.sb("Va", [128, NT, 4, 65], BF16); TVa = T()
                    k.op("pool", lambda e: e.memset(Va[:], 1.0), writes=[TVa])
                    um = [k.sb("um%d" % i, [128, 352]) for i in range(2)]; Tum = [T(), T()]
                    ps_prep = PScope(k); ps_prep.__enter__()
                    def dbl(name, shape, dt_=F32):
                        return [k.sb("%s_%d" % (name, i), shape, dt_) for i in range(2)], [T(), T()]
                    sqL, TsqL = dbl("sq5", [128, 512]); ss3L, Tss3L = dbl("ss3", [128, 3]); ss8L, Tss8L = dbl("ss8", [128, 12])
                    cnL, TcnL = dbl("cn", [128, 320], BF16); cTtL, TcTL = dbl("cTt", [128, 3, 128], BF16)
                    qfL, TqfL = dbl("qf", [128, 4, 96]); kvfL, TkvfL = dbl("kvf", [128, 4, 128])
                    rbL, TrbL = dbl("rb", [128, 5, 32]); raL, TraL = dbl("ra", [128, 4, 5, 16])
                    QbL, TQbL = dbl("Qb", [128, 4, 96], BF16); KbL, TKbL = dbl("Kb", [128, 4, 96], BF16)
                    p_trA = [k.ps("p5trA%d" % i, [128, 4, 128], BF16) for i in range(2)]; Tp_trA = [PT(), PT()]
                    p_trB = [k.ps("p5trB%d" % i, [128, 4, 128], BF16) for i in range(2)]; Tp_trB = [PT(), PT()]
                    p_qL = [k.ps("p_q%d" % i, [128, 384]) for i in range(2)]; Tp_qL = [PT(), PT()]
                    p_kvL = [k.ps("p_kv%d" % i, [128, 512]) for i in range(2)]; Tp_kvL = [PT(), PT()]
                    for ti in range(NT):
                        U = um[ti % 2]; tU = Tum[ti % 2]
                        j2 = ti % 2
                        sq = sqL[j2]; Tsq = TsqL[j2]; ss3 = ss3L[j2]; Tss3 = Tss3L[j2]; ss8 = ss8L[j2]; Tss8 = Tss8L[j2]
                        cn = cnL[j2]; Tcn = TcnL[j2]; cTt = cTtL[j2]; TcT = TcTL[j2]; qf = qfL[j2]; Tqf = TqfL[j2]; kvf = kvfL[j2]; Tkvf = TkvfL[j2]
                        rb = rbL[j2]; Trb = TrbL[j2]; ra = raL[j2]; Tra = TraL[j2]; Qb = QbL[j2]; TQb = TQbL[j2]; Kb = KbL[j2]; TKb = TKbL[j2]
                        p_q = p_qL[j2]; Tp_q = Tp_qL[j2]; p_kv = p_kvL[j2]; Tp_kv = Tp_kvL[j2]
                        p_tr = [p_trB[0], p_trB[1]]; Tp_tr = [Tp_trB[0], Tp_trB[1]]
                        k.dma("sp", U[:], ut_d[b, ti * 128:(ti + 1) * 128, 520:872], reads=[Tut[b]], writes=[tU])
                        k.op("pool", lambda e: e.tensor_tensor(out=sq[:, 0:352], in0=U[:], in1=U[:], op=ALU.mult), reads=[tU], writes=[Tsq])
                        for j, (a_, b_) in enumerate(((0, 192), (192, 320), (320, 352))):
                            k.op("dve", lambda e: e.reduce_sum(out=ss3[:, j:j + 1], in_=sq[:, a_:b_], axis=AX.X), reads=[Tsq], writes=[Tss3])
                        k.op("dve", lambda e: e.tensor_tensor(out=ss3[:], in0=ss3[:], in1=invn3[:], op=ALU.mult), reads=[Tss3, Tp5], writes=[Tss3])
                        k.op("act", lambda e: e.activation(out=ss3[:], in_=ss3[:], func=AF.Sqrt, bias=EPS), reads=[Tss3], writes=[Tss3])
                        k.op("dve", lambda e: e.reciprocal(out=ss3[:], in_=ss3[:]), reads=[Tss3], writes=[Tss3])
                        k.op("dve", lambda e: e.scalar_tensor_tensor(out=cn[:, 0:192], in0=U[:, 0:192], scalar=ss3[:, 0:1], in1=qan[:], op0=ALU.mult, op1=ALU.mult),
                             reads=[tU, Tss3, Tp5], writes=[Tcn])
                        k.op("dve", lambda e: e.scalar_tensor_tensor(out=cn[:, 192:320], in0=U[:, 192:320], scalar=ss3[:, 1:2], in1=kvan[:], op0=ALU.mult, op1=ALU.mult),
                             reads=[tU, Tss3, Tp5], writes=[Tcn])
                        k.op("dve", lambda e: e.scalar_tensor_tensor(out=rb[:, 4, :], in0=U[:, 320:352], scalar=ss3[:, 2:3], in1=knr[:, 64:96], op0=ALU.mult, op1=ALU.mult),
                             reads=[tU, Tss3, Tp5], writes=[Trb])
                        pt = p_trA[j2]; tpt = Tp_trA[j2]
                        k.op("pe", lambda e: e.transpose(pt[0:96, 0, :], cn[:, 0:96], ident_b[:]), reads=[Tcn, Tc], writes=[tpt])
                        k.op("pe", lambda e: e.transpose(pt[0:96, 1, :], cn[:, 96:192], ident_b[:]), reads=[Tcn, Tc], writes=[tpt])
                        k.op("pe", lambda e: e.transpose(pt[:, 2, :], cn[:, 192:320], ident_b[:]), reads=[Tcn, Tc], writes=[tpt])
                        k.op("act", lambda e: e.activation(out=cTt[0:96, 0:2, :], in_=pt[0:96, 0:2, :], func=AF.Copy), reads=[tpt], writes=[TcT])
                        k.op("act", lambda e: e.activation(out=cTt[:, 2, :], in_=pt[:, 2, :], func=AF.Copy), reads=[tpt], writes=[TcT])
                        for c_ in range(2):
                            k.op("pe", lambda e: e.matmul(p_q[:], lhsT=cTt[0:96, c_, :], rhs=wq[:, c_, :], start=(c_ == 0), stop=(c_ == 1)), reads=[TcT, Tp5], writes=[Tp_q])
                        k.op("pe", lambda e: e.matmul(p_kv[:], lhsT=cTt[:, 2, :], rhs=wkv[:], start=True, stop=True), reads=[TcT, Tp5], writes=[Tp_kv])
                        k.op("act", lambda e: e.activation(out=qf[:].rearrange("p h c -> p (h c)"), in_=p_q[:], func=AF.Copy), reads=[Tp_q], writes=[Tqf])
                        k.op("dve", lambda e: e.tensor_copy(out=kvf[:].rearrange("p h c -> p (h c)"), in_=p_kv[:]), reads=[Tp_kv], writes=[Tkvf])
                        sq4 = sq[:, 0:384].rearrange("p (h c) -> p h c", c=96)
                        k.op("pool", lambda e: e.tensor_tensor(out=sq4, in0=qf[:], in1=qf[:], op=ALU.mult), reads=[Tqf], writes=[Tsq])
                        k.op("dve", lambda e: e.reduce_sum(out=ss8[:, 0:4], in_=sq4[:, :, 0:64], axis=AX.X), reads=[Tsq], writes=[Tss8])
                        k.op("dve", lambda e: e.reduce_sum(out=ss8[:, 4:8], in_=sq4[:, :, 64:96], axis=AX.X), reads=[Tsq], writes=[Tss8])
                        sq5 = sq[:, 0:512].rearrange("p (h c) -> p h c", c=128)
                        k.op("pool", lambda e: e.tensor_tensor(out=sq5, in0=kvf[:], in1=kvf[:], op=ALU.mult), reads=[Tkvf, Tss8], writes=[Tsq])
                        k.op("dve", lambda e: e.reduce_sum(out=ss8[:, 8:12], in_=sq5[:, :, 0:64], axis=AX.X), reads=[Tsq], writes=[Tss8])
                        k.op("dve", lambda e: e.tensor_tensor(out=ss8[:, 0:8], in0=ss8[:, 0:8], in1=invn8[:], op=ALU.mult), reads=[Tss8, Tp5], writes=[Tss8])
                        k.op("dve", lambda e: e.tensor_tensor(out=ss8[:, 8:12], in0=ss8[:, 8:12], in1=invn8[:, 0:4], op=ALU.mult), reads=[Tss8, Tp5], writes=[Tss8])
                        k.op("act", lambda e: e.activation(out=ss8[:], in_=ss8[:], func=AF.Sqrt, bias=EPS), reads=[Tss8], writes=[Tss8])
                        k.op("dve", lambda e: e.reciprocal(out=ss8[:], in_=ss8[:]), reads=[Tss8], writes=[Tss8])
                        k.op("dve", lambda e: e.tensor_tensor(out=qf[:, :, 0:64], in0=qf[:, :, 0:64], in1=ss8[:, 0:4, None].to_broadcast([128, 4, 64]), op=ALU.mult),
                             reads=[Tqf, Tss8], writes=[Tqf])
                        k.op("dve", lambda e: e.tensor_tensor(out=Qb[:, :, 0:64], in0=qf[:, :, 0:64], in1=qnr[:, None, 0:64].to_broadcast([128, 4, 64]), op=ALU.mult),
                             reads=[Tqf, Tp5], writes=[TQb])
                        k.op("dve", lambda e: e.tensor_tensor(out=qf[:, :, 64:96], in0=qf[:, :, 64:96], in1=ss8[:, 4:8, None].to_broadcast([128, 4, 32]), op=ALU.mult),
                             reads=[Tqf, Tss8], writes=[Tqf])
                        k.op("dve", lambda e: e.tensor_tensor(out=rb[:, 0:4, :], in0=qf[:, :, 64:96], in1=qnr[:, None, 64:96].to_broadcast([128, 4, 32]), op=ALU.mult),
                             reads=[Tqf, Tp5], writes=[Trb])
                        k.op("dve", lambda e: e.tensor_tensor(out=kvf[:, :, 0:64], in0=kvf[:, :, 0:64], in1=ss8[:, 8:12, None].to_broadcast([128, 4, 64]), op=ALU.mult),
                             reads=[Tkvf, Tss8], writes=[Tkvf])
                        k.op("dve", lambda e: e.tensor_tensor(out=Kb[:, :, 0:64], in0=kvf[:, :, 0:64], in1=knr[:, None, 0:64].to_broadcast([128, 4, 64]), op=ALU.mult),
                             reads=[Tkvf, Tp5], writes=[TKb])
                        k.op("pool", lambda e: e.tensor_copy(out=Va[:, ti, :, 0:64], in_=kvf[:, :, 64:128]), reads=[Tkvf], writes=[TVa])
                        if ti >= 2:
                            rb4 = rb[:].rearrange("p h (j two) -> p h j two", two=2)
                            cs = rope[:, ti - 2, 0, None, :].to_broadcast([128, 5, 16]); sn = rope[:, ti - 2, 1, None, :].to_broadcast([128, 5, 16])
                            k.op("dve", lambda e: e.tensor_tensor(out=ra[:, 0], in0=rb4[:, :, :, 0], in1=cs, op=ALU.mult), reads=[Trb, Tp5], writes=[Tra])
                            k.op("dve", lambda e: e.tensor_tensor(out=ra[:, 1], in0=rb4[:, :, :, 1], in1=sn, op=ALU.mult), reads=[Trb, Tp5], writes=[Tra])
                            k.op("pool", lambda e: e.tensor_tensor(out=ra[:, 2], in0=rb4[:, :, :, 0], in1=sn, op=ALU.mult), reads=[Trb, Tp5], writes=[Tra])
                            k.op("pool", lambda e: e.tensor_tensor(out=ra[:, 3], in0=rb4[:, :, :, 1], in1=cs, op=ALU.mult), reads=[Trb, Tp5], writes=[Tra])
                            k.op("dve", lambda e: e.tensor_tensor(out=rb4[:, :, :, 0], in0=ra[:, 0], in1=ra[:, 1], op=ALU.subtract), reads=[Tra, Trb], writes=[Trb])
                            k.op("dve", lambda e: e.tensor_tensor(out=rb4[:, :, :, 1], in0=ra[:, 2], in1=ra[:, 3], op=ALU.add), reads=[Tra, Trb], writes=[Trb])
                        k.op("dve", lambda e: e.tensor_copy(out=Qb[:, :, 64:96], in_=rb[:, 0:4, :]), reads=[Trb], writes=[TQb])
                        k.op("dve", lambda e: e.tensor_copy(out=Kb[:, :, 64:96], in_=rb[:, 4:5, :].to_broadcast([128, 4, 32])), reads=[Trb], writes=[TKb])
                        for (src, tsrc, dstT, tdst, pi) in ((Qb, TQb, QT, TQT, 1), (Kb, TKb, KT, TKT, 0)):
                            pt = p_tr[pi]; tpt = Tp_tr[pi]
                            for h in range(4):
                                k.op("pe", lambda e: e.transpose(pt[0:96, h, :], src[:, h, :], ident_b[:]), reads=[tsrc, Tc], writes=[tpt])
                            if pi == 1:
                                k.op("act", lambda e: e.activation(out=dstT[:, :, ti * 128:(ti + 1) * 128], in_=pt[0:96, :, :], func=AF.Copy), reads=[tpt], writes=[tdst])
                            else:
                                k.op("dve", lambda e: e.tensor_copy(out=dstT[:, :, ti * 128:(ti + 1) * 128], in_=pt[0:96, :, :]), reads=[tpt], writes=[tdst])
                    ps_prep.__exit__(None, None, None)
                    ps_att = PScope(k); ps_att.__enter__()
                    p_tr = [k.ps("p5trC%d" % i, [128, 4, 128], BF16) for i in range(2)]; Tp_tr = [PT(), PT()]
                    PTt = [k.sb("PT%d" % i, [128, 512], BF16) for i in range(3)]; TPT = [T() for _ in range(3)]
                    p_s = [k.ps("p_s%d" % i, [128, 512]) for i in range(2)]; Tp_s = [PT(), PT()]
                    p_o = [k.ps("p_o%d" % i, [128, 4, 65]) for i in range(2)]; Tp_o = [PT(), PT()]
                    rec = k.sb("rec", [128, 4]); Trec = T()
                    ym = k.sb("ym", [128, NT, 256], BF16); Tym = T()
                    sc = 96.0 ** -0.5
                    nsc = 0; nh = 0
                    qblocks = BLOCKS if need_ctx else BLOCKS[1:]
                    its = []
                    for (q0, qn_) in qblocks:
                        keys = list(range(0, 2) if q0 == 0 else range(NT))
                        for h in range(4):
                            for ki, kt_ in enumerate(keys):
                                its.append((q0, qn_, h, ki, kt_, len(keys), len(its)))

                    def att_qk(q0, qn_, h, ki, kt_, nk, j):
                        ps_ = p_s[j % 2]; tps = Tp_s[j % 2]; P = PTt[j % 3]; tP = TPT[j % 3]
                        k.op("pe", lambda e: e.matmul(ps_[:, 0:qn_], lhsT=KT[:, h, kt_ * 128:(kt_ + 1) * 128], rhs=QT[:, h, q0:q0 + qn_], start=True, stop=True),
                             reads=[TKT, TQT], writes=[tps])
                        k.op("act", lambda e: e.activation(out=P[:, 0:qn_], in_=ps_[:, 0:qn_], func=AF.Exp, scale=sc), reads=[tps], writes=[tP])

                    def att_pv(q0, qn_, h, ki, kt_, nk, j):
                        P = PTt[j % 3]; tP = TPT[j % 3]
                        grp = j // 1
                        nq = qn_ // 128
                        gidx = (q0, h)
                        if ki == 0:
                            att_state["nh"] += 1
                        po = p_o[att_state["nh"] % 2]; tpo = Tp_o[att_state["nh"] % 2]
                        for qs_ in range(nq):
                            k.op("pe", lambda e: e.matmul(po[:, qs_, :], lhsT=P[:, qs_ * 128:(qs_ + 1) * 128], rhs=Va[:, kt_, h, :],
                                                          start=(ki == 0 and qs_ == 0), stop=(ki == nk - 1), skip_group_check=True),
                                 reads=[tP, TVa], writes=[tpo])
                        if ki == nk - 1:
                            k.op("dve", lambda e: e.reciprocal(out=rec[:, 0:nq], in_=po[:, 0:nq, 64]), reads=[tpo], writes=[Trec])
                            t_0 = q0 // 128
                            k.op("dve", lambda e: e.tensor_tensor(out=ym[:, t_0:t_0 + nq, h * 64:(h + 1) * 64], in0=po[:, 0:nq, 0:64],
                                                                  in1=rec[:, 0:nq, None].to_broadcast([128, nq, 64]), op=ALU.mult), reads=[tpo, Trec], writes=[Tym])

                    att_state = {"nh": 0}
                    att_qk(*its[0])
                    for j in range(len(its)):
                        if j + 1 < len(its):
                            att_qk(*its[j + 1])
                        att_pv(*its[j])
                    yoT = k.sb("yoT5", [128, 2, S], BF16); TyoT = T()
                    if not need_ctx:
                        k.op("pool", lambda e: e.memset(yoT[:, :, 0:NCTX], 0.0), writes=[TyoT])
                    ntr = 0
                    for ti in range(0 if need_ctx else 2, NT):
                        for ch in range(2):
                            pp = p_tr[ntr % 2]; tp = Tp_tr[ntr % 2]
                            k.op("pe", lambda e: e.transpose(pp[:, 0, :], ym[:, ti, ch * 128:(ch + 1) * 128], ident_b[:]), reads=[Tym, Tc], writes=[tp])
                            if ntr % 2 == 0:
                                k.op("act", lambda e: e.activation(out=yoT[:, ch, ti * 128:(ti + 1) * 128], in_=pp[:, 0, :], func=AF.Copy), reads=[tp], writes=[TyoT])
                            else:
                                k.op("dve", lambda e: e.tensor_copy(out=yoT[:, ch, ti * 128:(ti + 1) * 128], in_=pp[:, 0, :]), reads=[tp], writes=[TyoT])
                            ntr += 1
                    k.dma("sp", yT_d[b, 768:1024, :].rearrange("(c p) t -> p c t", p=128), yoT[:], reads=[TyoT], writes=[TyT[b]])
                    ps_att.__exit__(None, None, None)
                if cfg.upto < 6:
                    continue
                need_ctx = l < L - 1
                with Stage(k):
                    Tp6 = T()
                    wo = k.sb("wo", [128, 8, D], BF16); wrt = k.sb("wrt", [128, 8, NEXP], BF16)
                    cst = Caster(k, 128, D)
                    for kc in range(8):
                        cst.load(wo[:, kc, :], w_out_d[l, kc * 128:(kc + 1) * 128, :], 128, D, [Tp6])
                    wrs = k.sb("wrs", [128, 8, NEXP]); Twrs = T()
                    k.dma("sp", wrs[:], w_rt_d[l].rearrange("(c p) e -> p c e", p=128), writes=[Twrs])
                    k.op("pool", lambda e: e.tensor_copy(out=wrt[:], in_=wrs[:]), reads=[Twrs], writes=[Tp6])
                    G2 = k.sb("G2", [128, 2, 8]); Tg2 = T()
                    for i, mi in enumerate((b, 2)):
                        k.op("dve", lambda e: e.scalar_tensor_tensor(out=G2[:, i, :], in0=modT[:, l, 32:40, mi], scalar=1.0, in1=n2T[:, l, :],
                                                                    op0=ALU.add, op1=ALU.mult), reads=[Tmod, Tc], writes=[Tg2])
                    Yb = [k.sb("Yb%d" % i, [128, 8, 512], BF16) for i in range(2)]; TYb = [T(), T()]
                    Xb = [k.sb("Xb%d" % i, [128, 8, 512]) for i in range(2)]; TXb = [T(), T()]
                    Qs = k.sb("Qs", [128, 8, 512], BF16); TQs = T()
                    Rr6 = k.sb("Rr6", [128, 512]); TRr6 = T()
                    tm6 = [k.sb("tm6_%d" % i, [128, 512]) for i in range(2)]; Ttm6 = [T(), T()]
                    H2 = k.sb("H2", [128, 8, 512], BF16); TH2 = T()
                    h2o = [k.sb("h2o%d" % i, [128, D], BF16) for i in range(2)]; Th2o = [T(), T()]
                    Ee = k.sb("Ee", [16, 512]); TEe = T()
                    rc6 = k.sb("rc6", [16, 512]); Trc6 = T()
                    pwo = [k.ps("pwo%d" % i, [128, 512]) for i in range(2)]; Tpwo = [PT(), PT()]
                    pss = k.ps("pss6", [128, 512]); Tpss = PT()
                    ptr = [k.ps("ptr6_%d" % i, [128, 8, 128], BF16) for i in range(2)]; Tptr = [PT(), PT()]
                    prl = k.ps("prl", [16, 512]); Tprl = PT()
                    prs = k.ps("prs", [16, 512]); Tprs = PT()
                    ntr_box = [0]
                    blks6 = BLOCKS if need_ctx else BLOCKS[1:]

                    def s6_A(bi):
                        t0, n = blks6[bi]
                        isctx = (t0 == 0)
                        seg = 1 if isctx else 0
                        mi = 2 if isctx else b
                        Y = Yb[bi % 2]; tY = TYb[bi % 2]; X = Xb[bi % 2]; tX = TXb[bi % 2]
                        k.dma("sp", Y[:, :, 0:n], yT_d[b, :, t0:t0 + n].rearrange("(c p) t -> p c t", p=128), reads=[TyT[b]], writes=[tY])
                        k.dma("sp", X[:, :, 0:n], xT_d[b, :, t0:t0 + n].rearrange("(c p) t -> p c t", p=128), reads=[Tx[b]], writes=[tX])
                        for dch in range(8):
                            pp = pwo[dch % 2]; tp = Tpwo[dch % 2]
                            for c_ in range(8):
                                k.op("pe", lambda e: e.matmul(pp[:, 0:n], lhsT=wo[:, c_, dch * 128:(dch + 1) * 128], rhs=Y[:, c_, 0:n], start=(c_ == 0), stop=(c_ == 7)),
                                     reads=[Tp6, tY], writes=[tp])
                            k.op("dve", lambda e: e.scalar_tensor_tensor(out=X[:, dch, 0:n], in0=pp[:, 0:n], scalar=modT[:, l, 16 + dch, mi:mi + 1], in1=X[:, dch, 0:n],
                                                                        op0=ALU.mult, op1=ALU.add), reads=[tp, Tmod, tX], writes=[tX])
                        k.dma("sp", xT_d[b, :, t0:t0 + n].rearrange("(c p) t -> p c t", p=128), X[:, :, 0:n], reads=[tX], writes=[Tx[b]])

                    def s6_B(bi):
                        t0, n = blks6[bi]
                        isctx = (t0 == 0)
                        seg = 1 if isctx else 0
                        mi = 2 if isctx else b
                        X = Xb[bi % 2]; tX = TXb[bi % 2]
                        ntr = ntr_box[0]
                        k.op("act", lambda e: e.activation(out=Qs[:, :, 0:n], in_=X[:, :, 0:n], func=AF.Square), reads=[tX], writes=[TQs])
                        for kc in range(8):
                            k.op("pe", lambda e: e.matmul(pss[:, 0:n], lhsT=ones_b[:], rhs=Qs[:, kc, 0:n], start=(kc == 0), stop=(kc == 7)), reads=[TQs, Tc], writes=[Tpss])
                        k.op("act", lambda e: e.activation(out=Rr6[:, 0:n], in_=pss[:, 0:n], func=AF.Sqrt, scale=1.0 / D, bias=EPS), reads=[Tpss], writes=[TRr6])
                        k.op("dve", lambda e: e.reciprocal(out=Rr6[:, 0:n], in_=Rr6[:, 0:n]), reads=[TRr6], writes=[TRr6])
                        for kc in range(8):
                            tm = tm6[kc % 2]; ttm = Ttm6[kc % 2]
                            k.op("dve", lambda e: e.tensor_tensor(out=tm[:, 0:n], in0=X[:, kc, 0:n], in1=Rr6[:, 0:n], op=ALU.mult), reads=[tX, TRr6], writes=[ttm])
                            k.op("act", lambda e: e.activation(out=H2[:, kc, 0:n], in_=tm[:, 0:n], func=AF.Identity, scale=G2[:, seg, kc:kc + 1],
                                                               bias=modT[:, l, 24 + kc, mi:mi + 1]), reads=[ttm, Tg2, Tmod], writes=[TH2])
                        for tt in range(n // 128):
                            pp = ptr[ntr % 2]; tp = Tptr[ntr % 2]; ho = h2o[ntr % 2]; tho = Th2o[ntr % 2]
                            for kc in range(8):
                                k.op("pe", lambda e: e.transpose(pp[:, kc, :], H2[:, kc, tt * 128:(tt + 1) * 128], ident_b[:]), reads=[TH2, Tc], writes=[tp])
                            if ntr % 2 == 0:
                                k.op("act", lambda e: e.activation(out=ho[:], in_=pp[:].rearrange("p c t -> p (c t)"), func=AF.Copy), reads=[tp], writes=[tho])
                            else:
                                k.op("dve", lambda e: e.tensor_copy(out=ho[:], in_=pp[:].rearrange("p c t -> p (c t)")), reads=[tp], writes=[tho])
                            k.dma("sp", h2t_d[b, t0 + tt * 128:t0 + (tt + 1) * 128, :], ho[:], reads=[tho], writes=[Th2[b]])
                            ntr += 1
                        for kc in range(8):
                            k.op("pe", lambda e: e.matmul(prl[:, 0:n], lhsT=wrt[:, kc, :], rhs=H2[:, kc, 0:n], start=(kc == 0), stop=(kc == 7)), reads=[Tp6, TH2], writes=[Tprl])
                        k.op("act", lambda e: e.activation(out=Ee[:, 0:n], in_=prl[:, 0:n], func=AF.Exp), reads=[Tprl], writes=[TEe])
                        k.op("pe", lambda e: e.matmul(prs[:, 0:n], lhsT=ones_f[0:16, 0:16], rhs=Ee[:, 0:n], start=True, stop=True), reads=[TEe, Tc], writes=[Tprs])
                        k.op("dve", lambda e: e.reciprocal(out=rc6[:, 0:n], in_=prs[:, 0:n]), reads=[Tprs], writes=[Trc6])
                        k.op("dve", lambda e: e.tensor_tensor(out=rc6[:, 0:n], in0=rc6[:, 0:n], in1=Ee[:, 0:n], op=ALU.mult), reads=[Trc6, TEe], writes=[Trc6])
                        k.dma("sp", aff_d[b, :, t0:t0 + n], rc6[:, 0:n], reads=[Trc6], writes=[Taff[b]])
                        ntr_box[0] = ntr

                    s6_A(0)
                    for bi in range(len(blks6)):
                        if bi + 1 < len(blks6):
                            s6_A(bi + 1)
                        s6_B(bi)
                if cfg.upto < 7:
                    continue
                need_ctx = l < L - 1
                last = (l == cfg.layers - 1)
                segl = [(NCTX, NLAT, 256)] + ([(0, NCTX, 32)] if need_ctx else [])
                if l in getattr(cfg, 'skip_moe', ()) or b in getattr(cfg, 'skip_moe_b', ()):
                    segl = []
                segl = segl[:getattr(cfg, 'max_seg', 2)]
                for (t0, N, cap) in segl:
                  isctx = (t0 == 0)
                  mi = 2 if isctx else b
                  ntile = N // 128; nst = max(1, cap // 128); sp = min(cap, 128)
                  with Stage(k):
                    Tp7 = T()
                    iotaf = k.sb("iotaf", [128, 256]); iotap = k.sb("iotap", [128, 2])
                    for dst, src in ((iotaf, iotaf_d), (iotap, iotap_d)):
                        k.dma("sp", dst[:], src, writes=[Tp7])
                    slotm = k.sb("slotm", [16, N]); Tslot = T()
                    slotT = k.sb("slotT", [128, ntile, 16]); TslotT = T()
                    ghlT = k.sb("ghlT", [128, ntile, 16, 2], BF16); TghlT = T()
                    ysb = k.sb("ysb", [128, NEXP, nst, D], BF16); Tysb = T()
                    with Stage(k):
                        ones16 = k.sb("ones16", [16, NLAT])
                        k.dma("sp", ones16[:], ones16_d, writes=[Tp7])
                        affs = k.sb("affs", [16, N]); Taffs = T()
                        work = k.sb("work", [16, N]); Twork = T()
                        m8 = k.sb("m8", [16, 8]); Tm8 = T()
                        mask = k.sb("mask", [16, N]); Tmask = T()
                        ghi = k.sb("ghi", [16, N], BF16); glo = k.sb("glo", [16, N], BF16); Tg = T()
                        k.dma("sp", affs[:], aff_d[b, :, t0:t0 + N], reads=[Taff[b]], writes=[Taffs])
                        k.op("act", lambda e: e.activation(out=work[:], in_=affs[:], func=AF.Copy), reads=[Taffs], writes=[Twork])
                        for it_ in range(cap // 8):
                            k.op("dve", lambda e: e.max(out=m8[:], in_=work[:]), reads=[Twork], writes=[Tm8])
                            k.op("dve", lambda e: e.match_replace(out=work[:], in_to_replace=m8[:], in_values=work[:], imm_value=-1.0), reads=[Tm8, Twork], writes=[Twork])
                        k.op("dve", lambda e: e.tensor_scalar(out=mask[:], in0=work[:], scalar1=0.0, scalar2=None, op0=ALU.is_lt), reads=[Twork], writes=[Tmask])
                        k.op("dve", lambda e: e.tensor_tensor_scan(out=slotm[:], data0=ones16[:, 0:N], data1=mask[:], initial=0.0, op0=ALU.mult, op1=ALU.add),
                             reads=[Tmask, Tp7], writes=[Tslot])
                        k.op("dve", lambda e: e.tensor_tensor(out=slotm[:], in0=slotm[:], in1=mask[:], op=ALU.mult), reads=[Tslot, Tmask], writes=[Tslot])
                        k.op("dve", lambda e: e.tensor_scalar(out=slotm[:], in0=slotm[:], scalar1=-1.0, scalar2=None, op0=ALU.add), reads=[Tslot], writes=[Tslot])
                        k.op("dve", lambda e: e.tensor_tensor(out=affs[:], in0=affs[:], in1=mask[:], op=ALU.mult), reads=[Taffs, Tmask], writes=[Taffs])
                        k.op("dve", lambda e: e.tensor_copy(out=ghi[:], in_=affs[:]), reads=[Taffs], writes=[Tg])
                        k.op("dve", lambda e: e.tensor_tensor(out=affs[:], in0=affs[:], in1=ghi[:], op=ALU.subtract), reads=[Taffs, Tg], writes=[Taffs])
                        k.op("dve", lambda e: e.tensor_copy(out=glo[:], in_=affs[:]), reads=[Taffs], writes=[Tg])
                        pst = k.ps("pst", [128, ntile, 16]); Tpst = PT()
                        pgh = k.ps("pgh", [128, ntile, 2, 16], BF16); Tpgh = PT()
                        for ti in range(ntile):
                            cs_ = slice(ti * 128, (ti + 1) * 128)
                            k.op("pe", lambda e: e.transpose(pst[:, ti, :], slotm[:, cs_], ident_f[0:16, 0:16]), reads=[Tslot, Tc], writes=[Tpst])
                            k.op("pe", lambda e: e.transpose(pgh[:, ti, 0, :], ghi[:, cs_], ident_b[0:16, 0:16]), reads=[Tg, Tc], writes=[Tpgh])
                            k.op("pe", lambda e: e.transpose(pgh[:, ti, 1, :], glo[:, cs_], ident_b[0:16, 0:16]), reads=[Tg, Tc], writes=[Tpgh])
                        k.op("dve", lambda e: e.tensor_copy(out=slotT[:], in_=pst[:]), reads=[Tpst], writes=[TslotT])
                        k.op("dve", lambda e: e.tensor_copy(out=ghlT[:].rearrange("p n e h -> p n h e"), in_=pgh[:]), reads=[Tpgh], writes=[TghlT])
                    with Stage(k):
                        h2k = k.sb("h2k", [128, ntile, D], BF16); Th2k = T()
                        k.dma("sp", h2k[:], h2t_d[b, t0:t0 + N, :].rearrange("(n p) d -> p n d", p=128), reads=[Th2[b]], writes=[Th2k])
                        wg = [k.sb("wg%d" % i, [128, 8, FF], BF16) for i in range(2)]
                        wu = [k.sb("wu%d" % i, [128, 8, FF], BF16) for i in range(2)]
                        wd = [k.sb("wd%d" % i, [128, 4, D], BF16) for i in range(2)]
                        Tw = [T(), T()]
                        Sel = [k.sb("Sel%d" % i, [128, ntile, cap], BF16) for i in range(1)] * 2; TSel = [T()] * 2
                        cst7 = Caster(k, 128, 2048, nbuf=4)
                        xsTL = [k.sb("xsT%d" % i, [128, 8, cap], BF16) for i in range(1)] * 2; TxsTL = [T()] * 2
                        sg = [k.sb("sg%d" % i, [128, cap]) for i in range(2)]; Tsg = [T(), T()]
                        actTL = [k.sb("actT%d" % i, [128, 4, cap], BF16) for i in range(1)] * 2; TactTL = [T()] * 2
                        gs2L = [k.sb("gs2_%d" % i, [128, nst, 2]) for i in range(2)]; gsL = [k.sb("gs_%d" % i, [128, nst]) for i in range(2)]; TgsL = [T(), T()]
                        pg = [k.ps("pg7_%d" % i, [128, cap]) for i in range(3)]; Tpg = [PT() for _ in range(3)]
                        pG = k.ps("pG", [128, cap]); TpG = PT()
                        pU = k.ps("pU", [128, cap]); TpU = PT()
                        pY = [k.ps("pY%d" % i, [128, 512]) for i in range(2)]; TpY = [PT(), PT()]
                        pgs = k.ps("pgs", [128, nst, 2]); Tpgs = PT()
                        nev = 0
                        for ex in range(NEXP):
                            i2 = ex % 2
                            xsT = xsTL[i2]; TxsT = TxsTL[i2]; actT = actTL[i2]; TactT = TactTL[i2]
                            gs2 = gs2L[i2]; gs = gsL[i2]; Tgs = TgsL[i2]
                            for (wt_, src_, c_) in ((wg[i2], w_gate_d[l, ex], 8), (wu[i2], w_up_d[l, ex], 8), (wd[i2], w_down_d[l, ex], 4)):
                                s3 = src_.rearrange("(c p) n -> p c n", p=128)
                                hc_ = c_ // 2
                                for pc_ in range(2):
                                    cst7.load(wt_[:, pc_ * hc_:(pc_ + 1) * hc_, :], s3[:, pc_ * hc_:(pc_ + 1) * hc_, :], 128, 2048, [Tw[i2]], split=2048 // hc_)
                            SL = Sel[i2]; tSL = TSel[i2]
                            for ti in range(ntile):
                                k.op("dve", lambda e: e.tensor_scalar(out=SL[:, ti, :], in0=iotaf[:, 0:cap], scalar1=slotT[:, ti, ex:ex + 1], scalar2=None, op0=ALU.is_equal),
                                     reads=[TslotT, Tp7], writes=[tSL])
                            for dc in range(8):
                                pp = pg[dc % 3]; tp = Tpg[dc % 3]
                                for ti in range(ntile):
                                    k.op("pe", lambda e: e.matmul(pp[:], lhsT=h2k[:, ti, dc * 128:(dc + 1) * 128], rhs=SL[:, ti, :], start=(ti == 0), stop=(ti == ntile - 1)),
                                         reads=[Th2k, tSL], writes=[tp])
                                if dc % 2 == 0:
                                    k.op("act", lambda e: e.activation(out=xsT[:, dc, :], in_=pp[:], func=AF.Copy), reads=[tp], writes=[TxsT])
                                else:
                                    k.op("dve", lambda e: e.tensor_copy(out=xsT[:, dc, :], in_=pp[:]), reads=[tp], writes=[TxsT])
                            for st in range(nst):
                                for ti in range(ntile):
                                    k.op("pe", lambda e: e.matmul(pgs[0:sp, st, :], lhsT=SL[:, ti, st * 128:st * 128 + sp], rhs=ghlT[:, ti, ex, :], start=(ti == 0), stop=(ti == ntile - 1)),
                                         reads=[tSL, TghlT], writes=[Tpgs])
                            k.op("act", lambda e: e.activation(out=gs2[0:sp], in_=pgs[0:sp], func=AF.Copy), reads=[Tpgs], writes=[Tgs])
                            k.op("dve", lambda e: e.tensor_tensor(out=gs[0:sp], in0=gs2[0:sp, :, 0], in1=gs2[0:sp, :, 1], op=ALU.add), reads=[Tgs], writes=[Tgs])
                            for fc in range(4):
                                for kc in range(8):
                                    k.op("pe", lambda e: e.matmul(pG[:], lhsT=wg[i2][:, kc, fc * 128:(fc + 1) * 128], rhs=xsT[:, kc, :], start=(kc == 0), stop=(kc == 7)),
                                         reads=[Tw[i2], TxsT], writes=[TpG])
                                for kc in range(8):
                                    k.op("pe", lambda e: e.matmul(pU[:], lhsT=wu[i2][:, kc, fc * 128:(fc + 1) * 128], rhs=xsT[:, kc, :], start=(kc == 0), stop=(kc == 7)),
                                         reads=[Tw[i2], TxsT], writes=[TpU])
                                k.op("act", lambda e: e.activation(out=sg[fc % 2][:], in_=pG[:], func=AF.Silu), reads=[TpG], writes=[Tsg[fc % 2]])
                                k.op("dve", lambda e: e.tensor_tensor(out=actT[:, fc, :], in0=sg[fc % 2][:], in1=pU[:], op=ALU.mult), reads=[Tsg[fc % 2], TpU], writes=[TactT])
                            for st in range(nst):
                                for dh in range(2):
                                    pp = pY[nev % 2]; tp = TpY[nev % 2]
                                    for fc in range(4):
                                        k.op("pe", lambda e: e.matmul(pp[0:sp, :], lhsT=actT[:, fc, st * 128:st * 128 + sp], rhs=wd[i2][:, fc, dh * 512:(dh + 1) * 512],
                                                                      start=(fc == 0), stop=(fc == 3)), reads=[TactT, Tw[i2]], writes=[tp])
                                    dsto = ysb[0:sp, ex, st, dh * 512:(dh + 1) * 512]
                                    if nev % 2 == 0:
                                        k.op("act", lambda e: e.activation(out=dsto, in_=pp[0:sp, :], func=AF.Copy, scale=gs[0:sp, st:st + 1]), reads=[tp, Tgs], writes=[Tysb])
                                    else:
                                        k.op("dve", lambda e: e.tensor_scalar(out=dsto, in0=pp[0:sp, :], scalar1=gs[0:sp, st:st + 1], scalar2=None, op0=ALU.mult),
                                             reads=[tp, Tgs], writes=[Tysb])
                                    nev += 1
                    with Stage(k):
                        oneh = k.sb("oneh", [16, 16, 128]); Toneh = T()
                        k.dma("sp", oneh[:], oneh_d, writes=[Toneh])
                        SelTL = [k.sb("SelT%d" % i, [128, NEXP, nst, 512], BF16) for i in range(2)]; TSelTL = [T(), T()]
                        Xc = [k.sb("Xc%d" % i, [128, 8, 512]) for i in range(2)]; TXc = [T(), T()]
                        ot = [k.sb("ot%d" % i, [128, D]) for i in range(2)]; Tot = [T(), T()]
                        pb = [k.ps("pb%d" % i, [128, 512]) for i in range(2)]; Tpb = [PT(), PT()]
                        pc = [k.ps("pc%d" % i, [128, 512]) for i in range(4)]; Tpc = [PT() for _ in range(4)]
                        po_ = [k.ps("po7_%d" % i, [128, 4, 128]) for i in range(2)]; Tpo_ = [PT(), PT()]
                        nto = 0
                        nblk = max(1, N // 512)
                        def cmb_build(tb):
                            n = min(N, 512)
                            c0 = tb * 512
                            X = Xc[tb % 2]; tX = TXc[tb % 2]
                            SelT = SelTL[tb % 2]; TSelT = TSelTL[tb % 2]
                            k.dma("sp", X[:, :, 0:n], xT_d[b, :, t0 + c0:t0 + c0 + n].rearrange("(c p) t -> p c t", p=128), reads=[Tx[b]], writes=[tX])
                            for ex in range(NEXP):
                                pp = pb[ex % 2]; tp = Tpb[ex % 2]
                                k.op("pe", lambda e: e.matmul(pp[:, 0:n], lhsT=oneh[:, ex, :], rhs=slotm[:, c0:c0 + n], start=True, stop=True), reads=[Tslot, Toneh], writes=[tp])
                                for st in range(nst):
                                    k.op("dve", lambda e: e.tensor_scalar(out=SelT[:, ex, st, 0:n], in0=pp[:, 0:n], scalar1=iotap[:, st:st + 1], scalar2=None, op0=ALU.is_equal),
                                         reads=[tp, Tp7], writes=[TSelT])

                        cmb_build(0)
                        for tb in range(nblk):
                            n = min(N, 512)
                            c0 = tb * 512
                            X = Xc[tb % 2]; tX = TXc[tb % 2]
                            SelT = SelTL[tb % 2]; TSelT = TSelTL[tb % 2]
                            if tb + 1 < nblk:
                                cmb_build(tb + 1)
                            for dh in range(2):
                                for dcl in range(4):
                                    dc = dh * 4 + dcl
                                    pp = pc[dcl]; tp = Tpc[dcl]
                                    for ex in range(NEXP):
                                        for st in range(nst):
                                            k.op("pe", lambda e: e.matmul(pp[:, 0:n], lhsT=ysb[0:sp, ex, st, dc * 128:(dc + 1) * 128], rhs=SelT[0:sp, ex, st, 0:n],
                                                                          start=(ex == 0 and st == 0), stop=(ex == NEXP - 1 and st == nst - 1)),
                                                 reads=[Tysb, TSelT], writes=[tp])
                                    k.op("dve", lambda e: e.scalar_tensor_tensor(out=X[:, dc, 0:n], in0=pp[:, 0:n], scalar=modT[:, l, 40 + dc, mi:mi + 1], in1=X[:, dc, 0:n],
                                                                                op0=ALU.mult, op1=ALU.add), reads=[tp, Tmod, tX], writes=[tX])
                            if not last:
                                k.dma("sp", xT_d[b, :, t0 + c0:t0 + c0 + n].rearrange("(c p) t -> p c t", p=128), X[:, :, 0:n], reads=[tX], writes=[Tx[b]])
                            elif not isctx:
                                for tt in range(n // 128):
                                    O = ot[nto % 2]; tO = Tot[nto % 2]
                                    for half in range(2):
                                        pp = po_[half]; tp = Tpo_[half]
                                        for q in range(4):
                                            dc = half * 4 + q
                                            k.op("pe", lambda e: e.transpose(pp[:, q, :], X[:, dc, tt * 128:(tt + 1) * 128], ident_f[:]), reads=[tX, Tc], writes=[tp])
                                        if half == 0:
                                            k.op("act", lambda e: e.activation(out=O[:, 0:512], in_=pp[:].rearrange("p q t -> p (q t)"), func=AF.Copy), reads=[tp], writes=[tO])
                                        else:
                                            k.op("dve", lambda e: e.tensor_copy(out=O[:, 512:1024], in_=pp[:].rearrange("p q t -> p (q t)")), reads=[tp], writes=[tO])
                                    tok0 = c0 + tt * 128
                                    k.dma("sp", out_d[b, tok0:tok0 + 128, :], O[:], reads=[tO], writes=[Tout])
                                    nto += 1

        k.barrier()
        global LAST_K
        LAST_K = k
    return nc


def prep_inputs(inp, core, nb=2):
    b0 = core * 2
    m = {}
    m["x"] = np.ascontiguousarray(inp["x"][b0:b0 + nb])
    m["ctx"] = np.ascontiguousarray(inp["ctx"][b0:b0 + nb])
    cc = np.stack([inp["c"][b0], inp["c"][b0 + 1], inp["c_ctx"]], axis=0)
    m["cT"] = np.ascontiguousarray(fm(cc, 8).transpose(0, 2, 1))
    m["ada_w"] = inp["ada_w"]
    m["ada_bT"] = fm(inp["ada_b"], 48)
    m["n1T"] = fm(inp["norm1_w"], 8)
    m["n2T"] = fm(inp["norm2_w"], 8)
    m["w_in"] = inp["w_in"]
    m.update(prep_lru(inp))
    m["hg_lbT"] = fm(inp["hgrn_lb_logits"], 2)
    m["hg_nwT"] = fm(inp["hgrn_norm_w"], 2)
    scw = np.asarray(inp["ssd_conv_w"], np.float32)
    m["sd_cw"] = np.ascontiguousarray(scw.reshape(L, 4, 4, 128).transpose(3, 0, 2, 1))
    m["sd_cb"] = fm(inp["ssd_conv_b"], 4)
    rep = lambda v: np.ascontiguousarray(np.broadcast_to(np.asarray(v, np.float32)[None], (128,) + tuple(np.shape(v))))
    m["sd_alog"] = rep(np.asarray(inp["ssd_a_log"]).reshape(L, 8))
    m["sd_dtb"] = rep(np.asarray(inp["ssd_dt_bias"]).reshape(L, 8))
    m["sd_dsk"] = rep(np.repeat(np.asarray(inp["ssd_d_skip"]), 64, axis=-1))
    m["sd_nw"] = rep(inp["ssd_norm_w"])
    m["ml_qan"] = rep(inp["mla_q_a_norm"]); m["ml_kvan"] = rep(inp["mla_kv_a_norm"])
    m["ml_qn"] = rep(inp["mla_q_norm"]); m["ml_kn"] = rep(inp["mla_k_norm"])
    m["ml_wq"] = inp["mla_w_q_up"]; m["ml_wkv"] = inp["mla_w_kv_up"]
    m["w_out"] = inp["w_out"]; m["w_rt"] = inp["moe_router"]
    m["w_gate"] = inp["moe_w_gate"]; m["w_up"] = inp["moe_w_up"]; m["w_down"] = inp["moe_w_down"]
    m.update(host_consts())
    return m


def kernel(**inputs):
    inp = {k_: np.asarray(v) for k_, v in inputs.items()}
    cfg = Cfg(nb=2)
    nc = build(cfg)
    in_maps = [prep_inputs(inp, c) for c in range(8)]
    res = run_bass_kernel_spmd(nc, in_maps, core_ids=list(range(8)))
    out = np.concatenate([r["out"] for r in res.results], axis=0)
    return out.astype(np.float32)
```

```python
import math
from contextlib import ExitStack
import numpy as np
import ml_dtypes
import concourse.bass as bass
import concourse.mybir as mybir
from concourse.bass_utils import run_bass_kernel_spmd

F32 = mybir.dt.float32
BF16 = mybir.dt.bfloat16
I32 = mybir.dt.int32
U32 = mybir.dt.uint32
ALU = mybir.AluOpType
AF = mybir.ActivationFunctionType
AX = mybir.AxisListType

L = 2
D = 1024
NLAT = 2048
NCTX = 256
S = NCTX + NLAT
NT = S // 128
IN_COLS = 2920
EPS = 1e-6
NEXP = 16
FF = 512
TM_RANGES = [(1280, 1536), (1792, 2048), (2560, 2920)]
TM_COLS = sum(b - a for a, b in TM_RANGES)
FM_CHUNKS = list(range(0, 10)) + [12, 13] + [16, 17, 18, 19]
BLOCKS = [(0, 256)] + [(256 + 512 * i, 512) for i in range(4)]


class T:
    __slots__ = ("name", "w", "r", "x")

    def __init__(self, name="", x=False):
        self.name = name
        self.w = None
        self.r = []
        self.x = x


def PT():
    return T("psum", True)


class _Rec:
    def __init__(self):
        self.call = None

    def __getattr__(self, name):
        def f(*a, **kw):
            self.call = (name, a, kw)
            return self
        return f


class K:
    N_DMA_SEMS = 24

    def __init__(self, nc, stack):
        self.nc = nc
        self.stack = stack
        self.eng = {"pe": nc.tensor, "dve": nc.vector, "act": nc.scalar,
                    "pool": nc.gpsimd, "sp": nc.sync}
        self.sems = {}
        self.count = {}
        self.seen = {e: {} for e in self.eng}
        for e in self.eng:
            self.sems[e] = stack.enter_context(nc.semaphore("s_" + e))
            self.count[e] = 0
        for i in range(self.N_DMA_SEMS):
            k = "d%d" % i
            self.sems[k] = stack.enter_context(nc.semaphore("s_" + k))
            self.count[k] = 0
        self.dma_rr = 0
        self.n_inst = 0
        self.scope = stack

    def sb(self, name, shape, dtype=F32):
        self.n_alloc = getattr(self, "n_alloc", 0) + 1
        return self.scope.enter_context(self.nc.sbuf_tensor("sb%d_%s" % (self.n_alloc, name), list(shape), dtype))

    def ps(self, name, shape, dtype=F32):
        self.n_alloc = getattr(self, "n_alloc", 0) + 1
        nel = 512 if dtype == F32 else 1024
        scope = getattr(self, "pscope", None) or self.scope
        full = scope.enter_context(self.nc.psum_tensor("ps%d_%s" % (self.n_alloc, name), [128, nel], dtype))
        n = 1
        for d_ in shape[1:]:
            n *= d_
        assert n <= nel, (name, shape)
        v = full[0:shape[0], 0:n]
        if len(shape) == 3:
            v = v.rearrange("p (a b) -> p a b", b=shape[2])
        elif len(shape) == 4:
            v = v.rearrange("p (a b c) -> p a b c", b=shape[2], c=shape[3])
        return v

    def _waits(self, e, reads, writes):
        need = {}
        for t in reads:
            if t.w is not None:
                k, v, pe = t.w
                if not (pe == "pe" and e == "pe"):
                    need[k] = max(need.get(k, 0), v)
        for t in writes:
            if t.w is not None:
                k, v, pe = t.w
                if not (pe == "pe" and e == "pe"):
                    need[k] = max(need.get(k, 0), v)
            for (k, v, pe) in t.r:
                if pe == "pe" and e == "pe":
                    continue
                need[k] = max(need.get(k, 0), v)
        seen = self.seen[e]
        h = self.eng[e]
        for k, v in need.items():
            if seen.get(k, 0) < v:
                h.wait_ge(self.sems[k], v)
                seen[k] = v

    def _commit(self, tok, reads, writes):
        for t in writes:
            t.w = tok
            t.r = []
        for t in reads:
            if t not in writes:
                t.r.append(tok)
                if len(t.r) > 16:
                    best = {}
                    for (k, v, pe) in t.r:
                        if k not in best or best[k][1] < v:
                            best[k] = (k, v, pe)
                    t.r = list(best.values())

    def flush(self, pend, n=None):
        n = len(pend) if n is None else min(n, len(pend))
        for _ in range(n):
            it = pend.pop(0)
            if it[0] == "op":
                _, e, (name, a, kw), reads, writes = it
                self.op(e, lambda eng: getattr(eng, name)(*a, **kw), reads, writes)
            else:
                _, e, out, in_, reads, writes, kw = it
                self.dma(e, out, in_, reads, writes, **kw)

    def op(self, e, fn, reads=(), writes=()):
        reads = list(reads)
        writes = list(writes)
        if getattr(self, "defer", None) is not None:
            rec = _Rec()
            fn(rec)
            self.defer.append(("op", e, rec.call, reads, writes))
            return None
        xr = [t for t in reads if t.x]
        if xr:
            reads = [t for t in reads if not t.x]
            writes = writes + [t for t in xr if t not in writes]
        self._waits(e, reads, writes)
        ins = fn(self.eng[e])
        self.count[e] += 1
        ins.then_inc(self.sems[e], 1)
        self._commit((e, self.count[e], e), reads, writes)
        self.n_inst += 1
        return ins

    def dma(self, e, out, in_, reads=(), writes=(), **kw):
        reads = list(reads)
        writes = list(writes)
        if getattr(self, "defer", None) is not None:
            self.defer.append(("dma", e, out, in_, reads, writes, kw))
            return None
        self._waits(e, reads, writes)
        k = "d%d" % self.dma_rr
        self.dma_rr = (self.dma_rr + 1) % self.N_DMA_SEMS
        ins = self.eng[e].dma_start(out=out, in_=in_, **kw)
        self.count[k] += 16
        ins.then_inc(self.sems[k], 16)
        self._commit((k, self.count[k], "dma"), reads, writes)
        self.n_inst += 1
        return ins

    def barrier(self):
        for e, h in self.eng.items():
            seen = self.seen[e]
            for k, v in self.count.items():
                if v > 0 and seen.get(k, 0) < v and k != e:
                    h.wait_ge(self.sems[k], v)
                    seen[k] = v


class PScope:
    def __init__(self, k):
        self.k = k

    def __enter__(self):
        self.prev = getattr(self.k, "pscope", None)
        self.st = ExitStack()
        self.st.__enter__()
        self.k.pscope = self.st
        return self

    def __exit__(self, *a):
        self.k.barrier()
        self.k.pscope = self.prev
        return self.st.__exit__(*a)


class Caster:
    def __init__(self, k, npart, nfree, nbuf=2, eng="act"):
        self.k = k
        self.eng = eng
        self.st = [k.sb("stg%d" % i, [npart, nfree]) for i in range(nbuf)]
        self.T = [T() for _ in range(nbuf)]
        self.i = 0

    def load(self, dst, src, npart, nfree, writes, reads=(), split=None):
        k = self.k
        j = self.i % len(self.st)
        self.i += 1
        st = self.st[j][0:npart, 0:nfree]
        if split is not None:
            st = st.rearrange("p (a b) -> p a b", b=split)
        if getattr(self, "alt", False) and self.i % 2 == 0:
            k.dma("sp", st, src, reads=list(reads), writes=[self.T[j]])
            k.op("dve", lambda e: e.tensor_copy(out=dst, in_=st), reads=[self.T[j]], writes=list(writes))
            return
        k.dma("sp", st, src, reads=list(reads), writes=[self.T[j]])
        if self.eng == "act":
            k.op("act", lambda e: e.activation(out=dst, in_=st, func=AF.Copy), reads=[self.T[j]], writes=list(writes))
        else:
            k.op(self.eng, lambda e: e.tensor_copy(out=dst, in_=st), reads=[self.T[j]], writes=list(writes))


class Stage:
    def __init__(self, k):
        self.k = k

    def __enter__(self):
        self.prev = self.k.scope
        self.st = ExitStack()
        self.st.__enter__()
        self.k.scope = self.st
        return self

    def __exit__(self, *a):
        self.k.barrier()
        self.k.scope = self.prev
        return self.st.__exit__(*a)


def fm(v, nch):
    v = np.asarray(v, np.float32)
    lead = v.shape[:-1]
    r = v.reshape(lead + (nch, 128))
    r = np.moveaxis(r, -1, 0)
    return np.ascontiguousarray(r)


def host_consts():
    c = {}
    c["ident_f"] = np.eye(128, dtype=np.float32)
    c["ident_b"] = np.eye(128, dtype=np.float32).astype(ml_dtypes.bfloat16)
    c["ones_b"] = np.ones((128, 128), np.float32).astype(ml_dtypes.bfloat16)
    c["ones_f"] = np.ones((128, 128), np.float32)
    t = np.arange(S)
    c["mfwd"] = np.ascontiguousarray(np.broadcast_to((t % 128 != 0).astype(np.float32), (128, S)))
    c["mbwd"] = np.ascontiguousarray(np.broadcast_to((t % 128 != 127).astype(np.float32), (128, S)))
    i = np.arange(128)
    c["triU"] = (i[:, None] <= i[None, :]).astype(np.uint32)
    c["triL"] = (i[:, None] >= i[None, :]).astype(np.uint32)
    c["triUf"] = (i[:, None] <= i[None, :]).astype(np.float32)
    c["triLf"] = (i[:, None] >= i[None, :]).astype(np.float32)
    c["strLf"] = (i[:, None] > i[None, :]).astype(np.float32)
    c["strUf"] = (i[:, None] < i[None, :]).astype(np.float32)
    tt = np.arange(NLAT)
    inv = 10000.0 ** (-np.arange(0, 16, 2, dtype=np.float32) / 16)
    ang = np.concatenate([(tt // 64).astype(np.float32)[:, None] * inv, (tt % 64).astype(np.float32)[:, None] * inv], axis=-1).astype(np.float32)
    cs = np.stack([np.cos(ang), np.sin(ang)], axis=1).astype(np.float32)
    c["rope"] = np.ascontiguousarray(cs.reshape(16, 128, 2, 16).transpose(1, 0, 2, 3))
    c["invn3"] = np.ascontiguousarray(np.broadcast_to(np.array([1 / 192, 1 / 128, 1 / 32], np.float32), (128, 3)))
    c["invn8"] = np.ascontiguousarray(np.broadcast_to(np.array([1 / 64] * 4 + [1 / 32] * 4, np.float32), (128, 8)))
    c["iotaf"] = np.ascontiguousarray(np.broadcast_to(np.arange(256, dtype=np.float32), (128, 256)))
    c["iotap"] = np.stack([i.astype(np.float32), i.astype(np.float32) + 128], axis=1)
    oh = np.zeros((16, 16, 128), np.float32)
    for e_ in range(16):
        oh[e_, e_, :] = 1.0
    c["oneh"] = oh
    c["ones16"] = np.ones((16, NLAT), np.float32)
    rep4 = lambda a_: np.ascontiguousarray(np.broadcast_to(a_[:, None, :], (128, 4, 128))).astype(np.float32)
    c["triUf4"] = rep4(c["triUf"]); c["triLf4"] = rep4(c["triLf"])
    c["negmf"] = rep4(-1.0e4 * c["strLf"]); c["negmb"] = rep4(-1.0e4 * c["strUf"])
    c["blk64"] = (i[:, None] // 64 == i[None, :] // 64).astype(np.float32).astype(ml_dtypes.bfloat16)
    return c


def prep_lru(inp):
    m = {}
    cw = np.asarray(inp["lru_conv_w"], np.float32)
    m["lru_cw"] = np.ascontiguousarray(cw.reshape(L, 4, 2, 128).transpose(3, 0, 2, 1))
    m["lru_cb"] = fm(inp["lru_conv_b"], 2)
    for nm, key in (("lru_wr", "lru_w_r"), ("lru_wi", "lru_w_i")):
        w = np.asarray(inp[key], np.float32)
        o = np.zeros((128, L, 2, 2, 128), np.float32)
        for ch in range(2):
            for hh in range(2):
                o[hh * 64:(hh + 1) * 64, :, :, ch, hh * 64:(hh + 1) * 64] = w[:, :, 2 * ch + hh].transpose(2, 0, 1, 3)
        m[nm] = o
    m["lru_br"] = fm(inp["lru_b_r"], 2)
    m["lru_bi"] = fm(inp["lru_b_i"], 2)
    m["lru_lam"] = fm(inp["lru_lam"], 2)
    return m


class Cfg:
    def __init__(self, nb=2, upto=99, debug=False, layers=2):
        self.layers = layers
        self.nb = nb
        self.upto = upto
        self.debug = debug


def build(cfg):
    nc = bass.Bass("TRN2", target_bir_lowering=False)
    NB = cfg.nb
    dbg_kind = "ExternalOutput" if cfg.debug else "Internal"

    def din(name, shape, dt=F32):
        return nc.dram_tensor(name, list(shape), dt, kind="ExternalInput").ap()

    def dscr(name, shape, dt=F32):
        return nc.dram_tensor(name, list(shape), dt, kind=dbg_kind).ap()

    x_d = din("x", [NB, NLAT, D])
    ctx_d = din("ctx", [NB, NCTX, D])
    cT_d = din("cT", [128, 8, 3])
    ada_w_d = din("ada_w", [L, D, 6 * D])
    ada_bT_d = din("ada_bT", [128, L, 48])
    n1T_d = din("n1T", [128, L, 8])
    n2T_d = din("n2T", [128, L, 8])
    w_in_d = din("w_in", [L, D, IN_COLS])
    lru_cw_d = din("lru_cw", [128, L, 2, 4])
    lru_cb_d = din("lru_cb", [128, L, 2])
    lru_wr_d = din("lru_wr", [128, L, 2, 2, 128])
    lru_wi_d = din("lru_wi", [128, L, 2, 2, 128])
    lru_br_d = din("lru_br", [128, L, 2, 2])
    lru_bi_d = din("lru_bi", [128, L, 2, 2])
    lru_lam_d = din("lru_lam", [128, L, 2, 2])
    hg_lbT_d = din("hg_lbT", [128, L, 2])
    hg_nwT_d = din("hg_nwT", [128, L, 2])
    mfwd_d = din("mfwd", [128, S])
    mbwd_d = din("mbwd", [128, S])
    triU_d = din("triU", [128, 128], U32)
    triL_d = din("triL", [128, 128], U32)
    blk64_d = din("blk64", [128, 128], BF16)
    sd_cw_d = din("sd_cw", [128, L, 4, 4])
    sd_cb_d = din("sd_cb", [128, L, 4])
    sd_alog_d = din("sd_alog", [128, L, 8])
    sd_dtb_d = din("sd_dtb", [128, L, 8])
    sd_dsk_d = din("sd_dsk", [128, L, 256])
    sd_nw_d = din("sd_nw", [128, L, 256])
    triUf_d = din("triUf", [128, 128])
    triLf_d = din("triLf", [128, 128])
    strLf_d = din("strLf", [128, 128])
    strUf_d = din("strUf", [128, 128])
    ml_qan_d = din("ml_qan", [128, L, 192])
    ml_kvan_d = din("ml_kvan", [128, L, 128])
    ml_qn_d = din("ml_qn", [128, L, 96])
    ml_kn_d = din("ml_kn", [128, L, 96])
    ml_wq_d = din("ml_wq", [L, 192, 384])
    ml_wkv_d = din("ml_wkv", [L, 128, 512])
    rope_d = din("rope", [128, 16, 2, 16])
    invn3_d = din("invn3", [128, 3])
    invn8_d = din("invn8", [128, 8])
    w_out_d = din("w_out", [L, D, D])
    w_rt_d = din("w_rt", [L, D, NEXP])
    w_gate_d = din("w_gate", [L, NEXP, D, FF])
    w_up_d = din("w_up", [L, NEXP, D, FF])
    w_down_d = din("w_down", [L, NEXP, FF, D])
    iotaf_d = din("iotaf", [128, 256])
    iotap_d = din("iotap", [128, 2])
    oneh_d = din("oneh", [16, 16, 128])
    ones16_d = din("ones16", [16, NLAT])
    triUf4_d = din("triUf4", [128, 4, 128])
    triLf4_d = din("triLf4", [128, 4, 128])
    negmf_d = din("negmf", [128, 4, 128])
    negmb_d = din("negmb", [128, 4, 128])
    ident_f_d = din("ident_f", [128, 128])
    ident_b_d = din("ident_b", [128, 128], BF16)
    ones_b_d = din("ones_b", [128, 128], BF16)
    ones_f_d = din("ones_f", [128, 128])
    out_d = nc.dram_tensor("out", [NB, NLAT, D], F32, kind="ExternalOutput").ap()

    xT_d = dscr("xT", [NB, D, S])
    uT_d = dscr("uT", [NB, IN_COLS, S])
    ut_d = dscr("ut", [NB, S, TM_COLS])
    yT_d = dscr("yT", [NB, D, S], BF16)
    TyT = [T() for b in range(NB)]
    h2t_d = dscr("h2t", [NB, S, D], BF16)
    aff_d = dscr("aff", [NB, NEXP, S])
    Th2 = [T() for b in range(NB)]
    Taff = [T() for b in range(NB)]
    Tx = [T("xT%d" % b) for b in range(NB)]
    TuT = [T() for b in range(NB)]
    Tut = [T() for b in range(NB)]
    Tout = T("out")

    with ExitStack() as root:
        k = K(nc, root)
        ident_f = k.sb("ident_f", [128, 128]); ident_b = k.sb("ident_b", [128, 128], BF16)
        ones_b = k.sb("ones_b", [128, 128], BF16); ones_f = k.sb("ones_f", [128, 128])
        modT = k.sb("modT", [128, L, 48, 3])
        n1T = k.sb("n1T", [128, L, 8]); n2T = k.sb("n2T", [128, L, 8])
        Tc = T("consts")
        Tmod = T("mod")
        k.dma("sp", ident_f[:], ident_f_d, writes=[Tc])
        k.dma("sp", ident_b[:], ident_b_d, writes=[Tc])
        k.dma("sp", ones_b[:], ones_b_d, writes=[Tc])
        k.dma("sp", ones_f[:], ones_f_d, writes=[Tc])
        k.dma("sp", n1T[:], n1T_d, writes=[Tc])
        k.dma("sp", n2T[:], n2T_d, writes=[Tc])

        with Stage(k):
            cT = k.sb("cT", [128, 8, 3]); sT = k.sb("sT", [128, 8, 3])
            abT = k.sb("abT", [128, L, 48])
            Tct = T(); Tst = T()
            k.dma("sp", cT[:], cT_d, writes=[Tct])
            k.dma("sp", abT[:], ada_bT_d, writes=[Tct])
            k.op("act", lambda e: e.activation(out=sT[:], in_=cT[:], func=AF.Silu), reads=[Tct], writes=[Tst])
            wbuf = [k.sb("adaw%d" % i, [128, 8, 512]) for i in range(2)]
            Tw = [T(), T()]
            pm = [k.ps("pm%d" % i, [128, 4, 4]) for i in range(2)]
            Tpm = [PT(), PT()]
            it = 0
            for l in range(L):
                for j in range(12):
                    wb = wbuf[it % 2]; tw = Tw[it % 2]
                    k.dma("sp", wb[:], ada_w_d[l, :, j * 512:(j + 1) * 512].rearrange("(kc p) n -> p kc n", p=128), writes=[tw])
                    pp = pm[it % 2]; tp = Tpm[it % 2]
                    for sub in range(4):
                        for kc in range(8):
                            k.op("pe", lambda e: e.matmul(pp[:, sub, 0:3], lhsT=wb[:, kc, sub * 128:(sub + 1) * 128],
                                                          rhs=sT[:, kc, :], start=(kc == 0), stop=(kc == 7)),
                                 reads=[tw, Tst], writes=[tp])
                    for sub in range(4):
                        ch = j * 4 + sub
                        k.op("dve", lambda e: e.tensor_scalar(out=modT[:, l, ch, :], in0=pp[:, sub, 0:3],
                                                              scalar1=abT[:, l, ch:ch + 1], scalar2=None, op0=ALU.add),
                             reads=[tp, Tct], writes=[Tmod])
                    it += 1

        with Stage(k):
            xin = [k.sb("xin%d" % i, [128, D]) for i in range(3)]
            Txin = [T() for _ in range(3)]
            xo = [k.sb("xo%d" % i, [128, 8, 128]) for i in range(3)]
            Txo = [T() for _ in range(3)]
            pt = [k.ps("pt%d" % i, [128, 4, 128]) for i in range(4)]
            Tpt = [PT() for _ in range(4)]
            it = 0
            for b in range(NB):
                for ti in range(NT):
                    src = ctx_d[b, ti * 128:(ti + 1) * 128, :] if ti < 2 else x_d[b, (ti - 2) * 128:(ti - 1) * 128, :]
                    xi = xin[it % 3]; txi = Txin[it % 3]
                    k.dma("sp", xi[:], src, writes=[txi])
                    xx = xo[it % 3]; txo = Txo[it % 3]
                    for half in range(2):
                        pp = pt[(2 * it + half) % 4]; tp = Tpt[(2 * it + half) % 4]
                        for q in range(4):
                            kc = half * 4 + q
                            k.op("pe", lambda e: e.transpose(pp[:, q, :], xi[:, kc * 128:(kc + 1) * 128], ident_f[:]),
                                 reads=[txi, Tc], writes=[tp])
                        if half == 0:
                            k.op("act", lambda e: e.activation(out=xx[:, 0:4, :], in_=pp[:], func=AF.Copy), reads=[tp], writes=[txo])
                        else:
                            k.op("dve", lambda e: e.tensor_copy(out=xx[:, 4:8, :], in_=pp[:]), reads=[tp], writes=[txo])
                    k.dma("sp", xT_d[b, :, ti * 128:(ti + 1) * 128].rearrange("(kc p) t -> p kc t", p=128), xx[:],
                          reads=[txo], writes=[Tx[b]])
                    it += 1

        for l in range(cfg.layers):
            if cfg.upto < 1:
                break
            for b in range(NB):
                with Stage(k):
                    w_in = k.sb("w_in", [128, 8, IN_COLS], BF16); Tw = T()
                    cst = Caster(k, 128, IN_COLS)
                    for kc in range(8):
                        cst.load(w_in[:, kc, :], w_in_d[l, kc * 128:(kc + 1) * 128, :], 128, IN_COLS, [Tw])
                    G = k.sb("G", [128, 2, 8]); Tg = T()
                    for i, mi in enumerate((b, 2)):
                        k.op("dve", lambda e: e.scalar_tensor_tensor(out=G[:, i, :], in0=modT[:, l, 8:16, mi], scalar=1.0,
                                                                    in1=n1T[:, l, :], op0=ALU.add, op1=ALU.mult),
                             reads=[Tmod, Tc], writes=[Tg])
                    xb = [k.sb("xb%d" % i, [128, 8, 512]) for i in range(2)]; Txb = [T(), T()]
                    sq = [k.sb("sq%d" % i, [128, 8, 512], BF16) for i in range(2)]; Tsq = [T(), T()]
                    rs = [k.sb("rs%d" % i, [128, 512]) for i in range(2)]; Trs = [T(), T()]
                    tmp = [k.sb("tmp%d" % i, [128, 512]) for i in range(2)]; Ttmp = [T(), T()]
                    hT = [k.sb("hT%d" % i, [128, 8, 512], BF16) for i in range(2)]; ThT = [T(), T()]
                    ev = [k.sb("ev%d" % i, [128, 512]) for i in range(4)]; Tev = [T() for _ in range(4)]
                    evt = [k.sb("evt%d" % i, [128, TM_COLS]) for i in range(2)]; Tevt = [T(), T()]
                    pss = k.ps("pss", [128, 512]); Tpss = PT()
                    pu = [k.ps("pu%d" % i, [128, 512]) for i in range(4)]; Tpu = [PT() for _ in range(4)]
                    pv = [k.ps("pv%d" % i, [128, 512]) for i in range(3)]; Tpv = [PT() for _ in range(3)]
                    nev_box = [0]

                    def s1_norm(bi):
                        t0, n = BLOCKS[bi]
                        seg = 1 if bi == 0 else 0
                        mi = 2 if bi == 0 else b
                        X = xb[bi % 2]; tX = Txb[bi % 2]
                        k.dma("sp", X[:, :, 0:n], xT_d[b, :, t0:t0 + n].rearrange("(kc p) t -> p kc t", p=128),
                              reads=[Tx[b]], writes=[tX])
                        Q = sq[bi % 2]; tQ = Tsq[bi % 2]
                        k.op("act", lambda e: e.activation(out=Q[:, :, 0:n], in_=X[:, :, 0:n], func=AF.Square), reads=[tX], writes=[tQ])
                        for kc in range(8):
                            k.op("pe", lambda e: e.matmul(pss[:, 0:n], lhsT=ones_b[:], rhs=Q[:, kc, 0:n], start=(kc == 0), stop=(kc == 7)),
                                 reads=[tQ, Tc], writes=[Tpss])
                        R = rs[bi % 2]; tR = Trs[bi % 2]
                        k.op("act", lambda e: e.activation(out=R[:, 0:n], in_=pss[:, 0:n], func=AF.Sqrt, scale=1.0 / D, bias=EPS),
                             reads=[Tpss], writes=[tR])
                        k.op("dve", lambda e: e.reciprocal(out=R[:, 0:n], in_=R[:, 0:n]), reads=[tR], writes=[tR])
                        H = hT[bi % 2]; tH = ThT[bi % 2]
                        for kc in range(8):
                            tm = tmp[kc % 2]; ttm = Ttmp[kc % 2]
                            k.op("dve", lambda e: e.tensor_tensor(out=tm[:, 0:n], in0=X[:, kc, 0:n], in1=R[:, 0:n], op=ALU.mult),
                                 reads=[tX, tR], writes=[ttm])
                            k.op("act", lambda e: e.activation(out=H[:, kc, 0:n], in_=tm[:, 0:n], func=AF.Identity,
                                                               scale=G[:, seg, kc:kc + 1], bias=modT[:, l, kc, mi:mi + 1]),
                                 reads=[ttm, Tg, Tmod], writes=[tH])

                    def s1_proj(bi):
                        t0, n = BLOCKS[bi]
                        H = hT[bi % 2]; tH = ThT[bi % 2]
                        nev = nev_box[0]
                        for ci, ch in enumerate(FM_CHUNKS):
                            c0 = ch * 128
                            pp = pu[ci % 4]; tp = Tpu[ci % 4]
                            for kc in range(8):
                                k.op("pe", lambda e: e.matmul(pp[:, 0:n], lhsT=w_in[:, kc, c0:c0 + 128], rhs=H[:, kc, 0:n],
                                                              start=(kc == 0), stop=(kc == 7)), reads=[Tw, tH], writes=[tp])
                            E = ev[nev % 4]; tE = Tev[nev % 4]
                            if nev % 2 == 0:
                                k.op("act", lambda e: e.activation(out=E[:, 0:n], in_=pp[:, 0:n], func=AF.Copy), reads=[tp], writes=[tE])
                            else:
                                k.op("dve", lambda e: e.tensor_copy(out=E[:, 0:n], in_=pp[:, 0:n]), reads=[tp], writes=[tE])
                            k.dma("sp", uT_d[b, c0:c0 + 128, t0:t0 + n], E[:, 0:n], reads=[tE], writes=[TuT[b]])
                            nev += 1
                        for tt in range(n // 128):
                            ET = evt[tt % 2]; tET = Tevt[tt % 2]
                            off = 0
                            for ri, (a, bnd) in enumerate(TM_RANGES):
                                w = bnd - a
                                pp = pv[ri]; tp = Tpv[ri]
                                for kc in range(8):
                                    k.op("pe", lambda e: e.matmul(pp[:, 0:w], lhsT=H[:, kc, tt * 128:(tt + 1) * 128], rhs=w_in[:, kc, a:bnd],
                                                                  start=(kc == 0), stop=(kc == 7)), reads=[Tw, tH], writes=[tp])
                                if ri == 1:
                                    k.op("act", lambda e: e.activation(out=ET[:, off:off + w], in_=pp[:, 0:w], func=AF.Copy), reads=[tp], writes=[tET])
                                else:
                                    k.op("dve", lambda e: e.tensor_copy(out=ET[:, off:off + w], in_=pp[:, 0:w]), reads=[tp], writes=[tET])
                                off += w
                            k.dma("sp", ut_d[b, t0 + tt * 128:t0 + (tt + 1) * 128, :], ET[:], reads=[tET], writes=[Tut[b]])
                        nev_box[0] = nev

                    s1_norm(0)
                    for bi in range(len(BLOCKS)):
                        if bi + 1 < len(BLOCKS):
                            s1_norm(bi + 1)
                        s1_proj(bi)
                if cfg.upto < 2:
                    continue
                with Stage(k):
                    cw = k.sb("cw", [128, 2, 4]); cb = k.sb("cb", [128, 2])
                    wr = k.sb("wr", [128, 2, 2, 128], BF16); wi = k.sb("wi", [128, 2, 2, 128], BF16)
                    br = k.sb("br", [128, 2, 2]); bi_ = k.sb("bi", [128, 2, 2]); lam = k.sb("lam", [128, 2, 2])
                    cl = k.sb("cl", [128, 2, 2]); cl2 = k.sb("cl2", [128, 2, 2])
                    Tp2 = T()
                    k.dma("sp", cw[:], lru_cw_d[:, l], writes=[Tp2]); k.dma("sp", cb[:], lru_cb_d[:, l], writes=[Tp2])
                    cst = Caster(k, 128, 512)
                    cst.load(wr[:].rearrange("p a b c -> p (a b c)"), lru_wr_d[:, l].rearrange("p a b c -> p (a b c)"), 128, 512, [Tp2])
                    cst.load(wi[:].rearrange("p a b c -> p (a b c)"), lru_wi_d[:, l].rearrange("p a b c -> p (a b c)"), 128, 512, [Tp2])
                    k.dma("sp", br[:], lru_br_d[:, l], writes=[Tp2]); k.dma("sp", bi_[:], lru_bi_d[:, l], writes=[Tp2])
                    k.dma("sp", lam[:], lru_lam_d[:, l], writes=[Tp2])
                    k.op("act", lambda e: e.activation(out=cl[:], in_=lam[:], func=AF.Exp, scale=-1.0), reads=[Tp2], writes=[Tp2])
                    k.op("act", lambda e: e.activation(out=cl[:], in_=cl[:], func=AF.Ln, bias=1.0), reads=[Tp2], writes=[Tp2])
                    k.op("dve", lambda e: e.tensor_scalar(out=cl2[:], in0=cl[:], scalar1=-16.0, scalar2=None, op0=ALU.mult), reads=[Tp2], writes=[Tp2])
                    k.op("dve", lambda e: e.tensor_scalar(out=cl[:], in0=cl[:], scalar1=-8.0, scalar2=None, op0=ALU.mult), reads=[Tp2], writes=[Tp2])
                    xp = k.sb("xp", [128, S + 6]); Txp = T()
                    xc = k.sb("xc", [128, S]); Txc = T()
                    xcb = k.sb("xcb", [128, S], BF16); Txcb = T()
                    gt = k.sb("gt", [128, S]); Tgt = T()
                    Rr = k.sb("Rr", [128, S]); TR = T()
                    Ii = k.sb("Ii", [128, S]); TI = T()
                    Aa = k.sb("Aa", [128, S]); TA = T()
                    Bb = k.sb("Bb", [128, S]); TB = T()
                    Hh = [k.sb("Hh%d" % i, [128, S]) for i in range(2)]; TH = [T(), T()]
                    yo = k.sb("yo", [128, S], BF16); Tyo = T()
                    pg = [k.ps("pg%d" % i, [128, 512]) for i in range(4)]; Tpg = [PT() for _ in range(4)]
                    npg = 0
                    segs = [(0, NCTX, 2), (NCTX, NLAT, NCTX + 5)]
                    for ch in range(2):
                        k.op("pool", lambda e: e.memset(xp[:], 0.0), writes=[Txp])
                        for (t0, n, o) in segs:
                            k.dma("sp", xp[:, o:o + n], uT_d[b, ch * 128:(ch + 1) * 128, t0:t0 + n], reads=[TuT[b]], writes=[Txp])
                        k.dma("sp", gt[:], uT_d[b, 256 + ch * 128:256 + (ch + 1) * 128, :], reads=[TuT[b]], writes=[Tgt])
                        for (t0, n, o) in segs:
                            k.op("dve", lambda e: e.tensor_scalar(out=xc[:, t0:t0 + n], in0=xp[:, o - 2:o - 2 + n], scalar1=cw[:, ch, 0:1],
                                                                  scalar2=cb[:, ch:ch + 1], op0=ALU.mult, op1=ALU.add),
                                 reads=[Txp, Tp2], writes=[Txc])
                            for j in range(1, 4):
                                k.op("dve", lambda e: e.scalar_tensor_tensor(out=xc[:, t0:t0 + n], in0=xp[:, o - 2 + j:o - 2 + j + n],
                                                                            scalar=cw[:, ch, j:j + 1], in1=xc[:, t0:t0 + n],
                                                                            op0=ALU.mult, op1=ALU.add),
                                     reads=[Txp, Tp2, Txc], writes=[Txc])
                        k.op("pool", lambda e: e.tensor_copy(out=xcb[:], in_=xc[:]), reads=[Txc], writes=[Txcb])
                        for d in range(2):
                            for (t0, n) in BLOCKS:
                                for (W, bias, dst, tdst) in ((wr, br, Rr, TR), (wi, bi_, Ii, TI)):
                                    pp = pg[npg % 4]; tp = Tpg[npg % 4]; npg += 1
                                    k.op("pe", lambda e: e.matmul(pp[:, 0:n], lhsT=W[:, d, ch, :], rhs=xcb[:, t0:t0 + n], start=True, stop=True),
                                         reads=[Tp2, Txcb], writes=[tp])
                                    k.op("act", lambda e: e.activation(out=dst[:, t0:t0 + n], in_=pp[:, 0:n], func=AF.Sigmoid,
                                                                       bias=bias[:, d, ch:ch + 1]), reads=[tp, Tp2], writes=[tdst])
                            k.op("act", lambda e: e.activation(out=Aa[:], in_=Rr[:], func=AF.Exp, scale=cl[:, d, ch:ch + 1]),
                                 reads=[TR, Tp2], writes=[TA])
                            k.op("act", lambda e: e.activation(out=Bb[:], in_=Rr[:], func=AF.Exp, scale=cl2[:, d, ch:ch + 1]),
                                 reads=[TR, Tp2], writes=[TB])
                            k.op("act", lambda e: e.activation(out=Bb[:], in_=Bb[:], func=AF.Sqrt, scale=-1.0, bias=1.0), reads=[TB], writes=[TB])
                            k.op("dve", lambda e: e.tensor_tensor(out=Ii[:], in0=Ii[:], in1=xc[:], op=ALU.mult), reads=[TI, Txc], writes=[TI])
                            k.op("dve", lambda e: e.tensor_tensor(out=Bb[:], in0=Bb[:], in1=Ii[:], op=ALU.mult), reads=[TB, TI], writes=[TB])
                            H = Hh[d]
                            if d == 0:
                                k.op("dve", lambda e: e.tensor_tensor_scan(out=H[:], data0=Aa[:], data1=Bb[:], initial=0.0,
                                                                          op0=ALU.mult, op1=ALU.add), reads=[TA, TB], writes=[TH[d]])
                            else:
                                k.op("dve", lambda e: e.tensor_tensor_scan(out=H[:, NCTX - 1::-1], data0=Aa[:, NCTX - 1::-1], data1=Bb[:, NCTX - 1::-1],
                                                                          initial=0.0, op0=ALU.mult, op1=ALU.add), reads=[TA, TB], writes=[TH[d]])
                                k.op("dve", lambda e: e.tensor_tensor_scan(out=H[:, S - 1:NCTX - 1:-1], data0=Aa[:, S - 1:NCTX - 1:-1],
                                                                          data1=Bb[:, S - 1:NCTX - 1:-1], initial=H[:, 0:1],
                                                                          op0=ALU.mult, op1=ALU.add), reads=[TA, TB, TH[d]], writes=[TH[d]])
                        k.op("act", lambda e: e.activation(out=gt[:], in_=gt[:], func=AF.Gelu_apprx_tanh), reads=[Tgt], writes=[Tgt])
                        k.op("dve", lambda e: e.tensor_tensor(out=Hh[0][:], in0=Hh[0][:], in1=Hh[1][:], op=ALU.add), reads=TH, writes=[TH[0]])
                        k.op("dve", lambda e: e.tensor_tensor(out=yo[:], in0=Hh[0][:], in1=gt[:], op=ALU.mult), reads=[TH[0], Tgt], writes=[Tyo])
                        k.dma("sp", yT_d[b, ch * 128:(ch + 1) * 128, :], yo[:], reads=[Tyo], writes=[TyT[b]])
                if cfg.upto < 3:
                    continue
                with Stage(k):
                    lbz = k.sb("lbz", [128, L, 2]); lbe = k.sb("lbe", [128, L, 2]); lbs = k.sb("lbs", [128, 2]); lb = k.sb("lb", [128, 2])
                    oml = k.sb("oml", [128, 2]); hnw = k.sb("hnw", [128, 2]); Tp3 = T()
                    mf = k.sb("mf", [128, S]); mb = k.sb("mb", [128, S]); triU = k.sb("triU", [128, 128], U32); triL = k.sb("triL", [128, 128], U32)
                    blk = k.sb("blk", [128, 128], BF16)
                    k.dma("sp", lbz[:], hg_lbT_d, writes=[Tp3]); k.dma("sp", hnw[:], hg_nwT_d[:, l], writes=[Tp3])
                    k.dma("sp", mf[:], mfwd_d, writes=[Tp3]); k.dma("sp", mb[:], mbwd_d, writes=[Tp3])
                    k.dma("sp", triU[:], triU_d, writes=[Tp3]); k.dma("sp", triL[:], triL_d, writes=[Tp3]); k.dma("sp", blk[:], blk64_d, writes=[Tp3])
                    k.op("act", lambda e: e.activation(out=lbe[:], in_=lbz[:], func=AF.Exp), reads=[Tp3], writes=[Tp3])
                    k.op("dve", lambda e: e.tensor_tensor(out=lbs[:], in0=lbe[:, 0, :], in1=lbe[:, 1, :], op=ALU.add), reads=[Tp3], writes=[Tp3])
                    k.op("dve", lambda e: e.reciprocal(out=lbs[:], in_=lbs[:]), reads=[Tp3], writes=[Tp3])
                    for ll in range(L):
                        k.op("dve", lambda e: e.tensor_tensor(out=lbe[:, ll, :], in0=lbe[:, ll, :], in1=lbs[:], op=ALU.mult), reads=[Tp3], writes=[Tp3])
                    k.op("dve", lambda e: e.tensor_copy(out=lb[:], in_=lbe[:, 0, :]), reads=[Tp3], writes=[Tp3])
                    for ll in range(1, l + 1):
                        k.op("dve", lambda e: e.tensor_tensor(out=lb[:], in0=lb[:], in1=lbe[:, ll, :], op=ALU.add), reads=[Tp3], writes=[Tp3])
                    k.op("dve", lambda e: e.tensor_tensor(out=lb[:], in0=lb[:], in1=lbe[:, 0, :], op=ALU.subtract), reads=[Tp3], writes=[Tp3])
                    k.op("dve", lambda e: e.tensor_scalar(out=oml[:], in0=lb[:], scalar1=-1.0, scalar2=1.0, op0=ALU.mult, op1=ALU.add), reads=[Tp3], writes=[Tp3])
                    vt = k.sb("vt", [128, NT, 256], BF16); Tvt = T()
                    vstg = k.sb("vstg", [128, NT // 2, 256]); Tvstg = T()
                    for hf in range(2):
                        k.dma("sp", vstg[:], ut_d[b, hf * (S // 2):(hf + 1) * (S // 2), 0:256].rearrange("(n p) c -> p n c", p=128), reads=[Tut[b]], writes=[Tvstg])
                        k.op("pool", lambda e: e.tensor_copy(out=vt[:, hf * (NT // 2):(hf + 1) * (NT // 2), :], in_=vstg[:]), reads=[Tvstg], writes=[Tvt])
                    qh = k.sb("qh", [128, S]); Tqh = T()
                    gg = k.sb("gg", [128, S]); Tgg = T()
                    ff = k.sb("ff", [128, S]); Tff = T()
                    lf = k.sb("lf", [128, S]); Tlf = T()
                    cum = k.sb("cum", [128, S]); Tcum = T()
                    dd = k.sb("dd", [128, S]); Tdd = T()
                    EE = k.sb("EE", [128, S]); TEE = T()
                    qt = k.sb("qt", [128, S], BF16); Tqt = T()
                    kt = k.sb("kt", [128, S], BF16); Tkt = T()
                    qs = k.sb("qs", [128, S], BF16); Tqs = T()
                    ke = k.sb("ke", [128, S], BF16); Tke = T()
                    etot = k.sb("etot", [128, NT]); Tet = T()
                    OO = k.sb("OO", [128, S]); TOO = T()
                    ket = [k.sb("ket%d" % i, [128, 128], BF16) for i in range(2)]; Tket = [T(), T()]
                    Am = [[k.sb("Am%d_%d" % (d, i), [128, 128], BF16) for i in range(2)] for d in range(2)]
                    TAm = [[T(), T()] for d in range(2)]
                    S32 = k.sb("S32", [128, 64]); TS32 = T()
                    Sb = k.sb("Sb", [128, 64], BF16); TSb = T()
                    sqb = k.sb("sqb", [128, 512], BF16); Tsqb = T()
                    rsd = k.sb("rsd", [128, 512]); Trsd = T()
                    yo = k.sb("yo3", [128, S], BF16); Tyo = T()
                    p_sc = [k.ps("p_sc%d" % i, [128, 128]) for i in range(2)]; Tp_sc = [PT(), PT()]
                    p_y = [k.ps("p_y%d" % i, [128, 128]) for i in range(2)]; Tp_y = [PT(), PT()]
                    p_st = k.ps("p_st", [128, 64]); Tp_st = PT()
                    p_tr = k.ps("p_tr", [128, 128], BF16); Tp_tr = PT()
                    p_ss = k.ps("p_ss", [128, 512]); Tp_ss = PT()
                    for d in range(2):
                        for i in range(2):
                            k.op("pool", lambda e: e.memset(Am[d][i][:], 0.0), writes=[TAm[d][i]])
                    for i in range(2):
                        k.op("dve", lambda e: e.memset(p_sc[i][:], 0.0), writes=[Tp_sc[i]])
                    cum3 = cum[:].rearrange("p (n t) -> p n t", t=128)
                    dd3 = dd[:].rearrange("p (n t) -> p n t", t=128)
                    cum4 = cum[:].rearrange("p (n t) -> p n t", t=32)
                    dd4 = dd[:].rearrange("p (n t) -> p n t", t=32)
                    qx = k.sb("qx", [128, S], BF16); Tqx = T()
                    kx = [None] + [k.sb("kx%d" % i, [128, NT, 96], BF16) for i in range(1, 4)]; Tkx = T()
                    def mkset(i_):
                        d_ = {}
                        for nm in ("qt", "kt", "qx", "qs", "ke"):
                            d_[nm] = k.sb("%s_b%d" % (nm, i_), [128, S], BF16); d_["T" + nm] = T()
                        d_["kx"] = [None] + [k.sb("kx%d_b%d" % (j, i_), [128, NT, 96], BF16) for j in range(1, 4)]; d_["Tkx"] = T()
                        d_["etot"] = k.sb("etot_b%d" % i_, [128, NT]); d_["Tet"] = T()
                        return d_
                    sets = [dict(qt=qt, Tqt=Tqt, kt=kt, Tkt=Tkt, qx=qx, Tqx=Tqx, qs=qs, Tqs=Tqs, ke=ke, Tke=Tke, kx=kx, Tkx=Tkx, etot=etot, Tet=Tet), mkset(1)]

                    def hg_prep(hp, d, B):
                        r0 = 512 + hp * 128
                        qt, kt, qx, qs, ke, kx, etot = B["qt"], B["kt"], B["qx"], B["qs"], B["ke"], B["kx"], B["etot"]
                        Tqt, Tkt, Tqx, Tqs, Tke, Tkx, Tet = B["Tqt"], B["Tkt"], B["Tqx"], B["Tqs"], B["Tke"], B["Tkx"], B["Tet"]
                        if d == 0:
                            k.dma("sp", qh[:], uT_d[b, r0:r0 + 128, :], reads=[TuT[b]], writes=[Tqh])
                            k.op("act", lambda e: e.activation(out=qh[:], in_=qh[:], func=AF.Silu), reads=[Tqh], writes=[Tqh])
                        k.dma("sp", ff[:], uT_d[b, r0 + 256 * (d + 1):r0 + 256 * (d + 1) + 128, :], reads=[TuT[b]], writes=[Tff])
                        k.op("act", lambda e: e.activation(out=ff[:], in_=ff[:], func=AF.Sigmoid), reads=[Tff], writes=[Tff])
                        k.op("dve", lambda e: e.tensor_scalar(out=ff[:], in0=ff[:], scalar1=oml[:, hp:hp + 1], scalar2=lb[:, hp:hp + 1],
                                                              op0=ALU.mult, op1=ALU.add), reads=[Tff, Tp3], writes=[Tff])
                        k.op("act", lambda e: e.activation(out=lf[:], in_=ff[:], func=AF.Ln), reads=[Tff], writes=[Tlf])
                        k.op("dve", lambda e: e.tensor_scalar(out=ff[:], in0=ff[:], scalar1=-1.0, scalar2=1.0, op0=ALU.mult, op1=ALU.add),
                             reads=[Tff], writes=[Tff])
                        if d == 0:
                            k.op("dve", lambda e: e.tensor_tensor_scan(out=cum[:], data0=mf[:], data1=lf[:], initial=0.0, op0=ALU.mult, op1=ALU.add),
                                 reads=[Tp3, Tlf], writes=[Tcum])
                            mid, end = 63, 127
                        else:
                            k.op("dve", lambda e: e.tensor_tensor_scan(out=cum[:, ::-1], data0=mb[:, ::-1], data1=lf[:, ::-1], initial=0.0,
                                                                      op0=ALU.mult, op1=ALU.add), reads=[Tp3, Tlf], writes=[Tcum])
                            mid, end = 64, 0
                        mid4, first4 = (15, 0) if d == 0 else (16, 31)
                        k.op("dve", lambda e: e.tensor_tensor(out=dd4, in0=cum4, in1=cum4[:, :, mid4:mid4 + 1].to_broadcast([128, S // 32, 32]), op=ALU.subtract),
                             reads=[Tcum], writes=[Tdd])
                        k.op("act", lambda e: e.activation(out=EE[:], in_=dd[:], func=AF.Exp), reads=[Tdd], writes=[TEE])
                        k.op("dve", lambda e: e.tensor_tensor(out=qt[:], in0=qh[:], in1=EE[:], op=ALU.mult), reads=[Tqh, TEE], writes=[Tqt])
                        k.op("act", lambda e: e.activation(out=EE[:], in_=dd[:], func=AF.Exp, scale=-1.0), reads=[Tdd], writes=[TEE])
                        k.op("dve", lambda e: e.tensor_tensor(out=kt[:], in0=ff[:], in1=EE[:], op=ALU.mult), reads=[Tff, TEE], writes=[Tkt])
                        k.op("dve", lambda e: e.tensor_tensor(out=dd4, in0=cum4, in1=cum4[:, :, first4:first4 + 1].to_broadcast([128, S // 32, 32]), op=ALU.subtract),
                             reads=[Tcum], writes=[Tdd])
                        k.op("act", lambda e: e.activation(out=EE[:], in_=dd[:], func=AF.Exp), reads=[Tdd], writes=[TEE])
                        k.op("dve", lambda e: e.tensor_tensor(out=qx[:], in0=qh[:], in1=EE[:], op=ALU.mult), reads=[Tqh, TEE], writes=[Tqx])
                        ff3 = ff[:].rearrange("p (n t) -> p n t", t=128)
                        EE3 = EE[:].rearrange("p (n t) -> p n t", t=128)
                        for i in range(1, 4):
                            w_ = 32 * i
                            if d == 0:
                                srcs = slice(0, w_); refi = w_
                            else:
                                srcs = slice(128 - w_, 128); refi = 127 - w_
                            k.op("dve", lambda e: e.tensor_tensor(out=dd3[:, :, 0:w_], in0=cum3[:, :, refi:refi + 1].to_broadcast([128, NT, w_]),
                                                                  in1=cum3[:, :, srcs], op=ALU.subtract), reads=[Tcum], writes=[Tdd])
                            k.op("act", lambda e: e.activation(out=EE3[:, :, 0:w_], in_=dd3[:, :, 0:w_], func=AF.Exp), reads=[Tdd], writes=[TEE])
                            k.op("dve", lambda e: e.tensor_tensor(out=kx[i][:, :, 0:w_], in0=ff3[:, :, srcs], in1=EE3[:, :, 0:w_], op=ALU.mult),
                                 reads=[Tff, TEE], writes=[Tkx])
                        k.op("act", lambda e: e.activation(out=EE[:], in_=cum[:], func=AF.Exp), reads=[Tcum], writes=[TEE])
                        k.op("dve", lambda e: e.tensor_tensor(out=qs[:], in0=qh[:], in1=EE[:], op=ALU.mult), reads=[Tqh, TEE], writes=[Tqs])
                        k.op("act", lambda e: e.activation(out=etot[:], in_=cum3[:, :, end], func=AF.Exp), reads=[Tcum], writes=[Tet])
                        k.op("dve", lambda e: e.tensor_tensor(out=dd3, in0=cum3[:, :, end:end + 1].to_broadcast([128, NT, 128]), in1=cum3, op=ALU.subtract),
                             reads=[Tcum], writes=[Tdd])
                        k.op("act", lambda e: e.activation(out=EE[:], in_=dd[:], func=AF.Exp), reads=[Tdd], writes=[TEE])
                        k.op("dve", lambda e: e.tensor_tensor(out=ke[:], in0=ff[:], in1=EE[:], op=ALU.mult), reads=[Tff, TEE], writes=[Tke])

                    def hg_loop(hp, d, B, pend):
                        qt, kt, qx, qs, ke, kx, etot = B["qt"], B["kt"], B["qx"], B["qs"], B["ke"], B["kx"], B["etot"]
                        Tqt, Tkt, Tqx, Tqs, Tke, Tkx, Tet = B["Tqt"], B["Tkt"], B["Tqx"], B["Tqs"], B["Tke"], B["Tkx"], B["Tet"]
                        order = list(range(NT)) if d == 0 else [1, 0] + list(range(NT - 1, 1, -1))
                        tri = triU if d == 0 else triL
                        per = (len(pend) + NT - 3) // (NT - 2) if pend else 0
                        for oi, ti in enumerate(order):
                            ts_ = slice(ti * 128, (ti + 1) * 128)
                            k.op("pe", lambda e: e.transpose(p_tr[:], ke[:, ts_], ident_b[:]), reads=[Tke, Tc], writes=[Tp_tr])
                            KT = ket[oi % 2]; tKT = Tket[oi % 2]
                            k.op("act", lambda e: e.activation(out=KT[:], in_=p_tr[:], func=AF.Copy), reads=[Tp_tr], writes=[tKT])
                            py = p_y[oi % 2]; tpy = Tp_y[oi % 2]
                            for hh in range(2):
                                bs = slice(hh * 64, (hh + 1) * 64)
                                psc = p_sc[hh]; tps = Tp_sc[hh]
                                for tb in range(4):
                                    for sb_ in (range(0, tb + 1) if d == 0 else range(tb, 4)):
                                        tq = slice(ti * 128 + 32 * tb, ti * 128 + 32 * tb + 32)
                                        if sb_ == tb:
                                            lw = kt[bs, tq]; rq = qt[bs, tq]
                                        else:
                                            i = tb if d == 0 else 3 - tb
                                            o_ = 32 * sb_ if d == 0 else 32 * sb_ - (128 - 32 * i)
                                            lw = kx[i][bs, ti, o_:o_ + 32]; rq = qx[bs, tq]
                                        k.op("pe", lambda e: e.matmul(psc[32 * sb_:32 * sb_ + 32, 32 * tb:32 * tb + 32], lhsT=lw, rhs=rq, start=True, stop=True,
                                                                      tile_position=(bs.start, 32 * sb_)),
                                             reads=[Tkt, Tqt, Tkx, Tqx], writes=[tps])
                                A = Am[d][hh]; tA = TAm[d][hh]
                                k.op("dve", lambda e: e.copy_predicated(out=A[:], mask=tri[:], data=psc[:]), reads=[tps, Tp3], writes=[tA])
                                vs = vt[:, ti, (2 * hp + hh) * 64:(2 * hp + hh + 1) * 64]
                                k.op("pe", lambda e: e.matmul(py[bs, :], lhsT=vs, rhs=A[:], start=True, stop=(oi == 0)),
                                     reads=[Tvt, tA], writes=[tpy])
                                if oi > 0:
                                    k.op("pe", lambda e: e.matmul(py[bs, :], lhsT=Sb[bs, :], rhs=qs[bs, ts_], start=False, stop=True),
                                         reads=[TSb, Tqs], writes=[tpy])
                            if d == 0:
                                k.op("act", lambda e: e.activation(out=OO[:, ts_], in_=py[:], func=AF.Copy), reads=[tpy], writes=[TOO])
                            else:
                                k.op("dve", lambda e: e.tensor_tensor(out=OO[:, ts_], in0=OO[:, ts_], in1=py[:], op=ALU.add), reads=[tpy, TOO], writes=[TOO])
                            if oi < NT - 1:
                                for hh in range(2):
                                    bs = slice(hh * 64, (hh + 1) * 64)
                                    vs = vt[:, ti, (2 * hp + hh) * 64:(2 * hp + hh + 1) * 64]
                                    k.op("pe", lambda e: e.matmul(p_st[bs, :], lhsT=KT[:, bs], rhs=vs, start=True, stop=True),
                                         reads=[tKT, Tvt], writes=[Tp_st])
                                if oi == 0:
                                    k.op("dve", lambda e: e.tensor_copy(out=S32[:], in_=p_st[:]), reads=[Tp_st], writes=[TS32])
                                else:
                                    k.op("dve", lambda e: e.scalar_tensor_tensor(out=S32[:], in0=S32[:], scalar=etot[:, ti:ti + 1], in1=p_st[:],
                                                                                op0=ALU.mult, op1=ALU.add), reads=[Tp_st, Tet, TS32], writes=[TS32])
                                k.op("pool", lambda e: e.tensor_copy(out=Sb[:], in_=S32[:]), reads=[TS32], writes=[TSb])
                            if pend:
                                k.flush(pend, per)

                    chains = [(0, 0), (0, 1), (1, 0), (1, 1)]
                    hg_prep(0, 0, sets[0])
                    for ci, (hp, d) in enumerate(chains):
                        if d == 0:
                            r0 = 512 + hp * 128
                            k.dma("sp", gg[:], uT_d[b, r0 + 1024:r0 + 1024 + 128, :], reads=[TuT[b]], writes=[Tgg])
                            k.op("act", lambda e: e.activation(out=gg[:], in_=gg[:], func=AF.Silu), reads=[Tgg], writes=[Tgg])
                        pend = []
                        if ci + 1 < len(chains):
                            k.defer = pend
                            hg_prep(chains[ci + 1][0], chains[ci + 1][1], sets[(ci + 1) % 2])
                            k.defer = None
                        hg_loop(hp, d, sets[ci % 2], pend)
                        k.flush(pend)
                        if d == 1:
                            for (t0, n) in BLOCKS:
                                k.op("act", lambda e: e.activation(out=sqb[:, 0:n], in_=OO[:, t0:t0 + n], func=AF.Square), reads=[TOO], writes=[Tsqb])
                                k.op("pe", lambda e: e.matmul(p_ss[:, 0:n], lhsT=blk[:], rhs=sqb[:, 0:n], start=True, stop=True), reads=[Tsqb, Tp3], writes=[Tp_ss])
                                k.op("act", lambda e: e.activation(out=rsd[:, 0:n], in_=p_ss[:, 0:n], func=AF.Sqrt, scale=1.0 / 64, bias=EPS), reads=[Tp_ss], writes=[Trsd])
                                k.op("dve", lambda e: e.reciprocal(out=rsd[:, 0:n], in_=rsd[:, 0:n]), reads=[Trsd], writes=[Trsd])
                                k.op("dve", lambda e: e.tensor_tensor(out=rsd[:, 0:n], in0=rsd[:, 0:n], in1=OO[:, t0:t0 + n], op=ALU.mult), reads=[Trsd, TOO], writes=[Trsd])
                                k.op("dve", lambda e: e.scalar_tensor_tensor(out=yo[:, t0:t0 + n], in0=rsd[:, 0:n], scalar=hnw[:, hp:hp + 1], in1=gg[:, t0:t0 + n],
                                                                            op0=ALU.mult, op1=ALU.mult), reads=[Trsd, Tgg, Tp3], writes=[Tyo])
                            k.dma("sp", yT_d[b, 256 + hp * 128:256 + (hp + 1) * 128, :], yo[:], reads=[Tyo], writes=[TyT[b]])
                if cfg.upto < 4:
                    continue
                with Stage(k):
                    Tp4 = T()
                    cw = k.sb("scw", [128, 4, 4]); cb = k.sb("scb", [128, 4])
                    aneg = k.sb("aneg", [128, 8]); dtb = k.sb("dtb", [128, 8]); dsk = k.sb("dsk", [128, 256]); snw = k.sb("snw", [128, 256])
                    triUf = k.sb("triUf", [128, 128]); triLf = k.sb("triLf", [128, 128]); strLf = k.sb("strLf", [128, 128]); strUf = k.sb("strUf", [128, 128])
                    for dst, src in ((cw, sd_cw_d[:, l]), (cb, sd_cb_d[:, l]), (aneg, sd_alog_d[:, l]), (dtb, sd_dtb_d[:, l]), (dsk, sd_dsk_d[:, l]),
                                     (snw, sd_nw_d[:, l]), (triUf, triUf_d), (triLf, triLf_d), (strLf, strLf_d), (strUf, strUf_d)):
                        k.dma("sp", dst[:], src, writes=[Tp4])
                    k.op("act", lambda e: e.activation(out=aneg[:], in_=aneg[:], func=AF.Exp), reads=[Tp4], writes=[Tp4])
                    k.op("dve", lambda e: e.tensor_scalar(out=aneg[:], in0=aneg[:], scalar1=-1.0, scalar2=None, op0=ALU.mult), reads=[Tp4], writes=[Tp4])
                    xp = k.sb("sxp", [128, S + 6]); Txp = T()
                    xc = k.sb("sxc", [128, S]); Txc = T()
                    fmb = k.sb("fmb", [128, 4, S], BF16); Tfmb = T()
                    segs = [(0, NCTX, 2), (NCTX, NLAT, NCTX + 5)]
                    for ch in range(4):
                        k.op("pool", lambda e: e.memset(xp[:], 0.0), writes=[Txp])
                        for (t0, n, o) in segs:
                            k.dma("sp", xp[:, o:o + n], uT_d[b, 2048 + ch * 128:2048 + (ch + 1) * 128, t0:t0 + n], reads=[TuT[b]], writes=[Txp])
                        for (t0, n, o) in segs:
                            k.op("dve", lambda e: e.tensor_scalar(out=xc[:, t0:t0 + n], in0=xp[:, o - 2:o - 2 + n], scalar1=cw[:, ch, 0:1],
                                                                  scalar2=cb[:, ch:ch + 1], op0=ALU.mult, op1=ALU.add), reads=[Txp, Tp4], writes=[Txc])
                            for j in range(1, 4):
                                k.op("dve", lambda e: e.scalar_tensor_tensor(out=xc[:, t0:t0 + n], in0=xp[:, o - 2 + j:o - 2 + j + n], scalar=cw[:, ch, j:j + 1],
                                                                            in1=xc[:, t0:t0 + n], op0=ALU.mult, op1=ALU.add), reads=[Txp, Tp4, Txc], writes=[Txc])
                        k.op("act", lambda e: e.activation(out=fmb[:, ch, :], in_=xc[:], func=AF.Silu), reads=[Txc], writes=[Tfmb])
                    xst = k.sb("xst", [128, NT, 256], BF16); Txst = T()
                    Bt = k.sb("Bt", [128, NT, 128], BF16); TBt = T()
                    ps_pre = PScope(k); ps_pre.__enter__()
                    p_tr = [k.ps("p4tr%d" % i, [128, 128], BF16) for i in range(2)]; Tp_tr = [PT(), PT()]
                    ntr = 0
                    for ti in range(NT):
                        ts_ = slice(ti * 128, (ti + 1) * 128)
                        for ch in range(3):
                            pp = p_tr[ntr % 2]; tp = Tp_tr[ntr % 2]
                            k.op("pe", lambda e: e.transpose(pp[:], fmb[:, ch, ts_], ident_b[:]), reads=[Tfmb, Tc], writes=[tp])
                            dst = xst[:, ti, ch * 128:(ch + 1) * 128] if ch < 2 else Bt[:, ti, :]
                            tdst = Txst if ch < 2 else TBt
                            if ntr % 2 == 0:
                                k.op("act", lambda e: e.activation(out=dst, in_=pp[:], func=AF.Copy), reads=[tp], writes=[tdst])
                            else:
                                k.op("dve", lambda e: e.tensor_copy(out=dst, in_=pp[:]), reads=[tp], writes=[tdst])
                            ntr += 1
                    dt = k.sb("dt", [128, NT, 8]); Tdt = T()
                    la = k.sb("la", [128, NT, 8]); Tla = T()
                    k.dma("sp", dt[:], ut_d[b, :, 512:520].rearrange("(n p) c -> p n c", p=128), reads=[Tut[b]], writes=[Tdt])
                    k.op("dve", lambda e: e.tensor_tensor(out=dt[:], in0=dt[:], in1=dtb[:, None, :].to_broadcast([128, NT, 8]), op=ALU.add), reads=[Tdt, Tp4], writes=[Tdt])
                    k.op("act", lambda e: e.activation(out=dt[:], in_=dt[:], func=AF.Exp), reads=[Tdt], writes=[Tdt])
                    k.op("act", lambda e: e.activation(out=dt[:], in_=dt[:], func=AF.Ln, bias=1.0), reads=[Tdt], writes=[Tdt])
                    k.op("dve", lambda e: e.tensor_tensor(out=la[:], in0=dt[:], in1=aneg[:, None, :].to_broadcast([128, NT, 8]), op=ALU.mult), reads=[Tdt, Tp4], writes=[Tla])
                    p_ct = k.ps("p_ct", [128, 2, NT, 8]); Tp_ct = PT()
                    for ti in range(NT):
                        k.op("pe", lambda e: e.matmul(p_ct[:, 0, ti, 0:4], lhsT=triUf[:], rhs=la[:, ti, 0:4], start=True, stop=True), reads=[Tla, Tp4], writes=[Tp_ct])
                        k.op("pe", lambda e: e.matmul(p_ct[:, 0, ti, 4:8], lhsT=triLf[:], rhs=la[:, ti, 4:8], start=True, stop=True), reads=[Tla, Tp4], writes=[Tp_ct])
                        k.op("pe", lambda e: e.matmul(p_ct[:, 1, ti, :], lhsT=ones_f[:], rhs=la[:, ti, :], start=True, stop=True), reads=[Tla, Tc], writes=[Tp_ct])
                    cexp = k.sb("cexp", [128, NT, 8]); etot = k.sb("etot4", [128, NT, 8]); wend = k.sb("wend", [128, NT, 8]); Tce = T()
                    k.op("dve", lambda e: e.tensor_tensor(out=wend[:], in0=p_ct[:, 1], in1=p_ct[:, 0], op=ALU.subtract), reads=[Tp_ct], writes=[Tce]) if False else None
                    k.op("act", lambda e: e.activation(out=cexp[:], in_=p_ct[:, 0], func=AF.Copy), reads=[Tp_ct], writes=[Tce])
                    k.op("dve", lambda e: e.tensor_tensor(out=wend[:], in0=p_ct[:, 1], in1=cexp[:], op=ALU.subtract), reads=[Tp_ct, Tce], writes=[Tce])
                    k.op("act", lambda e: e.activation(out=wend[:], in_=wend[:], func=AF.Exp), reads=[Tce], writes=[Tce])
                    k.op("dve", lambda e: e.tensor_tensor(out=wend[:], in0=wend[:], in1=dt[:], op=ALU.mult), reads=[Tce, Tdt], writes=[Tce])
                    k.op("act", lambda e: e.activation(out=cexp[:], in_=cexp[:], func=AF.Exp), reads=[Tce], writes=[Tce])
                    k.op("act", lambda e: e.activation(out=etot[:], in_=p_ct[:, 1], func=AF.Exp), reads=[Tp_ct], writes=[Tce])
                    ps_pre.__exit__(None, None, None)
                    ps_loop = PScope(k); ps_loop.__enter__()
                    Yacc = k.sb("Yacc", [128, NT, 256]); TY = T()
                    inc4 = [k.sb("inc4_%d" % d, [128, 4, 128]) for d in range(2)]
                    ngm = [k.sb("ngm%d" % d, [128, 4, 128]) for d in range(2)]
                    k.dma("sp", inc4[0][:], triUf4_d, writes=[Tp4]); k.dma("sp", inc4[1][:], triLf4_d, writes=[Tp4])
                    k.dma("sp", ngm[0][:], negmf_d, writes=[Tp4]); k.dma("sp", ngm[1][:], negmb_d, writes=[Tp4])
                    ngmb = [k.sb("ngmb%d" % d, [128, 4, 128], BF16) for d in range(2)]
                    for d in range(2):
                        k.op("act", lambda e: e.activation(out=ngmb[d][:], in_=ngm[d][:], func=AF.Copy), reads=[Tp4], writes=[Tp4])
                    etH = k.sb("etH", [128, NT, 2, 2]); TetH = T()
                    et4 = etot[:].rearrange("p n (d h) -> p n d h", d=2)
                    for g in range(2):
                        gs = slice(g * 64, (g + 1) * 64)
                        k.op("dve", lambda e: e.tensor_copy(out=etH[gs], in_=et4[gs, :, :, 2 * g:2 * g + 2]), reads=[Tce], writes=[TetH])
                    Rall = k.sb("Rall", [128, NT, 4, 128]); TRall = T()
                    Bwall = k.sb("Bwall", [128, NT, 2, 2, 64], BF16); TBwall = T()
                    Es = [k.sb("Es%d" % i, [128, 4, 128]) for i in range(2)]; TEs = [T(), T()]
                    Ab = [k.sb("Ab%d" % i, [128, 4, 128], BF16) for i in range(2)]; TAb = [T(), T()]
                    Bw = [k.sb("Bw%d" % i, [128, 2, 2, 64], BF16) for i in range(2)]; TBw = [T(), T()]
                    tmpy = [k.sb("tmpy%d" % i, [128, 4, 64]) for i in range(2)]; Ttmpy = [T(), T()]
                    S32 = k.sb("S32_4", [128, 2, 64]); TS32 = T()
                    STb = k.sb("STb4", [128, 2, 64], BF16); TSTb = T()
                    p_g = [k.ps("p_g%d" % i, [128, 128]) for i in range(2)]; Tp_g = [PT(), PT()]
                    p_seg = [k.ps("p_seg%d" % i, [128, 4, 128]) for i in range(2)]; Tp_seg = [PT(), PT()]
                    p_y1 = k.ps("p_y1", [128, 4, 64]); Tp_y1 = PT()
                    p_y2 = [k.ps("p_y2%d" % i, [128, 2, 64]) for i in range(2)]; Tp_y2 = [PT(), PT()]
                    p_st = k.ps("p_st4", [128, 2, 64]); Tp_st = PT()
                    Bt4 = Bt[:].rearrange("p n (g c) -> p n g c", g=2)
                    def ssd_front(d, oi, ti, i2):
                        ts_ = slice(ti * 128, (ti + 1) * 128)
                        d4 = slice(d * 4, d * 4 + 4)
                        strm = strLf if d == 0 else strUf
                        for g in range(2):
                            gs = slice(g * 64, (g + 1) * 64)
                            k.op("pe", lambda e: e.matmul(p_g[g][:], lhsT=fmb[gs, 2, ts_], rhs=fmb[gs, 3, ts_], start=True, stop=True),
                                 reads=[Tfmb], writes=[Tp_g[g]])
                        if oi == 0:
                            for t2 in range(NT):
                                eng_ = "pool" if t2 % 2 == 0 else "dve"
                                k.op(eng_, lambda e: e.tensor_tensor(out=Rall[:, t2], in0=inc4[d][:], in1=la[:, t2, d4, None].to_broadcast([128, 4, 128]), op=ALU.mult),
                                     reads=[Tla, Tp4], writes=[TRall])
                            k.op("pool", lambda e: e.tensor_tensor(out=Bwall[:], in0=Bt4[:, :, :, None, :].to_broadcast([128, NT, 2, 2, 64]),
                                                                   in1=wend[:, :, d4].rearrange("p n (g j) -> p n g j", g=2)[:, :, :, :, None].to_broadcast([128, NT, 2, 2, 64]),
                                                                   op=ALU.mult), reads=[TBt, Tce], writes=[TBwall])
                        k.op("pe", lambda e: e.matmul(p_seg[i2][:].rearrange("p h t -> p (h t)"), lhsT=strm[:], rhs=Rall[:, ti].rearrange("p h t -> p (h t)"),
                                                      start=True, stop=False), reads=[TRall, Tp4], writes=[Tp_seg[i2]])
                        k.op("pe", lambda e: e.matmul(p_seg[i2][:].rearrange("p h t -> p (h t)"), lhsT=ident_b[:], rhs=ngmb[d][:].rearrange("p h t -> p (h t)"),
                                                      start=False, stop=True), reads=[Tc, Tp4], writes=[Tp_seg[i2]])
                        k.op("act", lambda e: e.activation(out=Es[i2][:], in_=p_seg[i2][:], func=AF.Exp), reads=[Tp_seg[i2]], writes=[TEs[i2]])
                        k.op("dve", lambda e: e.tensor_tensor(out=Es[i2][:], in0=Es[i2][:], in1=dt[:, ti, d4, None].to_broadcast([128, 4, 128]), op=ALU.mult),
                             reads=[TEs[i2], Tdt], writes=[TEs[i2]])
                        for g in range(2):
                            k.op("dve", lambda e: e.tensor_tensor(out=Ab[i2][:, 2 * g:2 * g + 2, :], in0=Es[i2][:, 2 * g:2 * g + 2, :],
                                                                  in1=p_g[g][:, None, :].to_broadcast([128, 2, 128]), op=ALU.mult),
                                 reads=[TEs[i2], Tp_g[g]], writes=[TAb[i2]])

                    def ssd_back(d, oi, ti, i2):
                        ts_ = slice(ti * 128, (ti + 1) * 128)
                        for h in range(4):
                            k.op("pe", lambda e: e.matmul(p_y1[:, h, :], lhsT=Ab[i2][:, h, :], rhs=xst[:, ti, h * 64:(h + 1) * 64], start=True, stop=True),
                                 reads=[TAb[i2], Txst], writes=[Tp_y1])
                        yacc = Yacc[:, ti, :].rearrange("p (h c) -> p h c", h=4)
                        if oi > 0:
                            for h in range(4):
                                g = h // 2; gs = slice(g * 64, (g + 1) * 64)
                                k.op("pe", lambda e: e.matmul(p_y2[g][:, h % 2, :], lhsT=fmb[gs, 3, ts_], rhs=STb[gs, h % 2, :], start=True, stop=True),
                                     reads=[Tfmb, TSTb], writes=[Tp_y2[g]])
                            for g in range(2):
                                k.op("dve", lambda e: e.tensor_tensor(out=tmpy[i2][:, 2 * g:2 * g + 2, :], in0=p_y2[g][:],
                                                                      in1=cexp[:, ti, d * 4 + 2 * g:d * 4 + 2 * g + 2, None].to_broadcast([128, 2, 64]), op=ALU.mult),
                                     reads=[Tp_y2[g], Tce], writes=[Ttmpy[i2]])
                            if d == 0:
                                k.op("dve", lambda e: e.tensor_tensor(out=yacc, in0=p_y1[:], in1=tmpy[i2][:], op=ALU.add), reads=[Tp_y1, Ttmpy[i2]], writes=[TY])
                            else:
                                k.op("pool", lambda e: e.tensor_tensor(out=yacc, in0=yacc, in1=tmpy[i2][:], op=ALU.add), reads=[TY, Ttmpy[i2]], writes=[TY])
                                k.op("dve", lambda e: e.tensor_tensor(out=yacc, in0=yacc, in1=p_y1[:], op=ALU.add), reads=[TY, Tp_y1], writes=[TY])
                        else:
                            if d == 0:
                                k.op("dve", lambda e: e.tensor_copy(out=yacc, in_=p_y1[:]), reads=[Tp_y1], writes=[TY])
                            else:
                                k.op("dve", lambda e: e.tensor_tensor(out=yacc, in0=yacc, in1=p_y1[:], op=ALU.add), reads=[TY, Tp_y1], writes=[TY])
                        if oi == NT - 1:
                            return
                        for h in range(4):
                            g = h // 2; gs = slice(g * 64, (g + 1) * 64)
                            k.op("pe", lambda e: e.matmul(p_st[gs, h % 2, :], lhsT=Bwall[:, ti, g, h % 2, :], rhs=xst[:, ti, h * 64:(h + 1) * 64], start=True, stop=True,
                                                          tile_position=(0, g * 64)), reads=[TBwall, Txst], writes=[Tp_st])
                        if oi == 0:
                            k.op("dve", lambda e: e.tensor_copy(out=S32[:], in_=p_st[:]), reads=[Tp_st], writes=[TS32])
                        else:
                            k.op("pool", lambda e: e.tensor_tensor(out=S32[:], in0=S32[:], in1=etH[:, ti, d, :, None].to_broadcast([128, 2, 64]), op=ALU.mult),
                                 reads=[TS32, TetH], writes=[TS32])
                            k.op("dve", lambda e: e.tensor_tensor(out=S32[:], in0=S32[:], in1=p_st[:], op=ALU.add), reads=[TS32, Tp_st], writes=[TS32])
                        k.op("pool", lambda e: e.tensor_copy(out=STb[:], in_=S32[:]), reads=[TS32], writes=[TSTb])

                    seq = []
                    for d in range(2):
                        order = list(range(NT)) if d == 0 else [1, 0] + list(range(NT - 1, 1, -1))
                        for oi, ti in enumerate(order):
                            seq.append((d, oi, ti, len(seq) % 2))
                    ssd_front(*seq[0])
                    for i_ in range(len(seq)):
                        if i_ + 1 < len(seq):
                            ssd_front(*seq[i_ + 1])
                        ssd_back(*seq[i_])
                    zz = k.sb("zz", [128, NT, 256]); Tzz = T()
                    k.dma("sp", zz[:], ut_d[b, :, 256:512].rearrange("(n p) c -> p n c", p=128), reads=[Tut[b]], writes=[Tzz])
                    k.op("act", lambda e: e.activation(out=zz[:], in_=zz[:], func=AF.Silu), reads=[Tzz], writes=[Tzz])
                    tq = k.sb("tq", [128, NT, 256]); Ttq = T()
                    k.op("dve", lambda e: e.tensor_tensor(out=tq[:], in0=xst[:], in1=dsk[:, None, :].to_broadcast([128, NT, 256]), op=ALU.mult), reads=[Txst, Tp4], writes=[Ttq])
                    k.op("dve", lambda e: e.tensor_tensor(out=Yacc[:], in0=Yacc[:], in1=tq[:], op=ALU.add), reads=[TY, Ttq], writes=[TY])
                    k.op("dve", lambda e: e.tensor_tensor(out=Yacc[:], in0=Yacc[:], in1=zz[:], op=ALU.mult), reads=[TY, Tzz], writes=[TY])
                    k.op("pool", lambda e: e.tensor_tensor(out=tq[:], in0=Yacc[:], in1=Yacc[:], op=ALU.mult), reads=[TY], writes=[Ttq])
                    ssq = k.sb("ssq", [128, NT]); Tssq = T()
                    k.op("dve", lambda e: e.reduce_sum(out=ssq[:], in_=tq[:], axis=AX.X), reads=[Ttq], writes=[Tssq])
                    k.op("act", lambda e: e.activation(out=ssq[:], in_=ssq[:], func=AF.Sqrt, scale=1.0 / 256, bias=EPS), reads=[Tssq], writes=[Tssq])
                    k.op("dve", lambda e: e.reciprocal(out=ssq[:], in_=ssq[:]), reads=[Tssq], writes=[Tssq])
                    k.op("dve", lambda e: e.tensor_tensor(out=Yacc[:], in0=Yacc[:], in1=ssq[:, :, None].to_broadcast([128, NT, 256]), op=ALU.mult), reads=[TY, Tssq], writes=[TY])
                    yob = k.sb("yob", [128, NT, 256], BF16); Tyob = T()
                    k.op("dve", lambda e: e.tensor_tensor(out=yob[:], in0=Yacc[:], in1=snw[:, None, :].to_broadcast([128, NT, 256]), op=ALU.mult), reads=[TY, Tp4], writes=[Tyob])
                    ps_loop.__exit__(None, None, None)
                    ps_epi = PScope(k); ps_epi.__enter__()
                    p_tr = [k.ps("p4tre%d" % i, [128, 128], BF16) for i in range(2)]; Tp_tr = [PT(), PT()]
                    yoT = k.sb("yoT", [128, 2, S], BF16); TyoT = T()
                    for ti in range(NT):
                        for ch in range(2):
                            pp = p_tr[ntr % 2]; tp = Tp_tr[ntr % 2]
                            k.op("pe", lambda e: e.transpose(pp[:], yob[:, ti, ch * 128:(ch + 1) * 128], ident_b[:]), reads=[Tyob, Tc], writes=[tp])
                            if ntr % 2 == 0:
                                k.op("act", lambda e: e.activation(out=yoT[:, ch, ti * 128:(ti + 1) * 128], in_=pp[:], func=AF.Copy), reads=[tp], writes=[TyoT])
                            else:
                                k.op("dve", lambda e: e.tensor_copy(out=yoT[:, ch, ti * 128:(ti + 1) * 128], in_=pp[:]), reads=[tp], writes=[TyoT])
                            ntr += 1
                    k.dma("sp", yT_d[b, 512:768, :].rearrange("(c p) t -> p c t", p=128), yoT[:], reads=[TyoT], writes=[TyT[b]])
                    ps_epi.__exit__(None, None, None)
                if cfg.upto < 5:
                    continue
                need_ctx = l < L - 1
                with Stage(k):
                    Tp5 = T()
                    qan = k.sb("qan", [128, 192]); kvan = k.sb("kvan", [128, 128]); qnr = k.sb("qnr", [128, 96]); knr = k.sb("knr", [128, 96])
                    wq = k.sb("wq", [96, 2, 384], BF16); wkv = k.sb("wkv", [128, 512], BF16)
                    rope = k.sb("rope", [128, 16, 2, 16]); invn3 = k.sb("invn3", [128, 3]); invn8 = k.sb("invn8", [128, 8])
                    for dst, src in ((qan, ml_qan_d[:, l]), (kvan, ml_kvan_d[:, l]), (qnr, ml_qn_d[:, l]), (knr, ml_kn_d[:, l]), (rope, rope_d),
                                     (invn3, invn3_d), (invn8, invn8_d)):
                        k.dma("sp", dst[:], src, writes=[Tp5])
                    wqs = k.sb("wqs", [96, 2, 384]); wkvs = k.sb("wkvs", [128, 512]); Twqs = T()
                    k.dma("sp", wqs[:], ml_wq_d[l].rearrange("(c p) n -> p c n", p=96), writes=[Twqs])
                    k.dma("sp", wkvs[:], ml_wkv_d[l], writes=[Twqs])
                    k.op("pool", lambda e: e.tensor_copy(out=wq[:], in_=wqs[:]), reads=[Twqs], writes=[Tp5])
                    k.op("pool", lambda e: e.tensor_copy(out=wkv[:], in_=wkvs[:]), reads=[Twqs], writes=[Tp5])
                    QT = k.sb("QT", [96, 4, S], BF16); TQT = T()
                    KT = k.sb("KT", [96, 4, S], BF16); TKT = T()
                    Va = k.sb("Va", [128, NT, 4, 65], BF16); TVa = T()
                    k.op("pool", lambda e: e.memset(Va[:], 1.0), writes=[TVa])
                    um = [k.sb("um%d" % i, [128, 352]) for i in range(2)]; Tum = [T(), T()]
                    ps_prep = PScope(k); ps_prep.__enter__()
                    def dbl(name, shape, dt_=F32):
                        return [k.sb("%s_%d" % (name, i), shape, dt_) for i in range(2)], [T(), T()]
                    sqL, TsqL = dbl("sq5", [128, 512]); ss3L, Tss3L = dbl("ss3", [128, 3]); ss8L, Tss8L = dbl("ss8", [128, 12])
                    cnL, TcnL = dbl("cn", [128, 320], BF16); cTtL, TcTL = dbl("cTt", [128, 3, 128], BF16)
                    qfL, TqfL = dbl("qf", [128, 4, 96]); kvfL, TkvfL = dbl("kvf", [128, 4, 128])
                    rbL, TrbL = dbl("rb", [128, 5, 32]); raL, TraL = dbl("ra", [128, 4, 5, 16])
                    QbL, TQbL = dbl("Qb", [128, 4, 96], BF16); KbL, TKbL = dbl("Kb", [128, 4, 96], BF16)
                    p_trA = [k.ps("p5trA%d" % i, [128, 4, 128], BF16) for i in range(2)]; Tp_trA = [PT(), PT()]
                    p_trB = [k.ps("p5trB%d" % i, [128, 4, 128], BF16) for i in range(2)]; Tp_trB = [PT(), PT()]
                    p_qL = [k.ps("p_q%d" % i, [128, 384]) for i in range(2)]; Tp_qL = [PT(), PT()]
                    p_kvL = [k.ps("p_kv%d" % i, [128, 512]) for i in range(2)]; Tp_kvL = [PT(), PT()]
                    for ti in range(NT):
                        U = um[ti % 2]; tU = Tum[ti % 2]
                        j2 = ti % 2
                        sq = sqL[j2]; Tsq = TsqL[j2]; ss3 = ss3L[j2]; Tss3 = Tss3L[j2]; ss8 = ss8L[j2]; Tss8 = Tss8L[j2]
                        cn = cnL[j2]; Tcn = TcnL[j2]; cTt = cTtL[j2]; TcT = TcTL[j2]; qf = qfL[j2]; Tqf = TqfL[j2]; kvf = kvfL[j2]; Tkvf = TkvfL[j2]
                        rb = rbL[j2]; Trb = TrbL[j2]; ra = raL[j2]; Tra = TraL[j2]; Qb = QbL[j2]; TQb = TQbL[j2]; Kb = KbL[j2]; TKb = TKbL[j2]
                        p_q = p_qL[j2]; Tp_q = Tp_qL[j2]; p_kv = p_kvL[j2]; Tp_kv = Tp_kvL[j2]
                        p_tr = [p_trB[0], p_trB[1]]; Tp_tr = [Tp_trB[0], Tp_trB[1]]
                        k.dma("sp", U[:], ut_d[b, ti * 128:(ti + 1) * 128, 520:872], reads=[Tut[b]], writes=[tU])
                        k.op("pool", lambda e: e.tensor_tensor(out=sq[:, 0:352], in0=U[:], in1=U[:], op=ALU.mult), reads=[tU], writes=[Tsq])
                        for j, (a_, b_) in enumerate(((0, 192), (192, 320), (320, 352))):
                            k.op("dve", lambda e: e.reduce_sum(out=ss3[:, j:j + 1], in_=sq[:, a_:b_], axis=AX.X), reads=[Tsq], writes=[Tss3])
                        k.op("dve", lambda e: e.tensor_tensor(out=ss3[:], in0=ss3[:], in1=invn3[:], op=ALU.mult), reads=[Tss3, Tp5], writes=[Tss3])
                        k.op("act", lambda e: e.activation(out=ss3[:], in_=ss3[:], func=AF.Sqrt, bias=EPS), reads=[Tss3], writes=[Tss3])
                        k.op("dve", lambda e: e.reciprocal(out=ss3[:], in_=ss3[:]), reads=[Tss3], writes=[Tss3])
                        k.op("dve", lambda e: e.scalar_tensor_tensor(out=cn[:, 0:192], in0=U[:, 0:192], scalar=ss3[:, 0:1], in1=qan[:], op0=ALU.mult, op1=ALU.mult),
                             reads=[tU, Tss3, Tp5], writes=[Tcn])
                        k.op("dve", lambda e: e.scalar_tensor_tensor(out=cn[:, 192:320], in0=U[:, 192:320], scalar=ss3[:, 1:2], in1=kvan[:], op0=ALU.mult, op1=ALU.mult),
                             reads=[tU, Tss3, Tp5], writes=[Tcn])
                        k.op("dve", lambda e: e.scalar_tensor_tensor(out=rb[:, 4, :], in0=U[:, 320:352], scalar=ss3[:, 2:3], in1=knr[:, 64:96], op0=ALU.mult, op1=ALU.mult),
                             reads=[tU, Tss3, Tp5], writes=[Trb])
                        pt = p_trA[j2]; tpt = Tp_trA[j2]
                        k.op("pe", lambda e: e.transpose(pt[0:96, 0, :], cn[:, 0:96], ident_b[:]), reads=[Tcn, Tc], writes=[tpt])
                        k.op("pe", lambda e: e.transpose(pt[0:96, 1, :], cn[:, 96:192], ident_b[:]), reads=[Tcn, Tc], writes=[tpt])
                        k.op("pe", lambda e: e.transpose(pt[:, 2, :], cn[:, 192:320], ident_b[:]), reads=[Tcn, Tc], writes=[tpt])
                        k.op("act", lambda e: e.activation(out=cTt[0:96, 0:2, :], in_=pt[0:96, 0:2, :], func=AF.Copy), reads=[tpt], writes=[TcT])
                        k.op("act", lambda e: e.activation(out=cTt[:, 2, :], in_=pt[:, 2, :], func=AF.Copy), reads=[tpt], writes=[TcT])
                        for c_ in range(2):
                            k.op("pe", lambda e: e.matmul(p_q[:], lhsT=cTt[0:96, c_, :], rhs=wq[:, c_, :], start=(c_ == 0), stop=(c_ == 1)), reads=[TcT, Tp5], writes=[Tp_q])
                        k.op("pe", lambda e: e.matmul(p_kv[:], lhsT=cTt[:, 2, :], rhs=wkv[:], start=True, stop=True), reads=[TcT, Tp5], writes=[Tp_kv])
                        k.op("act", lambda e: e.activation(out=qf[:].rearrange("p h c -> p (h c)"), in_=p_q[:], func=AF.Copy), reads=[Tp_q], writes=[Tqf])
                        k.op("dve", lambda e: e.tensor_copy(out=kvf[:].rearrange("p h c -> p (h c)"), in_=p_kv[:]), reads=[Tp_kv], writes=[Tkvf])
                        sq4 = sq[:, 0:384].rearrange("p (h c) -> p h c", c=96)
                        k.op("pool", lambda e: e.tensor_tensor(out=sq4, in0=qf[:], in1=qf[:], op=ALU.mult), reads=[Tqf], writes=[Tsq])
                        k.op("dve", lambda e: e.reduce_sum(out=ss8[:, 0:4], in_=sq4[:, :, 0:64], axis=AX.X), reads=[Tsq], writes=[Tss8])
                        k.op("dve", lambda e: e.reduce_sum(out=ss8[:, 4:8], in_=sq4[:, :, 64:96], axis=AX.X), reads=[Tsq], writes=[Tss8])
                        sq5 = sq[:, 0:512].rearrange("p (h c) -> p h c", c=128)
                        k.op("pool", lambda e: e.tensor_tensor(out=sq5, in0=kvf[:], in1=kvf[:], op=ALU.mult), reads=[Tkvf, Tss8], writes=[Tsq])
                        k.op("dve", lambda e: e.reduce_sum(out=ss8[:, 8:12], in_=sq5[:, :, 0:64], axis=AX.X), reads=[Tsq], writes=[Tss8])
                        k.op("dve", lambda e: e.tensor_tensor(out=ss8[:, 0:8], in0=ss8[:, 0:8], in1=invn8[:], op=ALU.mult), reads=[Tss8, Tp5], writes=[Tss8])
                        k.op("dve", lambda e: e.tensor_tensor(out=ss8[:, 8:12], in0=ss8[:, 8:12], in1=invn8[:, 0:4], op=ALU.mult), reads=[Tss8, Tp5], writes=[Tss8])
                        k.op("act", lambda e: e.activation(out=ss8[:], in_=ss8[:], func=AF.Sqrt, bias=EPS), reads=[Tss8], writes=[Tss8])
                        k.op("dve", lambda e: e.reciprocal(out=ss8[:], in_=ss8[:]), reads=[Tss8], writes=[Tss8])
                        k.op("dve", lambda e: e.tensor_tensor(out=qf[:, :, 0:64], in0=qf[:, :, 0:64], in1=ss8[:, 0:4, None].to_broadcast([128, 4, 64]), op=ALU.mult),
                             reads=[Tqf, Tss8], writes=[Tqf])
                        k.op("dve", lambda e: e.tensor_tensor(out=Qb[:, :, 0:64], in0=qf[:, :, 0:64], in1=qnr[:, None, 0:64].to_broadcast([128, 4, 64]), op=ALU.mult),
                             reads=[Tqf, Tp5], writes=[TQb])
                        k.op("dve", lambda e: e.tensor_tensor(out=qf[:, :, 64:96], in0=qf[:, :, 64:96], in1=ss8[:, 4:8, None].to_broadcast([128, 4, 32]), op=ALU.mult),
                             reads=[Tqf, Tss8], writes=[Tqf])
                        k.op("dve", lambda e: e.tensor_tensor(out=rb[:, 0:4, :], in0=qf[:, :, 64:96], in1=qnr[:, None, 64:96].to_broadcast([128, 4, 32]), op=ALU.mult),
                             reads=[Tqf, Tp5], writes=[Trb])
                        k.op("dve", lambda e: e.tensor_tensor(out=kvf[:, :, 0:64], in0=kvf[:, :, 0:64], in1=ss8[:, 8:12, None].to_broadcast([128, 4, 64]), op=ALU.mult),
                             reads=[Tkvf, Tss8], writes=[Tkvf])
                        k.op("dve", lambda e: e.tensor_tensor(out=Kb[:, :, 0:64], in0=kvf[:, :, 0:64], in1=knr[:, None, 0:64].to_broadcast([128, 4, 64]), op=ALU.mult),
                             reads=[Tkvf, Tp5], writes=[TKb])
                        k.op("pool", lambda e: e.tensor_copy(out=Va[:, ti, :, 0:64], in_=kvf[:, :, 64:128]), reads=[Tkvf], writes=[TVa])
                        if ti >= 2:
                            rb4 = rb[:].rearrange("p h (j two) -> p h j two", two=2)
                            cs = rope[:, ti - 2, 0, None, :].to_broadcast([128, 5, 16]); sn = rope[:, ti - 2, 1, None, :].to_broadcast([128, 5, 16])
                            k.op("dve", lambda e: e.tensor_tensor(out=ra[:, 0], in0=rb4[:, :, :, 0], in1=cs, op=ALU.mult), reads=[Trb, Tp5], writes=[Tra])
                            k.op("dve", lambda e: e.tensor_tensor(out=ra[:, 1], in0=rb4[:, :, :, 1], in1=sn, op=ALU.mult), reads=[Trb, Tp5], writes=[Tra])
                            k.op("pool", lambda e: e.tensor_tensor(out=ra[:, 2], in0=rb4[:, :, :, 0], in1=sn, op=ALU.mult), reads=[Trb, Tp5], writes=[Tra])
                            k.op("pool", lambda e: e.tensor_tensor(out=ra[:, 3], in0=rb4[:, :, :, 1], in1=cs, op=ALU.mult), reads=[Trb, Tp5], writes=[Tra])
                            k.op("dve", lambda e: e.tensor_tensor(out=rb4[:, :, :, 0], in0=ra[:, 0], in1=ra[:, 1], op=ALU.subtract), reads=[Tra, Trb], writes=[Trb])
                            k.op("dve", lambda e: e.tensor_tensor(out=rb4[:, :, :, 1], in0=ra[:, 2], in1=ra[:, 3], op=ALU.add), reads=[Tra, Trb], writes=[Trb])
                        k.op("dve", lambda e: e.tensor_copy(out=Qb[:, :, 64:96], in_=rb[:, 0:4, :]), reads=[Trb], writes=[TQb])
                        k.op("dve", lambda e: e.tensor_copy(out=Kb[:, :, 64:96], in_=rb[:, 4:5, :].to_broadcast([128, 4, 32])), reads=[Trb], writes=[TKb])
                        for (src, tsrc, dstT, tdst, pi) in ((Qb, TQb, QT, TQT, 1), (Kb, TKb, KT, TKT, 0)):
                            pt = p_tr[pi]; tpt = Tp_tr[pi]
                            for h in range(4):
                                k.op("pe", lambda e: e.transpose(pt[0:96, h, :], src[:, h, :], ident_b[:]), reads=[tsrc, Tc], writes=[tpt])
                            if pi == 1:
                                k.op("act", lambda e: e.activation(out=dstT[:, :, ti * 128:(ti + 1) * 128], in_=pt[0:96, :, :], func=AF.Copy), reads=[tpt], writes=[tdst])
                            else:
                                k.op("dve", lambda e: e.tensor_copy(out=dstT[:, :, ti * 128:(ti + 1) * 128], in_=pt[0:96, :, :]), reads=[tpt], writes=[tdst])
                    ps_prep.__exit__(None, None, None)
                    ps_att = PScope(k); ps_att.__enter__()
                    p_tr = [k.ps("p5trC%d" % i, [128, 4, 128], BF16) for i in range(2)]; Tp_tr = [PT(), PT()]
                    PTt = [k.sb("PT%d" % i, [128, 512], BF16) for i in range(3)]; TPT = [T() for _ in range(3)]
                    p_s = [k.ps("p_s%d" % i, [128, 512]) for i in range(2)]; Tp_s = [PT(), PT()]
                    p_o = [k.ps("p_o%d" % i, [128, 4, 65]) for i in range(2)]; Tp_o = [PT(), PT()]
                    rec = k.sb("rec", [128, 4]); Trec = T()
                    ym = k.sb("ym", [128, NT, 256], BF16); Tym = T()
                    sc = 96.0 ** -0.5
                    nsc = 0; nh = 0
                    qblocks = BLOCKS if need_ctx else BLOCKS[1:]
                    its = []
                    for (q0, qn_) in qblocks:
                        keys = list(range(0, 2) if q0 == 0 else range(NT))
                        for h in range(4):
                            for ki, kt_ in enumerate(keys):
                                its.append((q0, qn_, h, ki, kt_, len(keys), len(its)))

                    def att_qk(q0, qn_, h, ki, kt_, nk, j):
                        ps_ = p_s[j % 2]; tps = Tp_s[j % 2]; P = PTt[j % 3]; tP = TPT[j % 3]
                        k.op("pe", lambda e: e.matmul(ps_[:, 0:qn_], lhsT=KT[:, h, kt_ * 128:(kt_ + 1) * 128], rhs=QT[:, h, q0:q0 + qn_], start=True, stop=True),
                             reads=[TKT, TQT], writes=[tps])
                        k.op("act", lambda e: e.activation(out=P[:, 0:qn_], in_=ps_[:, 0:qn_], func=AF.Exp, scale=sc), reads=[tps], writes=[tP])

                    def att_pv(q0, qn_, h, ki, kt_, nk, j):
                        P = PTt[j % 3]; tP = TPT[j % 3]
                        grp = j // 1
                        nq = qn_ // 128
                        gidx = (q0, h)
                        if ki == 0:
                            att_state["nh"] += 1
                        po = p_o[att_state["nh"] % 2]; tpo = Tp_o[att_state["nh"] % 2]
                        for qs_ in range(nq):
                            k.op("pe", lambda e: e.matmul(po[:, qs_, :], lhsT=P[:, qs_ * 128:(qs_ + 1) * 128], rhs=Va[:, kt_, h, :],
                                                          start=(ki == 0 and qs_ == 0), stop=(ki == nk - 1), skip_group_check=True),
                                 reads=[tP, TVa], writes=[tpo])
                        if ki == nk - 1:
                            k.op("dve", lambda e: e.reciprocal(out=rec[:, 0:nq], in_=po[:, 0:nq, 64]), reads=[tpo], writes=[Trec])
                            t_0 = q0 // 128
                            k.op("dve", lambda e: e.tensor_tensor(out=ym[:, t_0:t_0 + nq, h * 64:(h + 1) * 64], in0=po[:, 0:nq, 0:64],
                                                                  in1=rec[:, 0:nq, None].to_broadcast([128, nq, 64]), op=ALU.mult), reads=[tpo, Trec], writes=[Tym])

                    att_state = {"nh": 0}
                    att_qk(*its[0])
                    for j in range(len(its)):
                        if j + 1 < len(its):
                            att_qk(*its[j + 1])
                        att_pv(*its[j])
                    yoT = k.sb("yoT5", [128, 2, S], BF16); TyoT = T()
                    if not need_ctx:
                        k.op("pool", lambda e: e.memset(yoT[:, :, 0:NCTX], 0.0), writes=[TyoT])
                    ntr = 0
                    for ti in range(0 if need_ctx else 2, NT):
                        for ch in range(2):
                            pp = p_tr[ntr % 2]; tp = Tp_tr[ntr % 2]
                            k.op("pe", lambda e: e.transpose(pp[:, 0, :], ym[:, ti, ch * 128:(ch + 1) * 128], ident_b[:]), reads=[Tym, Tc], writes=[tp])
                            if ntr % 2 == 0:
                                k.op("act", lambda e: e.activation(out=yoT[:, ch, ti * 128:(ti + 1) * 128], in_=pp[:, 0, :], func=AF.Copy), reads=[tp], writes=[TyoT])
                            else:
                                k.op("dve", lambda e: e.tensor_copy(out=yoT[:, ch, ti * 128:(ti + 1) * 128], in_=pp[:, 0, :]), reads=[tp], writes=[TyoT])
                            ntr += 1
                    k.dma("sp", yT_d[b, 768:1024, :].rearrange("(c p) t -> p c t", p=128), yoT[:], reads=[TyoT], writes=[TyT[b]])
                    ps_att.__exit__(None, None, None)
                if cfg.upto < 6:
                    continue
                need_ctx = l < L - 1
                with Stage(k):
                    Tp6 = T()
                    wo = k.sb("wo", [128, 8, D], BF16); wrt = k.sb("wrt", [128, 8, NEXP], BF16)
                    cst = Caster(k, 128, D)
                    for kc in range(8):
                        cst.load(wo[:, kc, :], w_out_d[l, kc * 128:(kc + 1) * 128, :], 128, D, [Tp6])
                    wrs = k.sb("wrs", [128, 8, NEXP]); Twrs = T()
                    k.dma("sp", wrs[:], w_rt_d[l].rearrange("(c p) e -> p c e", p=128), writes=[Twrs])
                    k.op("pool", lambda e: e.tensor_copy(out=wrt[:], in_=wrs[:]), reads=[Twrs], writes=[Tp6])
                    G2 = k.sb("G2", [128, 2, 8]); Tg2 = T()
                    for i, mi in enumerate((b, 2)):
                        k.op("dve", lambda e: e.scalar_tensor_tensor(out=G2[:, i, :], in0=modT[:, l, 32:40, mi], scalar=1.0, in1=n2T[:, l, :],
                                                                    op0=ALU.add, op1=ALU.mult), reads=[Tmod, Tc], writes=[Tg2])
                    Yb = [k.sb("Yb%d" % i, [128, 8, 512], BF16) for i in range(2)]; TYb = [T(), T()]
                    Xb = [k.sb("Xb%d" % i, [128, 8, 512]) for i in range(2)]; TXb = [T(), T()]
                    Qs = k.sb("Qs", [128, 8, 512], BF16); TQs = T()
                    Rr6 = k.sb("Rr6", [128, 512]); TRr6 = T()
                    tm6 = [k.sb("tm6_%d" % i, [128, 512]) for i in range(2)]; Ttm6 = [T(), T()]
                    H2 = k.sb("H2", [128, 8, 512], BF16); TH2 = T()
                    h2o = [k.sb("h2o%d" % i, [128, D], BF16) for i in range(2)]; Th2o = [T(), T()]
                    Ee = k.sb("Ee", [16, 512]); TEe = T()
                    rc6 = k.sb("rc6", [16, 512]); Trc6 = T()
                    pwo = [k.ps("pwo%d" % i, [128, 512]) for i in range(2)]; Tpwo = [PT(), PT()]
                    pss = k.ps("pss6", [128, 512]); Tpss = PT()
                    ptr = [k.ps("ptr6_%d" % i, [128, 8, 128], BF16) for i in range(2)]; Tptr = [PT(), PT()]
                    prl = k.ps("prl", [16, 512]); Tprl = PT()
                    prs = k.ps("prs", [16, 512]); Tprs = PT()
                    ntr_box = [0]
                    blks6 = BLOCKS if need_ctx else BLOCKS[1:]

                    def s6_A(bi):
                        t0, n = blks6[bi]
                        isctx = (t0 == 0)
                        seg = 1 if isctx else 0
                        mi = 2 if isctx else b
                        Y = Yb[bi % 2]; tY = TYb[bi % 2]; X = Xb[bi % 2]; tX = TXb[bi % 2]
                        k.dma("sp", Y[:, :, 0:n], yT_d[b, :, t0:t0 + n].rearrange("(c p) t -> p c t", p=128), reads=[TyT[b]], writes=[tY])
                        k.dma("sp", X[:, :, 0:n], xT_d[b, :, t0:t0 + n].rearrange("(c p) t -> p c t", p=128), reads=[Tx[b]], writes=[tX])
                        for dch in range(8):
                            pp = pwo[dch % 2]; tp = Tpwo[dch % 2]
                            for c_ in range(8):
                                k.op("pe", lambda e: e.matmul(pp[:, 0:n], lhsT=wo[:, c_, dch * 128:(dch + 1) * 128], rhs=Y[:, c_, 0:n], start=(c_ == 0), stop=(c_ == 7)),
                                     reads=[Tp6, tY], writes=[tp])
                            k.op("dve", lambda e: e.scalar_tensor_tensor(out=X[:, dch, 0:n], in0=pp[:, 0:n], scalar=modT[:, l, 16 + dch, mi:mi + 1], in1=X[:, dch, 0:n],
                                                                        op0=ALU.mult, op1=ALU.add), reads=[tp, Tmod, tX], writes=[tX])
                        k.dma("sp", xT_d[b, :, t0:t0 + n].rearrange("(c p) t -> p c t", p=128), X[:, :, 0:n], reads=[tX], writes=[Tx[b]])

                    def s6_B(bi):
                        t0, n = blks6[bi]
                        isctx = (t0 == 0)
                        seg = 1 if isctx else 0
                        mi = 2 if isctx else b
                        X = Xb[bi % 2]; tX = TXb[bi % 2]
                        ntr = ntr_box[0]
                        k.op("act", lambda e: e.activation(out=Qs[:, :, 0:n], in_=X[:, :, 0:n], func=AF.Square), reads=[tX], writes=[TQs])
                        for kc in range(8):
                            k.op("pe", lambda e: e.matmul(pss[:, 0:n], lhsT=ones_b[:], rhs=Qs[:, kc, 0:n], start=(kc == 0), stop=(kc == 7)), reads=[TQs, Tc], writes=[Tpss])
                        k.op("act", lambda e: e.activation(out=Rr6[:, 0:n], in_=pss[:, 0:n], func=AF.Sqrt, scale=1.0 / D, bias=EPS), reads=[Tpss], writes=[TRr6])
                        k.op("dve", lambda e: e.reciprocal(out=Rr6[:, 0:n], in_=Rr6[:, 0:n]), reads=[TRr6], writes=[TRr6])
                        for kc in range(8):
                            tm = tm6[kc % 2]; ttm = Ttm6[kc % 2]
                            k.op("dve", lambda e: e.tensor_tensor(out=tm[:, 0:n], in0=X[:, kc, 0:n], in1=Rr6[:, 0:n], op=ALU.mult), reads=[tX, TRr6], writes=[ttm])
                            k.op("act", lambda e: e.activation(out=H2[:, kc, 0:n], in_=tm[:, 0:n], func=AF.Identity, scale=G2[:, seg, kc:kc + 1],
                                                               bias=modT[:, l, 24 + kc, mi:mi + 1]), reads=[ttm, Tg2, Tmod], writes=[TH2])
                        for tt in range(n // 128):
                            pp = ptr[ntr % 2]; tp = Tptr[ntr % 2]; ho = h2o[ntr % 2]; tho = Th2o[ntr % 2]
                            for kc in range(8):
                                k.op("pe", lambda e: e.transpose(pp[:, kc, :], H2[:, kc, tt * 128:(tt + 1) * 128], ident_b[:]), reads=[TH2, Tc], writes=[tp])
                            if ntr % 2 == 0:
                                k.op("act", lambda e: e.activation(out=ho[:], in_=pp[:].rearrange("p c t -> p (c t)"), func=AF.Copy), reads=[tp], writes=[tho])
                            else:
                                k.op("dve", lambda e: e.tensor_copy(out=ho[:], in_=pp[:].rearrange("p c t -> p (c t)")), reads=[tp], writes=[tho])
                            k.dma("sp", h2t_d[b, t0 + tt * 128:t0 + (tt + 1) * 128, :], ho[:], reads=[tho], writes=[Th2[b]])
                            ntr += 1
                        for kc in range(8):
                            k.op("pe", lambda e: e.matmul(prl[:, 0:n], lhsT=wrt[:, kc, :], rhs=H2[:, kc, 0:n], start=(kc == 0), stop=(kc == 7)), reads=[Tp6, TH2], writes=[Tprl])
                        k.op("act", lambda e: e.activation(out=Ee[:, 0:n], in_=prl[:, 0:n], func=AF.Exp), reads=[Tprl], writes=[TEe])
                        k.op("pe", lambda e: e.matmul(prs[:, 0:n], lhsT=ones_f[0:16, 0:16], rhs=Ee[:, 0:n], start=True, stop=True), reads=[TEe, Tc], writes=[Tprs])
                        k.op("dve", lambda e: e.reciprocal(out=rc6[:, 0:n], in_=prs[:, 0:n]), reads=[Tprs], writes=[Trc6])
                        k.op("dve", lambda e: e.tensor_tensor(out=rc6[:, 0:n], in0=rc6[:, 0:n], in1=Ee[:, 0:n], op=ALU.mult), reads=[Trc6, TEe], writes=[Trc6])
                        k.dma("sp", aff_d[b, :, t0:t0 + n], rc6[:, 0:n], reads=[Trc6], writes=[Taff[b]])
                        ntr_box[0] = ntr

                    s6_A(0)
                    for bi in range(len(blks6)):
                        if bi + 1 < len(blks6):
                            s6_A(bi + 1)
                        s6_B(bi)
                if cfg.upto < 7:
                    continue
                need_ctx = l < L - 1
                last = (l == cfg.layers - 1)
                segl = [(NCTX, NLAT, 256)] + ([(0, NCTX, 32)] if need_ctx else [])
                if l in getattr(cfg, 'skip_moe', ()) or b in getattr(cfg, 'skip_moe_b', ()):
                    segl = []
                segl = segl[:getattr(cfg, 'max_seg', 2)]
                for (t0, N, cap) in segl:
                  isctx = (t0 == 0)
                  mi = 2 if isctx else b
                  ntile = N // 128; nst = max(1, cap // 128); sp = min(cap, 128)
                  with Stage(k):
                    Tp7 = T()
                    iotaf = k.sb("iotaf", [128, 256]); iotap = k.sb("iotap", [128, 2])
                    for dst, src in ((iotaf, iotaf_d), (iotap, iotap_d)):
                        k.dma("sp", dst[:], src, writes=[Tp7])
                    slotm = k.sb("slotm", [16, N]); Tslot = T()
                    slotT = k.sb("slotT", [128, ntile, 16]); TslotT = T()
                    ghlT = k.sb("ghlT", [128, ntile, 16, 2], BF16); TghlT = T()
                    ysb = k.sb("ysb", [128, NEXP, nst, D], BF16); Tysb = T()
                    with Stage(k):
                        ones16 = k.sb("ones16", [16, NLAT])
                        k.dma("sp", ones16[:], ones16_d, writes=[Tp7])
                        affs = k.sb("affs", [16, N]); Taffs = T()
                        work = k.sb("work", [16, N]); Twork = T()
                        m8 = k.sb("m8", [16, 8]); Tm8 = T()
                        mask = k.sb("mask", [16, N]); Tmask = T()
                        ghi = k.sb("ghi", [16, N], BF16); glo = k.sb("glo", [16, N], BF16); Tg = T()
                        k.dma("sp", affs[:], aff_d[b, :, t0:t0 + N], reads=[Taff[b]], writes=[Taffs])
                        k.op("act", lambda e: e.activation(out=work[:], in_=affs[:], func=AF.Copy), reads=[Taffs], writes=[Twork])
                        for it_ in range(cap // 8):
                            k.op("dve", lambda e: e.max(out=m8[:], in_=work[:]), reads=[Twork], writes=[Tm8])
                            k.op("dve", lambda e: e.match_replace(out=work[:], in_to_replace=m8[:], in_values=work[:], imm_value=-1.0), reads=[Tm8, Twork], writes=[Twork])
                        k.op("dve", lambda e: e.tensor_scalar(out=mask[:], in0=work[:], scalar1=0.0, scalar2=None, op0=ALU.is_lt), reads=[Twork], writes=[Tmask])
                        k.op("dve", lambda e: e.tensor_tensor_scan(out=slotm[:], data0=ones16[:, 0:N], data1=mask[:], initial=0.0, op0=ALU.mult, op1=ALU.add),
                             reads=[Tmask, Tp7], writes=[Tslot])
                        k.op("dve", lambda e: e.tensor_tensor(out=slotm[:], in0=slotm[:], in1=mask[:], op=ALU.mult), reads=[Tslot, Tmask], writes=[Tslot])
                        k.op("dve", lambda e: e.tensor_scalar(out=slotm[:], in0=slotm[:], scalar1=-1.0, scalar2=None, op0=ALU.add), reads=[Tslot], writes=[Tslot])
                        k.op("dve", lambda e: e.tensor_tensor(out=affs[:], in0=affs[:], in1=mask[:], op=ALU.mult), reads=[Taffs, Tmask], writes=[Taffs])
                        k.op("dve", lambda e: e.tensor_copy(out=ghi[:], in_=affs[:]), reads=[Taffs], writes=[Tg])
                        k.op("dve", lambda e: e.tensor_tensor(out=affs[:], in0=affs[:], in1=ghi[:], op=ALU.subtract), reads=[Taffs, Tg], writes=[Taffs])
                        k.op("dve", lambda e: e.tensor_copy(out=glo[:], in_=affs[:]), reads=[Taffs], writes=[Tg])
                        pst = k.ps("pst", [128, ntile, 16]); Tpst = PT()
                        pgh = k.ps("pgh", [128, ntile, 2, 16], BF16); Tpgh = PT()
                        for ti in range(ntile):
                            cs_ = slice(ti * 128, (ti + 1) * 128)
                            k.op("pe", lambda e: e.transpose(pst[:, ti, :], slotm[:, cs_], ident_f[0:16, 0:16]), reads=[Tslot, Tc], writes=[Tpst])
                            k.op("pe", lambda e: e.transpose(pgh[:, ti, 0, :], ghi[:, cs_], ident_b[0:16, 0:16]), reads=[Tg, Tc], writes=[Tpgh])
                            k.op("pe", lambda e: e.transpose(pgh[:, ti, 1, :], glo[:, cs_], ident_b[0:16, 0:16]), reads=[Tg, Tc], writes=[Tpgh])
                        k.op("dve", lambda e: e.tensor_copy(out=slotT[:], in_=pst[:]), reads=[Tpst], writes=[TslotT])
                        k.op("dve", lambda e: e.tensor_copy(out=ghlT[:].rearrange("p n e h -> p n h e"), in_=pgh[:]), reads=[Tpgh], writes=[TghlT])
                    with Stage(k):
                        h2k = k.sb("h2k", [128, ntile, D], BF16); Th2k = T()
                        k.dma("sp", h2k[:], h2t_d[b, t0:t0 + N, :].rearrange("(n p) d -> p n d", p=128), reads=[Th2[b]], writes=[Th2k])
                        wg = [k.sb("wg%d" % i, [128, 8, FF], BF16) for i in range(2)]
                        wu = [k.sb("wu%d" % i, [128, 8, FF], BF16) for i in range(2)]
                        wd = [k.sb("wd%d" % i, [128, 4, D], BF16) for i in range(2)]
                        Tw = [T(), T()]
                        Sel = [k.sb("Sel%d" % i, [128, ntile, cap], BF16) for i in range(1)] * 2; TSel = [T()] * 2
                        cst7 = Caster(k, 128, 2048, nbuf=4)
                        xsTL = [k.sb("xsT%d" % i, [128, 8, cap], BF16) for i in range(1)] * 2; TxsTL = [T()] * 2
                        sg = [k.sb("sg%d" % i, [128, cap]) for i in range(2)]; Tsg = [T(), T()]
                        actTL = [k.sb("actT%d" % i, [128, 4, cap], BF16) for i in range(1)] * 2; TactTL = [T()] * 2
                        gs2L = [k.sb("gs2_%d" % i, [128, nst, 2]) for i in range(2)]; gsL = [k.sb("gs_%d" % i, [128, nst]) for i in range(2)]; TgsL = [T(), T()]
                        pg = [k.ps("pg7_%d" % i, [128, cap]) for i in range(3)]; Tpg = [PT() for _ in range(3)]
                        pG = k.ps("pG", [128, cap]); TpG = PT()
                        pU = k.ps("pU", [128, cap]); TpU = PT()
                        pY = [k.ps("pY%d" % i, [128, 512]) for i in range(2)]; TpY = [PT(), PT()]
                        pgs = k.ps("pgs", [128, nst, 2]); Tpgs = PT()
                        nev = 0
                        for ex in range(NEXP):
                            i2 = ex % 2
                            xsT = xsTL[i2]; TxsT = TxsTL[i2]; actT = actTL[i2]; TactT = TactTL[i2]
                            gs2 = gs2L[i2]; gs = gsL[i2]; Tgs = TgsL[i2]
                            for (wt_, src_, c_) in ((wg[i2], w_gate_d[l, ex], 8), (wu[i2], w_up_d[l, ex], 8), (wd[i2], w_down_d[l, ex], 4)):
                                s3 = src_.rearrange("(c p) n -> p c n", p=128)
                                hc_ = c_ // 2
                                for pc_ in range(2):
                                    cst7.load(wt_[:, pc_ * hc_:(pc_ + 1) * hc_, :], s3[:, pc_ * hc_:(pc_ + 1) * hc_, :], 128, 2048, [Tw[i2]], split=2048 // hc_)
                            SL = Sel[i2]; tSL = TSel[i2]
                            for ti in range(ntile):
                                k.op("dve", lambda e: e.tensor_scalar(out=SL[:, ti, :], in0=iotaf[:, 0:cap], scalar1=slotT[:, ti, ex:ex + 1], scalar2=None, op0=ALU.is_equal),
                                     reads=[TslotT, Tp7], writes=[tSL])
                            for dc in range(8):
                                pp = pg[dc % 3]; tp = Tpg[dc % 3]
                                for ti in range(ntile):
                                    k.op("pe", lambda e: e.matmul(pp[:], lhsT=h2k[:, ti, dc * 128:(dc + 1) * 128], rhs=SL[:, ti, :], start=(ti == 0), stop=(ti == ntile - 1)),
                                         reads=[Th2k, tSL], writes=[tp])
                                if dc % 2 == 0:
                                    k.op("act", lambda e: e.activation(out=xsT[:, dc, :], in_=pp[:], func=AF.Copy), reads=[tp], writes=[TxsT])
                                else:
                                    k.op("dve", lambda e: e.tensor_copy(out=xsT[:, dc, :], in_=pp[:]), reads=[tp], writes=[TxsT])
                            for st in range(nst):
                                for ti in range(ntile):
                                    k.op("pe", lambda e: e.matmul(pgs[0:sp, st, :], lhsT=SL[:, ti, st * 128:st * 128 + sp], rhs=ghlT[:, ti, ex, :], start=(ti == 0), stop=(ti == ntile - 1)),
                                         reads=[tSL, TghlT], writes=[Tpgs])
                            k.op("act", lambda e: e.activation(out=gs2[0:sp], in_=pgs[0:sp], func=AF.Copy), reads=[Tpgs], writes=[Tgs])
                            k.op("dve", lambda e: e.tensor_tensor(out=gs[0:sp], in0=gs2[0:sp, :, 0], in1=gs2[0:sp, :, 1], op=ALU.add), reads=[Tgs], writes=[Tgs])
                            for fc in range(4):
                                for kc in range(8):
                                    k.op("pe", lambda e: e.matmul(pG[:], lhsT=wg[i2][:, kc, fc * 128:(fc + 1) * 128], rhs=xsT[:, kc, :], start=(kc == 0), stop=(kc == 7)),
                                         reads=[Tw[i2], TxsT], writes=[TpG])
                                for kc in range(8):
                                    k.op("pe", lambda e: e.matmul(pU[:], lhsT=wu[i2][:, kc, fc * 128:(fc + 1) * 128], rhs=xsT[:, kc, :], start=(kc == 0), stop=(kc == 7)),
                                         reads=[Tw[i2], TxsT], writes=[TpU])
                                k.op("act", lambda e: e.activation(out=sg[fc % 2][:], in_=pG[:], func=AF.Silu), reads=[TpG], writes=[Tsg[fc % 2]])
                                k.op("dve", lambda e: e.tensor_tensor(out=actT[:, fc, :], in0=sg[fc % 2][:], in1=pU[:], op=ALU.mult), reads=[Tsg[fc % 2], TpU], writes=[TactT])
                            for st in range(nst):
                                for dh in range(2):
                                    pp = pY[nev % 2]; tp = TpY[nev % 2]
                                    for fc in range(4):
                                        k.op("pe", lambda e: e.matmul(pp[0:sp, :], lhsT=actT[:, fc, st * 128:st * 128 + sp], rhs=wd[i2][:, fc, dh * 512:(dh + 1) * 512],
                                                                      start=(fc == 0), stop=(fc == 3)), reads=[TactT, Tw[i2]], writes=[tp])
                                    dsto = ysb[0:sp, ex, st, dh * 512:(dh + 1) * 512]
                                    if nev % 2 == 0:
                                        k.op("act", lambda e: e.activation(out=dsto, in_=pp[0:sp, :], func=AF.Copy, scale=gs[0:sp, st:st + 1]), reads=[tp, Tgs], writes=[Tysb])
                                    else:
                                        k.op("dve", lambda e: e.tensor_scalar(out=dsto, in0=pp[0:sp, :], scalar1=gs[0:sp, st:st + 1], scalar2=None, op0=ALU.mult),
                                             reads=[tp, Tgs], writes=[Tysb])
                                    nev += 1
                    with Stage(k):
                        oneh = k.sb("oneh", [16, 16, 128]); Toneh = T()
                        k.dma("sp", oneh[:], oneh_d, writes=[Toneh])
                        SelTL = [k.sb("SelT%d" % i, [128, NEXP, nst, 512], BF16) for i in range(2)]; TSelTL = [T(), T()]
                        Xc = [k.sb("Xc%d" % i, [128, 8, 512]) for i in range(2)]; TXc = [T(), T()]
                        ot = [k.sb("ot%d" % i, [128, D]) for i in range(2)]; Tot = [T(), T()]
                        pb = [k.ps("pb%d" % i, [128, 512]) for i in range(2)]; Tpb = [PT(), PT()]
                        pc = [k.ps("pc%d" % i, [128, 512]) for i in range(4)]; Tpc = [PT() for _ in range(4)]
                        po_ = [k.ps("po7_%d" % i, [128, 4, 128]) for i in range(2)]; Tpo_ = [PT(), PT()]
                        nto = 0
                        nblk = max(1, N // 512)
                        def cmb_build(tb):
                            n = min(N, 512)
                            c0 = tb * 512
                            X = Xc[tb % 2]; tX = TXc[tb % 2]
                            SelT = SelTL[tb % 2]; TSelT = TSelTL[tb % 2]
                            k.dma("sp", X[:, :, 0:n], xT_d[b, :, t0 + c0:t0 + c0 + n].rearrange("(c p) t -> p c t", p=128), reads=[Tx[b]], writes=[tX])
                            for ex in range(NEXP):
                                pp = pb[ex % 2]; tp = Tpb[ex % 2]
                                k.op("pe", lambda e: e.matmul(pp[:, 0:n], lhsT=oneh[:, ex, :], rhs=slotm[:, c0:c0 + n], start=True, stop=True), reads=[Tslot, Toneh], writes=[tp])
                                for st in range(nst):
                                    k.op("dve", lambda e: e.tensor_scalar(out=SelT[:, ex, st, 0:n], in0=pp[:, 0:n], scalar1=iotap[:, st:st + 1], scalar2=None, op0=ALU.is_equal),
                                         reads=[tp, Tp7], writes=[TSelT])

                        cmb_build(0)
                        for tb in range(nblk):
                            n = min(N, 512)
                            c0 = tb * 512
                            X = Xc[tb % 2]; tX = TXc[tb % 2]
                            SelT = SelTL[tb % 2]; TSelT = TSelTL[tb % 2]
                            if tb + 1 < nblk:
                                cmb_build(tb + 1)
                            for dh in range(2):
                                for dcl in range(4):
                                    dc = dh * 4 + dcl
                                    pp = pc[dcl]; tp = Tpc[dcl]
                                    for ex in range(NEXP):
                                        for st in range(nst):
                                            k.op("pe", lambda e: e.matmul(pp[:, 0:n], lhsT=ysb[0:sp, ex, st, dc * 128:(dc + 1) * 128], rhs=SelT[0:sp, ex, st, 0:n],
                                                                          start=(ex == 0 and st == 0), stop=(ex == NEXP - 1 and st == nst - 1)),
                                                 reads=[Tysb, TSelT], writes=[tp])
                                    k.op("dve", lambda e: e.scalar_tensor_tensor(out=X[:, dc, 0:n], in0=pp[:, 0:n], scalar=modT[:, l, 40 + dc, mi:mi + 1], in1=X[:, dc, 0:n],
                                                                                op0=ALU.mult, op1=ALU.add), reads=[tp, Tmod, tX], writes=[tX])
                            if not last:
                                k.dma("sp", xT_d[b, :, t0 + c0:t0 + c0 + n].rearrange("(c p) t -> p c t", p=128), X[:, :, 0:n], reads=[tX], writes=[Tx[b]])
                            elif not isctx:
                                for tt in range(n // 128):
                                    O = ot[nto % 2]; tO = Tot[nto % 2]
                                    for half in range(2):
                                        pp = po_[half]; tp = Tpo_[half]
                                        for q in range(4):
                                            dc = half * 4 + q
                                            k.op("pe", lambda e: e.transpose(pp[:, q, :], X[:, dc, tt * 128:(tt + 1) * 128], ident_f[:]), reads=[tX, Tc], writes=[tp])
                                        if half == 0:
                                            k.op("act", lambda e: e.activation(out=O[:, 0:512], in_=pp[:].rearrange("p q t -> p (q t)"), func=AF.Copy), reads=[tp], writes=[tO])
                                        else:
                                            k.op("dve", lambda e: e.tensor_copy(out=O[:, 512:1024], in_=pp[:].rearrange("p q t -> p (q t)")), reads=[tp], writes=[tO])
                                    tok0 = c0 + tt * 128
                                    k.dma("sp", out_d[b, tok0:tok0 + 128, :], O[:], reads=[tO], writes=[Tout])
                                    nto += 1

        k.barrier()
        global LAST_K
        LAST_K = k
    return nc


def prep_inputs(inp, core, nb=2):
    b0 = core * 2
    m = {}
    m["x"] = np.ascontiguousarray(inp["x"][b0:b0 + nb])
    m["ctx"] = np.ascontiguousarray(inp["ctx"][b0:b0 + nb])
    cc = np.stack([inp["c"][b0], inp["c"][b0 + 1], inp["c_ctx"]], axis=0)
    m["cT"] = np.ascontiguousarray(fm(cc, 8).transpose(0, 2, 1))
    m["ada_w"] = inp["ada_w"]
    m["ada_bT"] = fm(inp["ada_b"], 48)
    m["n1T"] = fm(inp["norm1_w"], 8)
    m["n2T"] = fm(inp["norm2_w"], 8)
    m["w_in"] = inp["w_in"]
    m.update(prep_lru(inp))
    m["hg_lbT"] = fm(inp["hgrn_lb_logits"], 2)
    m["hg_nwT"] = fm(inp["hgrn_norm_w"], 2)
    scw = np.asarray(inp["ssd_conv_w"], np.float32)
    m["sd_cw"] = np.ascontiguousarray(scw.reshape(L, 4, 4, 128).transpose(3, 0, 2, 1))
    m["sd_cb"] = fm(inp["ssd_conv_b"], 4)
    rep = lambda v: np.ascontiguousarray(np.broadcast_to(np.asarray(v, np.float32)[None], (128,) + tuple(np.shape(v))))
    m["sd_alog"] = rep(np.asarray(inp["ssd_a_log"]).reshape(L, 8))
    m["sd_dtb"] = rep(np.asarray(inp["ssd_dt_bias"]).reshape(L, 8))
    m["sd_dsk"] = rep(np.repeat(np.asarray(inp["ssd_d_skip"]), 64, axis=-1))
    m["sd_nw"] = rep(inp["ssd_norm_w"])
    m["ml_qan"] = rep(inp["mla_q_a_norm"]); m["ml_kvan"] = rep(inp["mla_kv_a_norm"])
    m["ml_qn"] = rep(inp["mla_q_norm"]); m["ml_kn"] = rep(inp["mla_k_norm"])
    m["ml_wq"] = inp["mla_w_q_up"]; m["ml_wkv"] = inp["mla_w_kv_up"]
    m["w_out"] = inp["w_out"]; m["w_rt"] = inp["moe_router"]
    m["w_gate"] = inp["moe_w_gate"]; m["w_up"] = inp["moe_w_up"]; m["w_down"] = inp["moe_w_down"]
    m.update(host_consts())
    return m


def kernel(**inputs):
    inp = {k_: np.asarray(v) for k_, v in inputs.items()}
    cfg = Cfg(nb=2)
    nc = build(cfg)
    in_maps = [prep_inputs(inp, c) for c in range(8)]
    res = run_bass_kernel_spmd(nc, in_maps, core_ids=list(range(8)))
    out = np.concatenate([r["out"] for r in res.results], axis=0)
    return out.astype(np.float32)
```

```python
import math
from contextlib import ExitStack
import numpy as np
import ml_dtypes
import concourse.bass as bass
import concourse.mybir as mybir
from concourse.bass_utils import run_bass_kernel_spmd

F32 = mybir.dt.float32
BF16 = mybir.dt.bfloat16
I32 = mybir.dt.int32
U32 = mybir.dt.uint32
ALU = mybir.AluOpType
AF = mybir.ActivationFunctionType
AX = mybir.AxisListType

L = 2
D = 1024
NLAT = 2048
NCTX = 256
S = NCTX + NLAT
NT = S // 128
IN_COLS = 2920
EPS = 1e-6
NEXP = 16
FF = 512
TM_RANGES = [(1280, 1536), (1792, 2048), (2560, 2920)]
TM_COLS = sum(b - a for a, b in TM_RANGES)
FM_CHUNKS = list(range(0, 10)) + [12, 13] + [16, 17, 18, 19]
BLOCKS = [(0, 256)] + [(256 + 512 * i, 512) for i in range(4)]


class T:
    __slots__ = ("name", "w", "r", "x")

    def __init__(self, name="", x=False):
        self.name = name
        self.w = None
        self.r = []
        self.x = x


def PT():
    return T("psum", True)


class _Rec:
    def __init__(self):
        self.call = None

    def __getattr__(self, name):
        def f(*a, **kw):
            self.call = (name, a, kw)
            return self
        return f


class K:
    N_DMA_SEMS = 24

    def __init__(self, nc, stack):
        self.nc = nc
        self.stack = stack
        self.eng = {"pe": nc.tensor, "dve": nc.vector, "act": nc.scalar,
                    "pool": nc.gpsimd, "sp": nc.sync}
        self.sems = {}
        self.count = {}
        self.seen = {e: {} for e in self.eng}
        for e in self.eng:
            self.sems[e] = stack.enter_context(nc.semaphore("s_" + e))
            self.count[e] = 0
        for i in range(self.N_DMA_SEMS):
            k = "d%d" % i
            self.sems[k] = stack.enter_context(nc.semaphore("s_" + k))
            self.count[k] = 0
        self.dma_rr = 0
        self.n_inst = 0
        self.scope = stack

    def sb(self, name, shape, dtype=F32):
        self.n_alloc = getattr(self, "n_alloc", 0) + 1
        return self.scope.enter_context(self.nc.sbuf_tensor("sb%d_%s" % (self.n_alloc, name), list(shape), dtype))

    def ps(self, name, shape, dtype=F32):
        self.n_alloc = getattr(self, "n_alloc", 0) + 1
        nel = 512 if dtype == F32 else 1024
        scope = getattr(self, "pscope", None) or self.scope
        full = scope.enter_context(self.nc.psum_tensor("ps%d_%s" % (self.n_alloc, name), [128, nel], dtype))
        n = 1
        for d_ in shape[1:]:
            n *= d_
        assert n <= nel, (name, shape)
        v = full[0:shape[0], 0:n]
        if len(shape) == 3:
            v = v.rearrange("p (a b) -> p a b", b=shape[2])
        elif len(shape) == 4:
            v = v.rearrange("p (a b c) -> p a b c", b=shape[2], c=shape[3])
        return v

    def _waits(self, e, reads, writes):
        need = {}
        for t in reads:
            if t.w is not None:
                k, v, pe = t.w
                if not (pe == "pe" and e == "pe"):
                    need[k] = max(need.get(k, 0), v)
        for t in writes:
            if t.w is not None:
                k, v, pe = t.w
                if not (pe == "pe" and e == "pe"):
                    need[k] = max(need.get(k, 0), v)
            for (k, v, pe) in t.r:
                if pe == "pe" and e == "pe":
                    continue
                need[k] = max(need.get(k, 0), v)
        seen = self.seen[e]
        h = self.eng[e]
        for k, v in need.items():
            if seen.get(k, 0) < v:
                h.wait_ge(self.sems[k], v)
                seen[k] = v

    def _commit(self, tok, reads, writes):
        for t in writes:
            t.w = tok
            t.r = []
        for t in reads:
            if t not in writes:
                t.r.append(tok)
                if len(t.r) > 16:
                    best = {}
                    for (k, v, pe) in t.r:
                        if k not in best or best[k][1] < v:
                            best[k] = (k, v, pe)
                    t.r = list(best.values())

    def flush(self, pend, n=None):
        n = len(pend) if n is None else min(n, len(pend))
        for _ in range(n):
            it = pend.pop(0)
            if it[0] == "op":
                _, e, (name, a, kw), reads, writes = it
                self.op(e, lambda eng: getattr(eng, name)(*a, **kw), reads, writes)
            else:
                _, e, out, in_, reads, writes, kw = it
                self.dma(e, out, in_, reads, writes, **kw)

    def op(self, e, fn, reads=(), writes=()):
        reads = list(reads)
        writes = list(writes)
        if getattr(self, "defer", None) is not None:
            rec = _Rec()
            fn(rec)
            self.defer.append(("op", e, rec.call, reads, writes))
            return None
        xr = [t for t in reads if t.x]
        if xr:
            reads = [t for t in reads if not t.x]
            writes = writes + [t for t in xr if t not in writes]
        self._waits(e, reads, writes)
        ins = fn(self.eng[e])
        self.count[e] += 1
        ins.then_inc(self.sems[e], 1)
        self._commit((e, self.count[e], e), reads, writes)
        self.n_inst += 1
        return ins

    def dma(self, e, out, in_, reads=(), writes=(), **kw):
        reads = list(reads)
        writes = list(writes)
        if getattr(self, "defer", None) is not None:
            self.defer.append(("dma", e, out, in_, reads, writes, kw))
            return None
        self._waits(e, reads, writes)
        k = "d%d" % self.dma_rr
        self.dma_rr = (self.dma_rr + 1) % self.N_DMA_SEMS
        ins = self.eng[e].dma_start(out=out, in_=in_, **kw)
        self.count[k] += 16
        ins.then_inc(self.sems[k], 16)
        self._commit((k, self.count[k], "dma"), reads, writes)
        self.n_inst += 1
        return ins

    def barrier(self):
        for e, h in self.eng.items():
            seen = self.seen[e]
            for k, v in self.count.items():
                if v > 0 and seen.get(k, 0) < v and k != e:
                    h.wait_ge(self.sems[k], v)
                    seen[k] = v


class PScope:
    def __init__(self, k):
        self.k = k

    def __enter__(self):
        self.prev = getattr(self.k, "pscope", None)
        self.st = ExitStack()
        self.st.__enter__()
        self.k.pscope = self.st
        return self

    def __exit__(self, *a):
        self.k.barrier()
        self.k.pscope = self.prev
        return self.st.__exit__(*a)


class Caster:
    def __init__(self, k, npart, nfree, nbuf=2, eng="act"):
        self.k = k
        self.eng = eng
        self.st = [k.sb("stg%d" % i, [npart, nfree]) for i in range(nbuf)]
        self.T = [T() for _ in range(nbuf)]
        self.i = 0

    def load(self, dst, src, npart, nfree, writes, reads=(), split=None):
        k = self.k
        j = self.i % len(self.st)
        self.i += 1
        st = self.st[j][0:npart, 0:nfree]
        if split is not None:
            st = st.rearrange("p (a b) -> p a b", b=split)
        if getattr(self, "alt", False) and self.i % 2 == 0:
            k.dma("sp", st, src, reads=list(reads), writes=[self.T[j]])
            k.op("dve", lambda e: e.tensor_copy(out=dst, in_=st), reads=[self.T[j]], writes=list(writes))
            return
        k.dma("sp", st, src, reads=list(reads), writes=[self.T[j]])
        if self.eng == "act":
            k.op("act", lambda e: e.activation(out=dst, in_=st, func=AF.Copy), reads=[self.T[j]], writes=list(writes))
        else:
            k.op(self.eng, lambda e: e.tensor_copy(out=dst, in_=st), reads=[self.T[j]], writes=list(writes))


class Stage:
    def __init__(self, k):
        self.k = k

    def __enter__(self):
        self.prev = self.k.scope
        self.st = ExitStack()
        self.st.__enter__()
        self.k.scope = self.st
        return self

    def __exit__(self, *a):
        self.k.barrier()
        self.k.scope = self.prev
        return self.st.__exit__(*a)


def fm(v, nch):
    v = np.asarray(v, np.float32)
    lead = v.shape[:-1]
    r = v.reshape(lead + (nch, 128))
    r = np.moveaxis(r, -1, 0)
    return np.ascontiguousarray(r)


def host_consts():
    c = {}
    c["ident_f"] = np.eye(128, dtype=np.float32)
    c["ident_b"] = np.eye(128, dtype=np.float32).astype(ml_dtypes.bfloat16)
    c["ones_b"] = np.ones((128, 128), np.float32).astype(ml_dtypes.bfloat16)
    c["ones_f"] = np.ones((128, 128), np.float32)
    t = np.arange(S)
    c["mfwd"] = np.ascontiguousarray(np.broadcast_to((t % 128 != 0).astype(np.float32), (128, S)))
    c["mbwd"] = np.ascontiguousarray(np.broadcast_to((t % 128 != 127).astype(np.float32), (128, S)))
    i = np.arange(128)
    c["triU"] = (i[:, None] <= i[None, :]).astype(np.uint32)
    c["triL"] = (i[:, None] >= i[None, :]).astype(np.uint32)
    c["triUf"] = (i[:, None] <= i[None, :]).astype(np.float32)
    c["triLf"] = (i[:, None] >= i[None, :]).astype(np.float32)
    c["strLf"] = (i[:, None] > i[None, :]).astype(np.float32)
    c["strUf"] = (i[:, None] < i[None, :]).astype(np.float32)
    tt = np.arange(NLAT)
    inv = 10000.0 ** (-np.arange(0, 16, 2, dtype=np.float32) / 16)
    ang = np.concatenate([(tt // 64).astype(np.float32)[:, None] * inv, (tt % 64).astype(np.float32)[:, None] * inv], axis=-1).astype(np.float32)
    cs = np.stack([np.cos(ang), np.sin(ang)], axis=1).astype(np.float32)
    c["rope"] = np.ascontiguousarray(cs.reshape(16, 128, 2, 16).transpose(1, 0, 2, 3))
    c["invn3"] = np.ascontiguousarray(np.broadcast_to(np.array([1 / 192, 1 / 128, 1 / 32], np.float32), (128, 3)))
    c["invn8"] = np.ascontiguousarray(np.broadcast_to(np.array([1 / 64] * 4 + [1 / 32] * 4, np.float32), (128, 8)))
    c["iotaf"] = np.ascontiguousarray(np.broadcast_to(np.arange(256, dtype=np.float32), (128, 256)))
    c["iotap"] = np.stack([i.astype(np.float32), i.astype(np.float32) + 128], axis=1)
    oh = np.zeros((16, 16, 128), np.float32)
    for e_ in range(16):
        oh[e_, e_, :] = 1.0
    c["oneh"] = oh
    c["ones16"] = np.ones((16, NLAT), np.float32)
    rep4 = lambda a_: np.ascontiguousarray(np.broadcast_to(a_[:, None, :], (128, 4, 128))).astype(np.float32)
    c["triUf4"] = rep4(c["triUf"]); c["triLf4"] = rep4(c["triLf"])
    c["negmf"] = rep4(-1.0e4 * c["strLf"]); c["negmb"] = rep4(-1.0e4 * c["strUf"])
    c["blk64"] = (i[:, None] // 64 == i[None, :] // 64).astype(np.float32).astype(ml_dtypes.bfloat16)
    return c


def prep_lru(inp):
    m = {}
    cw = np.asarray(inp["lru_conv_w"], np.float32)
    m["lru_cw"] = np.ascontiguousarray(cw.reshape(L, 4, 2, 128).transpose(3, 0, 2, 1))
    m["lru_cb"] = fm(inp["lru_conv_b"], 2)
    for nm, key in (("lru_wr", "lru_w_r"), ("lru_wi", "lru_w_i")):
        w = np.asarray(inp[key], np.float32)
        o = np.zeros((128, L, 2, 2, 128), np.float32)
        for ch in range(2):
            for hh in range(2):
                o[hh * 64:(hh + 1) * 64, :, :, ch, hh * 64:(hh + 1) * 64] = w[:, :, 2 * ch + hh].transpose(2, 0, 1, 3)
        m[nm] = o
    m["lru_br"] = fm(inp["lru_b_r"], 2)
    m["lru_bi"] = fm(inp["lru_b_i"], 2)
    m["lru_lam"] = fm(inp["lru_lam"], 2)
    return m


class Cfg:
    def __init__(self, nb=2, upto=99, debug=False, layers=2):
        self.layers = layers
        self.nb = nb
        self.upto = upto
        self.debug = debug


def build(cfg):
    nc = bass.Bass("TRN2", target_bir_lowering=False)
    NB = cfg.nb
    dbg_kind = "ExternalOutput" if cfg.debug else "Internal"

    def din(name, shape, dt=F32):
        return nc.dram_tensor(name, list(shape), dt, kind="ExternalInput").ap()

    def dscr(name, shape, dt=F32):
        return nc.dram_tensor(name, list(shape), dt, kind=dbg_kind).ap()

    x_d = din("x", [NB, NLAT, D])
    ctx_d = din("ctx", [NB, NCTX, D])
    cT_d = din("cT", [128, 8, 3])
    ada_w_d = din("ada_w", [L, D, 6 * D])
    ada_bT_d = din("ada_bT", [128, L, 48])
    n1T_d = din("n1T", [128, L, 8])
    n2T_d = din("n2T", [128, L, 8])
    w_in_d = din("w_in", [L, D, IN_COLS])
    lru_cw_d = din("lru_cw", [128, L, 2, 4])
    lru_cb_d = din("lru_cb", [128, L, 2])
    lru_wr_d = din("lru_wr", [128, L, 2, 2, 128])
    lru_wi_d = din("lru_wi", [128, L, 2, 2, 128])
    lru_br_d = din("lru_br", [128, L, 2, 2])
    lru_bi_d = din("lru_bi", [128, L, 2, 2])
    lru_lam_d = din("lru_lam", [128, L, 2, 2])
    hg_lbT_d = din("hg_lbT", [128, L, 2])
    hg_nwT_d = din("hg_nwT", [128, L, 2])
    mfwd_d = din("mfwd", [128, S])
    mbwd_d = din("mbwd", [128, S])
    triU_d = din("triU", [128, 128], U32)
    triL_d = din("triL", [128, 128], U32)
    blk64_d = din("blk64", [128, 128], BF16)
    sd_cw_d = din("sd_cw", [128, L, 4, 4])
    sd_cb_d = din("sd_cb", [128, L, 4])
    sd_alog_d = din("sd_alog", [128, L, 8])
    sd_dtb_d = din("sd_dtb", [128, L, 8])
    sd_dsk_d = din("sd_dsk", [128, L, 256])
    sd_nw_d = din("sd_nw", [128, L, 256])
    triUf_d = din("triUf", [128, 128])
    triLf_d = din("triLf", [128, 128])
    strLf_d = din("strLf", [128, 128])
    strUf_d = din("strUf", [128, 128])
    ml_qan_d = din("ml_qan", [128, L, 192])
    ml_kvan_d = din("ml_kvan", [128, L, 128])
    ml_qn_d = din("ml_qn", [128, L, 96])
    ml_kn_d = din("ml_kn", [128, L, 96])
    ml_wq_d = din("ml_wq", [L, 192, 384])
    ml_wkv_d = din("ml_wkv", [L, 128, 512])
    rope_d = din("rope", [128, 16, 2, 16])
    invn3_d = din("invn3", [128, 3])
    invn8_d = din("invn8", [128, 8])
    w_out_d = din("w_out", [L, D, D])
    w_rt_d = din("w_rt", [L, D, NEXP])
    w_gate_d = din("w_gate", [L, NEXP, D, FF])
    w_up_d = din("w_up", [L, NEXP, D, FF])
    w_down_d = din("w_down", [L, NEXP, FF, D])
    iotaf_d = din("iotaf", [128, 256])
    iotap_d = din("iotap", [128, 2])
    oneh_d = din("oneh", [16, 16, 128])
    ones16_d = din("ones16", [16, NLAT])
    triUf4_d = din("triUf4", [128, 4, 128])
    triLf4_d = din("triLf4", [128, 4, 128])
    negmf_d = din("negmf", [128, 4, 128])
    negmb_d = din("negmb", [128, 4, 128])
    ident_f_d = din("ident_f", [128, 128])
    ident_b_d = din("ident_b", [128, 128], BF16)
    ones_b_d = din("ones_b", [128, 128], BF16)
    ones_f_d = din("ones_f", [128, 128])
    out_d = nc.dram_tensor("out", [NB, NLAT, D], F32, kind="ExternalOutput").ap()

    xT_d = dscr("xT", [NB, D, S])
    uT_d = dscr("uT", [NB, IN_COLS, S])
    ut_d = dscr("ut", [NB, S, TM_COLS])
    yT_d = dscr("yT", [NB, D, S], BF16)
    TyT = [T() for b in range(NB)]
    h2t_d = dscr("h2t", [NB, S, D], BF16)
    aff_d = dscr("aff", [NB, NEXP, S])
    Th2 = [T() for b in range(NB)]
    Taff = [T() for b in range(NB)]
    Tx = [T("xT%d" % b) for b in range(NB)]
    TuT = [T() for b in range(NB)]
    Tut = [T() for b in range(NB)]
    Tout = T("out")

    with ExitStack() as root:
        k = K(nc, root)
        ident_f = k.sb("ident_f", [128, 128]); ident_b = k.sb("ident_b", [128, 128], BF16)
        ones_b = k.sb("ones_b", [128, 128], BF16); ones_f = k.sb("ones_f", [128, 128])
        modT = k.sb("modT", [128, L, 48, 3])
        n1T = k.sb("n1T", [128, L, 8]); n2T = k.sb("n2T", [128, L, 8])
        Tc = T("consts")
        Tmod = T("mod")
        k.dma("sp", ident_f[:], ident_f_d, writes=[Tc])
        k.dma("sp", ident_b[:], ident_b_d, writes=[Tc])
        k.dma("sp", ones_b[:], ones_b_d, writes=[Tc])
        k.dma("sp", ones_f[:], ones_f_d, writes=[Tc])
        k.dma("sp", n1T[:], n1T_d, writes=[Tc])
        k.dma("sp", n2T[:], n2T_d, writes=[Tc])

        with Stage(k):
            cT = k.sb("cT", [128, 8, 3]); sT = k.sb("sT", [128, 8, 3])
            abT = k.sb("abT", [128, L, 48])
            Tct = T(); Tst = T()
            k.dma("sp", cT[:], cT_d, writes=[Tct])
            k.dma("sp", abT[:], ada_bT_d, writes=[Tct])
            k.op("act", lambda e: e.activation(out=sT[:], in_=cT[:], func=AF.Silu), reads=[Tct], writes=[Tst])
            wbuf = [k.sb("adaw%d" % i, [128, 8, 512]) for i in range(2)]
            Tw = [T(), T()]
            pm = [k.ps("pm%d" % i, [128, 4, 4]) for i in range(2)]
            Tpm = [PT(), PT()]
            it = 0
            for l in range(L):
                for j in range(12):
                    wb = wbuf[it % 2]; tw = Tw[it % 2]
                    k.dma("sp", wb[:], ada_w_d[l, :, j * 512:(j + 1) * 512].rearrange("(kc p) n -> p kc n", p=128), writes=[tw])
                    pp = pm[it % 2]; tp = Tpm[it % 2]
                    for sub in range(4):
                        for kc in range(8):
                            k.op("pe", lambda e: e.matmul(pp[:, sub, 0:3], lhsT=wb[:, kc, sub * 128:(sub + 1) * 128],
                                                          rhs=sT[:, kc, :], start=(kc == 0), stop=(kc == 7)),
                                 reads=[tw, Tst], writes=[tp])
                    for sub in range(4):
                        ch = j * 4 + sub
                        k.op("dve", lambda e: e.tensor_scalar(out=modT[:, l, ch, :], in0=pp[:, sub, 0:3],
                                                              scalar1=abT[:, l, ch:ch + 1], scalar2=None, op0=ALU.add),
                             reads=[tp, Tct], writes=[Tmod])
                    it += 1

        with Stage(k):
            xin = [k.sb("xin%d" % i, [128, D]) for i in range(3)]
            Txin = [T() for _ in range(3)]
            xo = [k.sb("xo%d" % i, [128, 8, 128]) for i in range(3)]
            Txo = [T() for _ in range(3)]
            pt = [k.ps("pt%d" % i, [128, 4, 128]) for i in range(4)]
            Tpt = [PT() for _ in range(4)]
            it = 0
            for b in range(NB):
                for ti in range(NT):
                    src = ctx_d[b, ti * 128:(ti + 1) * 128, :] if ti < 2 else x_d[b, (ti - 2) * 128:(ti - 1) * 128, :]
                    xi = xin[it % 3]; txi = Txin[it % 3]
                    k.dma("sp", xi[:], src, writes=[txi])
                    xx = xo[it % 3]; txo = Txo[it % 3]
                    for half in range(2):
                        pp = pt[(2 * it + half) % 4]; tp = Tpt[(2 * it + half) % 4]
                        for q in range(4):
                            kc = half * 4 + q
                            k.op("pe", lambda e: e.transpose(pp[:, q, :], xi[:, kc * 128:(kc + 1) * 128], ident_f[:]),
                                 reads=[txi, Tc], writes=[tp])
                        if half == 0:
                            k.op("act", lambda e: e.activation(out=xx[:, 0:4, :], in_=pp[:], func=AF.Copy), reads=[tp], writes=[txo])
                        else:
                            k.op("dve", lambda e: e.tensor_copy(out=xx[:, 4:8, :], in_=pp[:]), reads=[tp], writes=[txo])
                    k.dma("sp", xT_d[b, :, ti * 128:(ti + 1) * 128].rearrange("(kc p) t -> p kc t", p=128), xx[:],
                          reads=[txo], writes=[Tx[b]])
                    it += 1

        for l in range(cfg.layers):
            if cfg.upto < 1:
                break
            for b in range(NB):
                with Stage(k):
                    w_in = k.sb("w_in", [128, 8, IN_COLS], BF16); Tw = T()
                    cst = Caster(k, 128, IN_COLS)
                    for kc in range(8):
                        cst.load(w_in[:, kc, :], w_in_d[l, kc * 128:(kc + 1) * 128, :], 128, IN_COLS, [Tw])
                    G = k.sb("G", [128, 2, 8]); Tg = T()
                    for i, mi in enumerate((b, 2)):
                        k.op("dve", lambda e: e.scalar_tensor_tensor(out=G[:, i, :], in0=modT[:, l, 8:16, mi], scalar=1.0,
                                                                    in1=n1T[:, l, :], op0=ALU.add, op1=ALU.mult),
                             reads=[Tmod, Tc], writes=[Tg])
                    xb = [k.sb("xb%d" % i, [128, 8, 512]) for i in range(2)]; Txb = [T(), T()]
                    sq = [k.sb("sq%d" % i, [128, 8, 512], BF16) for i in range(2)]; Tsq = [T(), T()]
                    rs = [k.sb("rs%d" % i, [128, 512]) for i in range(2)]; Trs = [T(), T()]
                    tmp = [k.sb("tmp%d" % i, [128, 512]) for i in range(2)]; Ttmp = [T(), T()]
                    hT = [k.sb("hT%d" % i, [128, 8, 512], BF16) for i in range(2)]; ThT = [T(), T()]
                    ev = [k.sb("ev%d" % i, [128, 512]) for i in range(4)]; Tev = [T() for _ in range(4)]
                    evt = [k.sb("evt%d" % i, [128, TM_COLS]) for i in range(2)]; Tevt = [T(), T()]
                    pss = k.ps("pss", [128, 512]); Tpss = PT()
                    pu = [k.ps("pu%d" % i, [128, 512]) for i in range(4)]; Tpu = [PT() for _ in range(4)]
                    pv = [k.ps("pv%d" % i, [128, 512]) for i in range(3)]; Tpv = [PT() for _ in range(3)]
                    nev_box = [0]

                    def s1_norm(bi):
                        t0, n = BLOCKS[bi]
                        seg = 1 if bi == 0 else 0
                        mi = 2 if bi == 0 else b
                        X = xb[bi % 2]; tX = Txb[bi % 2]
                        k.dma("sp", X[:, :, 0:n], xT_d[b, :, t0:t0 + n].rearrange("(kc p) t -> p kc t", p=128),
                              reads=[Tx[b]], writes=[tX])
                        Q = sq[bi % 2]; tQ = Tsq[bi % 2]
                        k.op("act", lambda e: e.activation(out=Q[:, :, 0:n], in_=X[:, :, 0:n], func=AF.Square), reads=[tX], writes=[tQ])
                        for kc in range(8):
                            k.op("pe", lambda e: e.matmul(pss[:, 0:n], lhsT=ones_b[:], rhs=Q[:, kc, 0:n], start=(kc == 0), stop=(kc == 7)),
                                 reads=[tQ, Tc], writes=[Tpss])
                        R = rs[bi % 2]; tR = Trs[bi % 2]
                        k.op("act", lambda e: e.activation(out=R[:, 0:n], in_=pss[:, 0:n], func=AF.Sqrt, scale=1.0 / D, bias=EPS),
                             reads=[Tpss], writes=[tR])
                        k.op("dve", lambda e: e.reciprocal(out=R[:, 0:n], in_=R[:, 0:n]), reads=[tR], writes=[tR])
                        H = hT[bi % 2]; tH = ThT[bi % 2]
                        for kc in range(8):
                            tm = tmp[kc % 2]; ttm = Ttmp[kc % 2]
                            k.op("dve", lambda e: e.tensor_tensor(out=tm[:, 0:n], in0=X[:, kc, 0:n], in1=R[:, 0:n], op=ALU.mult),
                                 reads=[tX, tR], writes=[ttm])
                            k.op("act", lambda e: e.activation(out=H[:, kc, 0:n], in_=tm[:, 0:n], func=AF.Identity,
                                                               scale=G[:, seg, kc:kc + 1], bias=modT[:, l, kc, mi:mi + 1]),
                                 reads=[ttm, Tg, Tmod], writes=[tH])

                    def s1_proj(bi):
                        t0, n = BLOCKS[bi]
                        H = hT[bi % 2]; tH = ThT[bi % 2]
                        nev = nev_box[0]
                        for ci, ch in enumerate(FM_CHUNKS):
                            c0 = ch * 128
                            pp = pu[ci % 4]; tp = Tpu[ci % 4]
                            for kc in range(8):
                                k.op("pe", lambda e: e.matmul(pp[:, 0:n], lhsT=w_in[:, kc, c0:c0 + 128], rhs=H[:, kc, 0:n],
                                                              start=(kc == 0), stop=(kc == 7)), reads=[Tw, tH], writes=[tp])
                            E = ev[nev % 4]; tE = Tev[nev % 4]
                            if nev % 2 == 0:
                                k.op("act", lambda e: e.activation(out=E[:, 0:n], in_=pp[:, 0:n], func=AF.Copy), reads=[tp], writes=[tE])
                            else:
                                k.op("dve", lambda e: e.tensor_copy(out=E[:, 0:n], in_=pp[:, 0:n]), reads=[tp], writes=[tE])
                            k.dma("sp", uT_d[b, c0:c0 + 128, t0:t0 + n], E[:, 0:n], reads=[tE], writes=[TuT[b]])
                            nev += 1
                        for tt in range(n // 128):
                            ET = evt[tt % 2]; tET = Tevt[tt % 2]
                            off = 0
                            for ri, (a, bnd) in enumerate(TM_RANGES):
                                w = bnd - a
                                pp = pv[ri]; tp = Tpv[ri]
                                for kc in range(8):
                                    k.op("pe", lambda e: e.matmul(pp[:, 0:w], lhsT=H[:, kc, tt * 128:(tt + 1) * 128], rhs=w_in[:, kc, a:bnd],
                                                                  start=(kc == 0), stop=(kc == 7)), reads=[Tw, tH], writes=[tp])
                                if ri == 1:
                                    k.op("act", lambda e: e.activation(out=ET[:, off:off + w], in_=pp[:, 0:w], func=AF.Copy), reads=[tp], writes=[tET])
                                else:
                                    k.op("dve", lambda e: e.tensor_copy(out=ET[:, off:off + w], in_=pp[:, 0:w]), reads=[tp], writes=[tET])
                                off += w
                            k.dma("sp", ut_d[b, t0 + tt * 128:t0 + (tt + 1) * 128, :], ET[:], reads=[tET], writes=[Tut[b]])
                        nev_box[0] = nev

                    s1_norm(0)
                    for bi in range(len(BLOCKS)):
                        if bi + 1 < len(BLOCKS):
                            s1_norm(bi + 1)
                        s1_proj(bi)
                if cfg.upto < 2:
                    continue
                with Stage(k):
                    cw = k.sb("cw", [128, 2, 4]); cb = k.sb("cb", [128, 2])
                    wr = k.sb("wr", [128, 2, 2, 128], BF16); wi = k.sb("wi", [128, 2, 2, 128], BF16)
                    br = k.sb("br", [128, 2, 2]); bi_ = k.sb("bi", [128, 2, 2]); lam = k.sb("lam", [128, 2, 2])
                    cl = k.sb("cl", [128, 2, 2]); cl2 = k.sb("cl2", [128, 2, 2])
                    Tp2 = T()
                    k.dma("sp", cw[:], lru_cw_d[:, l], writes=[Tp2]); k.dma("sp", cb[:], lru_cb_d[:, l], writes=[Tp2])
                    cst = Caster(k, 128, 512)
                    cst.load(wr[:].rearrange("p a b c -> p (a b c)"), lru_wr_d[:, l].rearrange("p a b c -> p (a b c)"), 128, 512, [Tp2])
                    cst.load(wi[:].rearrange("p a b c -> p (a b c)"), lru_wi_d[:, l].rearrange("p a b c -> p (a b c)"), 128, 512, [Tp2])
                    k.dma("sp", br[:], lru_br_d[:, l], writes=[Tp2]); k.dma("sp", bi_[:], lru_bi_d[:, l], writes=[Tp2])
                    k.dma("sp", lam[:], lru_lam_d[:, l], writes=[Tp2])
                    k.op("act", lambda e: e.activation(out=cl[:], in_=lam[:], func=AF.Exp, scale=-1.0), reads=[Tp2], writes=[Tp2])
                    k.op("act", lambda e: e.activation(out=cl[:], in_=cl[:], func=AF.Ln, bias=1.0), reads=[Tp2], writes=[Tp2])
                    k.op("dve", lambda e: e.tensor_scalar(out=cl2[:], in0=cl[:], scalar1=-16.0, scalar2=None, op0=ALU.mult), reads=[Tp2], writes=[Tp2])
                    k.op("dve", lambda e: e.tensor_scalar(out=cl[:], in0=cl[:], scalar1=-8.0, scalar2=None, op0=ALU.mult), reads=[Tp2], writes=[Tp2])
                    xp = k.sb("xp", [128, S + 6]); Txp = T()
                    xc = k.sb("xc", [128, S]); Txc = T()
                    xcb = k.sb("xcb", [128, S], BF16); Txcb = T()
                    gt = k.sb("gt", [128, S]); Tgt = T()
                    Rr = k.sb("Rr", [128, S]); TR = T()
                    Ii = k.sb("Ii", [128, S]); TI = T()
                    Aa = k.sb("Aa", [128, S]); TA = T()
                    Bb = k.sb("Bb", [128, S]); TB = T()
                    Hh = [k.sb("Hh%d" % i, [128, S]) for i in range(2)]; TH = [T(), T()]
                    yo = k.sb("yo", [128, S], BF16); Tyo = T()
                    pg = [k.ps("pg%d" % i, [128, 512]) for i in range(4)]; Tpg = [PT() for _ in range(4)]
                    npg = 0
                    segs = [(0, NCTX, 2), (NCTX, NLAT, NCTX + 5)]
                    for ch in range(2):
                        k.op("pool", lambda e: e.memset(xp[:], 0.0), writes=[Txp])
                        for (t0, n, o) in segs:
                            k.dma("sp", xp[:, o:o + n], uT_d[b, ch * 128:(ch + 1) * 128, t0:t0 + n], reads=[TuT[b]], writes=[Txp])
                        k.dma("sp", gt[:], uT_d[b, 256 + ch * 128:256 + (ch + 1) * 128, :], reads=[TuT[b]], writes=[Tgt])
                        for (t0, n, o) in segs:
                            k.op("dve", lambda e: e.tensor_scalar(out=xc[:, t0:t0 + n], in0=xp[:, o - 2:o - 2 + n], scalar1=cw[:, ch, 0:1],
                                                                  scalar2=cb[:, ch:ch + 1], op0=ALU.mult, op1=ALU.add),
                                 reads=[Txp, Tp2], writes=[Txc])
                            for j in range(1, 4):
                                k.op("dve", lambda e: e.scalar_tensor_tensor(out=xc[:, t0:t0 + n], in0=xp[:, o - 2 + j:o - 2 + j + n],
                                                                            scalar=cw[:, ch, j:j + 1], in1=xc[:, t0:t0 + n],
                                                                            op0=ALU.mult, op1=ALU.add),
                                     reads=[Txp, Tp2, Txc], writes=[Txc])
                        k.op("pool", lambda e: e.tensor_copy(out=xcb[:], in_=xc[:]), reads=[Txc], writes=[Txcb])
                        for d in range(2):
                            for (t0, n) in BLOCKS:
                                for (W, bias, dst, tdst) in ((wr, br, Rr, TR), (wi, bi_, Ii, TI)):
                                    pp = pg[npg % 4]; tp = Tpg[npg % 4]; npg += 1
                                    k.op("pe", lambda e: e.matmul(pp[:, 0:n], lhsT=W[:, d, ch, :], rhs=xcb[:, t0:t0 + n], start=True, stop=True),
                                         reads=[Tp2, Txcb], writes=[tp])
                                    k.op("act", lambda e: e.activation(out=dst[:, t0:t0 + n], in_=pp[:, 0:n], func=AF.Sigmoid,
                                                                       bias=bias[:, d, ch:ch + 1]), reads=[tp, Tp2], writes=[tdst])
                            k.op("act", lambda e: e.activation(out=Aa[:], in_=Rr[:], func=AF.Exp, scale=cl[:, d, ch:ch + 1]),
                                 reads=[TR, Tp2], writes=[TA])
                            k.op("act", lambda e: e.activation(out=Bb[:], in_=Rr[:], func=AF.Exp, scale=cl2[:, d, ch:ch + 1]),
                                 reads=[TR, Tp2], writes=[TB])
                            k.op("act", lambda e: e.activation(out=Bb[:], in_=Bb[:], func=AF.Sqrt, scale=-1.0, bias=1.0), reads=[TB], writes=[TB])
                            k.op("dve", lambda e: e.tensor_tensor(out=Ii[:], in0=Ii[:], in1=xc[:], op=ALU.mult), reads=[TI, Txc], writes=[TI])
                            k.op("dve", lambda e: e.tensor_tensor(out=Bb[:], in0=Bb[:], in1=Ii[:], op=ALU.mult), reads=[TB, TI], writes=[TB])
                            H = Hh[d]
                            if d == 0:
                                k.op("dve", lambda e: e.tensor_tensor_scan(out=H[:], data0=Aa[:], data1=Bb[:], initial=0.0,
                                                                          op0=ALU.mult, op1=ALU.add), reads=[TA, TB], writes=[TH[d]])
                            else:
                                k.op("dve", lambda e: e.tensor_tensor_scan(out=H[:, NCTX - 1::-1], data0=Aa[:, NCTX - 1::-1], data1=Bb[:, NCTX - 1::-1],
                                                                          initial=0.0, op0=ALU.mult, op1=ALU.add), reads=[TA, TB], writes=[TH[d]])
                                k.op("dve", lambda e: e.tensor_tensor_scan(out=H[:, S - 1:NCTX - 1:-1], data0=Aa[:, S - 1:NCTX - 1:-1],
                                                                          data1=Bb[:, S - 1:NCTX - 1:-1], initial=H[:, 0:1],
                                                                          op0=ALU.mult, op1=ALU.add), reads=[TA, TB, TH[d]], writes=[TH[d]])
                        k.op("act", lambda e: e.activation(out=gt[:], in_=gt[:], func=AF.Gelu_apprx_tanh), reads=[Tgt], writes=[Tgt])
                        k.op("dve", lambda e: e.tensor_tensor(out=Hh[0][:], in0=Hh[0][:], in1=Hh[1][:], op=ALU.add), reads=TH, writes=[TH[0]])
                        k.op("dve", lambda e: e.tensor_tensor(out=yo[:], in0=Hh[0][:], in1=gt[:], op=ALU.mult), reads=[TH[0], Tgt], writes=[Tyo])
                        k.dma("sp", yT_d[b, ch * 128:(ch + 1) * 128, :], yo[:], reads=[Tyo], writes=[TyT[b]])
                if cfg.upto < 3:
                    continue
                with Stage(k):
                    lbz = k.sb("lbz", [128, L, 2]); lbe = k.sb("lbe", [128, L, 2]); lbs = k.sb("lbs", [128, 2]); lb = k.sb("lb", [128, 2])
                    oml = k.sb("oml", [128, 2]); hnw = k.sb("hnw", [128, 2]); Tp3 = T()
                    mf = k.sb("mf", [128, S]); mb = k.sb("mb", [128, S]); triU = k.sb("triU", [128, 128], U32); triL = k.sb("triL", [128, 128], U32)
                    blk = k.sb("blk", [128, 128], BF16)
                    k.dma("sp", lbz[:], hg_lbT_d, writes=[Tp3]); k.dma("sp", hnw[:], hg_nwT_d[:, l], writes=[Tp3])
                    k.dma("sp", mf[:], mfwd_d, writes=[Tp3]); k.dma("sp", mb[:], mbwd_d, writes=[Tp3])
                    k.dma("sp", triU[:], triU_d, writes=[Tp3]); k.dma("sp", triL[:], triL_d, writes=[Tp3]); k.dma("sp", blk[:], blk64_d, writes=[Tp3])
                    k.op("act", lambda e: e.activation(out=lbe[:], in_=lbz[:], func=AF.Exp), reads=[Tp3], writes=[Tp3])
                    k.op("dve", lambda e: e.tensor_tensor(out=lbs[:], in0=lbe[:, 0, :], in1=lbe[:, 1, :], op=ALU.add), reads=[Tp3], writes=[Tp3])
                    k.op("dve", lambda e: e.reciprocal(out=lbs[:], in_=lbs[:]), reads=[Tp3], writes=[Tp3])
                    for ll in range(L):
                        k.op("dve", lambda e: e.tensor_tensor(out=lbe[:, ll, :], in0=lbe[:, ll, :], in1=lbs[:], op=ALU.mult), reads=[Tp3], writes=[Tp3])
                    k.op("dve", lambda e: e.tensor_copy(out=lb[:], in_=lbe[:, 0, :]), reads=[Tp3], writes=[Tp3])
                    for ll in range(1, l + 1):
                        k.op("dve", lambda e: e.tensor_tensor(out=lb[:], in0=lb[:], in1=lbe[:, ll, :], op=ALU.add), reads=[Tp3], writes=[Tp3])
                    k.op("dve", lambda e: e.tensor_tensor(out=lb[:], in0=lb[:], in1=lbe[:, 0, :], op=ALU.subtract), reads=[Tp3], writes=[Tp3])
                    k.op("dve", lambda e: e.tensor_scalar(out=oml[:], in0=lb[:], scalar1=-1.0, scalar2=1.0, op0=ALU.mult, op1=ALU.add), reads=[Tp3], writes=[Tp3])
                    vt = k.sb("vt", [128, NT, 256], BF16); Tvt = T()
                    vstg = k.sb("vstg", [128, NT // 2, 256]); Tvstg = T()
                    for hf in range(2):
                        k.dma("sp", vstg[:], ut_d[b, hf * (S // 2):(hf + 1) * (S // 2), 0:256].rearrange("(n p) c -> p n c", p=128), reads=[Tut[b]], writes=[Tvstg])
                        k.op("pool", lambda e: e.tensor_copy(out=vt[:, hf * (NT // 2):(hf + 1) * (NT // 2), :], in_=vstg[:]), reads=[Tvstg], writes=[Tvt])
                    qh = k.sb("qh", [128, S]); Tqh = T()
                    gg = k.sb("gg", [128, S]); Tgg = T()
                    ff = k.sb("ff", [128, S]); Tff = T()
                    lf = k.sb("lf", [128, S]); Tlf = T()
                    cum = k.sb("cum", [128, S]); Tcum = T()
                    dd = k.sb("dd", [128, S]); Tdd = T()
                    EE = k.sb("EE", [128, S]); TEE = T()
                    qt = k.sb("qt", [128, S], BF16); Tqt = T()
                    kt = k.sb("kt", [128, S], BF16); Tkt = T()
                    qs = k.sb("qs", [128, S], BF16); Tqs = T()
                    ke = k.sb("ke", [128, S], BF16); Tke = T()
                    etot = k.sb("etot", [128, NT]); Tet = T()
                    OO = k.sb("OO", [128, S]); TOO = T()
                    ket = [k.sb("ket%d" % i, [128, 128], BF16) for i in range(2)]; Tket = [T(), T()]
                    Am = [[k.sb("Am%d_%d" % (d, i), [128, 128], BF16) for i in range(2)] for d in range(2)]
                    TAm = [[T(), T()] for d in range(2)]
                    S32 = k.sb("S32", [128, 64]); TS32 = T()
                    Sb = k.sb("Sb", [128, 64], BF16); TSb = T()
                    sqb = k.sb("sqb", [128, 512], BF16); Tsqb = T()
                    rsd = k.sb("rsd", [128, 512]); Trsd = T()
                    yo = k.sb("yo3", [128, S], BF16); Tyo = T()
                    p_sc = [k.ps("p_sc%d" % i, [128, 128]) for i in range(2)]; Tp_sc = [PT(), PT()]
                    p_y = [k.ps("p_y%d" % i, [128, 128]) for i in range(2)]; Tp_y = [PT(), PT()]
                    p_st = k.ps("p_st", [128, 64]); Tp_st = PT()
                    p_tr = k.ps("p_tr", [128, 128], BF16); Tp_tr = PT()
                    p_ss = k.ps("p_ss", [128, 512]); Tp_ss = PT()
                    for d in range(2):
                        for i in range(2):
                            k.op("pool", lambda e: e.memset(Am[d][i][:], 0.0), writes=[TAm[d][i]])
                    for i in range(2):
                        k.op("dve", lambda e: e.memset(p_sc[i][:], 0.0), writes=[Tp_sc[i]])
                    cum3 = cum[:].rearrange("p (n t) -> p n t", t=128)
                    dd3 = dd[:].rearrange("p (n t) -> p n t", t=128)
                    cum4 = cum[:].rearrange("p (n t) -> p n t", t=32)
                    dd4 = dd[:].rearrange("p (n t) -> p n t", t=32)
                    qx = k.sb("qx", [128, S], BF16); Tqx = T()
                    kx = [None] + [k.sb("kx%d" % i, [128, NT, 96], BF16) for i in range(1, 4)]; Tkx = T()
                    def mkset(i_):
                        d_ = {}
                        for nm in ("qt", "kt", "qx", "qs", "ke"):
                            d_[nm] = k.sb("%s_b%d" % (nm, i_), [128, S], BF16); d_["T" + nm] = T()
                        d_["kx"] = [None] + [k.sb("kx%d_b%d" % (j, i_), [128, NT, 96], BF16) for j in range(1, 4)]; d_["Tkx"] = T()
                        d_["etot"] = k.sb("etot_b%d" % i_, [128, NT]); d_["Tet"] = T()
                        return d_
                    sets = [dict(qt=qt, Tqt=Tqt, kt=kt, Tkt=Tkt, qx=qx, Tqx=Tqx, qs=qs, Tqs=Tqs, ke=ke, Tke=Tke, kx=kx, Tkx=Tkx, etot=etot, Tet=Tet), mkset(1)]

                    def hg_prep(hp, d, B):
                        r0 = 512 + hp * 128
                        qt, kt, qx, qs, ke, kx, etot = B["qt"], B["kt"], B["qx"], B["qs"], B["ke"], B["kx"], B["etot"]
                        Tqt, Tkt, Tqx, Tqs, Tke, Tkx, Tet = B["Tqt"], B["Tkt"], B["Tqx"], B["Tqs"], B["Tke"], B["Tkx"], B["Tet"]
                        if d == 0:
                            k.dma("sp", qh[:], uT_d[b, r0:r0 + 128, :], reads=[TuT[b]], writes=[Tqh])
                            k.op("act", lambda e: e.activation(out=qh[:], in_=qh[:], func=AF.Silu), reads=[Tqh], writes=[Tqh])
                        k.dma("sp", ff[:], uT_d[b, r0 + 256 * (d + 1):r0 + 256 * (d + 1) + 128, :], reads=[TuT[b]], writes=[Tff])
                        k.op("act", lambda e: e.activation(out=ff[:], in_=ff[:], func=AF.Sigmoid), reads=[Tff], writes=[Tff])
                        k.op("dve", lambda e: e.tensor_scalar(out=ff[:], in0=ff[:], scalar1=oml[:, hp:hp + 1], scalar2=lb[:, hp:hp + 1],
                                                              op0=ALU.mult, op1=ALU.add), reads=[Tff, Tp3], writes=[Tff])
                        k.op("act", lambda e: e.activation(out=lf[:], in_=ff[:], func=AF.Ln), reads=[Tff], writes=[Tlf])
                        k.op("dve", lambda e: e.tensor_scalar(out=ff[:], in0=ff[:], scalar1=-1.0, scalar2=1.0, op0=ALU.mult, op1=ALU.add),
                             reads=[Tff], writes=[Tff])
                        if d == 0:
                            k.op("dve", lambda e: e.tensor_tensor_scan(out=cum[:], data0=mf[:], data1=lf[:], initial=0.0, op0=ALU.mult, op1=ALU.add),
                                 reads=[Tp3, Tlf], writes=[Tcum])
                            mid, end = 63, 127
                        else:
                            k.op("dve", lambda e: e.tensor_tensor_scan(out=cum[:, ::-1], data0=mb[:, ::-1], data1=lf[:, ::-1], initial=0.0,
                                                                      op0=ALU.mult, op1=ALU.add), reads=[Tp3, Tlf], writes=[Tcum])
                            mid, end = 64, 0
                        mid4, first4 = (15, 0) if d == 0 else (16, 31)
                        k.op("dve", lambda e: e.tensor_tensor(out=dd4, in0=cum4, in1=cum4[:, :, mid4:mid4 + 1].to_broadcast([128, S // 32, 32]), op=ALU.subtract),
                             reads=[Tcum], writes=[Tdd])
                        k.op("act", lambda e: e.activation(out=EE[:], in_=dd[:], func=AF.Exp), reads=[Tdd], writes=[TEE])
                        k.op("dve", lambda e: e.tensor_tensor(out=qt[:], in0=qh[:], in1=EE[:], op=ALU.mult), reads=[Tqh, TEE], writes=[Tqt])
                        k.op("act", lambda e: e.activation(out=EE[:], in_=dd[:], func=AF.Exp, scale=-1.0), reads=[Tdd], writes=[TEE])
                        k.op("dve", lambda e: e.tensor_tensor(out=kt[:], in0=ff[:], in1=EE[:], op=ALU.mult), reads=[Tff, TEE], writes=[Tkt])
                        k.op("dve", lambda e: e.tensor_tensor(out=dd4, in0=cum4, in1=cum4[:, :, first4:first4 + 1].to_broadcast([128, S // 32, 32]), op=ALU.subtract),
                             reads=[Tcum], writes=[Tdd])
                        k.op("act", lambda e: e.activation(out=EE[:], in_=dd[:], func=AF.Exp), reads=[Tdd], writes=[TEE])
                        k.op("dve", lambda e: e.tensor_tensor(out=qx[:], in0=qh[:], in1=EE[:], op=ALU.mult), reads=[Tqh, TEE], writes=[Tqx])
                        ff3 = ff[:].rearrange("p (n t) -> p n t", t=128)
                        EE3 = EE[:].rearrange("p (n t) -> p n t", t=128)
                        for i in range(1, 4):
                            w_ = 32 * i
                            if d == 0:
                                srcs = slice(0, w_); refi = w_
                            else:
                                srcs = slice(128 - w_, 128); refi = 127 - w_
                            k.op("dve", lambda e: e.tensor_tensor(out=dd3[:, :, 0:w_], in0=cum3[:, :, refi:refi + 1].to_broadcast([128, NT, w_]),
                                                                  in1=cum3[:, :, srcs], op=ALU.subtract), reads=[Tcum], writes=[Tdd])
                            k.op("act", lambda e: e.activation(out=EE3[:, :, 0:w_], in_=dd3[:, :, 0:w_], func=AF.Exp), reads=[Tdd], writes=[TEE])
                            k.op("dve", lambda e: e.tensor_tensor(out=kx[i][:, :, 0:w_], in0=ff3[:, :, srcs], in1=EE3[:, :, 0:w_], op=ALU.mult),
                                 reads=[Tff, TEE], writes=[Tkx])
                        k.op("act", lambda e: e.activation(out=EE[:], in_=cum[:], func=AF.Exp), reads=[Tcum], writes=[TEE])
                        k.op("dve", lambda e: e.tensor_tensor(out=qs[:], in0=qh[:], in1=EE[:], op=ALU.mult), reads=[Tqh, TEE], writes=[Tqs])
                        k.op("act", lambda e: e.activation(out=etot[:], in_=cum3[:, :, end], func=AF.Exp), reads=[Tcum], writes=[Tet])
                        k.op("dve", lambda e: e.tensor_tensor(out=dd3, in0=cum3[:, :, end:end + 1].to_broadcast([128, NT, 128]), in1=cum3, op=ALU.subtract),
                             reads=[Tcum], writes=[Tdd])
                        k.op("act", lambda e: e.activation(out=EE[:], in_=dd[:], func=AF.Exp), reads=[Tdd], writes=[TEE])
                        k.op("dve", lambda e: e.tensor_tensor(out=ke[:], in0=ff[:], in1=EE[:], op=ALU.mult), reads=[Tff, TEE], writes=[Tke])

                    def hg_loop(hp, d, B, pend):
                        qt, kt, qx, qs, ke, kx, etot = B["qt"], B["kt"], B["qx"], B["qs"], B["ke"], B["kx"], B["etot"]
                        Tqt, Tkt, Tqx, Tqs, Tke, Tkx, Tet = B["Tqt"], B["Tkt"], B["Tqx"], B["Tqs"], B["Tke"], B["Tkx"], B["Tet"]
                        order = list(range(NT)) if d == 0 else [1, 0] + list(range(NT - 1, 1, -1))
                        tri = triU if d == 0 else triL
                        per = (len(pend) + NT - 3) // (NT - 2) if pend else 0
                        for oi, ti in enumerate(order):
                            ts_ = slice(ti * 128, (ti + 1) * 128)
                            k.op("pe", lambda e: e.transpose(p_tr[:], ke[:, ts_], ident_b[:]), reads=[Tke, Tc], writes=[Tp_tr])
                            KT = ket[oi % 2]; tKT = Tket[oi % 2]
                            k.op("act", lambda e: e.activation(out=KT[:], in_=p_tr[:], func=AF.Copy), reads=[Tp_tr], writes=[tKT])
                            py = p_y[oi % 2]; tpy = Tp_y[oi % 2]
                            for hh in range(2):
                                bs = slice(hh * 64, (hh + 1) * 64)
                                psc = p_sc[hh]; tps = Tp_sc[hh]
                                for tb in range(4):
                                    for sb_ in (range(0, tb + 1) if d == 0 else range(tb, 4)):
                                        tq = slice(ti * 128 + 32 * tb, ti * 128 + 32 * tb + 32)
                                        if sb_ == tb:
                                            lw = kt[bs, tq]; rq = qt[bs, tq]
                                        else:
                                            i = tb if d == 0 else 3 - tb
                                            o_ = 32 * sb_ if d == 0 else 32 * sb_ - (128 - 32 * i)
                                            lw = kx[i][bs, ti, o_:o_ + 32]; rq = qx[bs, tq]
                                        k.op("pe", lambda e: e.matmul(psc[32 * sb_:32 * sb_ + 32, 32 * tb:32 * tb + 32], lhsT=lw, rhs=rq, start=True, stop=True,
                                                                      tile_position=(bs.start, 32 * sb_)),
                                             reads=[Tkt, Tqt, Tkx, Tqx], writes=[tps])
                                A = Am[d][hh]; tA = TAm[d][hh]
                                k.op("dve", lambda e: e.copy_predicated(out=A[:], mask=tri[:], data=psc[:]), reads=[tps, Tp3], writes=[tA])
                                vs = vt[:, ti, (2 * hp + hh) * 64:(2 * hp + hh + 1) * 64]
                                k.op("pe", lambda e: e.matmul(py[bs, :], lhsT=vs, rhs=A[:], start=True, stop=(oi == 0)),
                                     reads=[Tvt, tA], writes=[tpy])
                                if oi > 0:
                                    k.op("pe", lambda e: e.matmul(py[bs, :], lhsT=Sb[bs, :], rhs=qs[bs, ts_], start=False, stop=True),
                                         reads=[TSb, Tqs], writes=[tpy])
                            if d == 0:
                                k.op("act", lambda e: e.activation(out=OO[:, ts_], in_=py[:], func=AF.Copy), reads=[tpy], writes=[TOO])
                            else:
                                k.op("dve", lambda e: e.tensor_tensor(out=OO[:, ts_], in0=OO[:, ts_], in1=py[:], op=ALU.add), reads=[tpy, TOO], writes=[TOO])
                            if oi < NT - 1:
                                for hh in range(2):
                                    bs = slice(hh * 64, (hh + 1) * 64)
                                    vs = vt[:, ti, (2 * hp + hh) * 64:(2 * hp + hh + 1) * 64]
                                    k.op("pe", lambda e: e.matmul(p_st[bs, :], lhsT=KT[:, bs], rhs=vs, start=True, stop=True),
                                         reads=[tKT, Tvt], writes=[Tp_st])
                                if oi == 0:
                                    k.op("dve", lambda e: e.tensor_copy(out=S32[:], in_=p_st[:]), reads=[Tp_st], writes=[TS32])
                                else:
                                    k.op("dve", lambda e: e.scalar_tensor_tensor(out=S32[:], in0=S32[:], scalar=etot[:, ti:ti + 1], in1=p_st[:],
                                                                                op0=ALU.mult, op1=ALU.add), reads=[Tp_st, Tet, TS32], writes=[TS32])
                                k.op("pool", lambda e: e.tensor_copy(out=Sb[:], in_=S32[:]), reads=[TS32], writes=[TSb])
                            if pend:
                                k.flush(pend, per)

                    chains = [(0, 0), (0, 1), (1, 0), (1, 1)]
                    hg_prep(0, 0, sets[0])
                    for ci, (hp, d) in enumerate(chains):
                        if d == 0:
                            r0 = 512 + hp * 128
                            k.dma("sp", gg[:], uT_d[b, r0 + 1024:r0 + 1024 + 128, :], reads=[TuT[b]], writes=[Tgg])
                            k.op("act", lambda e: e.activation(out=gg[:], in_=gg[:], func=AF.Silu), reads=[Tgg], writes=[Tgg])
                        pend = []
                        if ci + 1 < len(chains):
                            k.defer = pend
                            hg_prep(chains[ci + 1][0], chains[ci + 1][1], sets[(ci + 1) % 2])
                            k.defer = None
                        hg_loop(hp, d, sets[ci % 2], pend)
                        k.flush(pend)
                        if d == 1:
                            for (t0, n) in BLOCKS:
                                k.op("act", lambda e: e.activation(out=sqb[:, 0:n], in_=OO[:, t0:t0 + n], func=AF.Square), reads=[TOO], writes=[Tsqb])
                                k.op("pe", lambda e: e.matmul(p_ss[:, 0:n], lhsT=blk[:], rhs=sqb[:, 0:n], start=True, stop=True), reads=[Tsqb, Tp3], writes=[Tp_ss])
                                k.op("act", lambda e: e.activation(out=rsd[:, 0:n], in_=p_ss[:, 0:n], func=AF.Sqrt, scale=1.0 / 64, bias=EPS), reads=[Tp_ss], writes=[Trsd])
                                k.op("dve", lambda e: e.reciprocal(out=rsd[:, 0:n], in_=rsd[:, 0:n]), reads=[Trsd], writes=[Trsd])
                                k.op("dve", lambda e: e.tensor_tensor(out=rsd[:, 0:n], in0=rsd[:, 0:n], in1=OO[:, t0:t0 + n], op=ALU.mult), reads=[Trsd, TOO], writes=[Trsd])
                                k.op("dve", lambda e: e.scalar_tensor_tensor(out=yo[:, t0:t0 + n], in0=rsd[:, 0:n], scalar=hnw[:, hp:hp + 1], in1=gg[:, t0:t0 + n],
                                                                            op0=ALU.mult, op1=ALU.mult), reads=[Trsd, Tgg, Tp3], writes=[Tyo])
                            k.dma("sp", yT_d[b, 256 + hp * 128:256 + (hp + 1) * 128, :], yo[:], reads=[Tyo], writes=[TyT[b]])
                if cfg.upto < 4:
                    continue
                with Stage(k):
                    Tp4 = T()
                    cw = k.sb("scw", [128, 4, 4]); cb = k.sb("scb", [128, 4])
                    aneg = k.sb("aneg", [128, 8]); dtb = k.sb("dtb", [128, 8]); dsk = k.sb("dsk", [128, 256]); snw = k.sb("snw", [128, 256])
                    triUf = k.sb("triUf", [128, 128]); triLf = k.sb("triLf", [128, 128]); strLf = k.sb("strLf", [128, 128]); strUf = k.sb("strUf", [128, 128])
                    for dst, src in ((cw, sd_cw_d[:, l]), (cb, sd_cb_d[:, l]), (aneg, sd_alog_d[:, l]), (dtb, sd_dtb_d[:, l]), (dsk, sd_dsk_d[:, l]),
                                     (snw, sd_nw_d[:, l]), (triUf, triUf_d), (triLf, triLf_d), (strLf, strLf_d), (strUf, strUf_d)):
                        k.dma("sp", dst[:], src, writes=[Tp4])
                    k.op("act", lambda e: e.activation(out=aneg[:], in_=aneg[:], func=AF.Exp), reads=[Tp4], writes=[Tp4])
                    k.op("dve", lambda e: e.tensor_scalar(out=aneg[:], in0=aneg[:], scalar1=-1.0, scalar2=None, op0=ALU.mult), reads=[Tp4], writes=[Tp4])
                    xp = k.sb("sxp", [128, S + 6]); Txp = T()
                    xc = k.sb("sxc", [128, S]); Txc = T()
                    fmb = k.sb("fmb", [128, 4, S], BF16); Tfmb = T()
                    segs = [(0, NCTX, 2), (NCTX, NLAT, NCTX + 5)]
                    for ch in range(4):
                        k.op("pool", lambda e: e.memset(xp[:], 0.0), writes=[Txp])
                        for (t0, n, o) in segs:
                            k.dma("sp", xp[:, o:o + n], uT_d[b, 2048 + ch * 128:2048 + (ch + 1) * 128, t0:t0 + n], reads=[TuT[b]], writes=[Txp])
                        for (t0, n, o) in segs:
                            k.op("dve", lambda e: e.tensor_scalar(out=xc[:, t0:t0 + n], in0=xp[:, o - 2:o - 2 + n], scalar1=cw[:, ch, 0:1],
                                                                  scalar2=cb[:, ch:ch + 1], op0=ALU.mult, op1=ALU.add), reads=[Txp, Tp4], writes=[Txc])
                            for j in range(1, 4):
                                k.op("dve", lambda e: e.scalar_tensor_tensor(out=xc[:, t0:t0 + n], in0=xp[:, o - 2 + j:o - 2 + j + n], scalar=cw[:, ch, j:j + 1],
                                                                            in1=xc[:, t0:t0 + n], op0=ALU.mult, op1=ALU.add), reads=[Txp, Tp4, Txc], writes=[Txc])
                        k.op("act", lambda e: e.activation(out=fmb[:, ch, :], in_=xc[:], func=AF.Silu), reads=[Txc], writes=[Tfmb])
                    xst = k.sb("xst", [128, NT, 256], BF16); Txst = T()
                    Bt = k.sb("Bt", [128, NT, 128], BF16); TBt = T()
                    ps_pre = PScope(k); ps_pre.__enter__()
                    p_tr = [k.ps("p4tr%d" % i, [128, 128], BF16) for i in range(2)]; Tp_tr = [PT(), PT()]
                    ntr = 0
                    for ti in range(NT):
                        ts_ = slice(ti * 128, (ti + 1) * 128)
                        for ch in range(3):
                            pp = p_tr[ntr % 2]; tp = Tp_tr[ntr % 2]
                            k.op("pe", lambda e: e.transpose(pp[:], fmb[:, ch, ts_], ident_b[:]), reads=[Tfmb, Tc], writes=[tp])
                            dst = xst[:, ti, ch * 128:(ch + 1) * 128] if ch < 2 else Bt[:, ti, :]
                            tdst = Txst if ch < 2 else TBt
                            if ntr % 2 == 0:
                                k.op("act", lambda e: e.activation(out=dst, in_=pp[:], func=AF.Copy), reads=[tp], writes=[tdst])
                            else:
                                k.op("dve", lambda e: e.tensor_copy(out=dst, in_=pp[:]), reads=[tp], writes=[tdst])
                            ntr += 1
                    dt = k.sb("dt", [128, NT, 8]); Tdt = T()
                    la = k.sb("la", [128, NT, 8]); Tla = T()
                    k.dma("sp", dt[:], ut_d[b, :, 512:520].rearrange("(n p) c -> p n c", p=128), reads=[Tut[b]], writes=[Tdt])
                    k.op("dve", lambda e: e.tensor_tensor(out=dt[:], in0=dt[:], in1=dtb[:, None, :].to_broadcast([128, NT, 8]), op=ALU.add), reads=[Tdt, Tp4], writes=[Tdt])
                    k.op("act", lambda e: e.activation(out=dt[:], in_=dt[:], func=AF.Exp), reads=[Tdt], writes=[Tdt])
                    k.op("act", lambda e: e.activation(out=dt[:], in_=dt[:], func=AF.Ln, bias=1.0), reads=[Tdt], writes=[Tdt])
                    k.op("dve", lambda e: e.tensor_tensor(out=la[:], in0=dt[:], in1=aneg[:, None, :].to_broadcast([128, NT, 8]), op=ALU.mult), reads=[Tdt, Tp4], writes=[Tla])
                    p_ct = k.ps("p_ct", [128, 2, NT, 8]); Tp_ct = PT()
                    for ti in range(NT):
                        k.op("pe", lambda e: e.matmul(p_ct[:, 0, ti, 0:4], lhsT=triUf[:], rhs=la[:, ti, 0:4], start=True, stop=True), reads=[Tla, Tp4], writes=[Tp_ct])
                        k.op("pe", lambda e: e.matmul(p_ct[:, 0, ti, 4:8], lhsT=triLf[:], rhs=la[:, ti, 4:8], start=True, stop=True), reads=[Tla, Tp4], writes=[Tp_ct])
                        k.op("pe", lambda e: e.matmul(p_ct[:, 1, ti, :], lhsT=ones_f[:], rhs=la[:, ti, :], start=True, stop=True), reads=[Tla, Tc], writes=[Tp_ct])
                    cexp = k.sb("cexp", [128, NT, 8]); etot = k.sb("etot4", [128, NT, 8]); wend = k.sb("wend", [128, NT, 8]); Tce = T()
                    k.op("dve", lambda e: e.tensor_tensor(out=wend[:], in0=p_ct[:, 1], in1=p_ct[:, 0], op=ALU.subtract), reads=[Tp_ct], writes=[Tce]) if False else None
                    k.op("act", lambda e: e.activation(out=cexp[:], in_=p_ct[:, 0], func=AF.Copy), reads=[Tp_ct], writes=[Tce])
                    k.op("dve", lambda e: e.tensor_tensor(out=wend[:], in0=p_ct[:, 1], in1=cexp[:], op=ALU.subtract), reads=[Tp_ct, Tce], writes=[Tce])
                    k.op("act", lambda e: e.activation(out=wend[:], in_=wend[:], func=AF.Exp), reads=[Tce], writes=[Tce])
                    k.op("dve", lambda e: e.tensor_tensor(out=wend[:], in0=wend[:], in1=dt[:], op=ALU.mult), reads=[Tce, Tdt], writes=[Tce])
                    k.op("act", lambda e: e.activation(out=cexp[:], in_=cexp[:], func=AF.Exp), reads=[Tce], writes=[Tce])
                    k.op("act", lambda e: e.activation(out=etot[:], in_=p_ct[:, 1], func=AF.Exp), reads=[Tp_ct], writes=[Tce])
                    ps_pre.__exit__(None, None, None)
                    ps_loop = PScope(k); ps_loop.__enter__()
                    Yacc = k.sb("Yacc", [128, NT, 256]); TY = T()
                    inc4 = [k.sb("inc4_%d" % d, [128, 4, 128]) for d in range(2)]
                    ngm = [k.sb("ngm%d" % d, [128, 4, 128]) for d in range(2)]
                    k.dma("sp", inc4[0][:], triUf4_d, writes=[Tp4]); k.dma("sp", inc4[1][:], triLf4_d, writes=[Tp4])
                    k.dma("sp", ngm[0][:], negmf_d, writes=[Tp4]); k.dma("sp", ngm[1][:], negmb_d, writes=[Tp4])
                    ngmb = [k.sb("ngmb%d" % d, [128, 4, 128], BF16) for d in range(2)]
                    for d in range(2):
                        k.op("act", lambda e: e.activation(out=ngmb[d][:], in_=ngm[d][:], func=AF.Copy), reads=[Tp4], writes=[Tp4])
                    etH = k.sb("etH", [128, NT, 2, 2]); TetH = T()
                    et4 = etot[:].rearrange("p n (d h) -> p n d h", d=2)
                    for g in range(2):
                        gs = slice(g * 64, (g + 1) * 64)
                        k.op("dve", lambda e: e.tensor_copy(out=etH[gs], in_=et4[gs, :, :, 2 * g:2 * g + 2]), reads=[Tce], writes=[TetH])
                    Rall = k.sb("Rall", [128, NT, 4, 128]); TRall = T()
                    Bwall = k.sb("Bwall", [128, NT, 2, 2, 64], BF16); TBwall = T()
                    Es = [k.sb("Es%d" % i, [128, 4, 128]) for i in range(2)]; TEs = [T(), T()]
                    Ab = [k.sb("Ab%d" % i, [128, 4, 128], BF16) for i in range(2)]; TAb = [T(), T()]
                    Bw = [k.sb("Bw%d" % i, [128, 2, 2, 64], BF16) for i in range(2)]; TBw = [T(), T()]
                    tmpy = [k.sb("tmpy%d" % i, [128, 4, 64]) for i in range(2)]; Ttmpy = [T(), T()]
                    S32 = k.sb("S32_4", [128, 2, 64]); TS32 = T()
                    STb = k.sb("STb4", [128, 2, 64], BF16); TSTb = T()
                    p_g = [k.ps("p_g%d" % i, [128, 128]) for i in range(2)]; Tp_g = [PT(), PT()]
                    p_seg = [k.ps("p_seg%d" % i, [128, 4, 128]) for i in range(2)]; Tp_seg = [PT(), PT()]
                    p_y1 = k.ps("p_y1", [128, 4, 64]); Tp_y1 = PT()
                    p_y2 = [k.ps("p_y2%d" % i, [128, 2, 64]) for i in range(2)]; Tp_y2 = [PT(), PT()]
                    p_st = k.ps("p_st4", [128, 2, 64]); Tp_st = PT()
                    Bt4 = Bt[:].rearrange("p n (g c) -> p n g c", g=2)
                    def ssd_front(d, oi, ti, i2):
                        ts_ = slice(ti * 128, (ti + 1) * 128)
                        d4 = slice(d * 4, d * 4 + 4)
                        strm = strLf if d == 0 else strUf
                        for g in range(2):
                            gs = slice(g * 64, (g + 1) * 64)
                            k.op("pe", lambda e: e.matmul(p_g[g][:], lhsT=fmb[gs, 2, ts_], rhs=fmb[gs, 3, ts_], start=True, stop=True),
                                 reads=[Tfmb], writes=[Tp_g[g]])
                        if oi == 0:
                            for t2 in range(NT):
                                eng_ = "pool" if t2 % 2 == 0 else "dve"
                                k.op(eng_, lambda e: e.tensor_tensor(out=Rall[:, t2], in0=inc4[d][:], in1=la[:, t2, d4, None].to_broadcast([128, 4, 128]), op=ALU.mult),
                                     reads=[Tla, Tp4], writes=[TRall])
                            k.op("pool", lambda e: e.tensor_tensor(out=Bwall[:], in0=Bt4[:, :, :, None, :].to_broadcast([128, NT, 2, 2, 64]),
                                                                   in1=wend[:, :, d4].rearrange("p n (g j) -> p n g j", g=2)[:, :, :, :, None].to_broadcast([128, NT, 2, 2, 64]),
                                                                   op=ALU.mult), reads=[TBt, Tce], writes=[TBwall])
                        k.op("pe", lambda e: e.matmul(p_seg[i2][:].rearrange("p h t -> p (h t)"), lhsT=strm[:], rhs=Rall[:, ti].rearrange("p h t -> p (h t)"),
                                                      start=True, stop=False), reads=[TRall, Tp4], writes=[Tp_seg[i2]])
                        k.op("pe", lambda e: e.matmul(p_seg[i2][:].rearrange("p h t -> p (h t)"), lhsT=ident_b[:], rhs=ngmb[d][:].rearrange("p h t -> p (h t)"),
                                                      start=False, stop=True), reads=[Tc, Tp4], writes=[Tp_seg[i2]])
                        k.op("act", lambda e: e.activation(out=Es[i2][:], in_=p_seg[i2][:], func=AF.Exp), reads=[Tp_seg[i2]], writes=[TEs[i2]])
                        k.op("dve", lambda e: e.tensor_tensor(out=Es[i2][:], in0=Es[i2][:], in1=dt[:, ti, d4, None].to_broadcast([128, 4, 128]), op=ALU.mult),
                             reads=[TEs[i2], Tdt], writes=[TEs[i2]])
                        for g in range(2):
                            k.op("dve", lambda e: e.tensor_tensor(out=Ab[i2][:, 2 * g:2 * g + 2, :], in0=Es[i2][:, 2 * g:2 * g + 2, :],
                                                                  in1=p_g[g][:, None, :].to_broadcast([128, 2, 128]), op=ALU.mult),
                                 reads=[TEs[i2], Tp_g[g]], writes=[TAb[i2]])

                    def ssd_back(d, oi, ti, i2):
                        ts_ = slice(ti * 128, (ti + 1) * 128)
                        for h in range(4):
                            k.op("pe", lambda e: e.matmul(p_y1[:, h, :], lhsT=Ab[i2][:, h, :], rhs=xst[:, ti, h * 64:(h + 1) * 64], start=True, stop=True),
                                 reads=[TAb[i2], Txst], writes=[Tp_y1])
                        yacc = Yacc[:, ti, :].rearrange("p (h c) -> p h c", h=4)
                        if oi > 0:
                            for h in range(4):
                                g = h // 2; gs = slice(g * 64, (g + 1) * 64)
                                k.op("pe", lambda e: e.matmul(p_y2[g][:, h % 2, :], lhsT=fmb[gs, 3, ts_], rhs=STb[gs, h % 2, :], start=True, stop=True),
                                     reads=[Tfmb, TSTb], writes=[Tp_y2[g]])
                            for g in range(2):
                                k.op("dve", lambda e: e.tensor_tensor(out=tmpy[i2][:, 2 * g:2 * g + 2, :], in0=p_y2[g][:],
                                                                      in1=cexp[:, ti, d * 4 + 2 * g:d * 4 + 2 * g + 2, None].to_broadcast([128, 2, 64]), op=ALU.mult),
                                     reads=[Tp_y2[g], Tce], writes=[Ttmpy[i2]])
                            if d == 0:
                                k.op("dve", lambda e: e.tensor_tensor(out=yacc, in0=p_y1[:], in1=tmpy[i2][:], op=ALU.add), reads=[Tp_y1, Ttmpy[i2]], writes=[TY])
                            else:
                                k.op("pool", lambda e: e.tensor_tensor(out=yacc, in0=yacc, in1=tmpy[i2][:], op=ALU.add), reads=[TY, Ttmpy[i2]], writes=[TY])
                                k.op("dve", lambda e: e.tensor_tensor(out=yacc, in0=yacc, in1=p_y1[:], op=ALU.add), reads=[TY, Tp_y1], writes=[TY])
                        else:
                            if d == 0:
                                k.op("dve", lambda e: e.tensor_copy(out=yacc, in_=p_y1[:]), reads=[Tp_y1], writes=[TY])
                            else:
                                k.op("dve", lambda e: e.tensor_tensor(out=yacc, in0=yacc, in1=p_y1[:], op=ALU.add), reads=[TY, Tp_y1], writes=[TY])
                        if oi == NT - 1:
                            return
                        for h in range(4):
                            g = h // 2; gs = slice(g * 64, (g + 1) * 64)
                            k.op("pe", lambda e: e.matmul(p_st[gs, h % 2, :], lhsT=Bwall[:, ti, g, h % 2, :], rhs=xst[:, ti, h * 64:(h + 1) * 64], start=True, stop=True,
                                                          tile_position=(0, g * 64)), reads=[TBwall, Txst], writes=[Tp_st])
                        if oi == 0:
                            k.op("dve", lambda e: e.tensor_copy(out=S32[:], in_=p_st[:]), reads=[Tp_st], writes=[TS32])
                        else:
                            k.op("pool", lambda e: e.tensor_tensor(out=S32[:], in0=S32[:], in1=etH[:, ti, d, :, None].to_broadcast([128, 2, 64]), op=ALU.mult),
                                 reads=[TS32, TetH], writes=[TS32])
                            k.op("dve", lambda e: e.tensor_tensor(out=S32[:], in0=S32[:], in1=p_st[:], op=ALU.add), reads=[TS32, Tp_st], writes=[TS32])
                        k.op("pool", lambda e: e.tensor_copy(out=STb[:], in_=S32[:]), reads=[TS32], writes=[TSTb])

                    seq = []
                    for d in range(2):
                        order = list(range(NT)) if d == 0 else [1, 0] + list(range(NT - 1, 1, -1))
                        for oi, ti in enumerate(order):
                            seq.append((d, oi, ti, len(seq) % 2))
                    ssd_front(*seq[0])
                    for i_ in range(len(seq)):
                        if i_ + 1 < len(seq):
                            ssd_front(*seq[i_ + 1])
                        ssd_back(*seq[i_])
                    zz = k.sb("zz", [128, NT, 256]); Tzz = T()
                    k.dma("sp", zz[:], ut_d[b, :, 256:512].rearrange("(n p) c -> p n c", p=128), reads=[Tut[b]], writes=[Tzz])
                    k.op("act", lambda e: e.activation(out=zz[:], in_=zz[:], func=AF.Silu), reads=[Tzz], writes=[Tzz])
                    tq = k.sb("tq", [128, NT, 256]); Ttq = T()
                    k.op("dve", lambda e: e.tensor_tensor(out=tq[:], in0=xst[:], in1=dsk[:, None, :].to_broadcast([128, NT, 256]), op=ALU.mult), reads=[Txst, Tp4], writes=[Ttq])
                    k.op("dve", lambda e: e.tensor_tensor(out=Yacc[:], in0=Yacc[:], in1=tq[:], op=ALU.add), reads=[TY, Ttq], writes=[TY])
                    k.op("dve", lambda e: e.tensor_tensor(out=Yacc[:], in0=Yacc[:], in1=zz[:], op=ALU.mult), reads=[TY, Tzz], writes=[TY])
                    k.op("pool", lambda e: e.tensor_tensor(out=tq[:], in0=Yacc[:], in1=Yacc[:], op=ALU.mult), reads=[TY], writes=[Ttq])
                    ssq = k.sb("ssq", [128, NT]); Tssq = T()
                    k.op("dve", lambda e: e.reduce_sum(out=ssq[:], in_=tq[:], axis=AX.X), reads=[Ttq], writes=[Tssq])
                    k.op("act", lambda e: e.activation(out=ssq[:], in_=ssq[:], func=AF.Sqrt, scale=1.0 / 256, bias=EPS), reads=[Tssq], writes=[Tssq])
                    k.op("dve", lambda e: e.reciprocal(out=ssq[:], in_=ssq[:]), reads=[Tssq], writes=[Tssq])
                    k.op("dve", lambda e: e.tensor_tensor(out=Yacc[:], in0=Yacc[:], in1=ssq[:, :, None].to_broadcast([128, NT, 256]), op=ALU.mult), reads=[TY, Tssq], writes=[TY])
                    yob = k.sb("yob", [128, NT, 256], BF16); Tyob = T()
                    k.op("dve", lambda e: e.tensor_tensor(out=yob[:], in0=Yacc[:], in1=snw[:, None, :].to_broadcast([128, NT, 256]), op=ALU.mult), reads=[TY, Tp4], writes=[Tyob])
                    ps_loop.__exit__(None, None, None)
                    ps_epi = PScope(k); ps_epi.__enter__()
                    p_tr = [k.ps("p4tre%d" % i, [128, 128], BF16) for i in range(2)]; Tp_tr = [PT(), PT()]
                    yoT = k.sb("yoT", [128, 2, S], BF16); TyoT = T()
                    for ti in range(NT):
                        for ch in range(2):
                            pp = p_tr[ntr % 2]; tp = Tp_tr[ntr % 2]
                            k.op("pe", lambda e: e.transpose(pp[:], yob[:, ti, ch * 128:(ch + 1) * 128], ident_b[:]), reads=[Tyob, Tc], writes=[tp])
                            if ntr % 2 == 0:
                                k.op("act", lambda e: e.activation(out=yoT[:, ch, ti * 128:(ti + 1) * 128], in_=pp[:], func=AF.Copy), reads=[tp], writes=[TyoT])
                            else:
                                k.op("dve", lambda e: e.tensor_copy(out=yoT[:, ch, ti * 128:(ti + 1) * 128], in_=pp[:]), reads=[tp], writes=[TyoT])
                            ntr += 1
                    k.dma("sp", yT_d[b, 512:768, :].rearrange("(c p) t -> p c t", p=128), yoT[:], reads=[TyoT], writes=[TyT[b]])
                    ps_epi.__exit__(None, None, None)
                if cfg.upto < 5:
                    continue
                need_ctx = l < L - 1
                with Stage(k):
                    Tp5 = T()
                    qan = k.sb("qan", [128, 192]); kvan = k.sb("kvan", [128, 128]); qnr = k.sb("qnr", [128, 96]); knr = k.sb("knr", [128, 96])
                    wq = k.sb("wq", [96, 2, 384], BF16); wkv = k.sb("wkv", [128, 512], BF16)
                    rope = k.sb("rope", [128, 16, 2, 16]); invn3 = k.sb("invn3", [128, 3]); invn8 = k.sb("invn8", [128, 8])
                    for dst, src in ((qan, ml_qan_d[:, l]), (kvan, ml_kvan_d[:, l]), (qnr, ml_qn_d[:, l]), (knr, ml_kn_d[:, l]), (rope, rope_d),
                                     (invn3, invn3_d), (invn8, invn8_d)):
                        k.dma("sp", dst[:], src, writes=[Tp5])
                    wqs = k.sb("wqs", [96, 2, 384]); wkvs = k.sb("wkvs", [128, 512]); Twqs = T()
                    k.dma("sp", wqs[:], ml_wq_d[l].rearrange("(c p) n -> p c n", p=96), writes=[Twqs])
                    k.dma("sp", wkvs[:], ml_wkv_d[l], writes=[Twqs])
                    k.op("pool", lambda e: e.tensor_copy(out=wq[:], in_=wqs[:]), reads=[Twqs], writes=[Tp5])
                    k.op("pool", lambda e: e.tensor_copy(out=wkv[:], in_=wkvs[:]), reads=[Twqs], writes=[Tp5])
                    QT = k.sb("QT", [96, 4, S], BF16); TQT = T()
                    KT = k.sb("KT", [96, 4, S], BF16); TKT = T()
                    Va = k.sb("Va", [128, NT, 4, 65], BF16); TVa = T()
                    k.op("pool", lambda e: e.memset(Va[:], 1.0), writes=[TVa])
                    um = [k.sb("um%d" % i, [128, 352]) for i in range(2)]; Tum = [T(), T()]
                    ps_prep = PScope(k); ps_prep.__enter__()
                    def dbl(name, shape, dt_=F32):
                        return [k.sb("%s_%d" % (name, i), shape, dt_) for i in range(2)], [T(), T()]
                    sqL, TsqL = dbl("sq5", [128, 512]); ss3L, Tss3L = dbl("ss3", [128, 3]); ss8L, Tss8L = dbl("ss8", [128, 12])
                    cnL, TcnL = dbl("cn", [128, 320], BF16); cTtL, TcTL = dbl("cTt", [128, 3, 128], BF16)
                    qfL, TqfL = dbl("qf", [128, 4, 96]); kvfL, TkvfL = dbl("kvf", [128, 4, 128])
                    rbL, TrbL = dbl("rb", [128, 5, 32]); raL, TraL = dbl("ra", [128, 4, 5, 16])
                    QbL, TQbL = dbl("Qb", [128, 4, 96], BF16); KbL, TKbL = dbl("Kb", [128, 4, 96], BF16)
                    p_trA = [k.ps("p5trA%d" % i, [128, 4, 128], BF16) for i in range(2)]; Tp_trA = [PT(), PT()]
                    p_trB = [k.ps("p5trB%d" % i, [128, 8, 128], BF16) for i in range(2)]; Tp_trB = [PT(), PT()]
                    p_qL = [k.ps("p_q%d" % i, [128, 384]) for i in range(2)]; Tp_qL = [PT(), PT()]
                    p_kvL = [k.ps("p_kv%d" % i, [128, 512]) for i in range(2)]; Tp_kvL = [PT(), PT()]
                    def mla_tile(ti):
                        U = um[ti % 2]; tU = Tum[ti % 2]
                        j2 = ti % 2
                        sq = sqL[j2]; Tsq = TsqL[j2]; ss3 = ss3L[j2]; Tss3 = Tss3L[j2]; ss8 = ss8L[j2]; Tss8 = Tss8L[j2]
                        cn = cnL[j2]; Tcn = TcnL[j2]; cTt = cTtL[j2]; TcT = TcTL[j2]; qf = qfL[j2]; Tqf = TqfL[j2]; kvf = kvfL[j2]; Tkvf = TkvfL[j2]
                        rb = rbL[j2]; Trb = TrbL[j2]; ra = raL[j2]; Tra = TraL[j2]; Qb = QbL[j2]; TQb = TQbL[j2]; Kb = KbL[j2]; TKb = TKbL[j2]
                        p_q = p_qL[j2]; Tp_q = Tp_qL[j2]; p_kv = p_kvL[j2]; Tp_kv = Tp_kvL[j2]
                        p_tr = [p_trB[j2][:, 0:4, :], p_trB[j2][:, 4:8, :]]; Tp_tr = [Tp_trB[j2], Tp_trB[j2]]
                        k.dma("sp", U[:], ut_d[b, ti * 128:(ti + 1) * 128, 520:872], reads=[Tut[b]], writes=[tU])
                        k.op("pool", lambda e: e.tensor_tensor(out=sq[:, 0:352], in0=U[:], in1=U[:], op=ALU.mult), reads=[tU], writes=[Tsq])
                        for j, (a_, b_) in enumerate(((0, 192), (192, 320), (320, 352))):
                            k.op("dve", lambda e: e.reduce_sum(out=ss3[:, j:j + 1], in_=sq[:, a_:b_], axis=AX.X), reads=[Tsq], writes=[Tss3])
                        k.op("dve", lambda e: e.tensor_tensor(out=ss3[:], in0=ss3[:], in1=invn3[:], op=ALU.mult), reads=[Tss3, Tp5], writes=[Tss3])
                        k.op("act", lambda e: e.activation(out=ss3[:], in_=ss3[:], func=AF.Sqrt, bias=EPS), reads=[Tss3], writes=[Tss3])
                        k.op("dve", lambda e: e.reciprocal(out=ss3[:], in_=ss3[:]), reads=[Tss3], writes=[Tss3])
                        k.op("dve", lambda e: e.scalar_tensor_tensor(out=cn[:, 0:192], in0=U[:, 0:192], scalar=ss3[:, 0:1], in1=qan[:], op0=ALU.mult, op1=ALU.mult),
                             reads=[tU, Tss3, Tp5], writes=[Tcn])
                        k.op("dve", lambda e: e.scalar_tensor_tensor(out=cn[:, 192:320], in0=U[:, 192:320], scalar=ss3[:, 1:2], in1=kvan[:], op0=ALU.mult, op1=ALU.mult),
                             reads=[tU, Tss3, Tp5], writes=[Tcn])
                        k.op("dve", lambda e: e.scalar_tensor_tensor(out=rb[:, 4, :], in0=U[:, 320:352], scalar=ss3[:, 2:3], in1=knr[:, 64:96], op0=ALU.mult, op1=ALU.mult),
                             reads=[tU, Tss3, Tp5], writes=[Trb])
                        pt = p_trA[j2]; tpt = Tp_trA[j2]
                        k.op("pe", lambda e: e.transpose(pt[0:96, 0, :], cn[:, 0:96], ident_b[:]), reads=[Tcn, Tc], writes=[tpt])
                        k.op("pe", lambda e: e.transpose(pt[0:96, 1, :], cn[:, 96:192], ident_b[:]), reads=[Tcn, Tc], writes=[tpt])
                        k.op("pe", lambda e: e.transpose(pt[:, 2, :], cn[:, 192:320], ident_b[:]), reads=[Tcn, Tc], writes=[tpt])
                        k.op("act", lambda e: e.activation(out=cTt[0:96, 0:2, :], in_=pt[0:96, 0:2, :], func=AF.Copy), reads=[tpt], writes=[TcT])
                        k.op("act", lambda e: e.activation(out=cTt[:, 2, :], in_=pt[:, 2, :], func=AF.Copy), reads=[tpt], writes=[TcT])
                        for c_ in range(2):
                            k.op("pe", lambda e: e.matmul(p_q[:], lhsT=cTt[0:96, c_, :], rhs=wq[:, c_, :], start=(c_ == 0), stop=(c_ == 1)), reads=[TcT, Tp5], writes=[Tp_q])
                        k.op("pe", lambda e: e.matmul(p_kv[:], lhsT=cTt[:, 2, :], rhs=wkv[:], start=True, stop=True), reads=[TcT, Tp5], writes=[Tp_kv])
                        k.op("act", lambda e: e.activation(out=qf[:].rearrange("p h c -> p (h c)"), in_=p_q[:], func=AF.Copy), reads=[Tp_q], writes=[Tqf])
                        k.op("dve", lambda e: e.tensor_copy(out=kvf[:].rearrange("p h c -> p (h c)"), in_=p_kv[:]), reads=[Tp_kv], writes=[Tkvf])
                        sq4 = sq[:, 0:384].rearrange("p (h c) -> p h c", c=96)
                        k.op("pool", lambda e: e.tensor_tensor(out=sq4, in0=qf[:], in1=qf[:], op=ALU.mult), reads=[Tqf], writes=[Tsq])
                        k.op("dve", lambda e: e.reduce_sum(out=ss8[:, 0:4], in_=sq4[:, :, 0:64], axis=AX.X), reads=[Tsq], writes=[Tss8])
                        k.op("dve", lambda e: e.reduce_sum(out=ss8[:, 4:8], in_=sq4[:, :, 64:96], axis=AX.X), reads=[Tsq], writes=[Tss8])
                        sq5 = sq[:, 0:512].rearrange("p (h c) -> p h c", c=128)
                        k.op("pool", lambda e: e.tensor_tensor(out=sq5, in0=kvf[:], in1=kvf[:], op=ALU.mult), reads=[Tkvf, Tss8], writes=[Tsq])
                        k.op("dve", lambda e: e.reduce_sum(out=ss8[:, 8:12], in_=sq5[:, :, 0:64], axis=AX.X), reads=[Tsq], writes=[Tss8])
                        k.op("dve", lambda e: e.tensor_tensor(out=ss8[:, 0:8], in0=ss8[:, 0:8], in1=invn8[:], op=ALU.mult), reads=[Tss8, Tp5], writes=[Tss8])
                        k.op("dve", lambda e: e.tensor_tensor(out=ss8[:, 8:12], in0=ss8[:, 8:12], in1=invn8[:, 0:4], op=ALU.mult), reads=[Tss8, Tp5], writes=[Tss8])
                        k.op("act", lambda e: e.activation(out=ss8[:], in_=ss8[:], func=AF.Sqrt, bias=EPS), reads=[Tss8], writes=[Tss8])
                        k.op("dve", lambda e: e.reciprocal(out=ss8[:], in_=ss8[:]), reads=[Tss8], writes=[Tss8])
                        k.op("dve", lambda e: e.tensor_tensor(out=qf[:, :, 0:64], in0=qf[:, :, 0:64], in1=ss8[:, 0:4, None].to_broadcast([128, 4, 64]), op=ALU.mult),
                             reads=[Tqf, Tss8], writes=[Tqf])
                        k.op("dve", lambda e: e.tensor_tensor(out=Qb[:, :, 0:64], in0=qf[:, :, 0:64], in1=qnr[:, None, 0:64].to_broadcast([128, 4, 64]), op=ALU.mult),
                             reads=[Tqf, Tp5], writes=[TQb])
                        k.op("dve", lambda e: e.tensor_tensor(out=qf[:, :, 64:96], in0=qf[:, :, 64:96], in1=ss8[:, 4:8, None].to_broadcast([128, 4, 32]), op=ALU.mult),
                             reads=[Tqf, Tss8], writes=[Tqf])
                        k.op("dve", lambda e: e.tensor_tensor(out=rb[:, 0:4, :], in0=qf[:, :, 64:96], in1=qnr[:, None, 64:96].to_broadcast([128, 4, 32]), op=ALU.mult),
                             reads=[Tqf, Tp5], writes=[Trb])
                        k.op("dve", lambda e: e.tensor_tensor(out=kvf[:, :, 0:64], in0=kvf[:, :, 0:64], in1=ss8[:, 8:12, None].to_broadcast([128, 4, 64]), op=ALU.mult),
                             reads=[Tkvf, Tss8], writes=[Tkvf])
                        k.op("dve", lambda e: e.tensor_tensor(out=Kb[:, :, 0:64], in0=kvf[:, :, 0:64], in1=knr[:, None, 0:64].to_broadcast([128, 4, 64]), op=ALU.mult),
                             reads=[Tkvf, Tp5], writes=[TKb])
                        k.op("pool", lambda e: e.tensor_copy(out=Va[:, ti, :, 0:64], in_=kvf[:, :, 64:128]), reads=[Tkvf], writes=[TVa])
                        if ti >= 2:
                            rb4 = rb[:].rearrange("p h (j two) -> p h j two", two=2)
                            cs = rope[:, ti - 2, 0, None, :].to_broadcast([128, 5, 16]); sn = rope[:, ti - 2, 1, None, :].to_broadcast([128, 5, 16])
                            k.op("dve", lambda e: e.tensor_tensor(out=ra[:, 0], in0=rb4[:, :, :, 0], in1=cs, op=ALU.mult), reads=[Trb, Tp5], writes=[Tra])
                            k.op("dve", lambda e: e.tensor_tensor(out=ra[:, 1], in0=rb4[:, :, :, 1], in1=sn, op=ALU.mult), reads=[Trb, Tp5], writes=[Tra])
                            k.op("pool", lambda e: e.tensor_tensor(out=ra[:, 2], in0=rb4[:, :, :, 0], in1=sn, op=ALU.mult), reads=[Trb, Tp5], writes=[Tra])
                            k.op("pool", lambda e: e.tensor_tensor(out=ra[:, 3], in0=rb4[:, :, :, 1], in1=cs, op=ALU.mult), reads=[Trb, Tp5], writes=[Tra])
                            k.op("dve", lambda e: e.tensor_tensor(out=rb4[:, :, :, 0], in0=ra[:, 0], in1=ra[:, 1], op=ALU.subtract), reads=[Tra, Trb], writes=[Trb])
                            k.op("dve", lambda e: e.tensor_tensor(out=rb4[:, :, :, 1], in0=ra[:, 2], in1=ra[:, 3], op=ALU.add), reads=[Tra, Trb], writes=[Trb])
                        k.op("dve", lambda e: e.tensor_copy(out=Qb[:, :, 64:96], in_=rb[:, 0:4, :]), reads=[Trb], writes=[TQb])
                        k.op("dve", lambda e: e.tensor_copy(out=Kb[:, :, 64:96], in_=rb[:, 4:5, :].to_broadcast([128, 4, 32])), reads=[Trb], writes=[TKb])
                        for (src, tsrc, dstT, tdst, pi) in ((Qb, TQb, QT, TQT, 1), (Kb, TKb, KT, TKT, 0)):
                            pt = p_tr[pi]; tpt = Tp_tr[pi]
                            for h in range(4):
                                k.op("pe", lambda e: e.transpose(pt[0:96, h, :], src[:, h, :], ident_b[:]), reads=[tsrc, Tc], writes=[tpt])
                            if pi == 1:
                                k.op("act", lambda e: e.activation(out=dstT[:, :, ti * 128:(ti + 1) * 128], in_=pt[0:96, :, :], func=AF.Copy), reads=[tpt], writes=[tdst])
                            else:
                                k.op("dve", lambda e: e.tensor_copy(out=dstT[:, :, ti * 128:(ti + 1) * 128], in_=pt[0:96, :, :]), reads=[tpt], writes=[tdst])
                    for ti0 in range(0, NT, 2):
                        LA = []; k.defer = LA; mla_tile(ti0)
                        LB = []; k.defer = LB; mla_tile(ti0 + 1)
                        k.defer = None
                        while LA or LB:
                            k.flush(LA, 1); k.flush(LB, 1)
                    ps_prep.__exit__(None, None, None)
                    ps_att = PScope(k); ps_att.__enter__()
                    p_tr = [k.ps("p5trC%d" % i, [128, 4, 128], BF16) for i in range(2)]; Tp_tr = [PT(), PT()]
                    PTt = [k.sb("PT%d" % i, [128, 512], BF16) for i in range(3)]; TPT = [T() for _ in range(3)]
                    p_s = [k.ps("p_s%d" % i, [128, 512]) for i in range(2)]; Tp_s = [PT(), PT()]
                    p_o = [k.ps("p_o%d" % i, [128, 4, 65]) for i in range(2)]; Tp_o = [PT(), PT()]
                    rec = k.sb("rec", [128, 4]); Trec = T()
                    ym = k.sb("ym", [128, NT, 256], BF16); Tym = T()
                    sc = 96.0 ** -0.5
                    nsc = 0; nh = 0
                    qblocks = BLOCKS if need_ctx else BLOCKS[1:]
                    its = []
                    for (q0, qn_) in qblocks:
                        keys = list(range(0, 2) if q0 == 0 else range(NT))
                        for h in range(4):
                            for ki, kt_ in enumerate(keys):
                                its.append((q0, qn_, h, ki, kt_, len(keys), len(its)))

                    def att_qk(q0, qn_, h, ki, kt_, nk, j):
                        ps_ = p_s[j % 2]; tps = Tp_s[j % 2]; P = PTt[j % 3]; tP = TPT[j % 3]
                        k.op("pe", lambda e: e.matmul(ps_[:, 0:qn_], lhsT=KT[:, h, kt_ * 128:(kt_ + 1) * 128], rhs=QT[:, h, q0:q0 + qn_], start=True, stop=True),
                             reads=[TKT, TQT], writes=[tps])
                        k.op("act", lambda e: e.activation(out=P[:, 0:qn_], in_=ps_[:, 0:qn_], func=AF.Exp, scale=sc), reads=[tps], writes=[tP])

                    def att_pv(q0, qn_, h, ki, kt_, nk, j):
                        P = PTt[j % 3]; tP = TPT[j % 3]
                        grp = j // 1
                        nq = qn_ // 128
                        gidx = (q0, h)
                        if ki == 0:
                            att_state["nh"] += 1
                        po = p_o[att_state["nh"] % 2]; tpo = Tp_o[att_state["nh"] % 2]
                        for qs_ in range(nq):
                            k.op("pe", lambda e: e.matmul(po[:, qs_, :], lhsT=P[:, qs_ * 128:(qs_ + 1) * 128], rhs=Va[:, kt_, h, :],
                                                          start=(ki == 0 and qs_ == 0), stop=(ki == nk - 1), skip_group_check=True),
                                 reads=[tP, TVa], writes=[tpo])
                        if ki == nk - 1:
                            k.op("dve", lambda e: e.reciprocal(out=rec[:, 0:nq], in_=po[:, 0:nq, 64]), reads=[tpo], writes=[Trec])
                            t_0 = q0 // 128
                            k.op("dve", lambda e: e.tensor_tensor(out=ym[:, t_0:t_0 + nq, h * 64:(h + 1) * 64], in0=po[:, 0:nq, 0:64],
                                                                  in1=rec[:, 0:nq, None].to_broadcast([128, nq, 64]), op=ALU.mult), reads=[tpo, Trec], writes=[Tym])

                    att_state = {"nh": 0}
                    att_qk(*its[0])
                    for j in range(len(its)):
                        if j + 1 < len(its):
                            att_qk(*its[j + 1])
                        att_pv(*its[j])
                    yoT = k.sb("yoT5", [128, 2, S], BF16); TyoT = T()
                    if not need_ctx:
                        k.op("pool", lambda e: e.memset(yoT[:, :, 0:NCTX], 0.0), writes=[TyoT])
                    ntr = 0
                    for ti in range(0 if need_ctx else 2, NT):
                        for ch in range(2):
                            pp = p_tr[ntr % 2]; tp = Tp_tr[ntr % 2]
                            k.op("pe", lambda e: e.transpose(pp[:, 0, :], ym[:, ti, ch * 128:(ch + 1) * 128], ident_b[:]), reads=[Tym, Tc], writes=[tp])
                            if ntr % 2 == 0:
                                k.op("act", lambda e: e.activation(out=yoT[:, ch, ti * 128:(ti + 1) * 128], in_=pp[:, 0, :], func=AF.Copy), reads=[tp], writes=[TyoT])
                            else:
                                k.op("dve", lambda e: e.tensor_copy(out=yoT[:, ch, ti * 128:(ti + 1) * 128], in_=pp[:, 0, :]), reads=[tp], writes=[TyoT])
                            ntr += 1
                    k.dma("sp", yT_d[b, 768:1024, :].rearrange("(c p) t -> p c t", p=128), yoT[:], reads=[TyoT], writes=[TyT[b]])
                    ps_att.__exit__(None, None, None)
                if cfg.upto < 6:
                    continue
                need_ctx = l < L - 1
                with Stage(k):
                    Tp6 = T()
                    wo = k.sb("wo", [128, 8, D], BF16); wrt = k.sb("wrt", [128, 8, NEXP], BF16)
                    cst = Caster(k, 128, D)
                    for kc in range(8):
                        cst.load(wo[:, kc, :], w_out_d[l, kc * 128:(kc + 1) * 128, :], 128, D, [Tp6])
                    wrs = k.sb("wrs", [128, 8, NEXP]); Twrs = T()
                    k.dma("sp", wrs[:], w_rt_d[l].rearrange("(c p) e -> p c e", p=128), writes=[Twrs])
                    k.op("pool", lambda e: e.tensor_copy(out=wrt[:], in_=wrs[:]), reads=[Twrs], writes=[Tp6])
                    G2 = k.sb("G2", [128, 2, 8]); Tg2 = T()
                    for i, mi in enumerate((b, 2)):
                        k.op("dve", lambda e: e.scalar_tensor_tensor(out=G2[:, i, :], in0=modT[:, l, 32:40, mi], scalar=1.0, in1=n2T[:, l, :],
                                                                    op0=ALU.add, op1=ALU.mult), reads=[Tmod, Tc], writes=[Tg2])
                    Yb = [k.sb("Yb%d" % i, [128, 8, 512], BF16) for i in range(2)]; TYb = [T(), T()]
                    Xb = [k.sb("Xb%d" % i, [128, 8, 512]) for i in range(2)]; TXb = [T(), T()]
                    Qs = k.sb("Qs", [128, 8, 512], BF16); TQs = T()
                    Rr6 = k.sb("Rr6", [128, 512]); TRr6 = T()
                    tm6 = [k.sb("tm6_%d" % i, [128, 512]) for i in range(2)]; Ttm6 = [T(), T()]
                    H2 = k.sb("H2", [128, 8, 512], BF16); TH2 = T()
                    h2o = [k.sb("h2o%d" % i, [128, D], BF16) for i in range(2)]; Th2o = [T(), T()]
                    Ee = k.sb("Ee", [16, 512]); TEe = T()
                    rc6 = k.sb("rc6", [16, 512]); Trc6 = T()
                    pwo = [k.ps("pwo%d" % i, [128, 512]) for i in range(2)]; Tpwo = [PT(), PT()]
                    pss = k.ps("pss6", [128, 512]); Tpss = PT()
                    ptr = [k.ps("ptr6_%d" % i, [128, 8, 128], BF16) for i in range(2)]; Tptr = [PT(), PT()]
                    prl = k.ps("prl", [16, 512]); Tprl = PT()
                    prs = k.ps("prs", [16, 512]); Tprs = PT()
                    ntr_box = [0]
                    blks6 = BLOCKS if need_ctx else BLOCKS[1:]

                    def s6_A(bi):
                        t0, n = blks6[bi]
                        isctx = (t0 == 0)
                        seg = 1 if isctx else 0
                        mi = 2 if isctx else b
                        Y = Yb[bi % 2]; tY = TYb[bi % 2]; X = Xb[bi % 2]; tX = TXb[bi % 2]
                        k.dma("sp", Y[:, :, 0:n], yT_d[b, :, t0:t0 + n].rearrange("(c p) t -> p c t", p=128), reads=[TyT[b]], writes=[tY])
                        k.dma("sp", X[:, :, 0:n], xT_d[b, :, t0:t0 + n].rearrange("(c p) t -> p c t", p=128), reads=[Tx[b]], writes=[tX])
                        for dch in range(8):
                            pp = pwo[dch % 2]; tp = Tpwo[dch % 2]
                            for c_ in range(8):
                                k.op("pe", lambda e: e.matmul(pp[:, 0:n], lhsT=wo[:, c_, dch * 128:(dch + 1) * 128], rhs=Y[:, c_, 0:n], start=(c_ == 0), stop=(c_ == 7)),
                                     reads=[Tp6, tY], writes=[tp])
                            k.op("dve", lambda e: e.scalar_tensor_tensor(out=X[:, dch, 0:n], in0=pp[:, 0:n], scalar=modT[:, l, 16 + dch, mi:mi + 1], in1=X[:, dch, 0:n],
                                                                        op0=ALU.mult, op1=ALU.add), reads=[tp, Tmod, tX], writes=[tX])
                        k.dma("sp", xT_d[b, :, t0:t0 + n].rearrange("(c p) t -> p c t", p=128), X[:, :, 0:n], reads=[tX], writes=[Tx[b]])

                    def s6_B(bi):
                        t0, n = blks6[bi]
                        isctx = (t0 == 0)
                        seg = 1 if isctx else 0
                        mi = 2 if isctx else b
                        X = Xb[bi % 2]; tX = TXb[bi % 2]
                        ntr = ntr_box[0]
                        k.op("act", lambda e: e.activation(out=Qs[:, :, 0:n], in_=X[:, :, 0:n], func=AF.Square), reads=[tX], writes=[TQs])
                        for kc in range(8):
                            k.op("pe", lambda e: e.matmul(pss[:, 0:n], lhsT=ones_b[:], rhs=Qs[:, kc, 0:n], start=(kc == 0), stop=(kc == 7)), reads=[TQs, Tc], writes=[Tpss])
                        k.op("act", lambda e: e.activation(out=Rr6[:, 0:n], in_=pss[:, 0:n], func=AF.Sqrt, scale=1.0 / D, bias=EPS), reads=[Tpss], writes=[TRr6])
                        k.op("dve", lambda e: e.reciprocal(out=Rr6[:, 0:n], in_=Rr6[:, 0:n]), reads=[TRr6], writes=[TRr6])
                        for kc in range(8):
                            tm = tm6[kc % 2]; ttm = Ttm6[kc % 2]
                            k.op("dve", lambda e: e.tensor_tensor(out=tm[:, 0:n], in0=X[:, kc, 0:n], in1=Rr6[:, 0:n], op=ALU.mult), reads=[tX, TRr6], writes=[ttm])
                            k.op("act", lambda e: e.activation(out=H2[:, kc, 0:n], in_=tm[:, 0:n], func=AF.Identity, scale=G2[:, seg, kc:kc + 1],
                                                               bias=modT[:, l, 24 + kc, mi:mi + 1]), reads=[ttm, Tg2, Tmod], writes=[TH2])
                        for tt in range(n // 128):
                            pp = ptr[ntr % 2]; tp = Tptr[ntr % 2]; ho = h2o[ntr % 2]; tho = Th2o[ntr % 2]
                            for kc in range(8):
                                k.op("pe", lambda e: e.transpose(pp[:, kc, :], H2[:, kc, tt * 128:(tt + 1) * 128], ident_b[:]), reads=[TH2, Tc], writes=[tp])
                            if ntr % 2 == 0:
                                k.op("act", lambda e: e.activation(out=ho[:], in_=pp[:].rearrange("p c t -> p (c t)"), func=AF.Copy), reads=[tp], writes=[tho])
                            else:
                                k.op("dve", lambda e: e.tensor_copy(out=ho[:], in_=pp[:].rearrange("p c t -> p (c t)")), reads=[tp], writes=[tho])
                            k.dma("sp", h2t_d[b, t0 + tt * 128:t0 + (tt + 1) * 128, :], ho[:], reads=[tho], writes=[Th2[b]])
                            ntr += 1
                        for kc in range(8):
                            k.op("pe", lambda e: e.matmul(prl[:, 0:n], lhsT=wrt[:, kc, :], rhs=H2[:, kc, 0:n], start=(kc == 0), stop=(kc == 7)), reads=[Tp6, TH2], writes=[Tprl])
                        k.op("act", lambda e: e.activation(out=Ee[:, 0:n], in_=prl[:, 0:n], func=AF.Exp), reads=[Tprl], writes=[TEe])
                        k.op("pe", lambda e: e.matmul(prs[:, 0:n], lhsT=ones_f[0:16, 0:16], rhs=Ee[:, 0:n], start=True, stop=True), reads=[TEe, Tc], writes=[Tprs])
                        k.op("dve", lambda e: e.reciprocal(out=rc6[:, 0:n], in_=prs[:, 0:n]), reads=[Tprs], writes=[Trc6])
                        k.op("dve", lambda e: e.tensor_tensor(out=rc6[:, 0:n], in0=rc6[:, 0:n], in1=Ee[:, 0:n], op=ALU.mult), reads=[Trc6, TEe], writes=[Trc6])
                        k.dma("sp", aff_d[b, :, t0:t0 + n], rc6[:, 0:n], reads=[Trc6], writes=[Taff[b]])
                        ntr_box[0] = ntr

                    s6_A(0)
                    for bi in range(len(blks6)):
                        if bi + 1 < len(blks6):
                            s6_A(bi + 1)
                        s6_B(bi)
                if cfg.upto < 7:
                    continue
                need_ctx = l < L - 1
                last = (l == cfg.layers - 1)
                segl = [(NCTX, NLAT, 256)] + ([(0, NCTX, 32)] if need_ctx else [])
                if l in getattr(cfg, 'skip_moe', ()) or b in getattr(cfg, 'skip_moe_b', ()):
                    segl = []
                segl = segl[:getattr(cfg, 'max_seg', 2)]
                for (t0, N, cap) in segl:
                  isctx = (t0 == 0)
                  mi = 2 if isctx else b
                  ntile = N // 128; nst = max(1, cap // 128); sp = min(cap, 128)
                  with Stage(k):
                    Tp7 = T()
                    iotaf = k.sb("iotaf", [128, 256]); iotap = k.sb("iotap", [128, 2])
                    for dst, src in ((iotaf, iotaf_d), (iotap, iotap_d)):
                        k.dma("sp", dst[:], src, writes=[Tp7])
                    slotm = k.sb("slotm", [16, N]); Tslot = T()
                    slotT = k.sb("slotT", [128, ntile, 16]); TslotT = T()
                    ghlT = k.sb("ghlT", [128, ntile, 16, 2], BF16); TghlT = T()
                    ysb = k.sb("ysb", [128, NEXP, nst, D], BF16); Tysb = T()
                    with Stage(k):
                        ones16 = k.sb("ones16", [16, NLAT])
                        k.dma("sp", ones16[:], ones16_d, writes=[Tp7])
                        affs = k.sb("affs", [16, N]); Taffs = T()
                        work = k.sb("work", [16, N]); Twork = T()
                        m8 = k.sb("m8", [16, 8]); Tm8 = T()
                        mask = k.sb("mask", [16, N]); Tmask = T()
                        ghi = k.sb("ghi", [16, N], BF16); glo = k.sb("glo", [16, N], BF16); Tg = T()
                        k.dma("sp", affs[:], aff_d[b, :, t0:t0 + N], reads=[Taff[b]], writes=[Taffs])
                        k.op("act", lambda e: e.activation(out=work[:], in_=affs[:], func=AF.Copy), reads=[Taffs], writes=[Twork])
                        for it_ in range(cap // 8):
                            k.op("dve", lambda e: e.max(out=m8[:], in_=work[:]), reads=[Twork], writes=[Tm8])
                            k.op("dve", lambda e: e.match_replace(out=work[:], in_to_replace=m8[:], in_values=work[:], imm_value=-1.0), reads=[Tm8, Twork], writes=[Twork])
                        k.op("dve", lambda e: e.tensor_scalar(out=mask[:], in0=work[:], scalar1=0.0, scalar2=None, op0=ALU.is_lt), reads=[Twork], writes=[Tmask])
                        k.op("dve", lambda e: e.tensor_tensor_scan(out=slotm[:], data0=ones16[:, 0:N], data1=mask[:], initial=0.0, op0=ALU.mult, op1=ALU.add),
                             reads=[Tmask, Tp7], writes=[Tslot])
                        k.op("dve", lambda e: e.tensor_tensor(out=slotm[:], in0=slotm[:], in1=mask[:], op=ALU.mult), reads=[Tslot, Tmask], writes=[Tslot])
                        k.op("dve", lambda e: e.tensor_scalar(out=slotm[:], in0=slotm[:], scalar1=-1.0, scalar2=None, op0=ALU.add), reads=[Tslot], writes=[Tslot])
                        k.op("dve", lambda e: e.tensor_tensor(out=affs[:], in0=affs[:], in1=mask[:], op=ALU.mult), reads=[Taffs, Tmask], writes=[Taffs])
                        k.op("dve", lambda e: e.tensor_copy(out=ghi[:], in_=affs[:]), reads=[Taffs], writes=[Tg])
                        k.op("dve", lambda e: e.tensor_tensor(out=affs[:], in0=affs[:], in1=ghi[:], op=ALU.subtract), reads=[Taffs, Tg], writes=[Taffs])
                        k.op("dve", lambda e: e.tensor_copy(out=glo[:], in_=affs[:]), reads=[Taffs], writes=[Tg])
                        pst = k.ps("pst", [128, ntile, 16]); Tpst = PT()
                        pgh = k.ps("pgh", [128, ntile, 2, 16], BF16); Tpgh = PT()
                        for ti in range(ntile):
                            cs_ = slice(ti * 128, (ti + 1) * 128)
                            k.op("pe", lambda e: e.transpose(pst[:, ti, :], slotm[:, cs_], ident_f[0:16, 0:16]), reads=[Tslot, Tc], writes=[Tpst])
                            k.op("pe", lambda e: e.transpose(pgh[:, ti, 0, :], ghi[:, cs_], ident_b[0:16, 0:16]), reads=[Tg, Tc], writes=[Tpgh])
                            k.op("pe", lambda e: e.transpose(pgh[:, ti, 1, :], glo[:, cs_], ident_b[0:16, 0:16]), reads=[Tg, Tc], writes=[Tpgh])
                        k.op("dve", lambda e: e.tensor_copy(out=slotT[:], in_=pst[:]), reads=[Tpst], writes=[TslotT])
                        k.op("dve", lambda e: e.tensor_copy(out=ghlT[:].rearrange("p n e h -> p n h e"), in_=pgh[:]), reads=[Tpgh], writes=[TghlT])
                    with Stage(k):
                        h2k = k.sb("h2k", [128, ntile, D], BF16); Th2k = T()
                        k.dma("sp", h2k[:], h2t_d[b, t0:t0 + N, :].rearrange("(n p) d -> p n d", p=128), reads=[Th2[b]], writes=[Th2k])
                        wg = [k.sb("wg%d" % i, [128, 8, FF], BF16) for i in range(2)]
                        wu = [k.sb("wu%d" % i, [128, 8, FF], BF16) for i in range(2)]
                        wd = [k.sb("wd%d" % i, [128, 4, D], BF16) for i in range(2)]
                        Tw = [T(), T()]
                        Sel = [k.sb("Sel%d" % i, [128, ntile, cap], BF16) for i in range(1)] * 2; TSel = [T()] * 2
                        cst7 = Caster(k, 128, 2048, nbuf=4)
                        xsTL = [k.sb("xsT%d" % i, [128, 8, cap], BF16) for i in range(1)] * 2; TxsTL = [T()] * 2
                        sg = [k.sb("sg%d" % i, [128, cap]) for i in range(2)]; Tsg = [T(), T()]
                        actTL = [k.sb("actT%d" % i, [128, 4, cap], BF16) for i in range(1)] * 2; TactTL = [T()] * 2
                        gs2L = [k.sb("gs2_%d" % i, [128, nst, 2]) for i in range(2)]; gsL = [k.sb("gs_%d" % i, [128, nst]) for i in range(2)]; TgsL = [T(), T()]
                        pg = [k.ps("pg7_%d" % i, [128, cap]) for i in range(3)]; Tpg = [PT() for _ in range(3)]
                        pG = k.ps("pG", [128, cap]); TpG = PT()
                        pU = k.ps("pU", [128, cap]); TpU = PT()
                        pY = [k.ps("pY%d" % i, [128, 512]) for i in range(2)]; TpY = [PT(), PT()]
                        pgs = k.ps("pgs", [128, nst, 2]); Tpgs = PT()
                        nev = 0
                        for ex in range(NEXP):
                            i2 = ex % 2
                            xsT = xsTL[i2]; TxsT = TxsTL[i2]; actT = actTL[i2]; TactT = TactTL[i2]
                            gs2 = gs2L[i2]; gs = gsL[i2]; Tgs = TgsL[i2]
                            for (wt_, src_, c_) in ((wg[i2], w_gate_d[l, ex], 8), (wu[i2], w_up_d[l, ex], 8), (wd[i2], w_down_d[l, ex], 4)):
                                s3 = src_.rearrange("(c p) n -> p c n", p=128)
                                hc_ = c_ // 2
                                for pc_ in range(2):
                                    cst7.load(wt_[:, pc_ * hc_:(pc_ + 1) * hc_, :], s3[:, pc_ * hc_:(pc_ + 1) * hc_, :], 128, 2048, [Tw[i2]], split=2048 // hc_)
                            SL = Sel[i2]; tSL = TSel[i2]
                            for ti in range(ntile):
                                k.op("dve", lambda e: e.tensor_scalar(out=SL[:, ti, :], in0=iotaf[:, 0:cap], scalar1=slotT[:, ti, ex:ex + 1], scalar2=None, op0=ALU.is_equal),
                                     reads=[TslotT, Tp7], writes=[tSL])
                            for dc in range(8):
                                pp = pg[dc % 3]; tp = Tpg[dc % 3]
                                for ti in range(ntile):
                                    k.op("pe", lambda e: e.matmul(pp[:], lhsT=h2k[:, ti, dc * 128:(dc + 1) * 128], rhs=SL[:, ti, :], start=(ti == 0), stop=(ti == ntile - 1)),
                                         reads=[Th2k, tSL], writes=[tp])
                                if dc % 2 == 0:
                                    k.op("act", lambda e: e.activation(out=xsT[:, dc, :], in_=pp[:], func=AF.Copy), reads=[tp], writes=[TxsT])
                                else:
                                    k.op("dve", lambda e: e.tensor_copy(out=xsT[:, dc, :], in_=pp[:]), reads=[tp], writes=[TxsT])
                            for st in range(nst):
                                for ti in range(ntile):
                                    k.op("pe", lambda e: e.matmul(pgs[0:sp, st, :], lhsT=SL[:, ti, st * 128:st * 128 + sp], rhs=ghlT[:, ti, ex, :], start=(ti == 0), stop=(ti == ntile - 1)),
                                         reads=[tSL, TghlT], writes=[Tpgs])
                            k.op("act", lambda e: e.activation(out=gs2[0:sp], in_=pgs[0:sp], func=AF.Copy), reads=[Tpgs], writes=[Tgs])
                            k.op("dve", lambda e: e.tensor_tensor(out=gs[0:sp], in0=gs2[0:sp, :, 0], in1=gs2[0:sp, :, 1], op=ALU.add), reads=[Tgs], writes=[Tgs])
                            for fc in range(4):
                                for kc in range(8):
                                    k.op("pe", lambda e: e.matmul(pG[:], lhsT=wg[i2][:, kc, fc * 128:(fc + 1) * 128], rhs=xsT[:, kc, :], start=(kc == 0), stop=(kc == 7)),
                                         reads=[Tw[i2], TxsT], writes=[TpG])
                                for kc in range(8):
                                    k.op("pe", lambda e: e.matmul(pU[:], lhsT=wu[i2][:, kc, fc * 128:(fc + 1) * 128], rhs=xsT[:, kc, :], start=(kc == 0), stop=(kc == 7)),
                                         reads=[Tw[i2], TxsT], writes=[TpU])
                                k.op("act", lambda e: e.activation(out=sg[fc % 2][:], in_=pG[:], func=AF.Silu), reads=[TpG], writes=[Tsg[fc % 2]])
                                k.op("dve", lambda e: e.tensor_tensor(out=actT[:, fc, :], in0=sg[fc % 2][:], in1=pU[:], op=ALU.mult), reads=[Tsg[fc % 2], TpU], writes=[TactT])
                            for st in range(nst):
                                for dh in range(2):
                                    pp = pY[nev % 2]; tp = TpY[nev % 2]
                                    for fc in range(4):
                                        k.op("pe", lambda e: e.matmul(pp[0:sp, :], lhsT=actT[:, fc, st * 128:st * 128 + sp], rhs=wd[i2][:, fc, dh * 512:(dh + 1) * 512],
                                                                      start=(fc == 0), stop=(fc == 3)), reads=[TactT, Tw[i2]], writes=[tp])
                                    dsto = ysb[0:sp, ex, st, dh * 512:(dh + 1) * 512]
                                    if nev % 2 == 0:
                                        k.op("act", lambda e: e.activation(out=dsto, in_=pp[0:sp, :], func=AF.Copy, scale=gs[0:sp, st:st + 1]), reads=[tp, Tgs], writes=[Tysb])
                                    else:
                                        k.op("dve", lambda e: e.tensor_scalar(out=dsto, in0=pp[0:sp, :], scalar1=gs[0:sp, st:st + 1], scalar2=None, op0=ALU.mult),
                                             reads=[tp, Tgs], writes=[Tysb])
                                    nev += 1
                    with Stage(k):
                        oneh = k.sb("oneh", [16, 16, 128]); Toneh = T()
                        k.dma("sp", oneh[:], oneh_d, writes=[Toneh])
                        SelTL = [k.sb("SelT%d" % i, [128, NEXP, nst, 512], BF16) for i in range(2)]; TSelTL = [T(), T()]
                        Xc = [k.sb("Xc%d" % i, [128, 8, 512]) for i in range(2)]; TXc = [T(), T()]
                        ot = [k.sb("ot%d" % i, [128, D]) for i in range(2)]; Tot = [T(), T()]
                        pb = [k.ps("pb%d" % i, [128, 512]) for i in range(2)]; Tpb = [PT(), PT()]
                        pc = [k.ps("pc%d" % i, [128, 512]) for i in range(4)]; Tpc = [PT() for _ in range(4)]
                        po_ = [k.ps("po7_%d" % i, [128, 4, 128]) for i in range(2)]; Tpo_ = [PT(), PT()]
                        nto = 0
                        nblk = max(1, N // 512)
                        def cmb_build(tb):
                            n = min(N, 512)
                            c0 = tb * 512
                            X = Xc[tb % 2]; tX = TXc[tb % 2]
                            SelT = SelTL[tb % 2]; TSelT = TSelTL[tb % 2]
                            k.dma("sp", X[:, :, 0:n], xT_d[b, :, t0 + c0:t0 + c0 + n].rearrange("(c p) t -> p c t", p=128), reads=[Tx[b]], writes=[tX])
                            for ex in range(NEXP):
                                pp = pb[ex % 2]; tp = Tpb[ex % 2]
                                k.op("pe", lambda e: e.matmul(pp[:, 0:n], lhsT=oneh[:, ex, :], rhs=slotm[:, c0:c0 + n], start=True, stop=True), reads=[Tslot, Toneh], writes=[tp])
                                for st in range(nst):
                                    k.op("dve", lambda e: e.tensor_scalar(out=SelT[:, ex, st, 0:n], in0=pp[:, 0:n], scalar1=iotap[:, st:st + 1], scalar2=None, op0=ALU.is_equal),
                                         reads=[tp, Tp7], writes=[TSelT])

                        cmb_build(0)
                        for tb in range(nblk):
                            n = min(N, 512)
                            c0 = tb * 512
                            X = Xc[tb % 2]; tX = TXc[tb % 2]
                            SelT = SelTL[tb % 2]; TSelT = TSelTL[tb % 2]
                            if tb + 1 < nblk:
                                cmb_build(tb + 1)
                            for dh in range(2):
                                for dcl in range(4):
                                    dc = dh * 4 + dcl
                                    pp = pc[dcl]; tp = Tpc[dcl]
                                    for ex in range(NEXP):
                                        for st in range(nst):
                                            k.op("pe", lambda e: e.matmul(pp[:, 0:n], lhsT=ysb[0:sp, ex, st, dc * 128:(dc + 1) * 128], rhs=SelT[0:sp, ex, st, 0:n],
                                                                          start=(ex == 0 and st == 0), stop=(ex == NEXP - 1 and st == nst - 1)),
                                                 reads=[Tysb, TSelT], writes=[tp])
                                    k.op("dve", lambda e: e.scalar_tensor_tensor(out=X[:, dc, 0:n], in0=pp[:, 0:n], scalar=modT[:, l, 40 + dc, mi:mi + 1], in1=X[:, dc, 0:n],
                                                                                op0=ALU.mult, op1=ALU.add), reads=[tp, Tmod, tX], writes=[tX])
                            if not last:
                                k.dma("sp", xT_d[b, :, t0 + c0:t0 + c0 + n].rearrange("(c p) t -> p c t", p=128), X[:, :, 0:n], reads=[tX], writes=[Tx[b]])
                            elif not isctx:
                                for tt in range(n // 128):
                                    O = ot[nto % 2]; tO = Tot[nto % 2]
                                    for half in range(2):
                                        pp = po_[half]; tp = Tpo_[half]
                                        for q in range(4):
                                            dc = half * 4 + q
                                            k.op("pe", lambda e: e.transpose(pp[:, q, :], X[:, dc, tt * 128:(tt + 1) * 128], ident_f[:]), reads=[tX, Tc], writes=[tp])
                                        if half == 0:
                                            k.op("act", lambda e: e.activation(out=O[:, 0:512], in_=pp[:].rearrange("p q t -> p (q t)"), func=AF.Copy), reads=[tp], writes=[tO])
                                        else:
                                            k.op("dve", lambda e: e.tensor_copy(out=O[:, 512:1024], in_=pp[:].rearrange("p q t -> p (q t)")), reads=[tp], writes=[tO])
                                    tok0 = c0 + tt * 128
                                    k.dma("sp", out_d[b, tok0:tok0 + 128, :], O[:], reads=[tO], writes=[Tout])
                                    nto += 1

        k.barrier()
        global LAST_K
        LAST_K = k
    return nc


def prep_inputs(inp, core, nb=2):
    b0 = core * 2
    m = {}
    m["x"] = np.ascontiguousarray(inp["x"][b0:b0 + nb])
    m["ctx"] = np.ascontiguousarray(inp["ctx"][b0:b0 + nb])
    cc = np.stack([inp["c"][b0], inp["c"][b0 + 1], inp["c_ctx"]], axis=0)
    m["cT"] = np.ascontiguousarray(fm(cc, 8).transpose(0, 2, 1))
    m["ada_w"] = inp["ada_w"]
    m["ada_bT"] = fm(inp["ada_b"], 48)
    m["n1T"] = fm(inp["norm1_w"], 8)
    m["n2T"] = fm(inp["norm2_w"], 8)
    m["w_in"] = inp["w_in"]
    m.update(prep_lru(inp))
    m["hg_lbT"] = fm(inp["hgrn_lb_logits"], 2)
    m["hg_nwT"] = fm(inp["hgrn_norm_w"], 2)
    scw = np.asarray(inp["ssd_conv_w"], np.float32)
    m["sd_cw"] = np.ascontiguousarray(scw.reshape(L, 4, 4, 128).transpose(3, 0, 2, 1))
    m["sd_cb"] = fm(inp["ssd_conv_b"], 4)
    rep = lambda v: np.ascontiguousarray(np.broadcast_to(np.asarray(v, np.float32)[None], (128,) + tuple(np.shape(v))))
    m["sd_alog"] = rep(np.asarray(inp["ssd_a_log"]).reshape(L, 8))
    m["sd_dtb"] = rep(np.asarray(inp["ssd_dt_bias"]).reshape(L, 8))
    m["sd_dsk"] = rep(np.repeat(np.asarray(inp["ssd_d_skip"]), 64, axis=-1))
    m["sd_nw"] = rep(inp["ssd_norm_w"])
    m["ml_qan"] = rep(inp["mla_q_a_norm"]); m["ml_kvan"] = rep(inp["mla_kv_a_norm"])
    m["ml_qn"] = rep(inp["mla_q_norm"]); m["ml_kn"] = rep(inp["mla_k_norm"])
    m["ml_wq"] = inp["mla_w_q_up"]; m["ml_wkv"] = inp["mla_w_kv_up"]
    m["w_out"] = inp["w_out"]; m["w_rt"] = inp["moe_router"]
    m["w_gate"] = inp["moe_w_gate"]; m["w_up"] = inp["moe_w_up"]; m["w_down"] = inp["moe_w_down"]
    m.update(host_consts())
    return m


def kernel(**inputs):
    inp = {k_: np.asarray(v) for k_, v in inputs.items()}
    cfg = Cfg(nb=2)
    nc = build(cfg)
    in_maps = [prep_inputs(inp, c) for c in range(8)]
    res = run_bass_kernel_spmd(nc, in_maps, core_ids=list(range(8)))
    out = np.concatenate([r["out"] for r in res.results], axis=0)
    return out.astype(np.float32)
```
